# Optimizing a Trainium2 kernel written in Bass

```python
import math
import jax, jax.numpy as jnp
from jax import lax
import numpy as np

D_MODEL = 1024
BATCH = 32
SEQ = 2048
DEPTH = 1

SSM_WIDTH = D_MODEL // 2
SSM_GROUP_CH = 16
SSM_GROUPS = SSM_WIDTH // SSM_GROUP_CH
SSM_STATE = 64
SGU_WIDTH = D_MODEL // 2
SGU_HEADS = 8
SGU_HEAD_DIM = SGU_WIDTH // SGU_HEADS
SGU_CHUNK = 128
IN_WIDTH = SSM_WIDTH + 2 * SGU_WIDTH
N_GROUPS = 4
EXPERTS_PER_GROUP = 8
N_EXPERTS = N_GROUPS * EXPERTS_PER_GROUP
TOP_K = 2
D_FF_EXPERT = D_MODEL // 2
EXPERT_BLOCK = 128
RMS_EPS = 1e-6
LN_EPS = 1e-5

kernel_name = "hybrid_s5_sgu_hmoe_block"


def rmsnorm(x, g):
    xf = x.astype(jnp.float32)
    y = xf * lax.rsqrt(jnp.mean(xf * xf, axis=-1, keepdims=True) + RMS_EPS)
    return (y * g.astype(jnp.float32)).astype(x.dtype)


def layernorm(x, g, b):
    xf = x.astype(jnp.float32)
    mu = jnp.mean(xf, axis=-1, keepdims=True)
    var = jnp.mean(jnp.square(xf - mu), axis=-1, keepdims=True)
    y = (xf - mu) * lax.rsqrt(var + LN_EPS) * g.astype(jnp.float32) + b.astype(jnp.float32)
    return y.astype(x.dtype)


def modulate(x, shift, scale):
    return x * (1.0 + scale[:, None, :]) + shift[:, None, :]


def s5_branch(u, a_re, a_im, b_re, b_im, c_re, c_im, d_skip, log_step, w_glu, b_glu):
    f32 = jnp.float32
    bq, sq, _ = u.shape
    uf = u.astype(f32).reshape(bq, sq, SSM_GROUPS, SSM_GROUP_CH)
    lam_re = jnp.minimum(a_re.astype(f32), -1e-4)
    lam_im = a_im.astype(f32)
    dt = jnp.exp(log_step.astype(f32))[:, None]
    mag = jnp.exp(lam_re * dt)
    ab_re = mag * jnp.cos(lam_im * dt)
    ab_im = mag * jnp.sin(lam_im * dt)
    den = lam_re * lam_re + lam_im * lam_im
    nr = ab_re - 1.0
    f_re = (nr * lam_re + ab_im * lam_im) / den
    f_im = (ab_im * lam_re - nr * lam_im) / den
    bu_re = jnp.einsum('bsgh,gnh->bsgn', uf, b_re.astype(f32))
    bu_im = jnp.einsum('bsgh,gnh->bsgn', uf, b_im.astype(f32))
    x_re = f_re * bu_re - f_im * bu_im
    x_im = f_re * bu_im + f_im * bu_re
    a_r = jnp.broadcast_to(ab_re[None, None], (1, sq, SSM_GROUPS, SSM_STATE))
    a_i = jnp.broadcast_to(ab_im[None, None], (1, sq, SSM_GROUPS, SSM_STATE))

    def combine(e1, e2):
        ar1, ai1, br1, bi1 = e1
        ar2, ai2, br2, bi2 = e2
        ar = ar2 * ar1 - ai2 * ai1
        ai = ar2 * ai1 + ai2 * ar1
        br = ar2 * br1 - ai2 * bi1 + br2
        bi = ar2 * bi1 + ai2 * br1 + bi2
        return (ar, ai, br, bi)

    _, _, h_re, h_im = lax.associative_scan(combine, (a_r, a_i, x_re, x_im), axis=1)
    y = (jnp.einsum('bsgn,ghn->bsgh', h_re, c_re.astype(f32))
         - jnp.einsum('bsgn,ghn->bsgh', h_im, c_im.astype(f32))
         + d_skip.astype(f32) * uf)
    y = jax.nn.gelu(y.reshape(bq, sq, SSM_WIDTH))
    y = y * jax.nn.sigmoid(y @ w_glu.astype(f32) + b_glu.astype(f32))
    return y.astype(u.dtype)


def sgu_branch(z_u, z_v, ln_g, ln_b, w_s, b_s):
    bq, sq, _ = z_u.shape
    u = jax.nn.gelu(z_u)
    v = layernorm(jax.nn.gelu(z_v), ln_g, ln_b)
    n_chunks = sq // SGU_CHUNK
    vh = v.reshape(bq, n_chunks, SGU_CHUNK, SGU_HEADS, SGU_HEAD_DIM)
    causal = jnp.tril(jnp.ones((SGU_CHUNK, SGU_CHUNK), w_s.dtype))
    ws = w_s * causal[None]
    mixed = jnp.einsum('hts,bcshd->bcthd', ws, vh) + b_s.T[None, None, :, :, None]
    return u * mixed.reshape(bq, sq, SGU_WIDTH)


def hierarchical_moe(xn, w_rg, b_rg, w_re, b_re, w1, w3, w2):
    f32 = jnp.float32
    bq, sq, dm = xn.shape
    n_tok = bq * sq
    xt = xn.reshape(n_tok, dm)
    lg = (xt @ w_rg + b_rg).astype(f32)
    pg = jax.nn.softmax(lg, axis=-1)
    grp = jnp.argmax(lg, axis=-1).astype(jnp.int32)
    pg_sel = jnp.take_along_axis(pg, grp[:, None], axis=1)[:, 0]
    le_all = (jnp.einsum('td,gde->tge', xt, w_re) + b_re[None]).astype(f32)
    le = jnp.take_along_axis(le_all, grp[:, None, None], axis=1)[:, 0]
    top_v, top_i = lax.top_k(le, TOP_K)
    pe = jax.nn.softmax(top_v, axis=-1)
    eid = grp[:, None] * EXPERTS_PER_GROUP + top_i.astype(jnp.int32)
    wgt = pg_sel[:, None] * pe

    n_asg = n_tok * TOP_K
    e_flat = eid.reshape(n_asg)
    w_flat = wgt.reshape(n_asg)
    tok_flat = jnp.repeat(jnp.arange(n_tok, dtype=jnp.int32), TOP_K)
    order = jnp.argsort(e_flat)
    e_s = e_flat[order]
    tok_s = tok_flat[order]
    w_s = w_flat[order]
    counts = jnp.bincount(e_flat, length=N_EXPERTS).astype(jnp.int32)
    padded = (counts + EXPERT_BLOCK - 1) // EXPERT_BLOCK * EXPERT_BLOCK
    starts = jnp.cumsum(counts) - counts
    ends_p = jnp.cumsum(padded)
    pstarts = ends_p - padded
    dest = pstarts[e_s] + (jnp.arange(n_asg, dtype=jnp.int32) - starts[e_s])
    n_buf = n_asg + N_EXPERTS * EXPERT_BLOCK
    n_blocks = n_buf // EXPERT_BLOCK
    buf_tok = jnp.full((n_buf,), n_tok, jnp.int32).at[dest].set(tok_s)
    buf_w = jnp.zeros((n_buf,), f32).at[dest].set(w_s)
    block_start = jnp.arange(n_blocks, dtype=jnp.int32) * EXPERT_BLOCK
    block_e = jnp.clip(jnp.searchsorted(ends_p, block_start, side='right'), 0, N_EXPERTS - 1)
    x_pad = jnp.concatenate([xt, jnp.zeros((1, dm), xt.dtype)], axis=0)
    xb = x_pad[buf_tok].reshape(n_blocks, EXPERT_BLOCK, dm)

    def expert_block(args):
        xblk, e = args
        hid = jax.nn.silu(xblk @ w1[e]) * (xblk @ w3[e])
        return hid @ w2[e]

    yb = lax.map(expert_block, (xb, block_e)).reshape(n_buf, dm)
    y = jnp.zeros((n_tok + 1, dm), yb.dtype).at[buf_tok].add(yb * buf_w[:, None].astype(yb.dtype))
    return y[:n_tok].reshape(bq, sq, dm)


def setup_inputs(seed: int = 0) -> dict:
    key = jax.random.key(seed)
    ks = iter(jax.random.split(key, 40))
    f32 = jnp.float32
    L = DEPTH

    def nrm(shape, scale):
        return jax.random.normal(next(ks), shape, f32) * scale

    d_inv = D_MODEL ** -0.5
    x = nrm((BATCH, SEQ, D_MODEL), 1.0)
    c = nrm((BATCH, D_MODEL), 1.0)
    w_ada = nrm((L, D_MODEL, 6 * D_MODEL), 0.2 * d_inv)
    b_ada = nrm((L, 6 * D_MODEL), 0.01)
    norm1_g = 1.0 + nrm((L, D_MODEL), 0.01)
    w_in = nrm((L, D_MODEL, IN_WIDTH), d_inv)
    w_gate = nrm((L, D_MODEL, 2 * D_MODEL), d_inv)
    b_gate = nrm((L, 2 * D_MODEL), 0.01)
    ssm_a_re = -0.5 + nrm((L, SSM_GROUPS, SSM_STATE), 0.01)
    ssm_a_im = jnp.pi * jnp.arange(SSM_STATE, dtype=f32) + nrm((L, SSM_GROUPS, SSM_STATE), 0.01)
    ssm_b_re = nrm((L, SSM_GROUPS, SSM_STATE, SSM_GROUP_CH), (2 * SSM_GROUP_CH) ** -0.5)
    ssm_b_im = nrm((L, SSM_GROUPS, SSM_STATE, SSM_GROUP_CH), (2 * SSM_GROUP_CH) ** -0.5)
    ssm_c_re = nrm((L, SSM_GROUPS, SSM_GROUP_CH, SSM_STATE), 0.5)
    ssm_c_im = nrm((L, SSM_GROUPS, SSM_GROUP_CH, SSM_STATE), 0.5)
    ssm_d = nrm((L, SSM_GROUPS, SSM_GROUP_CH), 1.0)
    ssm_log_step = jax.random.uniform(next(ks), (L, SSM_GROUPS), f32, math.log(1e-3), math.log(1e-1))
    w_glu = nrm((L, SSM_WIDTH, SSM_WIDTH), SSM_WIDTH ** -0.5)
    b_glu = nrm((L, SSM_WIDTH), 0.01)
    sgu_ln_g = 1.0 + nrm((L, SGU_WIDTH), 0.01)
    sgu_ln_b = nrm((L, SGU_WIDTH), 0.01)
    sgu_w = nrm((L, SGU_HEADS, SGU_CHUNK, SGU_CHUNK), SGU_CHUNK ** -0.5)
    sgu_b = 1.0 + nrm((L, SGU_HEADS, SGU_CHUNK), 0.01)
    w_branch_a = nrm((L, SSM_WIDTH, D_MODEL), SSM_WIDTH ** -0.5)
    w_branch_b = nrm((L, SGU_WIDTH, D_MODEL), SGU_WIDTH ** -0.5)
    w_out = nrm((L, D_MODEL, D_MODEL), d_inv)
    norm2_g = 1.0 + nrm((L, D_MODEL), 0.01)
    w_router_group = nrm((L, D_MODEL, N_GROUPS), d_inv)
    b_router_group = nrm((L, N_GROUPS), 0.01)
    w_router_expert = nrm((L, N_GROUPS, D_MODEL, EXPERTS_PER_GROUP), d_inv)
    b_router_expert = nrm((L, N_GROUPS, EXPERTS_PER_GROUP), 0.01)
    w1 = nrm((L, N_EXPERTS, D_MODEL, D_FF_EXPERT), d_inv)
    w3 = nrm((L, N_EXPERTS, D_MODEL, D_FF_EXPERT), d_inv)
    w2 = nrm((L, N_EXPERTS, D_FF_EXPERT, D_MODEL), D_FF_EXPERT ** -0.5)
    norm_f_g = 1.0 + nrm((D_MODEL,), 0.01)
    return {"x": x, "c": c, "w_ada": w_ada, "b_ada": b_ada, "norm1_g": norm1_g,
            "w_in": w_in, "w_gate": w_gate, "b_gate": b_gate,
            "ssm_a_re": ssm_a_re, "ssm_a_im": ssm_a_im, "ssm_b_re": ssm_b_re, "ssm_b_im": ssm_b_im,
            "ssm_c_re": ssm_c_re, "ssm_c_im": ssm_c_im, "ssm_d": ssm_d, "ssm_log_step": ssm_log_step,
            "w_glu": w_glu, "b_glu": b_glu, "sgu_ln_g": sgu_ln_g, "sgu_ln_b": sgu_ln_b,
            "sgu_w": sgu_w, "sgu_b": sgu_b, "w_branch_a": w_branch_a, "w_branch_b": w_branch_b,
            "w_out": w_out, "norm2_g": norm2_g, "w_router_group": w_router_group,
            "b_router_group": b_router_group, "w_router_expert": w_router_expert,
            "b_router_expert": b_router_expert, "w1": w1, "w3": w3, "w2": w2, "norm_f_g": norm_f_g}


def reference(x, c, w_ada, b_ada, norm1_g, w_in, w_gate, b_gate,
              ssm_a_re, ssm_a_im, ssm_b_re, ssm_b_im, ssm_c_re, ssm_c_im, ssm_d, ssm_log_step,
              w_glu, b_glu, sgu_ln_g, sgu_ln_b, sgu_w, sgu_b, w_branch_a, w_branch_b,
              w_out, norm2_g, w_router_group, b_router_group, w_router_expert,
              b_router_expert, w1, w3, w2, norm_f_g):
    h = x
    c_act = jax.nn.silu(c)
    for l in range(DEPTH):
        mod = c_act @ w_ada[l] + b_ada[l]
        sh1, sc1, gt1, sh2, sc2, gt2 = jnp.split(mod, 6, axis=-1)
        xn = modulate(rmsnorm(h, norm1_g[l]), sh1, sc1)
        proj = xn @ w_in[l]
        z_a = proj[..., :SSM_WIDTH]
        z_u = proj[..., SSM_WIDTH:SSM_WIDTH + SGU_WIDTH]
        z_v = proj[..., SSM_WIDTH + SGU_WIDTH:]
        y_a = s5_branch(z_a, ssm_a_re[l], ssm_a_im[l], ssm_b_re[l], ssm_b_im[l],
                        ssm_c_re[l], ssm_c_im[l], ssm_d[l], ssm_log_step[l], w_glu[l], b_glu[l])
        y_b = sgu_branch(z_u, z_v, sgu_ln_g[l], sgu_ln_b[l], sgu_w[l], sgu_b[l])
        gates = jax.nn.sigmoid(xn @ w_gate[l] + b_gate[l])
        g_a = gates[..., :D_MODEL]
        g_b = gates[..., D_MODEL:]
        merged = g_a * (y_a @ w_branch_a[l]) + g_b * (y_b @ w_branch_b[l])
        h = h + gt1[:, None, :] * (merged @ w_out[l])
        xn2 = modulate(rmsnorm(h, norm2_g[l]), sh2, sc2)
        y_m = hierarchical_moe(xn2, w_router_group[l], b_router_group[l], w_router_expert[l],
                               b_router_expert[l], w1[l], w3[l], w2[l])
        h = h + gt2[:, None, :] * y_m
    return rmsnorm(h, norm_f_g)
```

```python
import math
from contextlib import ExitStack
import numpy as np
import concourse.bass as bass
import concourse.mybir as mybir
from concourse.bass_utils import run_bass_kernel_spmd

F32 = mybir.dt.float32
BF16 = mybir.dt.bfloat16
I32 = mybir.dt.int32
U8 = mybir.dt.uint8
AF = mybir.ActivationFunctionType
ALU = mybir.AluOpType
AX = mybir.AxisListType

ENGS = ("pe", "act", "dve", "pool", "sp")
N_CORES = 8
D = 1024
TWO_PI = 2.0 * math.pi


class Res:
    __slots__ = ("name", "lastw", "readers")

    def __init__(self, name):
        self.name = name
        self.lastw = None
        self.readers = []


class Op:
    __slots__ = ("eng", "fn", "deps", "dma_key", "dma_val", "signal", "sig_idx")

    def __init__(self, eng, fn, dma_key):
        self.eng = eng
        self.fn = fn
        self.deps = []
        self.dma_key = dma_key
        self.dma_val = None
        self.signal = False
        self.sig_idx = None


class Sched:
    def __init__(self, nc):
        self.nc = nc
        self.ops = {e: [] for e in ENGS}
        self.dma_cnt = {}
        self.finals = []
        self.pending_barrier = {}

    def add(self, eng, fn, reads=(), writes=(), dma_key=None):
        op = Op(eng, fn, dma_key)
        deps = []
        for r in reads:
            if r.lastw is not None:
                deps.append(r.lastw)
        for w in writes:
            if w.lastw is not None:
                deps.append(w.lastw)
            deps.extend(w.readers)
        if eng in self.pending_barrier:
            deps.extend(self.pending_barrier.pop(eng))
        seen = set()
        for d in deps:
            if d is op or id(d) in seen:
                continue
            seen.add(id(d))
            if d.eng == "pe" and eng == "pe" and d.dma_key is None and dma_key is None:
                continue
            op.deps.append(d)
            if d.dma_key is None:
                d.signal = True
        if dma_key is not None:
            self.dma_cnt[dma_key] = self.dma_cnt.get(dma_key, 0) + 16
            op.dma_val = self.dma_cnt[dma_key]
        for r in reads:
            r.readers.append(op)
        for w in writes:
            w.lastw = op
            w.readers = []
        self.ops[eng].append(op)
        return op

    def barrier(self, dma_ops=()):
        lasts = [self.ops[e][-1] for e in ENGS if self.ops[e]]
        lasts = [o for o in lasts if o.dma_key is None] + list(dma_ops)
        for e in ENGS:
            self.pending_barrier.setdefault(e, []).extend(lasts)

    def finish(self, ops):
        self.finals.extend(ops)

    def emit(self, stack):
        nc = self.nc
        sems = {e: stack.enter_context(nc.semaphore("sem_" + e)) for e in ENGS}
        dsem = {k: stack.enter_context(nc.semaphore("dsem_%s" % (k,))) for k in self.dma_cnt}
        for e in ENGS:
            n = 0
            for op in self.ops[e]:
                if op.dma_key is None and op.signal:
                    n += 1
                    op.sig_idx = n
        block = stack.enter_context(nc.Block())
        engobj = {"pe": "tensor", "act": "scalar", "dve": "vector", "pool": "gpsimd", "sp": "sync"}
        finals = self.finals

        def body_for(e):
            def body(eng):
                seen = {}
                for op in self.ops[e]:
                    need = {}
                    for d in op.deps:
                        if d.dma_key is not None:
                            s, v, key = dsem[d.dma_key], d.dma_val, ("d", d.dma_key)
                        else:
                            s, v, key = sems[d.eng], d.sig_idx, ("e", d.eng)
                        if key not in need or need[key][1] < v:
                            need[key] = (s, v)
                    for key, (s, v) in need.items():
                        if seen.get(key, 0) >= v:
                            continue
                        seen[key] = v
                        eng.wait_ge(s, v)
                    inst = op.fn(eng)
                    if op.dma_key is not None:
                        inst.then_inc(dsem[op.dma_key], 16)
                    elif op.signal:
                        inst.then_inc(sems[e], 1)
                if e == "sp":
                    for d in finals:
                        eng.wait_ge(dsem[d.dma_key], d.dma_val)
            return body

        for e in ENGS:
            getattr(block, engobj[e])(body_for(e))


class Tl:
    __slots__ = ("ap", "r")

    def __init__(self, ap, name):
        self.ap = ap
        self.r = Res(name)


class Arena:
    def __init__(self, nc, stack, nbytes):
        self.buf = stack.enter_context(nc.sbuf_tensor("arena", [128, nbytes], U8))
        self.off = 0
        self.cap = nbytes
        self.live = []

    def alloc(self, name, shape, dt):
        esz = {F32: 4, BF16: 2, I32: 4}[dt]
        n = 1
        for s in shape[1:]:
            n *= s
        nb = (n * esz + 31) // 32 * 32
        assert self.off + nb <= self.cap, "SBUF arena overflow at %s: %d + %d > %d" % (name, self.off, nb, self.cap)
        v = self.buf[0:shape[0], self.off:self.off + n * esz].bitcast(dt)
        if len(shape) == 3:
            v = v.rearrange("p (a b) -> p a b", a=shape[1])
        elif len(shape) == 4:
            v = v.rearrange("p (a b c) -> p a b c", a=shape[1], b=shape[2])
        t = Tl(v, name)
        lo, hi = self.off, self.off + nb
        keep = []
        for (a, b, o) in self.live:
            if a < hi and lo < b:
                if o.r.lastw is not None:
                    t.r.readers.append(o.r.lastw)
                t.r.readers.extend(o.r.readers)
                if a >= lo and b <= hi:
                    continue
            keep.append((a, b, o))
        keep.append((lo, hi, t))
        self.live = keep
        self.off += nb
        return t

    def mark(self):
        return self.off

    def reset(self, m):
        self.off = m


def build(NSEQ=4, SEQ=2048, CAP=768, dbg=None, stop_after=None):
    nc = bass.Bass("TRN2", target_bir_lowering=False)
    NT = SEQ // 128
    NTOK = NSEQ * SEQ
    NTILES = NSEQ * NT
    NSLOT = 32 * CAP
    NBLK = CAP // 128
    dbg = dbg or {}
    dbg_outs = {}

    def din(name, shape, dt=F32):
        return nc.dram_tensor(name, list(shape), dt, kind="ExternalInput").ap()

    x_d = din("x", [NTOK, D])
    cT_d = din("cT", [128, 8, NSEQ])
    w_ada_d = din("w_ada", [D, 6 * D])
    b_ada_d = din("b_ada_rep", [NSEQ, 6 * D])
    g1T_d = din("g1T", [128, 8])
    w_in_d = din("w_in", [D, 1536])
    w_gate_d = din("w_gate", [D, 2048])
    b_gateT_d = din("b_gateT", [128, 16])
    lam_sm_d = din("lam_sm", [128, 3, 16])
    lam_c_d = din("lam_c", [128, 3, 4, 2, 128])
    Bc_d = din("Bc", [128, 2, 4, 2, 128])
    Cc_d = din("Cc", [128, 2, 16, 64])
    dT_d = din("dT", [128, 4])
    w_glu_d = din("w_glu", [512, 512])
    b_gluT_d = din("b_gluT", [128, 4])
    wsT_d = din("wsT", [128, 8, 128])
    lngT_d = din("lngT", [128, 4])
    lnbT_d = din("lnbT", [128, 4])
    bsT_d = din("bsT", [128, 4, 128])
    w_bra_d = din("w_bra", [512, D])
    w_brb_d = din("w_brb", [512, D])
    w_out_d = din("w_out", [D, D])
    g2_d = din("g2", [1, D])
    w_r_d = din("w_r", [128, 8, 36])
    b_r_d = din("b_r", [1, 36])
    w1_d = din("w1", [32, D, 512])
    w3_d = din("w3", [32, D, 512])
    w2_d = din("w2", [32, 512, D])
    gf_d = din("gf", [1, D])
    out_d = nc.dram_tensor("out", [NTOK, D], F32, kind="ExternalOutput").ap()
    mod_d = Tl(nc.dram_tensor("mod_d", [NSEQ, 6 * D], F32, kind="Internal").ap(), "mod_d")
    H_d = nc.dram_tensor("H_d", [NTOK, D], F32, kind="Internal").ap()
    X_d = nc.dram_tensor("X_d", [NSLOT, D], BF16, kind="Internal").ap()
    Y_d = nc.dram_tensor("Y_d", [NSLOT, D], F32, kind="Internal").ap()
    r_X = Res("X_d")
    r_Y = Res("Y_d")
    r_H = Res("H_d")

    S = Sched(nc)
    final_ops = []
    with ExitStack() as st:
        A = Arena(nc, st, 206 * 1024)
        ps_all = st.enter_context(nc.psum_tensor("ps_all", [128, 8, 512], F32))
        PSB = [Tl(ps_all[:, b, :], "psb%d" % b) for b in range(8)]

        def psv(b, shape, dt=F32):
            v = PSB[b].ap
            if dt == BF16:
                v = v.bitcast(BF16)
            n = 1
            for s in shape[1:]:
                n *= s
            v = v[0:shape[0], 0:n]
            if len(shape) == 3:
                v = v.rearrange("p (a b) -> p a b", a=shape[1])
            elif len(shape) == 4:
                v = v.rearrange("p (a b c) -> p a b c", a=shape[1], b=shape[2])
            return v

        def psv2(b, shape):
            v = ps_all[:, b:b + 2, :].rearrange("p a b -> p (a b)")
            n = 1
            for s in shape[1:]:
                n *= s
            v = v[0:shape[0], 0:n]
            if len(shape) == 3:
                v = v.rearrange("p (a b) -> p a b", a=shape[1])
            elif len(shape) == 4:
                v = v.rearrange("p (a b c) -> p a b c", a=shape[1], b=shape[2])
            return v

        def R(*ts):
            return [t.r if isinstance(t, Tl) else t for t in ts]

        def dma(eng, out, in_, reads, writes, key, **kw):
            return S.add(eng, lambda e: e.dma_start(out=out, in_=in_, **kw), R(*reads), R(*writes), dma_key=key)

        def tt(eng, out, in0, in1, op, reads, writes):
            return S.add(eng, lambda e: e.tensor_tensor(out=out, in0=in0, in1=in1, op=op), R(*reads), R(*writes))

        def ts(eng, out, in0, s1, s2, op0, op1, reads, writes, accum=None):
            if op1 is None:
                return S.add(eng, lambda e: e.tensor_scalar(out=out, in0=in0, scalar1=s1, scalar2=None, op0=op0), R(*reads), R(*writes))
            if accum is not None:
                return S.add(eng, lambda e: e.tensor_scalar(out=out, in0=in0, scalar1=s1, scalar2=s2, op0=op0, op1=op1, accum_out=accum), R(*reads), R(*writes))
            return S.add(eng, lambda e: e.tensor_scalar(out=out, in0=in0, scalar1=s1, scalar2=s2, op0=op0, op1=op1), R(*reads), R(*writes))

        def stt(out, in0, scalar, in1, op0, op1, reads, writes):
            return S.add("dve", lambda e: e.scalar_tensor_tensor(out=out, in0=in0, scalar=scalar, in1=in1, op0=op0, op1=op1), R(*reads), R(*writes))

        def act(out, in_, func, reads, writes, bias=None, scale=None, accum=None):
            kw = {}
            if bias is not None:
                kw["bias"] = bias
            if scale is not None:
                kw["scale"] = scale
            if accum is not None:
                kw["accum_out"] = accum
            return S.add("act", lambda e: e.activation(out=out, in_=in_, func=func, **kw), R(*reads), R(*writes))

        def cp(eng, out, in_, reads, writes):
            if eng == "act":
                return S.add("act", lambda e: e.copy(out=out, in_=in_), R(*reads), R(*writes))
            return S.add(eng, lambda e: e.tensor_copy(out=out, in_=in_), R(*reads), R(*writes))

        def mm(out, lhsT, rhs, start, stop, reads, writes):
            return S.add("pe", lambda e: e.matmul(out, lhsT=lhsT, rhs=rhs, start=start, stop=stop), R(*reads), R(*writes))

        def tr(out, in_, ident, reads, writes):
            return S.add("pe", lambda e: e.transpose(out=out, in_=in_, identity=ident), R(*reads), R(*writes))

        def memset(eng, ap, val, writes):
            return S.add(eng, lambda e: e.memset(ap, val), [], R(*writes))

        _regs = {}

        def breg(e, val):
            if val not in _regs:
                _regs[val] = e.to_reg(val)
            return _regs[val]

        def dump(name, t, ap=None):
            ap = t.ap if ap is None else ap
            shp = list(ap.shape)
            o = nc.dram_tensor("dbg_" + name, shp, ap.dtype, kind="ExternalOutput").ap()
            dbg_outs[name] = "dbg_" + name
            final_ops.append(dma("sp", o, ap, [t], [], "dbg_" + name))

        ident_f = A.alloc("ident_f", [128, 128], F32)
        ident_b = A.alloc("ident_b", [128, 128], BF16)
        tri_b = A.alloc("tri_b", [128, 128], BF16)
        ones_b = A.alloc("ones_b", [128, 128], BF16)
        memset("pool", ident_f.ap, 0.0, [ident_f])
        S.add("pool", lambda e: e.affine_select(out=ident_f.ap, in_=ident_f.ap, pattern=[[-1, 128]], compare_op=ALU.not_equal,
                                                  fill=1.0, base=0, channel_multiplier=1), R(ident_f), R(ident_f))
        cp("pool", ident_b.ap, ident_f.ap, [ident_f], [ident_b])
        memset("pool", ones_b.ap, 1.0, [ones_b])
        S.add("pool", lambda e: e.affine_select(out=tri_b.ap, in_=ones_b.ap, pattern=[[1, 128]], compare_op=ALU.is_gt,
                                                  fill=0.0, base=0, channel_multiplier=-1), R(ones_b), R(tri_b))

        wgt = A.alloc("wgt", [128, NTILES, 2], F32)
        sloti = A.alloc("sloti", [128, NTILES, 2], I32)
        base = A.alloc("base", [128, 32], F32)
        ecap = A.alloc("ecap", [128, 32], F32)
        memset("pool", base.ap, 0.0, [base])
        ecap_i = A.alloc("ecap_i", [128, 32], I32)
        S.add("pool", lambda e: e.iota(ecap_i.ap, pattern=[[CAP, 32]], base=0, channel_multiplier=0), [], R(ecap_i))
        cp("pool", ecap.ap, ecap_i.ap, [ecap_i], [ecap])
        A1T = A.alloc("A1T", [128, 8, NSEQ], F32)
        sh1T = A.alloc("sh1T", [128, 8, NSEQ], F32)
        eps_rms = A.alloc("eps_rms", [128, 1], F32)
        eps_ln = A.alloc("eps_ln", [128, 1], F32)
        memset("pool", eps_rms.ap, 1e-6, [eps_rms])
        memset("pool", eps_ln.ap, 1e-5, [eps_ln])

        mark_persist = A.mark()

        cact = A.alloc("cact", [128, 8, NSEQ], F32)
        dma("sp", cact.ap, cT_d, [], [cact], "cact")
        act(cact.ap, cact.ap, AF.Silu, [cact], [cact])
        modrow = A.alloc("modrow", [NSEQ, 6 * D], F32)
        bada = A.alloc("bada", [NSEQ, 6 * D], F32)
        dma("sp", bada.ap, b_ada_d, [], [bada], "bada")
        wa = [A.alloc("wa%d" % i, [128, 8, 512], F32) for i in range(2)]
        wa_view = w_ada_d.rearrange("(kc p) n -> p kc n", p=128)
        for cb in range(12):
            w = wa[cb % 2]
            dma("sp", w.ap, wa_view[:, :, cb * 512:(cb + 1) * 512], [], [w], "wa%d" % (cb % 2))
            pb = PSB[cb % 2]
            for kc in range(8):
                mm(pb.ap[0:NSEQ, :], cact.ap[:, kc, :], w.ap[:, kc, :], kc == 0, kc == 7, [cact, w], [pb])
            tt("dve", modrow.ap[:, cb * 512:(cb + 1) * 512], pb.ap[0:NSEQ, :], bada.ap[:, cb * 512:(cb + 1) * 512], ALU.add,
               [pb, bada], [modrow])
        dma("sp", mod_d.ap, modrow.ap, [modrow], [mod_d], "mod_d")
        sc1T = A.alloc("sc1T", [128, 8, NSEQ], F32)
        g1T = A.alloc("g1T", [128, 8], F32)
        dma("sp", g1T.ap, g1T_d, [], [g1T], "g1T")
        for b in range(NSEQ):
            S.add("sp", lambda e, b=b: e.dma_start(out=sh1T.ap[:, :, b], in_=mod_d.ap[b, 0:D].rearrange("(kc p) -> p kc", p=128),
                                                  allow_slow_non_contiguous=True), R(mod_d), R(sh1T), dma_key="sh1T")
            S.add("sp", lambda e, b=b: e.dma_start(out=sc1T.ap[:, :, b], in_=mod_d.ap[b, D:2 * D].rearrange("(kc p) -> p kc", p=128),
                                                  allow_slow_non_contiguous=True), R(mod_d), R(sc1T), dma_key="sc1T")
        for b in range(NSEQ):
            stt(A1T.ap[:, :, b], sc1T.ap[:, :, b], 1.0, g1T.ap, ALU.add, ALU.mult, [sc1T, g1T], [A1T])
        if "mod" in dbg:
            dump("modrow", modrow)
            dump("A1T", A1T)
        A.reset(mark_persist)
        S.barrier()
        if stop_after == "mod":
            S.finish(final_ops)
            S.emit(st)
            return nc, dbg_outs

        w_r = A.alloc("w_r", [128, 8, 36], F32)
        dma("sp", w_r.ap, w_r_d, [], [w_r], "w_r")
        b_r = A.alloc("b_r", [128, 36], F32)
        dma("sp", b_r.ap, b_r_d.partition_broadcast(128), [], [b_r], "b_r")
        b_gateT = A.alloc("b_gateT", [128, 16], F32)
        dma("sp", b_gateT.ap, b_gateT_d, [], [b_gateT], "b_gateT")
        b_gluT = A.alloc("b_gluT", [128, 4], F32)
        dma("sp", b_gluT.ap, b_gluT_d, [], [b_gluT], "b_gluT")
        dT = A.alloc("dT", [128, 4], F32)
        dma("sp", dT.ap, dT_d, [], [dT], "dT")
        lngT = A.alloc("lngT", [128, 4], F32)
        dma("sp", lngT.ap, lngT_d, [], [lngT], "lngT")
        lnbT = A.alloc("lnbT", [128, 4], F32)
        dma("sp", lnbT.ap, lnbT_d, [], [lnbT], "lnbT")

        tabC = A.alloc("tabC", [128, 16, 129], F32)
        tabD = A.alloc("tabD", [128, 16, 129], F32)
        rmag = A.alloc("rmag", [128, 16], F32)
        Bt = A.alloc("Bt", [128, 2, 8, 128], BF16)
        Ct = A.alloc("Ct", [128, 2, 16, 64], BF16)
        mark_setup = A.mark()

        def range_reduce(ph, tmpf, tmpi, n):
            ts("dve", tmpi, ph, 1.0 / TWO_PI, None, ALU.mult, None, [tabC], [tabC])
            cp("dve", tmpf, tmpi, [tabC], [tabC])
            stt(ph, tmpf, -TWO_PI, ph, ALU.mult, ALU.add, [tabC], [tabC])
            wrap(ph, tmpf)

        def wrap(ph, tmpf):
            ts("dve", tmpf, ph, math.pi, None, ALU.is_gt, None, [tabC], [tabC])
            stt(ph, tmpf, -TWO_PI, ph, ALU.mult, ALU.add, [tabC], [tabC])
            ts("dve", tmpf, ph, -math.pi, None, ALU.is_lt, None, [tabC], [tabC])
            stt(ph, tmpf, TWO_PI, ph, ALU.mult, ALU.add, [tabC], [tabC])

        def scr(name, shape, dt):
            t = A.alloc(name, shape, dt)
            t.r = tabC.r
            return t

        lam = scr("lam", [128, 3, 16], F32)
        dma("sp", lam.ap, lam_sm_d, [], [tabC], "lam")
        dt_s = scr("dt_s", [128, 16], F32)
        th_s = scr("th_s", [128, 16], F32)
        lre_s = scr("lre_s", [128, 16], F32)
        sv_i = scr("sv_i", [128, 129], I32)
        sv = scr("sv", [128, 129], F32)
        tmpf = scr("tmpf", [128, 16 * 129], F32)
        tmpi = scr("tmpi", [128, 16 * 129], I32)
        S.add("pool", lambda e: e.iota(sv_i.ap, pattern=[[1, 129]], base=0, channel_multiplier=0), [], R(tabC))
        cp("dve", sv.ap, sv_i.ap, [tabC], [tabC])
        ts("dve", lre_s.ap, lam.ap[:, 0, :], -1e-4, None, ALU.min, None, [tabC], [tabC])
        act(dt_s.ap, lam.ap[:, 2, :], AF.Exp, [tabC], [tabC])
        tt("dve", rmag.ap, lre_s.ap, dt_s.ap, ALU.mult, [tabC], [tabC, rmag])
        act(rmag.ap, rmag.ap, AF.Exp, [tabC, rmag], [tabC, rmag])
        tt("dve", th_s.ap, lam.ap[:, 1, :], dt_s.ap, ALU.mult, [tabC], [tabC])
        for j in range(16):
            ts("dve", tabD.ap[:, j, :], sv.ap, th_s.ap[:, j:j + 1], None, ALU.mult, None, [tabC], [tabC])
        phD = tabD.ap.rearrange("p a b -> p (a b)")
        phC = tabC.ap.rearrange("p a b -> p (a b)")
        range_reduce(phD, tmpf.ap, tmpi.ap, 16 * 129)
        ts("dve", phC, phD, math.pi / 2, None, ALU.add, None, [tabC], [tabC])
        wrap(phC, tmpf.ap)
        act(phD, phD, AF.Sin, [tabC], [tabC, tabD])
        act(phC, phC, AF.Sin, [tabC], [tabC, tabD])

        A.reset(mark_setup)
        lamc = scr("lamc", [128, 3, 1024], F32)
        dma("sp", lamc.ap, lam_c_d.rearrange("p a b c d -> p a (b c d)"), [], [tabC], "lamc")
        Bc = scr("Bc", [128, 2, 1024], F32)
        dma("sp", Bc.ap, Bc_d.rearrange("p a b c d -> p a (b c d)"), [], [tabC], "Bc")
        NQ = 1024
        zz = [scr("zz%d" % i, [128, NQ], F32) for i in range(10)]
        zi = scr("zzi", [128, NQ], I32)
        lre, dtc, mag, thc, cs, sn, den, nr, fre, fim = [z.ap for z in zz]
        ts("dve", lre, lamc.ap[:, 0, :], -1e-4, None, ALU.min, None, [tabC], [tabC])
        act(dtc, lamc.ap[:, 2, :], AF.Exp, [tabC], [tabC])
        tt("dve", mag, lre, dtc, ALU.mult, [tabC], [tabC])
        act(mag, mag, AF.Exp, [tabC], [tabC])
        tt("dve", thc, lamc.ap[:, 1, :], dtc, ALU.mult, [tabC], [tabC])
        cp("dve", sn, thc, [tabC], [tabC])
        range_reduce(sn, den, zi.ap, NQ)
        ts("dve", cs, sn, math.pi / 2, None, ALU.add, None, [tabC], [tabC])
        wrap(cs, den)
        act(sn, sn, AF.Sin, [tabC], [tabC])
        act(cs, cs, AF.Sin, [tabC], [tabC])
        tt("dve", cs, cs, mag, ALU.mult, [tabC], [tabC])
        tt("dve", sn, sn, mag, ALU.mult, [tabC], [tabC])
        tt("dve", den, lre, lre, ALU.mult, [tabC], [tabC])
        tt("dve", nr, lamc.ap[:, 1, :], lamc.ap[:, 1, :], ALU.mult, [tabC], [tabC])
        tt("dve", den, den, nr, ALU.add, [tabC], [tabC])
        S.add("dve", lambda e: e.reciprocal(out=den, in_=den), R(tabC), R(tabC))
        ts("dve", nr, cs, -1.0, None, ALU.add, None, [tabC], [tabC])
        tt("dve", fre, nr, lre, ALU.mult, [tabC], [tabC])
        tt("dve", fim, sn, lamc.ap[:, 1, :], ALU.mult, [tabC], [tabC])
        tt("dve", fre, fre, fim, ALU.add, [tabC], [tabC])
        tt("dve", fre, fre, den, ALU.mult, [tabC], [tabC])
        tt("dve", fim, sn, lre, ALU.mult, [tabC], [tabC])
        tt("dve", mag, nr, lamc.ap[:, 1, :], ALU.mult, [tabC], [tabC])
        tt("dve", fim, fim, mag, ALU.subtract, [tabC], [tabC])
        tt("dve", fim, fim, den, ALU.mult, [tabC], [tabC])
        Btf = Bt.ap.rearrange("p a b c -> p a (b c)")
        tt("dve", mag, fre, Bc.ap[:, 0, :], ALU.mult, [tabC], [tabC])
        tt("dve", thc, fim, Bc.ap[:, 1, :], ALU.mult, [tabC], [tabC])
        tt("dve", Btf[:, 0, :], mag, thc, ALU.subtract, [tabC], [tabC, Bt])
        tt("dve", mag, fre, Bc.ap[:, 1, :], ALU.mult, [tabC], [tabC])
        tt("dve", thc, fim, Bc.ap[:, 0, :], ALU.mult, [tabC], [tabC])
        tt("dve", Btf[:, 1, :], mag, thc, ALU.add, [tabC], [tabC, Bt])
        Ccf = scr("Ccf", [128, 2, 1024], F32)
        dma("sp", Ccf.ap, Cc_d.rearrange("p a b c -> p a (b c)"), [], [tabC], "Ccf")
        Ctf = Ct.ap.rearrange("p a b c -> p a (b c)")
        cp("dve", Ctf[:, 0, :], Ccf.ap[:, 0, :], [tabC], [tabC, Ct])
        ts("dve", Ctf[:, 1, :], Ccf.ap[:, 1, :], -1.0, None, ALU.mult, None, [tabC], [tabC, Ct])
        if "s5setup" in dbg:
            dump("tabC", tabC)
            dump("tabD", tabD)
            dump("rmag", rmag)
            dump("Bt", Bt)
            dump("fre", zz[8])
            dump("fim", zz[9])
        A.reset(mark_setup)

        wsT_b = A.alloc("wsT_b", [128, 8, 128], BF16)
        sgub = A.alloc("sgub", [128, 4, 128], F32)
        mark_sgu = A.mark()
        wsf = A.alloc("wsf", [128, 8, 128], F32)
        dma("sp", wsf.ap, wsT_d, [], [wsf], "wsf")
        for h in range(8):
            S.add("pool", lambda e, h=h: e.affine_select(out=wsf.ap[:, h, :], in_=wsf.ap[:, h, :], pattern=[[1, 128]],
                                                           compare_op=ALU.is_ge, fill=0.0, base=0, channel_multiplier=-1),
                  R(wsf), R(wsf))
        cp("pool", wsT_b.ap, wsf.ap, [wsf], [wsT_b])
        bsT = A.alloc("bsT", [128, 4, 128], F32)
        dma("sp", bsT.ap, bsT_d, [], [bsT], "bsT")
        pmix0 = psv(3, [128, 4, 128])
        for h in range(8):
            po = (h % 2) * 64
            mm(pmix0[po:po + 64, h // 2, :], ones_b.ap[:, 0:64], wsT_b.ap[:, h, :], True, True, [ones_b, wsT_b], [PSB[3]])
        for q in range(4):
            stt(sgub.ap[:, q, :], pmix0[:, q, :], lnbT.ap[:, q:q + 1], bsT.ap[:, q, :], ALU.mult, ALU.add,
                [PSB[3], lnbT, bsT], [sgub])
        if "sgusetup" in dbg:
            dump("sgub", sgub)
            dump("wsT_b", wsT_b)
        A.reset(mark_sgu)

        def wload(name, src_view, shape, key=None):
            t = A.alloc(name, shape, BF16)
            dma("pool", t.ap, src_view, [], [t], key or name)
            return t

        w_in_b = wload("w_in_b", w_in_d.rearrange("(kc p) n -> p kc n", p=128), [128, 8, 1536])
        w_gate_b = wload("w_gate_b", w_gate_d.rearrange("(kc p) n -> p kc n", p=128), [128, 8, 2048])
        w_glu_b = wload("w_glu_b", w_glu_d.rearrange("(kc p) n -> p kc n", p=128), [128, 4, 512])
        w_bra_b = wload("w_bra_b", w_bra_d.rearrange("(kc p) n -> p kc n", p=128), [128, 4, D])
        w_brb_b = wload("w_brb_b", w_brb_d.rearrange("(kc p) n -> p kc n", p=128), [128, 4, D])
        w_out_b = wload("w_out_b", w_out_d.rearrange("(kc p) n -> p kc n", p=128), [128, 8, D])
        NB2 = 2
        xt = [A.alloc("xt%d" % i, [128, D], F32) for i in range(NB2)]
        xsb = A.alloc("xsb", [128, D], BF16)
        ssq = A.alloc("ssq", [128, 1], F32)
        rstd = A.alloc("rstd", [128, 1], F32)
        xnT = A.alloc("xnT", [128, 8, 128], BF16)
        uf = A.alloc("uf", [128, 4, 128], F32)
        uT = A.alloc("uT", [128, 4, 128], BF16)
        guT = A.alloc("guT", [128, 4, 128], F32)
        gv = A.alloc("gv", [128, 512], F32)
        vst = A.alloc("vst", [128, 6], F32)
        vmv = A.alloc("vmv", [128, 2], F32)
        vrs = A.alloc("vrs", [128, 1], F32)
        vhat = A.alloc("vhat", [128, 512], BF16)
        mixT = A.alloc("mixT", [128, 4, 128], F32)
        ybT = A.alloc("ybT", [128, 4, 128], BF16)
        xtil = A.alloc("xtil", [128, 4, 2, 128], F32)
        s5a = A.alloc("s5a", [128, 4, 128], F32)
        s5b = A.alloc("s5b", [128, 4, 128], F32)
        gsc = A.alloc("gsc", [128, 4, 2, 128], F32)
        gp = A.alloc("gp", [128, 16, 2], F32)
        carry = A.alloc("carry", [128, 16, 2], F32)
        c4 = [A.alloc("c4_%d" % i, [128, 16], F32) for i in range(4)]
        hT = A.alloc("hT", [128, 4, 2, 128], BF16)
        ypre = A.alloc("ypre", [128, 4, 128], F32)
        ygf = A.alloc("ygf", [128, 4, 128], F32)
        ygT = A.alloc("ygT", [128, 4, 128], BF16)
        sg = A.alloc("sg", [128, 4, 128], F32)
        yaT = A.alloc("yaT", [128, 4, 128], BF16)
        gates = A.alloc("gates", [128, 16, 128], F32)
        mergedT = A.alloc("mergedT", [128, 8, 128], BF16)
        xn2 = A.alloc("xn2", [128, D], F32)
        xn2T = A.alloc("xn2T", [128, 8, 128], F32)
        gt1b = A.alloc("gt1b", [128, D], F32)
        A2b = A.alloc("A2b", [128, D], F32)
        sh2b = A.alloc("sh2b", [128, D], F32)
        ssq2 = A.alloc("ssq2", [128, 1], F32)
        rstd2 = A.alloc("rstd2", [128, 1], F32)
        lg = A.alloc("lg", [128, 36], F32)
        rt = {n: A.alloc("rt_" + n, [128, w_], F32) for n, w_ in
              [("gmax", 1), ("ngmax", 1), ("maskg", 4), ("eg", 4), ("sume", 1), ("pgs", 1), ("pen", 4), ("lem", 32),
               ("m1", 1), ("oh1", 32), ("lem2", 32), ("m2", 1), ("oh2", 32), ("dm", 1), ("e2", 1), ("p1", 1), ("p2", 1),
               ("rank", 32), ("slotv", 32), ("junk", 32), ("sl", 2), ("val", 32), ("vk", 2)]}
        ohb = A.alloc("ohb", [128, 32], BF16)

        def rms_rstd(src, ssq_t, rstd_t, junk_ap, junk_t):
            act(junk_ap, src.ap, AF.Square, [src], [junk_t, ssq_t], accum=ssq_t.ap)
            act(ssq_t.ap, ssq_t.ap, AF.Sqrt, [ssq_t, eps_rms], [ssq_t], bias=eps_rms.ap, scale=1.0 / D)
            S.add("dve", lambda e: e.reciprocal(out=rstd_t.ap, in_=ssq_t.ap), R(ssq_t), R(rstd_t))

        store_ops = []
        scat_ops = []
        for b in range(NSEQ):
            dma("sp", gt1b.ap, mod_d.ap[b:b + 1, 2 * D:3 * D].partition_broadcast(128), [mod_d], [gt1b], "gt1b")
            dma("sp", sh2b.ap, mod_d.ap[b:b + 1, 3 * D:4 * D].partition_broadcast(128), [mod_d], [sh2b], "sh2b")
            dma("sp", A2b.ap, mod_d.ap[b:b + 1, 4 * D:5 * D].partition_broadcast(128), [mod_d], [A2b], "A2b")
            dma("sp", xn2.ap, g2_d.partition_broadcast(128), [], [xn2], "g2tmp")
            stt(A2b.ap, A2b.ap, 1.0, xn2.ap, ALU.add, ALU.mult, [A2b, xn2], [A2b])
            memset("dve", carry.ap, 0.0, [carry])
            for tau in range(NT):
                i = b * NT + tau
                X = xt[i % NB2]
                tok0 = i * 128
                dbg_here = dbg.get("tile") == i
                dma("sp", X.ap, x_d[tok0:tok0 + 128, :], [], [X], "xt%d" % (i % NB2))
                rms_rstd(X, ssq, rstd, xsb.ap, xsb)
                act(xsb.ap, X.ap, AF.Copy, [X, rstd], [xsb], scale=rstd.ap[:, 0:1])
                pX = psv(0, [128, 8, 128], BF16)
                for kc in range(8):
                    tr(pX[:, kc, :], xsb.ap[:, kc * 128:(kc + 1) * 128], ident_b.ap, [xsb, ident_b], [PSB[0]])
                for kc in range(8):
                    act(xnT.ap[:, kc, :], pX[:, kc, :], AF.Identity, [PSB[0], A1T, sh1T], [xnT],
                        bias=sh1T.ap[:, kc, b:b + 1], scale=A1T.ap[:, kc, b:b + 1])
                if dbg_here:
                    dump("xnT", xnT)
                pZa = psv(1, [128, 4, 128])
                pZu = psv(2, [128, 4, 128])
                pV = psv(3, [128, 512])
                for m in range(4):
                    for kc in range(8):
                        mm(pZa[:, m, :], w_in_b.ap[:, kc, m * 128:(m + 1) * 128], xnT.ap[:, kc, :], kc == 0, kc == 7,
                           [w_in_b, xnT], [PSB[1]])
                for m in range(4):
                    for kc in range(8):
                        mm(pZu[:, m, :], w_in_b.ap[:, kc, 512 + m * 128:512 + (m + 1) * 128], xnT.ap[:, kc, :], kc == 0, kc == 7,
                           [w_in_b, xnT], [PSB[2]])
                for kc in range(8):
                    mm(pV, xnT.ap[:, kc, :], w_in_b.ap[:, kc, 1024:1536], kc == 0, kc == 7, [w_in_b, xnT], [PSB[3]])
                cp("act", uf.ap, pZa, [PSB[1]], [uf])
                cp("pool", uT.ap, uf.ap, [uf], [uT])
                act(guT.ap, pZu, AF.Gelu_apprx_tanh, [PSB[2]], [guT])
                act(gv.ap, pV, AF.Gelu_apprx_tanh, [PSB[3]], [gv])
                S.add("dve", lambda e: e.bn_stats(out=vst.ap, in_=gv.ap), R(gv), R(vst))
                S.add("dve", lambda e: e.bn_aggr(out=vmv.ap, in_=vst.ap), R(vst), R(vmv))
                act(vrs.ap, vmv.ap[:, 1:2], AF.Sqrt, [vmv, eps_ln], [vrs], bias=eps_ln.ap, scale=1.0)
                S.add("dve", lambda e: e.reciprocal(out=vrs.ap, in_=vrs.ap), R(vrs), R(vrs))
                ts("dve", vhat.ap, gv.ap, vmv.ap[:, 0:1], vrs.ap[:, 0:1], ALU.subtract, ALU.mult, [gv, vmv, vrs], [vhat])
                pMix = psv(3, [128, 4, 128])
                for h in range(8):
                    po = (h % 2) * 64
                    mm(pMix[po:po + 64, h // 2, :], vhat.ap[:, h * 64:(h + 1) * 64], wsT_b.ap[:, h, :], True, True,
                       [vhat, wsT_b], [PSB[3]])
                for q in range(4):
                    stt(mixT.ap[:, q, :], pMix[:, q, :], lngT.ap[:, q:q + 1], sgub.ap[:, q, :], ALU.mult, ALU.add,
                        [PSB[3], lngT, sgub], [mixT])
                tt("pool", ybT.ap, guT.ap, mixT.ap, ALU.mult, [guT, mixT], [ybT])
                if dbg_here:
                    dump("uf", uf)
                    dump("ybT", ybT)
                pYs = psv(6, [128, 4, 128])
                for q in range(4):
                    pXs = psv2(4, [128, 4, 2, 128])
                    for jj in range(4):
                        j = 4 * q + jj
                        ro = 64 * (jj // 2)
                        for part in range(2):
                            mm(pXs[:, jj, part, :], Bt.ap[ro:ro + 64, part, 2 * q + jj % 2, :], uT.ap[ro:ro + 64, q, :], True, True,
                               [Bt, uT], [PSB[4], PSB[5]])
                    tc_ = tabC.ap[:, 4 * q:4 * q + 4, 0:128]
                    td_ = tabD.ap[:, 4 * q:4 * q + 4, 0:128]
                    tt("dve", s5a.ap, pXs[:, :, 0, :], tc_, ALU.mult, [PSB[4], PSB[5], tabC], [s5a])
                    tt("dve", s5b.ap, pXs[:, :, 1, :], td_, ALU.mult, [PSB[4], PSB[5], tabD], [s5b])
                    tt("dve", xtil.ap[:, :, 0, :], s5a.ap, s5b.ap, ALU.add, [s5a, s5b], [xtil])
                    tt("dve", s5a.ap, pXs[:, :, 1, :], tc_, ALU.mult, [PSB[4], PSB[5], tabC, xtil], [s5a])
                    tt("dve", s5b.ap, pXs[:, :, 0, :], td_, ALU.mult, [PSB[4], PSB[5], tabD, xtil], [s5b])
                    tt("dve", xtil.ap[:, :, 1, :], s5a.ap, s5b.ap, ALU.subtract, [s5a, s5b], [xtil])
                    for jj in range(4):
                        j = 4 * q + jj
                        for part in range(2):
                            S.add("dve", lambda e, jj=jj, j=j, part=part: e.tensor_tensor_scan(
                                out=gsc.ap[:, jj, part, :], data0=rmag.ap[:, j:j + 1].to_broadcast([128, 128]),
                                data1=xtil.ap[:, jj, part, :], initial=carry.ap[:, j, part:part + 1],
                                op0=ALU.mult, op1=ALU.add), R(rmag, xtil, carry), R(gsc))
                    cp("dve", gp.ap[:, 4 * q:4 * q + 4, :], gsc.ap[:, :, :, 127], [gsc], [gp])
                    tt("dve", s5a.ap, gsc.ap[:, :, 0, :], tc_, ALU.mult, [gsc, tabC], [s5a])
                    tt("dve", s5b.ap, gsc.ap[:, :, 1, :], td_, ALU.mult, [gsc, tabD], [s5b])
                    tt("dve", hT.ap[:, :, 0, :], s5a.ap, s5b.ap, ALU.subtract, [s5a, s5b], [hT])
                    tt("dve", s5a.ap, gsc.ap[:, :, 1, :], tc_, ALU.mult, [gsc, tabC, hT], [s5a])
                    tt("dve", s5b.ap, gsc.ap[:, :, 0, :], td_, ALU.mult, [gsc, tabD, hT], [s5b])
                    tt("dve", hT.ap[:, :, 1, :], s5a.ap, s5b.ap, ALU.add, [s5a, s5b], [hT])
                    if dbg_here and q == 0:
                        dump("xtil0", xtil)
                        dump("gsc0", gsc)
                    for jj in range(4):
                        j = 4 * q + jj
                        ro = 64 * (jj // 2)
                        for part in range(2):
                            mm(pYs[ro:ro + 64, q, :], Ct.ap[:, part, j, :], hT.ap[:, jj, part, :],
                               jj % 2 == 0 and part == 0, jj % 2 == 1 and part == 1, [Ct, hT], [PSB[6]])
                    stt(ypre.ap[:, q, :], uf.ap[:, q, :], dT.ap[:, q:q + 1], pYs[:, q, :], ALU.mult, ALU.add,
                        [uf, dT, PSB[6]], [ypre])
                c128 = tabC.ap[:, :, 128]
                d128 = tabD.ap[:, :, 128]
                tt("dve", c4[0].ap, gp.ap[:, :, 0], c128, ALU.mult, [gp, tabC], [c4[0]])
                tt("dve", c4[1].ap, gp.ap[:, :, 1], d128, ALU.mult, [gp, tabD], [c4[1]])
                tt("dve", c4[2].ap, gp.ap[:, :, 1], c128, ALU.mult, [gp, tabC], [c4[2]])
                tt("dve", c4[3].ap, gp.ap[:, :, 0], d128, ALU.mult, [gp, tabD], [c4[3]])
                tt("dve", carry.ap[:, :, 0], c4[0].ap, c4[1].ap, ALU.subtract, [c4[0], c4[1]], [carry])
                tt("dve", carry.ap[:, :, 1], c4[2].ap, c4[3].ap, ALU.add, [c4[2], c4[3]], [carry])
                act(ygf.ap, ypre.ap, AF.Gelu_apprx_tanh, [ypre], [ygf])
                cp("pool", ygT.ap, ygf.ap, [ygf], [ygT])
                pG = psv(1, [128, 4, 128])
                for m in range(4):
                    for kc in range(4):
                        mm(pG[:, m, :], w_glu_b.ap[:, kc, m * 128:(m + 1) * 128], ygT.ap[:, kc, :], kc == 0, kc == 3,
                           [w_glu_b, ygT], [PSB[1]])
                for m in range(4):
                    act(sg.ap[:, m, :], pG[:, m, :], AF.Sigmoid, [PSB[1], b_gluT], [sg], bias=b_gluT.ap[:, m:m + 1], scale=1.0)
                tt("pool", yaT.ap, ygf.ap, sg.ap, ALU.mult, [ygf, sg], [yaT])
                if dbg_here:
                    dump("ypre", ypre)
                    dump("yaT", yaT)
                for mg in range(4):
                    bk = 2 if mg % 2 == 0 else 7
                    pGt = psv(bk, [128, 4, 128])
                    for mm_ in range(4):
                        m = mg * 4 + mm_
                        for kc in range(8):
                            mm(pGt[:, mm_, :], w_gate_b.ap[:, kc, m * 128:(m + 1) * 128], xnT.ap[:, kc, :], kc == 0, kc == 7,
                               [w_gate_b, xnT], [PSB[bk]])
                    for mm_ in range(4):
                        m = mg * 4 + mm_
                        act(gates.ap[:, m, :], pGt[:, mm_, :], AF.Sigmoid, [PSB[bk], b_gateT], [gates],
                            bias=b_gateT.ap[:, m:m + 1], scale=1.0)
                for half in range(2):
                    pA = psv(4, [128, 4, 128])
                    pB = psv(5, [128, 4, 128])
                    for mm_ in range(4):
                        m = half * 4 + mm_
                        for kc in range(4):
                            mm(pA[:, mm_, :], w_bra_b.ap[:, kc, m * 128:(m + 1) * 128], yaT.ap[:, kc, :], kc == 0, kc == 3,
                               [w_bra_b, yaT], [PSB[4]])
                    for mm_ in range(4):
                        m = half * 4 + mm_
                        for kc in range(4):
                            mm(pB[:, mm_, :], w_brb_b.ap[:, kc, m * 128:(m + 1) * 128], ybT.ap[:, kc, :], kc == 0, kc == 3,
                               [w_brb_b, ybT], [PSB[5]])
                    ga = gates.ap[:, half * 4:half * 4 + 4, :]
                    gb = gates.ap[:, 8 + half * 4:8 + half * 4 + 4, :]
                    tt("dve", ga, ga, pA, ALU.mult, [gates, PSB[4]], [gates])
                    tt("dve", gb, gb, pB, ALU.mult, [gates, PSB[5]], [gates])
                    tt("pool", mergedT.ap[:, half * 4:half * 4 + 4, :], ga, gb, ALU.add, [gates], [mergedT])
                if dbg_here:
                    dump("mergedT", mergedT)
                for half in range(2):
                    bk = 6 + half
                    for kc in range(8):
                        mm(PSB[bk].ap, mergedT.ap[:, kc, :], w_out_b.ap[:, kc, half * 512:(half + 1) * 512], kc == 0, kc == 7,
                           [mergedT, w_out_b], [PSB[bk]])
                    sl = slice(half * 512, (half + 1) * 512)
                    tt("dve", xn2.ap[:, sl], PSB[bk].ap, gt1b.ap[:, sl], ALU.mult, [PSB[bk], gt1b], [xn2])
                tt("pool", X.ap, X.ap, xn2.ap, ALU.add, [X, xn2], [X])
                store_ops.append(dma("sp", H_d[tok0:tok0 + 128, :], X.ap, [X], [r_H], "hst%d" % (i % NB2)))
                rms_rstd(X, ssq2, rstd2, xsb.ap, xsb)
                stt(xn2.ap, X.ap, rstd2.ap[:, 0:1], A2b.ap, ALU.mult, ALU.mult, [X, rstd2, A2b], [xn2])
                tt("pool", xn2.ap, xn2.ap, sh2b.ap, ALU.add, [xn2, sh2b], [xn2])
                if dbg_here:
                    dump("h", X)
                    dump("xn2", xn2)
                pX2 = psv2(0, [128, 8, 128])
                for kc in range(8):
                    tr(pX2[:, kc, :], xn2.ap[:, kc * 128:(kc + 1) * 128], ident_f.ap, [xn2, ident_f], [PSB[0], PSB[1]])
                cp("act", xn2T.ap, pX2, [PSB[0], PSB[1]], [xn2T])
                pL = psv(2, [128, 36])
                for kc in range(8):
                    mm(pL, xn2T.ap[:, kc, :], w_r.ap[:, kc, :], kc == 0, kc == 7, [xn2T, w_r], [PSB[2]])
                tt("dve", lg.ap, pL, b_r.ap, ALU.add, [PSB[2], b_r], [lg])
                r_ = rt
                BIG = 1.0e9
                S.add("dve", lambda e: e.tensor_reduce(out=r_["gmax"].ap, in_=lg.ap[:, 0:4], axis=AX.X, op=ALU.max), R(lg), R(r_["gmax"]))
                ts("dve", r_["maskg"].ap, lg.ap[:, 0:4], r_["gmax"].ap[:, 0:1], None, ALU.is_equal, None, [lg, r_["gmax"]], [r_["maskg"]])
                ts("dve", r_["ngmax"].ap, r_["gmax"].ap, -1.0, None, ALU.mult, None, [r_["gmax"]], [r_["ngmax"]])
                act(r_["eg"].ap, lg.ap[:, 0:4], AF.Exp, [lg, r_["ngmax"]], [r_["eg"], r_["sume"]], bias=r_["ngmax"].ap[:, 0:1], scale=1.0,
                    accum=r_["sume"].ap)
                S.add("dve", lambda e: e.reciprocal(out=r_["pgs"].ap, in_=r_["sume"].ap), R(r_["sume"]), R(r_["pgs"]))
                ts("dve", r_["pen"].ap, r_["maskg"].ap, BIG, -BIG, ALU.mult, ALU.add, [r_["maskg"]], [r_["pen"]])
                for g in range(4):
                    ts("dve", r_["lem"].ap[:, g * 8:(g + 1) * 8], lg.ap[:, 4 + g * 8:4 + (g + 1) * 8], r_["pen"].ap[:, g:g + 1], None,
                       ALU.add, None, [lg, r_["pen"]], [r_["lem"]])
                S.add("dve", lambda e: e.tensor_reduce(out=r_["m1"].ap, in_=r_["lem"].ap, axis=AX.X, op=ALU.max), R(r_["lem"]), R(r_["m1"]))
                ts("dve", r_["oh1"].ap, r_["lem"].ap, r_["m1"].ap[:, 0:1], None, ALU.is_equal, None, [r_["lem"], r_["m1"]], [r_["oh1"]])
                stt(r_["lem2"].ap, r_["oh1"].ap, -BIG, r_["lem"].ap, ALU.mult, ALU.add, [r_["oh1"], r_["lem"]], [r_["lem2"]])
                S.add("dve", lambda e: e.tensor_reduce(out=r_["m2"].ap, in_=r_["lem2"].ap, axis=AX.X, op=ALU.max), R(r_["lem2"]), R(r_["m2"]))
                ts("dve", r_["oh2"].ap, r_["lem2"].ap, r_["m2"].ap[:, 0:1], None, ALU.is_equal, None, [r_["lem2"], r_["m2"]], [r_["oh2"]])
                tt("dve", r_["dm"].ap, r_["m2"].ap, r_["m1"].ap, ALU.subtract, [r_["m1"], r_["m2"]], [r_["dm"]])
                act(r_["e2"].ap, r_["dm"].ap, AF.Exp, [r_["dm"]], [r_["e2"]])
                ts("dve", r_["p1"].ap, r_["e2"].ap, 1.0, None, ALU.add, None, [r_["e2"]], [r_["p1"]])
                S.add("dve", lambda e: e.reciprocal(out=r_["p1"].ap, in_=r_["p1"].ap), R(r_["p1"]), R(r_["p1"]))
                tt("dve", r_["p2"].ap, r_["e2"].ap, r_["p1"].ap, ALU.mult, [r_["e2"], r_["p1"]], [r_["p2"]])
                tt("dve", ohb.ap, r_["oh1"].ap, r_["oh2"].ap, ALU.add, [r_["oh1"], r_["oh2"]], [ohb])
                pR = psv(2, [128, 128])
                mm(pR[:, 64:96], tri_b.ap, ohb.ap, True, True, [tri_b, ohb], [PSB[2]])
                mm(pR[:, 96:128], ones_b.ap, ohb.ap, True, True, [ones_b, ohb], [PSB[2]])
                tt("dve", r_["rank"].ap, pR[:, 64:96], base.ap, ALU.add, [PSB[2], base], [r_["rank"]])
                tt("dve", base.ap, pR[:, 96:128], base.ap, ALU.add, [PSB[2], base, r_["rank"]], [base])
                ts("dve", r_["val"].ap, r_["rank"].ap, float(CAP), None, ALU.is_lt, None, [r_["rank"]], [r_["val"]])
                tt("dve", r_["slotv"].ap, r_["rank"].ap, ecap.ap, ALU.add, [r_["rank"], ecap], [r_["slotv"]])
                stt(r_["slotv"].ap, r_["val"].ap, -4.0e6, r_["slotv"].ap, ALU.mult, ALU.add, [r_["val"], r_["slotv"]], [r_["slotv"]])
                ts("dve", r_["slotv"].ap, r_["slotv"].ap, 4.0e6, None, ALU.add, None, [r_["slotv"]], [r_["slotv"]])
                for k, ohn in enumerate(("oh1", "oh2")):
                    tt("dve", r_["junk"].ap, r_[ohn].ap, r_["slotv"].ap, ALU.mult, [r_[ohn], r_["slotv"]], [r_["junk"]])
                    S.add("dve", lambda e, k=k: e.tensor_reduce(out=r_["sl"].ap[:, k:k + 1], in_=r_["junk"].ap, axis=AX.X, op=ALU.add),
                          R(r_["junk"]), R(r_["sl"]))
                    tt("dve", r_["junk"].ap, r_[ohn].ap, r_["val"].ap, ALU.mult, [r_[ohn], r_["val"]], [r_["junk"]])
                    S.add("dve", lambda e, k=k: e.tensor_reduce(out=r_["vk"].ap[:, k:k + 1], in_=r_["junk"].ap, axis=AX.X, op=ALU.add),
                          R(r_["junk"]), R(r_["vk"]))
                cp("dve", sloti.ap[:, i, :], r_["sl"].ap, [r_["sl"]], [sloti])
                stt(wgt.ap[:, i, 0:1], r_["p1"].ap, r_["pgs"].ap[:, 0:1], r_["vk"].ap[:, 0:1], ALU.mult, ALU.mult,
                    [r_["p1"], r_["pgs"], r_["vk"]], [wgt])
                stt(wgt.ap[:, i, 1:2], r_["p2"].ap, r_["pgs"].ap[:, 0:1], r_["vk"].ap[:, 1:2], ALU.mult, ALU.mult,
                    [r_["p2"], r_["pgs"], r_["vk"]], [wgt])
                if dbg_here:
                    dump("lg", lg)
                    dump("rank", r_["rank"])
                for k in range(2):
                    scat_ops.append(S.add("pool", lambda e, i=i, k=k: e.indirect_dma_start(
                        out=X_d, out_offset=bass.IndirectOffsetOnAxis(ap=sloti.ap[:, i, k:k + 1], axis=0),
                        in_=xn2.ap, in_offset=None, bounds_check=breg(e, NSLOT - 1), oob_is_err=False),
                        R(xn2, sloti), [r_X], dma_key="scat"))
        if "route" in dbg:
            dump("wgt", wgt)
            dump("sloti", sloti)
        if stop_after == "A":
            S.finish(final_ops + store_ops + scat_ops)
            S.emit(st)
            return nc, dbg_outs

        S.barrier(dma_ops=[scat_ops[-1], store_ops[-1]] + ([store_ops[-2]] if len(store_ops) > 1 else []))
        A.reset(mark_persist)
        w1b = [A.alloc("w1b%d" % i, [128, 8, 512], BF16) for i in range(2)]
        w3b = [A.alloc("w3b%d" % i, [128, 8, 512], BF16) for i in range(2)]
        w2b = [A.alloc("w2b%d" % i, [128, 4, D], BF16) for i in range(2)]
        Xblk = [A.alloc("Xblk%d" % i, [128, D], BF16) for i in range(2)]
        XT = A.alloc("XT", [128, 8, CAP], BF16)
        hidT = A.alloc("hidT", [128, 4, CAP], BF16)
        s1 = [A.alloc("s1_%d" % i, [128, 512], F32) for i in range(2)]
        Yblk = [A.alloc("Yblk%d" % i, [128, D], F32) for i in range(2)]
        NH = (CAP + 511) // 512
        HW_ = CAP // NH
        ystore = []
        nblk_global = 0
        for e_ in range(32):
            sl_ = e_ % 2
            dma("pool", w1b[sl_].ap, w1_d[e_].rearrange("(kc p) n -> p kc n", p=128), [], [w1b[sl_]], "w1b%d" % sl_)
            dma("pool", w3b[sl_].ap, w3_d[e_].rearrange("(kc p) n -> p kc n", p=128), [], [w3b[sl_]], "w3b%d" % sl_)
            dma("pool", w2b[sl_].ap, w2_d[e_].rearrange("(kc p) n -> p kc n", p=128), [], [w2b[sl_]], "w2b%d" % sl_)
            for blk in range(NBLK):
                xb_ = Xblk[nblk_global % 2]
                r0 = e_ * CAP + blk * 128
                dma("sp", xb_.ap, X_d[r0:r0 + 128, :], [r_X], [xb_], "xblk%d" % (nblk_global % 2))
                bk = nblk_global % 2
                pXT = psv(bk, [128, 8, 128], BF16)
                for kc in range(8):
                    tr(pXT[:, kc, :], xb_.ap[:, kc * 128:(kc + 1) * 128], ident_b.ap, [xb_, ident_b], [PSB[bk]])
                cp("act" if blk % 2 == 0 else "dve", XT.ap[:, :, blk * 128:(blk + 1) * 128], pXT, [PSB[bk]], [XT])
                nblk_global += 1
            for m in range(4):
                for nh in range(NH):
                    cs_ = slice(nh * HW_, (nh + 1) * HW_)
                    b1 = 2 + (m * NH + nh) % 2
                    b3 = 4 + (m * NH + nh) % 2
                    p1_ = PSB[b1].ap[:, 0:HW_]
                    p3_ = PSB[b3].ap[:, 0:HW_]
                    for kc in range(8):
                        mm(p1_, w1b[sl_].ap[:, kc, m * 128:(m + 1) * 128], XT.ap[:, kc, cs_], kc == 0, kc == 7,
                           [w1b[sl_], XT], [PSB[b1]])
                    for kc in range(8):
                        mm(p3_, w3b[sl_].ap[:, kc, m * 128:(m + 1) * 128], XT.ap[:, kc, cs_], kc == 0, kc == 7,
                           [w3b[sl_], XT], [PSB[b3]])
                    s1_ = s1[(m * NH + nh) % 2]
                    act(s1_.ap[:, 0:HW_], p1_, AF.Silu, [PSB[b1]], [s1_])
                    tt("dve", hidT.ap[:, m, cs_], s1_.ap[:, 0:HW_], p3_, ALU.mult, [s1_, PSB[b3]], [hidT])
            for blk in range(NBLK):
                yb_ = Yblk[blk % 2]
                for half in range(2):
                    bk = 6 + half
                    for kc in range(4):
                        mm(PSB[bk].ap, hidT.ap[:, kc, blk * 128:(blk + 1) * 128], w2b[sl_].ap[:, kc, half * 512:(half + 1) * 512],
                           kc == 0, kc == 3, [hidT, w2b[sl_]], [PSB[bk]])
                    cp("act" if half == 0 else "dve", yb_.ap[:, half * 512:(half + 1) * 512], PSB[bk].ap, [PSB[bk]], [yb_])
                r0 = e_ * CAP + blk * 128
                ystore.append(dma("sp", Y_d[r0:r0 + 128, :], yb_.ap, [yb_], [r_Y], "yst%d" % (blk % 2)))
        if stop_after == "B":
            S.finish(final_ops + ystore[-2:])
            S.emit(st)
            return nc, dbg_outs

        S.barrier(dma_ops=ystore[-2:])
        A.reset(mark_persist)
        Hc = [A.alloc("Hc%d" % i, [128, D], F32) for i in range(2)]
        Y0 = [A.alloc("Y0_%d" % i, [128, D], F32) for i in range(2)]
        Y1 = [A.alloc("Y1_%d" % i, [128, D], F32) for i in range(2)]
        acc = A.alloc("acc", [128, D], F32)
        ob = [A.alloc("ob%d" % i, [128, D], F32) for i in range(2)]
        gt2b = A.alloc("gt2b", [128, D], F32)
        gfb = A.alloc("gfb", [128, D], F32)
        jk = A.alloc("jk", [128, D], BF16)
        ssq3 = A.alloc("ssq3", [128, 1], F32)
        rstd3 = A.alloc("rstd3", [128, 1], F32)
        dma("sp", gfb.ap, gf_d.partition_broadcast(128), [], [gfb], "gfb")
        for b in range(NSEQ):
            dma("sp", gt2b.ap, mod_d.ap[b:b + 1, 5 * D:6 * D].partition_broadcast(128), [mod_d], [gt2b], "gt2b")
            for tau in range(NT):
                i = b * NT + tau
                s_ = i % 2
                tok0 = i * 128
                memset("pool", Y0[s_].ap, 0.0, [Y0[s_]])
                memset("pool", Y1[s_].ap, 0.0, [Y1[s_]])
                dma("sp", Hc[s_].ap, H_d[tok0:tok0 + 128, :], [r_H], [Hc[s_]], "hc%d" % s_)
                for k, Yk in enumerate((Y0[s_], Y1[s_])):
                    S.add("pool", lambda e, i=i, k=k, Yk=Yk: e.indirect_dma_start(
                        out=Yk.ap, out_offset=None, in_=Y_d, in_offset=bass.IndirectOffsetOnAxis(ap=sloti.ap[:, i, k:k + 1], axis=0),
                        bounds_check=breg(e, NSLOT - 1), oob_is_err=False), R(r_Y, sloti), R(Yk), dma_key="gath%d_%d" % (k, s_))
                ts("dve", acc.ap, Y0[s_].ap, wgt.ap[:, i, 0:1], None, ALU.mult, None, [Y0[s_], wgt], [acc])
                stt(acc.ap, Y1[s_].ap, wgt.ap[:, i, 1:2], acc.ap, ALU.mult, ALU.add, [Y1[s_], wgt, acc], [acc])
                tt("dve", acc.ap, acc.ap, gt2b.ap, ALU.mult, [acc, gt2b], [acc])
                tt("pool", acc.ap, acc.ap, Hc[s_].ap, ALU.add, [acc, Hc[s_]], [acc])
                rms_rstd(acc, ssq3, rstd3, jk.ap, jk)
                stt(ob[s_].ap, acc.ap, rstd3.ap[:, 0:1], gfb.ap, ALU.mult, ALU.mult, [acc, rstd3, gfb], [ob[s_]])
                final_ops.append(dma("sp", out_d[tok0:tok0 + 128, :], ob[s_].ap, [ob[s_]], [], "ost%d" % s_))
        S.finish(final_ops)
        S.emit(st)
    return nc, dbg_outs


def prep_shared(inp):
    f = np.float32
    g = {}
    L = 0
    g["w_ada"] = np.ascontiguousarray(inp["w_ada"][L], f)
    g["g1T"] = np.ascontiguousarray(inp["norm1_g"][L].reshape(8, 128).T, f)
    g["w_in"] = np.ascontiguousarray(inp["w_in"][L], f)
    g["w_gate"] = np.ascontiguousarray(inp["w_gate"][L], f)
    g["b_gateT"] = np.ascontiguousarray(inp["b_gate"][L].reshape(16, 128).T, f)
    a_re, a_im, ls = inp["ssm_a_re"][L], inp["ssm_a_im"][L], inp["ssm_log_step"][L]
    def sm(v):
        return v.reshape(16, 2, 64).transpose(1, 2, 0).reshape(128, 16)
    lsx = np.repeat(ls[:, None], 64, axis=1)
    g["lam_sm"] = np.ascontiguousarray(np.stack([sm(a_re), sm(a_im), sm(lsx)], axis=1), f)
    def cl(v):
        s = sm(v)
        o = np.zeros((128, 4, 2, 128), f)
        for p in range(128):
            for q in range(4):
                for jo in range(2):
                    o[p, q, jo, :] = s[:, 4 * q + 2 * (p // 64) + jo]
        return o
    g["lam_c"] = np.ascontiguousarray(np.stack([cl(a_re), cl(a_im), cl(lsx)], axis=1), f)
    Bc = np.zeros((128, 2, 4, 2, 128), f)
    for pi, Bsrc in enumerate((inp["ssm_b_re"][L], inp["ssm_b_im"][L])):
        for gi in range(32):
            j = gi // 2
            p0 = 32 * (j % 4) + 16 * (gi % 2)
            n0 = 64 * (gi % 2)
            Bc[p0:p0 + 16, pi, j // 4, j % 2, n0:n0 + 64] = Bsrc[gi].T
    g["Bc"] = Bc
    Cc = np.zeros((128, 2, 16, 64), f)
    for pi, Csrc in enumerate((inp["ssm_c_re"][L], inp["ssm_c_im"][L])):
        for gi in range(32):
            j = gi // 2
            n0 = 64 * (gi % 2)
            c0 = 32 * (j % 2) + 16 * (gi % 2)
            Cc[n0:n0 + 64, pi, j, c0:c0 + 16] = Csrc[gi].T
    g["Cc"] = Cc
    g["dT"] = np.ascontiguousarray(inp["ssm_d"][L].reshape(4, 128).T, f)
    g["w_glu"] = np.ascontiguousarray(inp["w_glu"][L], f)
    g["b_gluT"] = np.ascontiguousarray(inp["b_glu"][L].reshape(4, 128).T, f)
    g["wsT"] = np.ascontiguousarray(inp["sgu_w"][L].transpose(2, 0, 1), f)
    g["lngT"] = np.ascontiguousarray(inp["sgu_ln_g"][L].reshape(4, 128).T, f)
    g["lnbT"] = np.ascontiguousarray(inp["sgu_ln_b"][L].reshape(4, 128).T, f)
    bs = inp["sgu_b"][L]
    bsT = np.zeros((128, 4, 128), f)
    for q in range(4):
        bsT[0:64, q, :] = bs[2 * q][None, :]
        bsT[64:128, q, :] = bs[2 * q + 1][None, :]
    g["bsT"] = bsT
    g["w_bra"] = np.ascontiguousarray(inp["w_branch_a"][L], f)
    g["w_brb"] = np.ascontiguousarray(inp["w_branch_b"][L], f)
    g["w_out"] = np.ascontiguousarray(inp["w_out"][L], f)
    g["g2"] = np.ascontiguousarray(inp["norm2_g"][L].reshape(1, D), f)
    wr = np.concatenate([inp["w_router_group"][L], inp["w_router_expert"][L].transpose(1, 0, 2).reshape(D, 32)], axis=1)
    g["w_r"] = np.ascontiguousarray(wr.reshape(8, 128, 36).transpose(1, 0, 2), f)
    g["b_r"] = np.ascontiguousarray(np.concatenate([inp["b_router_group"][L], inp["b_router_expert"][L].reshape(32)]).reshape(1, 36), f)
    g["w1"] = np.ascontiguousarray(inp["w1"][L], f)
    g["w3"] = np.ascontiguousarray(inp["w3"][L], f)
    g["w2"] = np.ascontiguousarray(inp["w2"][L], f)
    g["gf"] = np.ascontiguousarray(inp["norm_f_g"].reshape(1, D), f)
    return g


def prep_core(inp, shared, b0, nseq):
    m = dict(shared)
    xs = inp["x"][b0:b0 + nseq]
    m["x"] = np.ascontiguousarray(xs.reshape(-1, D), np.float32)
    c = inp["c"][b0:b0 + nseq]
    m["cT"] = np.ascontiguousarray(c.reshape(nseq, 8, 128).transpose(2, 1, 0), np.float32)
    m["b_ada_rep"] = np.ascontiguousarray(np.repeat(inp["b_ada"][0][None, :], nseq, axis=0), np.float32)
    return m


_CACHE = {}


def kernel(**inputs):
    inp = {k: np.asarray(v) for k, v in inputs.items()}
    B, SEQ = inp["x"].shape[0], inp["x"].shape[1]
    nseq = B // N_CORES
    key = (nseq, SEQ)
    if key not in _CACHE:
        _CACHE[key] = build(NSEQ=nseq, SEQ=SEQ, CAP=768)[0]
    nc = _CACHE[key]
    shared = prep_shared(inp)
    in_maps = [prep_core(inp, shared, c * nseq, nseq) for c in range(N_CORES)]
    res = run_bass_kernel_spmd(nc, in_maps, core_ids=list(range(N_CORES)))
    outs = [np.asarray(r["out"]).reshape(nseq, SEQ, D) for r in res.results]
    return np.concatenate(outs, axis=0).astype(np.float32)
```

```python
import math
from contextlib import ExitStack
import numpy as np
import concourse.bass as bass
import concourse.mybir as mybir
from concourse.bass_utils import run_bass_kernel_spmd

F32 = mybir.dt.float32
BF16 = mybir.dt.bfloat16
I32 = mybir.dt.int32
U8 = mybir.dt.uint8
AF = mybir.ActivationFunctionType
ALU = mybir.AluOpType
AX = mybir.AxisListType

ENGS = ("pe", "act", "dve", "pool", "sp")
N_CORES = 8
D = 1024
TWO_PI = 2.0 * math.pi


class Res:
    __slots__ = ("name", "lastw", "readers")

    def __init__(self, name):
        self.name = name
        self.lastw = None
        self.readers = []


class Op:
    __slots__ = ("eng", "fn", "deps", "dma_key", "dma_val", "signal", "sig_idx")

    def __init__(self, eng, fn, dma_key):
        self.eng = eng
        self.fn = fn
        self.deps = []
        self.dma_key = dma_key
        self.dma_val = None
        self.signal = False
        self.sig_idx = None


class Sched:
    def __init__(self, nc):
        self.nc = nc
        self.ops = {e: [] for e in ENGS}
        self.dma_cnt = {}
        self.finals = []
        self.pending_barrier = {}

    def add(self, eng, fn, reads=(), writes=(), dma_key=None):
        op = Op(eng, fn, dma_key)
        deps = []
        for r in reads:
            if r.lastw is not None:
                deps.append(r.lastw)
        for w in writes:
            if w.lastw is not None:
                deps.append(w.lastw)
            deps.extend(w.readers)
        if eng in self.pending_barrier:
            deps.extend(self.pending_barrier.pop(eng))
        seen = set()
        for d in deps:
            if d is op or id(d) in seen:
                continue
            seen.add(id(d))
            if d.eng == "pe" and eng == "pe" and d.dma_key is None and dma_key is None:
                continue
            op.deps.append(d)
            if d.dma_key is None:
                d.signal = True
        if dma_key is not None:
            self.dma_cnt[dma_key] = self.dma_cnt.get(dma_key, 0) + 16
            op.dma_val = self.dma_cnt[dma_key]
        for r in reads:
            r.readers.append(op)
        for w in writes:
            w.lastw = op
            w.readers = []
        self.ops[eng].append(op)
        return op

    def barrier(self, dma_ops=()):
        lasts = [self.ops[e][-1] for e in ENGS if self.ops[e]]
        lasts = [o for o in lasts if o.dma_key is None] + list(dma_ops)
        for e in ENGS:
            self.pending_barrier.setdefault(e, []).extend(lasts)

    def finish(self, ops):
        self.finals.extend(ops)

    def emit(self, stack):
        nc = self.nc
        sems = {e: stack.enter_context(nc.semaphore("sem_" + e)) for e in ENGS}
        dsem = {k: stack.enter_context(nc.semaphore("dsem_%s" % (k,))) for k in self.dma_cnt}
        for e in ENGS:
            n = 0
            for op in self.ops[e]:
                if op.dma_key is None and op.signal:
                    n += 1
                    op.sig_idx = n
        block = stack.enter_context(nc.Block())
        engobj = {"pe": "tensor", "act": "scalar", "dve": "vector", "pool": "gpsimd", "sp": "sync"}
        finals = self.finals

        def body_for(e):
            def body(eng):
                seen = {}
                for op in self.ops[e]:
                    need = {}
                    for d in op.deps:
                        if d.dma_key is not None:
                            s, v, key = dsem[d.dma_key], d.dma_val, ("d", d.dma_key)
                        else:
                            s, v, key = sems[d.eng], d.sig_idx, ("e", d.eng)
                        if key not in need or need[key][1] < v:
                            need[key] = (s, v)
                    for key, (s, v) in need.items():
                        if seen.get(key, 0) >= v:
                            continue
                        seen[key] = v
                        eng.wait_ge(s, v)
                    inst = op.fn(eng)
                    if op.dma_key is not None:
                        inst.then_inc(dsem[op.dma_key], 16)
                    elif op.signal:
                        inst.then_inc(sems[e], 1)
                if e == "sp":
                    for d in finals:
                        eng.wait_ge(dsem[d.dma_key], d.dma_val)
            return body

        for e in ENGS:
            getattr(block, engobj[e])(body_for(e))


class Tl:
    __slots__ = ("ap", "r")

    def __init__(self, ap, name):
        self.ap = ap
        self.r = Res(name)


class Arena:
    def __init__(self, nc, stack, nbytes):
        self.buf = stack.enter_context(nc.sbuf_tensor("arena", [128, nbytes], U8))
        self.off = 0
        self.cap = nbytes
        self.live = []

    def alloc(self, name, shape, dt):
        esz = {F32: 4, BF16: 2, I32: 4}[dt]
        n = 1
        for s in shape[1:]:
            n *= s
        nb = (n * esz + 31) // 32 * 32
        assert self.off + nb <= self.cap, "SBUF arena overflow at %s: %d + %d > %d" % (name, self.off, nb, self.cap)
        v = self.buf[0:shape[0], self.off:self.off + n * esz].bitcast(dt)
        if len(shape) == 3:
            v = v.rearrange("p (a b) -> p a b", a=shape[1])
        elif len(shape) == 4:
            v = v.rearrange("p (a b c) -> p a b c", a=shape[1], b=shape[2])
        t = Tl(v, name)
        lo, hi = self.off, self.off + nb
        keep = []
        for (a, b, o) in self.live:
            if a < hi and lo < b:
                if o.r.lastw is not None:
                    t.r.readers.append(o.r.lastw)
                t.r.readers.extend(o.r.readers)
                if a >= lo and b <= hi:
                    continue
            keep.append((a, b, o))
        keep.append((lo, hi, t))
        self.live = keep
        self.off += nb
        return t

    def mark(self):
        return self.off

    def reset(self, m):
        self.off = m


def build(NSEQ=4, SEQ=2048, CAP=768, dbg=None, stop_after=None):
    nc = bass.Bass("TRN2", target_bir_lowering=False)
    NT = SEQ // 128
    NTOK = NSEQ * SEQ
    NTILES = NSEQ * NT
    NSLOT = 32 * CAP
    NBLK = CAP // 128
    dbg = dbg or {}
    dbg_outs = {}

    def din(name, shape, dt=F32):
        return nc.dram_tensor(name, list(shape), dt, kind="ExternalInput").ap()

    x_d = din("x", [NTOK, D])
    cT_d = din("cT", [128, 8, NSEQ])
    w_ada_d = din("w_ada", [D, 6 * D])
    b_ada_d = din("b_ada_rep", [NSEQ, 6 * D])
    g1T_d = din("g1T", [128, 8])
    w_in_d = din("w_in", [D, 1536])
    w_gate_d = din("w_gate", [D, 2048])
    b_gateT_d = din("b_gateT", [128, 16])
    lam_sm_d = din("lam_sm", [128, 3, 16])
    lam_c_d = din("lam_c", [128, 3, 4, 2, 128])
    Bc_d = din("Bc", [128, 2, 4, 2, 128])
    Cc_d = din("Cc", [128, 2, 16, 64])
    dT_d = din("dT", [128, 4])
    w_glu_d = din("w_glu", [512, 512])
    b_gluT_d = din("b_gluT", [128, 4])
    wsT_d = din("wsT", [128, 8, 128])
    lngT_d = din("lngT", [128, 4])
    lnbT_d = din("lnbT", [128, 4])
    bsT_d = din("bsT", [128, 4, 128])
    w_bra_d = din("w_bra", [512, D])
    w_brb_d = din("w_brb", [512, D])
    w_out_d = din("w_out", [D, D])
    g2_d = din("g2", [1, D])
    w_r_d = din("w_r", [128, 8, 36])
    b_r_d = din("b_r", [1, 36])
    w1_d = din("w1", [32, D, 512])
    w3_d = din("w3", [32, D, 512])
    w2_d = din("w2", [32, 512, D])
    gf_d = din("gf", [1, D])
    out_d = nc.dram_tensor("out", [NTOK, D], F32, kind="ExternalOutput").ap()
    mod_d = Tl(nc.dram_tensor("mod_d", [NSEQ, 6 * D], F32, kind="Internal").ap(), "mod_d")
    H_d = nc.dram_tensor("H_d", [NTOK, D], F32, kind="Internal").ap()
    X_d = nc.dram_tensor("X_d", [NSLOT, D], BF16, kind="Internal").ap()
    Y_d = nc.dram_tensor("Y_d", [NSLOT, D], F32, kind="Internal").ap()
    r_X = Res("X_d")
    r_Y = Res("Y_d")
    r_H = Res("H_d")

    S = Sched(nc)
    final_ops = []
    with ExitStack() as st:
        A = Arena(nc, st, 206 * 1024)
        ps_all = st.enter_context(nc.psum_tensor("ps_all", [128, 8, 512], F32))
        PSB = [Tl(ps_all[:, b, :], "psb%d" % b) for b in range(8)]

        def psv(b, shape, dt=F32):
            v = PSB[b].ap
            if dt == BF16:
                v = v.bitcast(BF16)
            n = 1
            for s in shape[1:]:
                n *= s
            v = v[0:shape[0], 0:n]
            if len(shape) == 3:
                v = v.rearrange("p (a b) -> p a b", a=shape[1])
            elif len(shape) == 4:
                v = v.rearrange("p (a b c) -> p a b c", a=shape[1], b=shape[2])
            return v

        def psv2(b, shape):
            v = ps_all[:, b:b + 2, :].rearrange("p a b -> p (a b)")
            n = 1
            for s in shape[1:]:
                n *= s
            v = v[0:shape[0], 0:n]
            if len(shape) == 3:
                v = v.rearrange("p (a b) -> p a b", a=shape[1])
            elif len(shape) == 4:
                v = v.rearrange("p (a b c) -> p a b c", a=shape[1], b=shape[2])
            return v

        def R(*ts):
            return [t.r if isinstance(t, Tl) else t for t in ts]

        def dma(eng, out, in_, reads, writes, key, **kw):
            return S.add(eng, lambda e: e.dma_start(out=out, in_=in_, **kw), R(*reads), R(*writes), dma_key=key)

        def tt(eng, out, in0, in1, op, reads, writes):
            return S.add(eng, lambda e: e.tensor_tensor(out=out, in0=in0, in1=in1, op=op), R(*reads), R(*writes))

        def ts(eng, out, in0, s1, s2, op0, op1, reads, writes, accum=None):
            if op1 is None:
                return S.add(eng, lambda e: e.tensor_scalar(out=out, in0=in0, scalar1=s1, scalar2=None, op0=op0), R(*reads), R(*writes))
            if accum is not None:
                return S.add(eng, lambda e: e.tensor_scalar(out=out, in0=in0, scalar1=s1, scalar2=s2, op0=op0, op1=op1, accum_out=accum), R(*reads), R(*writes))
            return S.add(eng, lambda e: e.tensor_scalar(out=out, in0=in0, scalar1=s1, scalar2=s2, op0=op0, op1=op1), R(*reads), R(*writes))

        def stt(out, in0, scalar, in1, op0, op1, reads, writes):
            return S.add("dve", lambda e: e.scalar_tensor_tensor(out=out, in0=in0, scalar=scalar, in1=in1, op0=op0, op1=op1), R(*reads), R(*writes))

        def act(out, in_, func, reads, writes, bias=None, scale=None, accum=None):
            kw = {}
            if bias is not None:
                kw["bias"] = bias
            if scale is not None:
                kw["scale"] = scale
            if accum is not None:
                kw["accum_out"] = accum
            return S.add("act", lambda e: e.activation(out=out, in_=in_, func=func, **kw), R(*reads), R(*writes))

        def cp(eng, out, in_, reads, writes):
            if eng == "act":
                return S.add("act", lambda e: e.copy(out=out, in_=in_), R(*reads), R(*writes))
            return S.add(eng, lambda e: e.tensor_copy(out=out, in_=in_), R(*reads), R(*writes))

        def mm(out, lhsT, rhs, start, stop, reads, writes):
            return S.add("pe", lambda e: e.matmul(out, lhsT=lhsT, rhs=rhs, start=start, stop=stop), R(*reads), R(*writes))

        def tr(out, in_, ident, reads, writes):
            return S.add("pe", lambda e: e.transpose(out=out, in_=in_, identity=ident), R(*reads), R(*writes))

        def memset(eng, ap, val, writes):
            return S.add(eng, lambda e: e.memset(ap, val), [], R(*writes))

        _regs = {}

        def breg(e, val):
            if val not in _regs:
                _regs[val] = e.to_reg(val)
            return _regs[val]

        def dump(name, t, ap=None):
            ap = t.ap if ap is None else ap
            shp = list(ap.shape)
            o = nc.dram_tensor("dbg_" + name, shp, ap.dtype, kind="ExternalOutput").ap()
            dbg_outs[name] = "dbg_" + name
            final_ops.append(dma("sp", o, ap, [t], [], "dbg_" + name))

        ident_f = A.alloc("ident_f", [128, 128], F32)
        ident_b = A.alloc("ident_b", [128, 128], BF16)
        tri_b = A.alloc("tri_b", [128, 128], BF16)
        ones_b = A.alloc("ones_b", [128, 128], BF16)
        memset("pool", ident_f.ap, 0.0, [ident_f])
        S.add("pool", lambda e: e.affine_select(out=ident_f.ap, in_=ident_f.ap, pattern=[[-1, 128]], compare_op=ALU.not_equal,
                                                  fill=1.0, base=0, channel_multiplier=1), R(ident_f), R(ident_f))
        cp("pool", ident_b.ap, ident_f.ap, [ident_f], [ident_b])
        memset("pool", ones_b.ap, 1.0, [ones_b])
        S.add("pool", lambda e: e.affine_select(out=tri_b.ap, in_=ones_b.ap, pattern=[[1, 128]], compare_op=ALU.is_gt,
                                                  fill=0.0, base=0, channel_multiplier=-1), R(ones_b), R(tri_b))

        wgt = A.alloc("wgt", [128, NTILES, 2], F32)
        sloti = A.alloc("sloti", [128, NTILES, 2], I32)
        base = A.alloc("base", [128, 32], F32)
        ecap = A.alloc("ecap", [128, 32], F32)
        memset("pool", base.ap, 0.0, [base])
        ecap_i = A.alloc("ecap_i", [128, 32], I32)
        S.add("pool", lambda e: e.iota(ecap_i.ap, pattern=[[CAP, 32]], base=0, channel_multiplier=0), [], R(ecap_i))
        cp("pool", ecap.ap, ecap_i.ap, [ecap_i], [ecap])
        A1T = A.alloc("A1T", [128, 8, NSEQ], F32)
        sh1T = A.alloc("sh1T", [128, 8, NSEQ], F32)
        eps_rms = A.alloc("eps_rms", [128, 1], F32)
        eps_ln = A.alloc("eps_ln", [128, 1], F32)
        memset("pool", eps_rms.ap, 1e-6, [eps_rms])
        memset("pool", eps_ln.ap, 1e-5, [eps_ln])

        mark_persist = A.mark()

        cact = A.alloc("cact", [128, 8, NSEQ], F32)
        dma("sp", cact.ap, cT_d, [], [cact], "cact")
        act(cact.ap, cact.ap, AF.Silu, [cact], [cact])
        modrow = A.alloc("modrow", [NSEQ, 6 * D], F32)
        bada = A.alloc("bada", [NSEQ, 6 * D], F32)
        dma("sp", bada.ap, b_ada_d, [], [bada], "bada")
        wa = [A.alloc("wa%d" % i, [128, 8, 512], F32) for i in range(2)]
        wa_view = w_ada_d.rearrange("(kc p) n -> p kc n", p=128)
        for cb in range(12):
            w = wa[cb % 2]
            dma("sp", w.ap, wa_view[:, :, cb * 512:(cb + 1) * 512], [], [w], "wa%d" % (cb % 2))
            pb = PSB[cb % 2]
            for kc in range(8):
                mm(pb.ap[0:NSEQ, :], cact.ap[:, kc, :], w.ap[:, kc, :], kc == 0, kc == 7, [cact, w], [pb])
            tt("dve", modrow.ap[:, cb * 512:(cb + 1) * 512], pb.ap[0:NSEQ, :], bada.ap[:, cb * 512:(cb + 1) * 512], ALU.add,
               [pb, bada], [modrow])
        dma("sp", mod_d.ap, modrow.ap, [modrow], [mod_d], "mod_d")
        sc1T = A.alloc("sc1T", [128, 8, NSEQ], F32)
        g1T = A.alloc("g1T", [128, 8], F32)
        dma("sp", g1T.ap, g1T_d, [], [g1T], "g1T")
        for b in range(NSEQ):
            S.add("sp", lambda e, b=b: e.dma_start(out=sh1T.ap[:, :, b], in_=mod_d.ap[b, 0:D].rearrange("(kc p) -> p kc", p=128),
                                                  allow_slow_non_contiguous=True), R(mod_d), R(sh1T), dma_key="sh1T")
            S.add("sp", lambda e, b=b: e.dma_start(out=sc1T.ap[:, :, b], in_=mod_d.ap[b, D:2 * D].rearrange("(kc p) -> p kc", p=128),
                                                  allow_slow_non_contiguous=True), R(mod_d), R(sc1T), dma_key="sc1T")
        for b in range(NSEQ):
            stt(A1T.ap[:, :, b], sc1T.ap[:, :, b], 1.0, g1T.ap, ALU.add, ALU.mult, [sc1T, g1T], [A1T])
        if "mod" in dbg:
            dump("modrow", modrow)
            dump("A1T", A1T)
        A.reset(mark_persist)
        S.barrier()
        if stop_after == "mod":
            S.finish(final_ops)
            S.emit(st)
            return nc, dbg_outs

        w_r = A.alloc("w_r", [128, 8, 36], F32)
        dma("sp", w_r.ap, w_r_d, [], [w_r], "w_r")
        b_r = A.alloc("b_r", [128, 36], F32)
        dma("sp", b_r.ap, b_r_d.partition_broadcast(128), [], [b_r], "b_r")
        b_gateT = A.alloc("b_gateT", [128, 16], F32)
        dma("sp", b_gateT.ap, b_gateT_d, [], [b_gateT], "b_gateT")
        b_gluT = A.alloc("b_gluT", [128, 4], F32)
        dma("sp", b_gluT.ap, b_gluT_d, [], [b_gluT], "b_gluT")
        dT = A.alloc("dT", [128, 4], F32)
        dma("sp", dT.ap, dT_d, [], [dT], "dT")
        lngT = A.alloc("lngT", [128, 4], F32)
        dma("sp", lngT.ap, lngT_d, [], [lngT], "lngT")
        lnbT = A.alloc("lnbT", [128, 4], F32)
        dma("sp", lnbT.ap, lnbT_d, [], [lnbT], "lnbT")

        tabC = A.alloc("tabC", [128, 16, 129], F32)
        tabD = A.alloc("tabD", [128, 16, 129], F32)
        rmag = A.alloc("rmag", [128, 16], F32)
        Bt = A.alloc("Bt", [128, 2, 8, 128], BF16)
        Ct = A.alloc("Ct", [128, 2, 16, 64], BF16)
        mark_setup = A.mark()

        def range_reduce(ph, tmpf, tmpi, n):
            ts("dve", tmpi, ph, 1.0 / TWO_PI, None, ALU.mult, None, [tabC], [tabC])
            cp("dve", tmpf, tmpi, [tabC], [tabC])
            stt(ph, tmpf, -TWO_PI, ph, ALU.mult, ALU.add, [tabC], [tabC])
            wrap(ph, tmpf)

        def wrap(ph, tmpf):
            ts("dve", tmpf, ph, math.pi, None, ALU.is_gt, None, [tabC], [tabC])
            stt(ph, tmpf, -TWO_PI, ph, ALU.mult, ALU.add, [tabC], [tabC])
            ts("dve", tmpf, ph, -math.pi, None, ALU.is_lt, None, [tabC], [tabC])
            stt(ph, tmpf, TWO_PI, ph, ALU.mult, ALU.add, [tabC], [tabC])

        def scr(name, shape, dt):
            t = A.alloc(name, shape, dt)
            t.r = tabC.r
            return t

        lam = scr("lam", [128, 3, 16], F32)
        dma("sp", lam.ap, lam_sm_d, [], [tabC], "lam")
        dt_s = scr("dt_s", [128, 16], F32)
        th_s = scr("th_s", [128, 16], F32)
        lre_s = scr("lre_s", [128, 16], F32)
        sv_i = scr("sv_i", [128, 129], I32)
        sv = scr("sv", [128, 129], F32)
        tmpf = scr("tmpf", [128, 16 * 129], F32)
        tmpi = scr("tmpi", [128, 16 * 129], I32)
        S.add("pool", lambda e: e.iota(sv_i.ap, pattern=[[1, 129]], base=0, channel_multiplier=0), [], R(tabC))
        cp("dve", sv.ap, sv_i.ap, [tabC], [tabC])
        ts("dve", lre_s.ap, lam.ap[:, 0, :], -1e-4, None, ALU.min, None, [tabC], [tabC])
        act(dt_s.ap, lam.ap[:, 2, :], AF.Exp, [tabC], [tabC])
        tt("dve", rmag.ap, lre_s.ap, dt_s.ap, ALU.mult, [tabC], [tabC, rmag])
        act(rmag.ap, rmag.ap, AF.Exp, [tabC, rmag], [tabC, rmag])
        tt("dve", th_s.ap, lam.ap[:, 1, :], dt_s.ap, ALU.mult, [tabC], [tabC])
        for j in range(16):
            ts("dve", tabD.ap[:, j, :], sv.ap, th_s.ap[:, j:j + 1], None, ALU.mult, None, [tabC], [tabC])
        phD = tabD.ap.rearrange("p a b -> p (a b)")
        phC = tabC.ap.rearrange("p a b -> p (a b)")
        range_reduce(phD, tmpf.ap, tmpi.ap, 16 * 129)
        ts("dve", phC, phD, math.pi / 2, None, ALU.add, None, [tabC], [tabC])
        wrap(phC, tmpf.ap)
        act(phD, phD, AF.Sin, [tabC], [tabC, tabD])
        act(phC, phC, AF.Sin, [tabC], [tabC, tabD])

        A.reset(mark_setup)
        lamc = scr("lamc", [128, 3, 1024], F32)
        dma("sp", lamc.ap, lam_c_d.rearrange("p a b c d -> p a (b c d)"), [], [tabC], "lamc")
        Bc = scr("Bc", [128, 2, 1024], F32)
        dma("sp", Bc.ap, Bc_d.rearrange("p a b c d -> p a (b c d)"), [], [tabC], "Bc")
        NQ = 1024
        zz = [scr("zz%d" % i, [128, NQ], F32) for i in range(10)]
        zi = scr("zzi", [128, NQ], I32)
        lre, dtc, mag, thc, cs, sn, den, nr, fre, fim = [z.ap for z in zz]
        ts("dve", lre, lamc.ap[:, 0, :], -1e-4, None, ALU.min, None, [tabC], [tabC])
        act(dtc, lamc.ap[:, 2, :], AF.Exp, [tabC], [tabC])
        tt("dve", mag, lre, dtc, ALU.mult, [tabC], [tabC])
        act(mag, mag, AF.Exp, [tabC], [tabC])
        tt("dve", thc, lamc.ap[:, 1, :], dtc, ALU.mult, [tabC], [tabC])
        cp("dve", sn, thc, [tabC], [tabC])
        range_reduce(sn, den, zi.ap, NQ)
        ts("dve", cs, sn, math.pi / 2, None, ALU.add, None, [tabC], [tabC])
        wrap(cs, den)
        act(sn, sn, AF.Sin, [tabC], [tabC])
        act(cs, cs, AF.Sin, [tabC], [tabC])
        tt("dve", cs, cs, mag, ALU.mult, [tabC], [tabC])
        tt("dve", sn, sn, mag, ALU.mult, [tabC], [tabC])
        tt("dve", den, lre, lre, ALU.mult, [tabC], [tabC])
        tt("dve", nr, lamc.ap[:, 1, :], lamc.ap[:, 1, :], ALU.mult, [tabC], [tabC])
        tt("dve", den, den, nr, ALU.add, [tabC], [tabC])
        S.add("dve", lambda e: e.reciprocal(out=den, in_=den), R(tabC), R(tabC))
        ts("dve", nr, cs, -1.0, None, ALU.add, None, [tabC], [tabC])
        tt("dve", fre, nr, lre, ALU.mult, [tabC], [tabC])
        tt("dve", fim, sn, lamc.ap[:, 1, :], ALU.mult, [tabC], [tabC])
        tt("dve", fre, fre, fim, ALU.add, [tabC], [tabC])
        tt("dve", fre, fre, den, ALU.mult, [tabC], [tabC])
        tt("dve", fim, sn, lre, ALU.mult, [tabC], [tabC])
        tt("dve", mag, nr, lamc.ap[:, 1, :], ALU.mult, [tabC], [tabC])
        tt("dve", fim, fim, mag, ALU.subtract, [tabC], [tabC])
        tt("dve", fim, fim, den, ALU.mult, [tabC], [tabC])
        Btf = Bt.ap.rearrange("p a b c -> p a (b c)")
        tt("dve", mag, fre, Bc.ap[:, 0, :], ALU.mult, [tabC], [tabC])
        tt("dve", thc, fim, Bc.ap[:, 1, :], ALU.mult, [tabC], [tabC])
        tt("dve", Btf[:, 0, :], mag, thc, ALU.subtract, [tabC], [tabC, Bt])
        tt("dve", mag, fre, Bc.ap[:, 1, :], ALU.mult, [tabC], [tabC])
        tt("dve", thc, fim, Bc.ap[:, 0, :], ALU.mult, [tabC], [tabC])
        tt("dve", Btf[:, 1, :], mag, thc, ALU.add, [tabC], [tabC, Bt])
        Ccf = scr("Ccf", [128, 2, 1024], F32)
        dma("sp", Ccf.ap, Cc_d.rearrange("p a b c -> p a (b c)"), [], [tabC], "Ccf")
        Ctf = Ct.ap.rearrange("p a b c -> p a (b c)")
        cp("dve", Ctf[:, 0, :], Ccf.ap[:, 0, :], [tabC], [tabC, Ct])
        ts("dve", Ctf[:, 1, :], Ccf.ap[:, 1, :], -1.0, None, ALU.mult, None, [tabC], [tabC, Ct])
        if "s5setup" in dbg:
            dump("tabC", tabC)
            dump("tabD", tabD)
            dump("rmag", rmag)
            dump("Bt", Bt)
            dump("fre", zz[8])
            dump("fim", zz[9])
        A.reset(mark_setup)

        wsT_b = A.alloc("wsT_b", [128, 8, 128], BF16)
        sgub = A.alloc("sgub", [128, 4, 128], F32)
        mark_sgu = A.mark()
        wsf = A.alloc("wsf", [128, 8, 128], F32)
        dma("sp", wsf.ap, wsT_d, [], [wsf], "wsf")
        for h in range(8):
            S.add("pool", lambda e, h=h: e.affine_select(out=wsf.ap[:, h, :], in_=wsf.ap[:, h, :], pattern=[[1, 128]],
                                                           compare_op=ALU.is_ge, fill=0.0, base=0, channel_multiplier=-1),
                  R(wsf), R(wsf))
        cp("pool", wsT_b.ap, wsf.ap, [wsf], [wsT_b])
        bsT = A.alloc("bsT", [128, 4, 128], F32)
        dma("sp", bsT.ap, bsT_d, [], [bsT], "bsT")
        pmix0 = psv(3, [128, 4, 128])
        for h in range(8):
            po = (h % 2) * 64
            mm(pmix0[po:po + 64, h // 2, :], ones_b.ap[:, 0:64], wsT_b.ap[:, h, :], True, True, [ones_b, wsT_b], [PSB[3]])
        for q in range(4):
            stt(sgub.ap[:, q, :], pmix0[:, q, :], lnbT.ap[:, q:q + 1], bsT.ap[:, q, :], ALU.mult, ALU.add,
                [PSB[3], lnbT, bsT], [sgub])
        if "sgusetup" in dbg:
            dump("sgub", sgub)
            dump("wsT_b", wsT_b)
        A.reset(mark_sgu)

        def wload(name, src_view, shape, key=None):
            t = A.alloc(name, shape, BF16)
            dma("pool", t.ap, src_view, [], [t], key or name)
            return t

        w_in_b = wload("w_in_b", w_in_d.rearrange("(kc p) n -> p kc n", p=128), [128, 8, 1536])
        w_gate_b = wload("w_gate_b", w_gate_d.rearrange("(kc p) n -> p kc n", p=128), [128, 8, 2048])
        w_glu_b = wload("w_glu_b", w_glu_d.rearrange("(kc p) n -> p kc n", p=128), [128, 4, 512])
        w_bra_b = wload("w_bra_b", w_bra_d.rearrange("(kc p) n -> p kc n", p=128), [128, 4, D])
        w_brb_b = wload("w_brb_b", w_brb_d.rearrange("(kc p) n -> p kc n", p=128), [128, 4, D])
        w_out_b = wload("w_out_b", w_out_d.rearrange("(kc p) n -> p kc n", p=128), [128, 8, D])
        def alias(name, shape, dt, of):
            t = Tl(None, name)
            n = 1
            for s_ in shape[1:]:
                n *= s_
            base_ = of.ap
            if len(base_.shape) == 3:
                base_ = base_.rearrange("p a b -> p (a b)")
            elif len(base_.shape) == 4:
                base_ = base_.rearrange("p a b c -> p (a b c)")
            if base_.dtype != dt:
                base_ = base_.bitcast(dt)
            v = base_[:, 0:n]
            if len(shape) == 3:
                v = v.rearrange("p (a b) -> p a b", a=shape[1])
            t.ap = v
            t.r = of.r
            return t

        xt = [A.alloc("xt%d" % i, [128, D], F32) for i in range(2)]
        xr = A.alloc("xr", [128, D], F32)
        xsb = A.alloc("xsb", [128, D], BF16)
        ssq = A.alloc("ssq", [128, 1], F32)
        rstd = A.alloc("rstd", [128, 1], F32)
        xnT = [A.alloc("xnT%d" % i, [128, 8, 128], BF16) for i in range(3)]
        uf = [A.alloc("uf%d" % i, [128, 4, 128], F32) for i in range(2)]
        uT = [A.alloc("uT%d" % i, [128, 4, 128], BF16) for i in range(2)]
        guT = A.alloc("guT", [128, 4, 128], F32)
        gv = A.alloc("gv", [128, 512], F32)
        vst = A.alloc("vst", [128, 6], F32)
        vmv = A.alloc("vmv", [128, 2], F32)
        vrs = A.alloc("vrs", [128, 1], F32)
        vhat = A.alloc("vhat", [128, 512], BF16)
        mixT = alias("mixT", [128, 4, 128], F32, gv)
        ybT = [A.alloc("ybT%d" % i, [128, 4, 128], BF16) for i in range(3)]
        xtil = A.alloc("xtil", [128, 4, 2, 128], F32)
        s5a = A.alloc("s5a", [128, 4, 128], F32)
        s5b = A.alloc("s5b", [128, 4, 128], F32)
        gsc = A.alloc("gsc", [128, 4, 2, 128], F32)
        gp = A.alloc("gp", [128, 16, 2], F32)
        carry = A.alloc("carry", [128, 16, 2], F32)
        c4 = [A.alloc("c4_%d" % i, [128, 16], F32) for i in range(4)]
        hT = A.alloc("hT", [128, 4, 2, 128], BF16)
        ypre = A.alloc("ypre", [128, 4, 128], F32)
        ygT = A.alloc("ygT", [128, 4, 128], BF16)
        sg = alias("sg", [128, 4, 128], F32, s5a)
        yaT = [A.alloc("yaT%d" % i, [128, 4, 128], BF16) for i in range(2)]
        gates = A.alloc("gates", [128, 16, 128], F32)
        xn2T = alias("xn2T", [128, 8, 128], F32, gates)
        mergedT = A.alloc("mergedT", [128, 8, 128], BF16)
        jk2 = alias("jk2", [128, D], BF16, mergedT)
        xn2 = A.alloc("xn2", [128, D], F32)
        gt1b = A.alloc("gt1b", [128, D], F32)
        A2b = A.alloc("A2b", [128, D], F32)
        sh2b = A.alloc("sh2b", [128, D], F32)
        ssq2 = A.alloc("ssq2", [128, 1], F32)
        rstd2 = A.alloc("rstd2", [128, 1], F32)
        lg = A.alloc("lg", [128, 36], F32)
        rt = {n: A.alloc("rt_" + n, [128, w_], F32) for n, w_ in
              [("gmax", 1), ("ngmax", 1), ("maskg", 4), ("eg", 4), ("sume", 1), ("pgs", 1), ("pen", 4), ("lem", 32),
               ("m1", 1), ("oh1", 32), ("lem2", 32), ("m2", 1), ("oh2", 32), ("dm", 1), ("e2", 1), ("p1", 1), ("p2", 1),
               ("rank", 32), ("slotv", 32), ("junk", 32), ("sl", 2), ("val", 32), ("vk", 2)]}
        ohb = A.alloc("ohb", [128, 32], BF16)

        def rms_rstd(src, ssq_t, rstd_t, junk_ap, junk_t):
            act(junk_ap, src.ap, AF.Square, [src], [junk_t, ssq_t], accum=ssq_t.ap)
            act(ssq_t.ap, ssq_t.ap, AF.Sqrt, [ssq_t, eps_rms], [ssq_t], bias=eps_rms.ap, scale=1.0 / D)
            S.add("dve", lambda e: e.reciprocal(out=rstd_t.ap, in_=ssq_t.ap), R(ssq_t), R(rstd_t))

        store_ops = []
        scat_ops = []
        c128 = tabC.ap[:, :, 128]
        d128 = tabD.ap[:, :, 128]

        def seqof(i):
            return i // NT

        def P1(i):
            b = seqof(i)
            X = xt[i % 2]
            XN = xnT[i % 3]
            dma("sp", X.ap, x_d[i * 128:(i + 1) * 128, :], [], [X], "xt%d" % (i % 2))
            rms_rstd(X, ssq, rstd, xsb.ap, xsb)
            act(xsb.ap, X.ap, AF.Copy, [X, rstd], [xsb], scale=rstd.ap[:, 0:1])
            pX = psv(0, [128, 8, 128], BF16)
            for kc in range(8):
                tr(pX[:, kc, :], xsb.ap[:, kc * 128:(kc + 1) * 128], ident_b.ap, [xsb, ident_b], [PSB[0]])
            for kc in range(8):
                act(XN.ap[:, kc, :], pX[:, kc, :], AF.Identity, [PSB[0], A1T, sh1T], [XN],
                    bias=sh1T.ap[:, kc, b:b + 1], scale=A1T.ap[:, kc, b:b + 1])
            if dbg.get("tile") == i:
                dump("xnT", XN)

        def P2(i):
            XN = xnT[i % 3]
            UF, UT = uf[i % 2], uT[i % 2]
            pZa = psv(1, [128, 4, 128])
            pZu = psv(0, [128, 4, 128])
            for m in range(4):
                for kc in range(8):
                    mm(pZa[:, m, :], w_in_b.ap[:, kc, m * 128:(m + 1) * 128], XN.ap[:, kc, :], kc == 0, kc == 7,
                       [w_in_b, XN], [PSB[1]])
            cp("act", UF.ap, pZa, [PSB[1]], [UF])
            cp("pool", UT.ap, UF.ap, [UF], [UT])
            for m in range(4):
                for kc in range(8):
                    mm(pZu[:, m, :], w_in_b.ap[:, kc, 512 + m * 128:512 + (m + 1) * 128], XN.ap[:, kc, :], kc == 0, kc == 7,
                       [w_in_b, XN], [PSB[0]])
            act(guT.ap, pZu, AF.Gelu_apprx_tanh, [PSB[0]], [guT])
            pV = psv(1, [128, 512])
            for kc in range(8):
                mm(pV, XN.ap[:, kc, :], w_in_b.ap[:, kc, 1024:1536], kc == 0, kc == 7, [w_in_b, XN], [PSB[1]])
            act(gv.ap, pV, AF.Gelu_apprx_tanh, [PSB[1]], [gv])
            if dbg.get("tile") == i:
                dump("uf", UF)

        def P3(i):
            YB = ybT[i % 3]
            S.add("dve", lambda e: e.bn_stats(out=vst.ap, in_=gv.ap), R(gv), R(vst))
            S.add("dve", lambda e: e.bn_aggr(out=vmv.ap, in_=vst.ap), R(vst), R(vmv))
            act(vrs.ap, vmv.ap[:, 1:2], AF.Sqrt, [vmv, eps_ln], [vrs], bias=eps_ln.ap, scale=1.0)
            S.add("dve", lambda e: e.reciprocal(out=vrs.ap, in_=vrs.ap), R(vrs), R(vrs))
            ts("dve", vhat.ap, gv.ap, vmv.ap[:, 0:1], vrs.ap[:, 0:1], ALU.subtract, ALU.mult, [gv, vmv, vrs], [vhat])
            pMix = psv(0, [128, 4, 128])
            for h in range(8):
                po = (h % 2) * 64
                mm(pMix[po:po + 64, h // 2, :], vhat.ap[:, h * 64:(h + 1) * 64], wsT_b.ap[:, h, :], True, True,
                   [vhat, wsT_b], [PSB[0]])
            for q in range(4):
                stt(mixT.ap[:, q, :], pMix[:, q, :], lngT.ap[:, q:q + 1], sgub.ap[:, q, :], ALU.mult, ALU.add,
                    [PSB[0], lngT, sgub], [mixT])
            tt("pool", YB.ap, guT.ap, mixT.ap, ALU.mult, [guT, mixT], [YB])
            if dbg.get("tile") == i:
                dump("ybT", YB)

        def QB(i, q):
            UT = uT[i % 2]
            if q == 0 and i % NT == 0:
                memset("dve", carry.ap, 0.0, [carry])
            pXs = psv2(2, [128, 4, 2, 128])
            for jj in range(4):
                ro = 64 * (jj // 2)
                for part in range(2):
                    mm(pXs[:, jj, part, :], Bt.ap[ro:ro + 64, part, 2 * q + jj % 2, :], UT.ap[ro:ro + 64, q, :], True, True,
                       [Bt, UT], [PSB[2], PSB[3]])

        def QD(i, q):
            pXs = psv2(2, [128, 4, 2, 128])
            tc_ = tabC.ap[:, 4 * q:4 * q + 4, 0:128]
            td_ = tabD.ap[:, 4 * q:4 * q + 4, 0:128]
            PB = [PSB[2], PSB[3]]
            tt("dve", s5a.ap, pXs[:, :, 0, :], tc_, ALU.mult, PB + [tabC], [s5a])
            tt("dve", s5b.ap, pXs[:, :, 1, :], td_, ALU.mult, PB + [tabD], [s5b])
            tt("dve", xtil.ap[:, :, 0, :], s5a.ap, s5b.ap, ALU.add, [s5a, s5b], [xtil])
            tt("dve", s5a.ap, pXs[:, :, 1, :], tc_, ALU.mult, PB + [tabC, xtil], [s5a])
            tt("dve", s5b.ap, pXs[:, :, 0, :], td_, ALU.mult, PB + [tabD, xtil], [s5b])
            tt("dve", xtil.ap[:, :, 1, :], s5a.ap, s5b.ap, ALU.subtract, [s5a, s5b], [xtil])
            for jj in range(4):
                j = 4 * q + jj
                for part in range(2):
                    S.add("dve", lambda e, jj=jj, j=j, part=part: e.tensor_tensor_scan(
                        out=gsc.ap[:, jj, part, :], data0=rmag.ap[:, j:j + 1].to_broadcast([128, 128]),
                        data1=xtil.ap[:, jj, part, :], initial=carry.ap[:, j, part:part + 1],
                        op0=ALU.mult, op1=ALU.add), R(rmag, xtil, carry), R(gsc))
            cp("dve", gp.ap[:, 4 * q:4 * q + 4, :], gsc.ap[:, :, :, 127], [gsc], [gp])
            tt("dve", s5a.ap, gsc.ap[:, :, 0, :], tc_, ALU.mult, [gsc, tabC], [s5a])
            tt("dve", s5b.ap, gsc.ap[:, :, 1, :], td_, ALU.mult, [gsc, tabD], [s5b])
            tt("dve", hT.ap[:, :, 0, :], s5a.ap, s5b.ap, ALU.subtract, [s5a, s5b], [hT])
            tt("dve", s5a.ap, gsc.ap[:, :, 1, :], tc_, ALU.mult, [gsc, tabC, hT], [s5a])
            tt("dve", s5b.ap, gsc.ap[:, :, 0, :], td_, ALU.mult, [gsc, tabD, hT], [s5b])
            tt("dve", hT.ap[:, :, 1, :], s5a.ap, s5b.ap, ALU.add, [s5a, s5b], [hT])
            if dbg.get("tile") == i and q == 0:
                dump("xtil0", xtil)
                dump("gsc0", gsc)

        def QC(i, q):
            UF = uf[i % 2]
            pYs = psv(4, [128, 4, 128])
            for jj in range(4):
                j = 4 * q + jj
                ro = 64 * (jj // 2)
                for part in range(2):
                    mm(pYs[ro:ro + 64, q, :], Ct.ap[:, part, j, :], hT.ap[:, jj, part, :],
                       jj % 2 == 0 and part == 0, jj % 2 == 1 and part == 1, [Ct, hT], [PSB[4]])
            stt(ypre.ap[:, q, :], UF.ap[:, q, :], dT.ap[:, q:q + 1], pYs[:, q, :], ALU.mult, ALU.add,
                [UF, dT, PSB[4]], [ypre])

        def Qcarry(i):
            tt("dve", c4[0].ap, gp.ap[:, :, 0], c128, ALU.mult, [gp, tabC], [c4[0]])
            tt("dve", c4[1].ap, gp.ap[:, :, 1], d128, ALU.mult, [gp, tabD], [c4[1]])
            tt("dve", c4[2].ap, gp.ap[:, :, 1], c128, ALU.mult, [gp, tabC], [c4[2]])
            tt("dve", c4[3].ap, gp.ap[:, :, 0], d128, ALU.mult, [gp, tabD], [c4[3]])
            tt("dve", carry.ap[:, :, 0], c4[0].ap, c4[1].ap, ALU.subtract, [c4[0], c4[1]], [carry])
            tt("dve", carry.ap[:, :, 1], c4[2].ap, c4[3].ap, ALU.add, [c4[2], c4[3]], [carry])

        def Qtail(i):
            YA = yaT[i % 2]
            if dbg.get("tile") == i:
                dump("ypre", ypre)
            act(ypre.ap, ypre.ap, AF.Gelu_apprx_tanh, [ypre], [ypre])
            cp("pool", ygT.ap, ypre.ap, [ypre], [ygT])
            pG = psv(4, [128, 4, 128])
            for m in range(4):
                for kc in range(4):
                    mm(pG[:, m, :], w_glu_b.ap[:, kc, m * 128:(m + 1) * 128], ygT.ap[:, kc, :], kc == 0, kc == 3,
                       [w_glu_b, ygT], [PSB[4]])
            for m in range(4):
                act(sg.ap[:, m, :], pG[:, m, :], AF.Sigmoid, [PSB[4], b_gluT], [sg], bias=b_gluT.ap[:, m:m + 1], scale=1.0)
            tt("pool", YA.ap, ypre.ap, sg.ap, ALU.mult, [ypre, sg], [YA])
            if dbg.get("tile") == i:
                dump("yaT", YA)

        def R0(i):
            b = seqof(i)
            if i % NT == 0:
                dma("sp", gt1b.ap, mod_d.ap[b:b + 1, 2 * D:3 * D].partition_broadcast(128), [mod_d], [gt1b], "gt1b")
                dma("sp", sh2b.ap, mod_d.ap[b:b + 1, 3 * D:4 * D].partition_broadcast(128), [mod_d], [sh2b], "sh2b")
                dma("sp", A2b.ap, mod_d.ap[b:b + 1, 4 * D:5 * D].partition_broadcast(128), [mod_d], [A2b], "A2b")
                dma("sp", xn2.ap, g2_d.partition_broadcast(128), [], [xn2], "g2tmp")
                stt(A2b.ap, A2b.ap, 1.0, xn2.ap, ALU.add, ALU.mult, [A2b, xn2], [A2b])
            dma("sp", xr.ap, x_d[i * 128:(i + 1) * 128, :], [], [xr], "xr")

        def R1(i, mg):
            XN = xnT[i % 3]
            bk = 5 if mg % 2 == 0 else 7
            pGt = psv(bk, [128, 4, 128])
            for mm_ in range(4):
                m = mg * 4 + mm_
                for kc in range(8):
                    mm(pGt[:, mm_, :], w_gate_b.ap[:, kc, m * 128:(m + 1) * 128], XN.ap[:, kc, :], kc == 0, kc == 7,
                       [w_gate_b, XN], [PSB[bk]])
            for mm_ in range(4):
                m = mg * 4 + mm_
                act(gates.ap[:, m, :], pGt[:, mm_, :], AF.Sigmoid, [PSB[bk], b_gateT], [gates],
                    bias=b_gateT.ap[:, m:m + 1], scale=1.0)

        def R2(i, half):
            YA, YB = yaT[i % 2], ybT[i % 3]
            pA = psv(5, [128, 4, 128])
            pB = psv(6, [128, 4, 128])
            for mm_ in range(4):
                m = half * 4 + mm_
                for kc in range(4):
                    mm(pA[:, mm_, :], w_bra_b.ap[:, kc, m * 128:(m + 1) * 128], YA.ap[:, kc, :], kc == 0, kc == 3,
                       [w_bra_b, YA], [PSB[5]])
            for mm_ in range(4):
                m = half * 4 + mm_
                for kc in range(4):
                    mm(pB[:, mm_, :], w_brb_b.ap[:, kc, m * 128:(m + 1) * 128], YB.ap[:, kc, :], kc == 0, kc == 3,
                       [w_brb_b, YB], [PSB[6]])
            ga = gates.ap[:, half * 4:half * 4 + 4, :]
            gb = gates.ap[:, 8 + half * 4:8 + half * 4 + 4, :]
            tt("dve", ga, ga, pA, ALU.mult, [gates, PSB[5]], [gates])
            tt("dve", gb, gb, pB, ALU.mult, [gates, PSB[6]], [gates])
            tt("pool", mergedT.ap[:, half * 4:half * 4 + 4, :], ga, gb, ALU.add, [gates], [mergedT])
            if dbg.get("tile") == i and half == 1:
                dump("mergedT", mergedT)

        def R3(i):
            for half in range(2):
                bk = 6 + half
                for kc in range(8):
                    mm(PSB[bk].ap, mergedT.ap[:, kc, :], w_out_b.ap[:, kc, half * 512:(half + 1) * 512], kc == 0, kc == 7,
                       [mergedT, w_out_b], [PSB[bk]])
                sl = slice(half * 512, (half + 1) * 512)
                tt("dve", xn2.ap[:, sl], PSB[bk].ap, gt1b.ap[:, sl], ALU.mult, [PSB[bk], gt1b], [xn2])
            tt("pool", xr.ap, xr.ap, xn2.ap, ALU.add, [xr, xn2], [xr])
            store_ops.append(dma("sp", H_d[i * 128:(i + 1) * 128, :], xr.ap, [xr], [r_H], "hst"))
            rms_rstd(xr, ssq2, rstd2, jk2.ap, jk2)
            stt(xn2.ap, xr.ap, rstd2.ap[:, 0:1], A2b.ap, ALU.mult, ALU.mult, [xr, rstd2, A2b], [xn2])
            tt("pool", xn2.ap, xn2.ap, sh2b.ap, ALU.add, [xn2, sh2b], [xn2])
            if dbg.get("tile") == i:
                dump("h", xr)
                dump("xn2", xn2)

        def R4(i):
            pX2 = psv2(6, [128, 8, 128])
            for kc in range(8):
                tr(pX2[:, kc, :], xn2.ap[:, kc * 128:(kc + 1) * 128], ident_f.ap, [xn2, ident_f], [PSB[6], PSB[7]])
            cp("act", xn2T.ap, pX2, [PSB[6], PSB[7]], [xn2T])
            pL = psv(5, [128, 36])
            for kc in range(8):
                mm(pL, xn2T.ap[:, kc, :], w_r.ap[:, kc, :], kc == 0, kc == 7, [xn2T, w_r], [PSB[5]])
            tt("dve", lg.ap, pL, b_r.ap, ALU.add, [PSB[5], b_r], [lg])
            r_ = rt
            BIG = 1.0e9
            S.add("dve", lambda e: e.tensor_reduce(out=r_["gmax"].ap, in_=lg.ap[:, 0:4], axis=AX.X, op=ALU.max), R(lg), R(r_["gmax"]))
            ts("dve", r_["maskg"].ap, lg.ap[:, 0:4], r_["gmax"].ap[:, 0:1], None, ALU.is_equal, None, [lg, r_["gmax"]], [r_["maskg"]])
            ts("dve", r_["ngmax"].ap, r_["gmax"].ap, -1.0, None, ALU.mult, None, [r_["gmax"]], [r_["ngmax"]])
            act(r_["eg"].ap, lg.ap[:, 0:4], AF.Exp, [lg, r_["ngmax"]], [r_["eg"], r_["sume"]], bias=r_["ngmax"].ap[:, 0:1], scale=1.0,
                accum=r_["sume"].ap)
            S.add("dve", lambda e: e.reciprocal(out=r_["pgs"].ap, in_=r_["sume"].ap), R(r_["sume"]), R(r_["pgs"]))
            ts("dve", r_["pen"].ap, r_["maskg"].ap, BIG, -BIG, ALU.mult, ALU.add, [r_["maskg"]], [r_["pen"]])
            for g in range(4):
                ts("dve", r_["lem"].ap[:, g * 8:(g + 1) * 8], lg.ap[:, 4 + g * 8:4 + (g + 1) * 8], r_["pen"].ap[:, g:g + 1], None,
                   ALU.add, None, [lg, r_["pen"]], [r_["lem"]])
            S.add("dve", lambda e: e.tensor_reduce(out=r_["m1"].ap, in_=r_["lem"].ap, axis=AX.X, op=ALU.max), R(r_["lem"]), R(r_["m1"]))
            ts("dve", r_["oh1"].ap, r_["lem"].ap, r_["m1"].ap[:, 0:1], None, ALU.is_equal, None, [r_["lem"], r_["m1"]], [r_["oh1"]])
            stt(r_["lem2"].ap, r_["oh1"].ap, -BIG, r_["lem"].ap, ALU.mult, ALU.add, [r_["oh1"], r_["lem"]], [r_["lem2"]])
            S.add("dve", lambda e: e.tensor_reduce(out=r_["m2"].ap, in_=r_["lem2"].ap, axis=AX.X, op=ALU.max), R(r_["lem2"]), R(r_["m2"]))
            ts("dve", r_["oh2"].ap, r_["lem2"].ap, r_["m2"].ap[:, 0:1], None, ALU.is_equal, None, [r_["lem2"], r_["m2"]], [r_["oh2"]])
            tt("dve", r_["dm"].ap, r_["m2"].ap, r_["m1"].ap, ALU.subtract, [r_["m1"], r_["m2"]], [r_["dm"]])
            act(r_["e2"].ap, r_["dm"].ap, AF.Exp, [r_["dm"]], [r_["e2"]])
            ts("dve", r_["p1"].ap, r_["e2"].ap, 1.0, None, ALU.add, None, [r_["e2"]], [r_["p1"]])
            S.add("dve", lambda e: e.reciprocal(out=r_["p1"].ap, in_=r_["p1"].ap), R(r_["p1"]), R(r_["p1"]))
            tt("dve", r_["p2"].ap, r_["e2"].ap, r_["p1"].ap, ALU.mult, [r_["e2"], r_["p1"]], [r_["p2"]])
            tt("dve", ohb.ap, r_["oh1"].ap, r_["oh2"].ap, ALU.add, [r_["oh1"], r_["oh2"]], [ohb])
            pR = psv(5, [128, 128])
            mm(pR[:, 64:96], tri_b.ap, ohb.ap, True, True, [tri_b, ohb], [PSB[5]])
            mm(pR[:, 96:128], ones_b.ap, ohb.ap, True, True, [ones_b, ohb], [PSB[5]])
            tt("dve", r_["rank"].ap, pR[:, 64:96], base.ap, ALU.add, [PSB[5], base], [r_["rank"]])
            tt("dve", base.ap, pR[:, 96:128], base.ap, ALU.add, [PSB[5], base, r_["rank"]], [base])
            ts("dve", r_["val"].ap, r_["rank"].ap, float(CAP), None, ALU.is_lt, None, [r_["rank"]], [r_["val"]])
            tt("dve", r_["slotv"].ap, r_["rank"].ap, ecap.ap, ALU.add, [r_["rank"], ecap], [r_["slotv"]])
            stt(r_["slotv"].ap, r_["val"].ap, -4.0e6, r_["slotv"].ap, ALU.mult, ALU.add, [r_["val"], r_["slotv"]], [r_["slotv"]])
            ts("dve", r_["slotv"].ap, r_["slotv"].ap, 4.0e6, None, ALU.add, None, [r_["slotv"]], [r_["slotv"]])
            for k, ohn in enumerate(("oh1", "oh2")):
                tt("dve", r_["junk"].ap, r_[ohn].ap, r_["slotv"].ap, ALU.mult, [r_[ohn], r_["slotv"]], [r_["junk"]])
                S.add("dve", lambda e, k=k: e.tensor_reduce(out=r_["sl"].ap[:, k:k + 1], in_=r_["junk"].ap, axis=AX.X, op=ALU.add),
                      R(r_["junk"]), R(r_["sl"]))
                tt("dve", r_["junk"].ap, r_[ohn].ap, r_["val"].ap, ALU.mult, [r_[ohn], r_["val"]], [r_["junk"]])
                S.add("dve", lambda e, k=k: e.tensor_reduce(out=r_["vk"].ap[:, k:k + 1], in_=r_["junk"].ap, axis=AX.X, op=ALU.add),
                      R(r_["junk"]), R(r_["vk"]))
            cp("dve", sloti.ap[:, i, :], r_["sl"].ap, [r_["sl"]], [sloti])
            stt(wgt.ap[:, i, 0:1], r_["p1"].ap, r_["pgs"].ap[:, 0:1], r_["vk"].ap[:, 0:1], ALU.mult, ALU.mult,
                [r_["p1"], r_["pgs"], r_["vk"]], [wgt])
            stt(wgt.ap[:, i, 1:2], r_["p2"].ap, r_["pgs"].ap[:, 0:1], r_["vk"].ap[:, 1:2], ALU.mult, ALU.mult,
                [r_["p2"], r_["pgs"], r_["vk"]], [wgt])
            if dbg.get("tile") == i:
                dump("lg", lg)
                dump("rank", r_["rank"])
            for k in range(2):
                scat_ops.append(S.add("pool", lambda e, i=i, k=k: e.indirect_dma_start(
                    out=X_d, out_offset=bass.IndirectOffsetOnAxis(ap=sloti.ap[:, i, k:k + 1], axis=0),
                    in_=xn2.ap, in_offset=None, bounds_check=breg(e, NSLOT - 1), oob_is_err=False),
                    R(xn2, sloti), [r_X], dma_key="scat"))

        for s_ in range(NTILES + 2):
            ip, iq, ir = s_, s_ - 1, s_ - 2
            hp = 0 <= ip < NTILES
            hq = 0 <= iq < NTILES
            hr = 0 <= ir < NTILES
            if hr:
                R0(ir)
            if hq:
                QB(iq, 0)
            if hp:
                P1(ip)
            if hq:
                QD(iq, 0)
            if hr:
                R1(ir, 0)
            if hq:
                QB(iq, 1)
                QC(iq, 0)
                QD(iq, 1)
            if hp:
                P2(ip)
            if hr:
                R1(ir, 1)
            if hq:
                QB(iq, 2)
                QC(iq, 1)
                QD(iq, 2)
            if hr:
                R1(ir, 2)
                R1(ir, 3)
            if hq:
                QB(iq, 3)
                QC(iq, 2)
                QD(iq, 3)
            if hp:
                P3(ip)
            if hr:
                R2(ir, 0)
                R2(ir, 1)
            if hq:
                QC(iq, 3)
                Qcarry(iq)
                Qtail(iq)
            if hr:
                R3(ir)
                R4(ir)
        if "route" in dbg:
            dump("wgt", wgt)
            dump("sloti", sloti)
        if stop_after == "A":
            S.finish(final_ops + store_ops + scat_ops)
            S.emit(st)
            return nc, dbg_outs

        S.barrier(dma_ops=[scat_ops[-1], store_ops[-1]])
        A.reset(mark_persist)
        w1b = [A.alloc("w1b%d" % i, [128, 8, 512], BF16) for i in range(2)]
        w3b = [A.alloc("w3b%d" % i, [128, 8, 512], BF16) for i in range(2)]
        w2b = [A.alloc("w2b%d" % i, [128, 4, D], BF16) for i in range(2)]
        Xblk = [A.alloc("Xblk%d" % i, [128, D], BF16) for i in range(2)]
        XT = A.alloc("XT", [128, 8, CAP], BF16)
        hidT = A.alloc("hidT", [128, 4, CAP], BF16)
        s1 = [A.alloc("s1_%d" % i, [128, 512], F32) for i in range(2)]
        Yblk = [A.alloc("Yblk%d" % i, [128, D], F32) for i in range(2)]
        NH = (CAP + 511) // 512
        HW_ = CAP // NH
        ystore = []
        nblk_global = 0
        for e_ in range(32):
            sl_ = e_ % 2
            dma("pool", w1b[sl_].ap, w1_d[e_].rearrange("(kc p) n -> p kc n", p=128), [], [w1b[sl_]], "w1b%d" % sl_)
            dma("pool", w3b[sl_].ap, w3_d[e_].rearrange("(kc p) n -> p kc n", p=128), [], [w3b[sl_]], "w3b%d" % sl_)
            dma("pool", w2b[sl_].ap, w2_d[e_].rearrange("(kc p) n -> p kc n", p=128), [], [w2b[sl_]], "w2b%d" % sl_)
            for blk in range(NBLK):
                xb_ = Xblk[nblk_global % 2]
                r0 = e_ * CAP + blk * 128
                dma("sp", xb_.ap, X_d[r0:r0 + 128, :], [r_X], [xb_], "xblk%d" % (nblk_global % 2))
                bk = nblk_global % 2
                pXT = psv(bk, [128, 8, 128], BF16)
                for kc in range(8):
                    tr(pXT[:, kc, :], xb_.ap[:, kc * 128:(kc + 1) * 128], ident_b.ap, [xb_, ident_b], [PSB[bk]])
                cp("act" if blk % 2 == 0 else "dve", XT.ap[:, :, blk * 128:(blk + 1) * 128], pXT, [PSB[bk]], [XT])
                nblk_global += 1
            for m in range(4):
                for nh in range(NH):
                    cs_ = slice(nh * HW_, (nh + 1) * HW_)
                    b1 = 2 + (m * NH + nh) % 2
                    b3 = 4 + (m * NH + nh) % 2
                    p1_ = PSB[b1].ap[:, 0:HW_]
                    p3_ = PSB[b3].ap[:, 0:HW_]
                    for kc in range(8):
                        mm(p1_, w1b[sl_].ap[:, kc, m * 128:(m + 1) * 128], XT.ap[:, kc, cs_], kc == 0, kc == 7,
                           [w1b[sl_], XT], [PSB[b1]])
                    for kc in range(8):
                        mm(p3_, w3b[sl_].ap[:, kc, m * 128:(m + 1) * 128], XT.ap[:, kc, cs_], kc == 0, kc == 7,
                           [w3b[sl_], XT], [PSB[b3]])
                    s1_ = s1[(m * NH + nh) % 2]
                    act(s1_.ap[:, 0:HW_], p1_, AF.Silu, [PSB[b1]], [s1_])
                    tt("dve", hidT.ap[:, m, cs_], s1_.ap[:, 0:HW_], p3_, ALU.mult, [s1_, PSB[b3]], [hidT])
            for blk in range(NBLK):
                yb_ = Yblk[blk % 2]
                for half in range(2):
                    bk = 6 + half
                    for kc in range(4):
                        mm(PSB[bk].ap, hidT.ap[:, kc, blk * 128:(blk + 1) * 128], w2b[sl_].ap[:, kc, half * 512:(half + 1) * 512],
                           kc == 0, kc == 3, [hidT, w2b[sl_]], [PSB[bk]])
                    cp("act" if half == 0 else "dve", yb_.ap[:, half * 512:(half + 1) * 512], PSB[bk].ap, [PSB[bk]], [yb_])
                r0 = e_ * CAP + blk * 128
                ystore.append(dma("sp", Y_d[r0:r0 + 128, :], yb_.ap, [yb_], [r_Y], "yst%d" % (blk % 2)))
        if stop_after == "B":
            S.finish(final_ops + ystore[-2:])
            S.emit(st)
            return nc, dbg_outs

        S.barrier(dma_ops=ystore[-2:])
        A.reset(mark_persist)
        Hc = [A.alloc("Hc%d" % i, [128, D], F32) for i in range(2)]
        Y0 = [A.alloc("Y0_%d" % i, [128, D], F32) for i in range(2)]
        Y1 = [A.alloc("Y1_%d" % i, [128, D], F32) for i in range(2)]
        acc = A.alloc("acc", [128, D], F32)
        ob = [A.alloc("ob%d" % i, [128, D], F32) for i in range(2)]
        gt2b = A.alloc("gt2b", [128, D], F32)
        gfb = A.alloc("gfb", [128, D], F32)
        jk = A.alloc("jk", [128, D], BF16)
        ssq3 = A.alloc("ssq3", [128, 1], F32)
        rstd3 = A.alloc("rstd3", [128, 1], F32)
        dma("sp", gfb.ap, gf_d.partition_broadcast(128), [], [gfb], "gfb")
        for b in range(NSEQ):
            dma("sp", gt2b.ap, mod_d.ap[b:b + 1, 5 * D:6 * D].partition_broadcast(128), [mod_d], [gt2b], "gt2b")
            for tau in range(NT):
                i = b * NT + tau
                s_ = i % 2
                tok0 = i * 128
                memset("pool", Y0[s_].ap, 0.0, [Y0[s_]])
                memset("pool", Y1[s_].ap, 0.0, [Y1[s_]])
                dma("sp", Hc[s_].ap, H_d[tok0:tok0 + 128, :], [r_H], [Hc[s_]], "hc%d" % s_)
                for k, Yk in enumerate((Y0[s_], Y1[s_])):
                    S.add("pool", lambda e, i=i, k=k, Yk=Yk: e.indirect_dma_start(
                        out=Yk.ap, out_offset=None, in_=Y_d, in_offset=bass.IndirectOffsetOnAxis(ap=sloti.ap[:, i, k:k + 1], axis=0),
                        bounds_check=breg(e, NSLOT - 1), oob_is_err=False), R(r_Y, sloti), R(Yk), dma_key="gath%d_%d" % (k, s_))
                ts("dve", acc.ap, Y0[s_].ap, wgt.ap[:, i, 0:1], None, ALU.mult, None, [Y0[s_], wgt], [acc])
                stt(acc.ap, Y1[s_].ap, wgt.ap[:, i, 1:2], acc.ap, ALU.mult, ALU.add, [Y1[s_], wgt, acc], [acc])
                tt("dve", acc.ap, acc.ap, gt2b.ap, ALU.mult, [acc, gt2b], [acc])
                tt("pool", acc.ap, acc.ap, Hc[s_].ap, ALU.add, [acc, Hc[s_]], [acc])
                rms_rstd(acc, ssq3, rstd3, jk.ap, jk)
                stt(ob[s_].ap, acc.ap, rstd3.ap[:, 0:1], gfb.ap, ALU.mult, ALU.mult, [acc, rstd3, gfb], [ob[s_]])
                final_ops.append(dma("sp", out_d[tok0:tok0 + 128, :], ob[s_].ap, [ob[s_]], [], "ost%d" % s_))
        S.finish(final_ops)
        S.emit(st)
    return nc, dbg_outs


def prep_shared(inp):
    f = np.float32
    g = {}
    L = 0
    g["w_ada"] = np.ascontiguousarray(inp["w_ada"][L], f)
    g["g1T"] = np.ascontiguousarray(inp["norm1_g"][L].reshape(8, 128).T, f)
    g["w_in"] = np.ascontiguousarray(inp["w_in"][L], f)
    g["w_gate"] = np.ascontiguousarray(inp["w_gate"][L], f)
    g["b_gateT"] = np.ascontiguousarray(inp["b_gate"][L].reshape(16, 128).T, f)
    a_re, a_im, ls = inp["ssm_a_re"][L], inp["ssm_a_im"][L], inp["ssm_log_step"][L]
    def sm(v):
        return v.reshape(16, 2, 64).transpose(1, 2, 0).reshape(128, 16)
    lsx = np.repeat(ls[:, None], 64, axis=1)
    g["lam_sm"] = np.ascontiguousarray(np.stack([sm(a_re), sm(a_im), sm(lsx)], axis=1), f)
    def cl(v):
        s = sm(v)
        o = np.zeros((128, 4, 2, 128), f)
        for p in range(128):
            for q in range(4):
                for jo in range(2):
                    o[p, q, jo, :] = s[:, 4 * q + 2 * (p // 64) + jo]
        return o
    g["lam_c"] = np.ascontiguousarray(np.stack([cl(a_re), cl(a_im), cl(lsx)], axis=1), f)
    Bc = np.zeros((128, 2, 4, 2, 128), f)
    for pi, Bsrc in enumerate((inp["ssm_b_re"][L], inp["ssm_b_im"][L])):
        for gi in range(32):
            j = gi // 2
            p0 = 32 * (j % 4) + 16 * (gi % 2)
            n0 = 64 * (gi % 2)
            Bc[p0:p0 + 16, pi, j // 4, j % 2, n0:n0 + 64] = Bsrc[gi].T
    g["Bc"] = Bc
    Cc = np.zeros((128, 2, 16, 64), f)
    for pi, Csrc in enumerate((inp["ssm_c_re"][L], inp["ssm_c_im"][L])):
        for gi in range(32):
            j = gi // 2
            n0 = 64 * (gi % 2)
            c0 = 32 * (j % 2) + 16 * (gi % 2)
            Cc[n0:n0 + 64, pi, j, c0:c0 + 16] = Csrc[gi].T
    g["Cc"] = Cc
    g["dT"] = np.ascontiguousarray(inp["ssm_d"][L].reshape(4, 128).T, f)
    g["w_glu"] = np.ascontiguousarray(inp["w_glu"][L], f)
    g["b_gluT"] = np.ascontiguousarray(inp["b_glu"][L].reshape(4, 128).T, f)
    g["wsT"] = np.ascontiguousarray(inp["sgu_w"][L].transpose(2, 0, 1), f)
    g["lngT"] = np.ascontiguousarray(inp["sgu_ln_g"][L].reshape(4, 128).T, f)
    g["lnbT"] = np.ascontiguousarray(inp["sgu_ln_b"][L].reshape(4, 128).T, f)
    bs = inp["sgu_b"][L]
    bsT = np.zeros((128, 4, 128), f)
    for q in range(4):
        bsT[0:64, q, :] = bs[2 * q][None, :]
        bsT[64:128, q, :] = bs[2 * q + 1][None, :]
    g["bsT"] = bsT
    g["w_bra"] = np.ascontiguousarray(inp["w_branch_a"][L], f)
    g["w_brb"] = np.ascontiguousarray(inp["w_branch_b"][L], f)
    g["w_out"] = np.ascontiguousarray(inp["w_out"][L], f)
    g["g2"] = np.ascontiguousarray(inp["norm2_g"][L].reshape(1, D), f)
    wr = np.concatenate([inp["w_router_group"][L], inp["w_router_expert"][L].transpose(1, 0, 2).reshape(D, 32)], axis=1)
    g["w_r"] = np.ascontiguousarray(wr.reshape(8, 128, 36).transpose(1, 0, 2), f)
    g["b_r"] = np.ascontiguousarray(np.concatenate([inp["b_router_group"][L], inp["b_router_expert"][L].reshape(32)]).reshape(1, 36), f)
    g["w1"] = np.ascontiguousarray(inp["w1"][L], f)
    g["w3"] = np.ascontiguousarray(inp["w3"][L], f)
    g["w2"] = np.ascontiguousarray(inp["w2"][L], f)
    g["gf"] = np.ascontiguousarray(inp["norm_f_g"].reshape(1, D), f)
    return g


def prep_core(inp, shared, b0, nseq):
    m = dict(shared)
    xs = inp["x"][b0:b0 + nseq]
    m["x"] = np.ascontiguousarray(xs.reshape(-1, D), np.float32)
    c = inp["c"][b0:b0 + nseq]
    m["cT"] = np.ascontiguousarray(c.reshape(nseq, 8, 128).transpose(2, 1, 0), np.float32)
    m["b_ada_rep"] = np.ascontiguousarray(np.repeat(inp["b_ada"][0][None, :], nseq, axis=0), np.float32)
    return m


_CACHE = {}


def kernel(**inputs):
    inp = {k: np.asarray(v) for k, v in inputs.items()}
    B, SEQ = inp["x"].shape[0], inp["x"].shape[1]
    nseq = B // N_CORES
    key = (nseq, SEQ)
    if key not in _CACHE:
        _CACHE[key] = build(NSEQ=nseq, SEQ=SEQ, CAP=768)[0]
    nc = _CACHE[key]
    shared = prep_shared(inp)
    in_maps = [prep_core(inp, shared, c * nseq, nseq) for c in range(N_CORES)]
    res = run_bass_kernel_spmd(nc, in_maps, core_ids=list(range(N_CORES)))
    outs = [np.asarray(r["out"]).reshape(nseq, SEQ, D) for r in res.results]
    return np.concatenate(outs, axis=0).astype(np.float32)
```

```python
import math
from contextlib import ExitStack
import numpy as np
import concourse.bass as bass
import concourse.mybir as mybir
from concourse.bass_utils import run_bass_kernel_spmd

F32 = mybir.dt.float32
BF16 = mybir.dt.bfloat16
I32 = mybir.dt.int32
U8 = mybir.dt.uint8
AF = mybir.ActivationFunctionType
ALU = mybir.AluOpType
AX = mybir.AxisListType

ENGS = ("pe", "act", "dve", "pool", "sp")
N_CORES = 8
D = 1024
TWO_PI = 2.0 * math.pi


class Res:
    __slots__ = ("name", "lastw", "readers")

    def __init__(self, name):
        self.name = name
        self.lastw = None
        self.readers = []


class Op:
    __slots__ = ("eng", "fn", "deps", "dma_key", "dma_val", "signal", "sig_idx")

    def __init__(self, eng, fn, dma_key):
        self.eng = eng
        self.fn = fn
        self.deps = []
        self.dma_key = dma_key
        self.dma_val = None
        self.signal = False
        self.sig_idx = None


class Sched:
    def __init__(self, nc):
        self.nc = nc
        self.ops = {e: [] for e in ENGS}
        self.dma_cnt = {}
        self.finals = []
        self.pending_barrier = {}

    def add(self, eng, fn, reads=(), writes=(), dma_key=None):
        op = Op(eng, fn, dma_key)
        deps = []
        for r in reads:
            if r.lastw is not None:
                deps.append(r.lastw)
        for w in writes:
            if w.lastw is not None:
                deps.append(w.lastw)
            deps.extend(w.readers)
        if eng in self.pending_barrier:
            deps.extend(self.pending_barrier.pop(eng))
        seen = set()
        for d in deps:
            if d is op or id(d) in seen:
                continue
            seen.add(id(d))
            if d.eng == "pe" and eng == "pe" and d.dma_key is None and dma_key is None:
                continue
            op.deps.append(d)
            if d.dma_key is None:
                d.signal = True
        if dma_key is not None:
            self.dma_cnt[dma_key] = self.dma_cnt.get(dma_key, 0) + 16
            op.dma_val = self.dma_cnt[dma_key]
        for r in reads:
            r.readers.append(op)
        for w in writes:
            w.lastw = op
            w.readers = []
        self.ops[eng].append(op)
        return op

    def barrier(self, dma_ops=()):
        lasts = [self.ops[e][-1] for e in ENGS if self.ops[e]]
        lasts = [o for o in lasts if o.dma_key is None] + list(dma_ops)
        for e in ENGS:
            self.pending_barrier.setdefault(e, []).extend(lasts)

    def finish(self, ops):
        self.finals.extend(ops)

    def emit(self, stack):
        nc = self.nc
        sems = {e: stack.enter_context(nc.semaphore("sem_" + e)) for e in ENGS}
        dsem = {k: stack.enter_context(nc.semaphore("dsem_%s" % (k,))) for k in self.dma_cnt}
        for e in ENGS:
            n = 0
            for op in self.ops[e]:
                if op.dma_key is None and op.signal:
                    n += 1
                    op.sig_idx = n
        block = stack.enter_context(nc.Block())
        engobj = {"pe": "tensor", "act": "scalar", "dve": "vector", "pool": "gpsimd", "sp": "sync"}
        finals = self.finals

        def body_for(e):
            def body(eng):
                seen = {}
                for op in self.ops[e]:
                    need = {}
                    for d in op.deps:
                        if d.dma_key is not None:
                            s, v, key = dsem[d.dma_key], d.dma_val, ("d", d.dma_key)
                        else:
                            s, v, key = sems[d.eng], d.sig_idx, ("e", d.eng)
                        if key not in need or need[key][1] < v:
                            need[key] = (s, v)
                    for key, (s, v) in need.items():
                        if seen.get(key, 0) >= v:
                            continue
                        seen[key] = v
                        eng.wait_ge(s, v)
                    inst = op.fn(eng)
                    if op.dma_key is not None:
                        inst.then_inc(dsem[op.dma_key], 16)
                    elif op.signal:
                        inst.then_inc(sems[e], 1)
                if e == "sp":
                    for d in finals:
                        eng.wait_ge(dsem[d.dma_key], d.dma_val)
            return body

        for e in ENGS:
            getattr(block, engobj[e])(body_for(e))


class Tl:
    __slots__ = ("ap", "r")

    def __init__(self, ap, name):
        self.ap = ap
        self.r = Res(name)


class Arena:
    def __init__(self, nc, stack, nbytes):
        self.buf = stack.enter_context(nc.sbuf_tensor("arena", [128, nbytes], U8))
        self.off = 0
        self.cap = nbytes
        self.live = []

    def alloc(self, name, shape, dt):
        esz = {F32: 4, BF16: 2, I32: 4}[dt]
        n = 1
        for s in shape[1:]:
            n *= s
        nb = (n * esz + 31) // 32 * 32
        assert self.off + nb <= self.cap, "SBUF arena overflow at %s: %d + %d > %d" % (name, self.off, nb, self.cap)
        v = self.buf[0:shape[0], self.off:self.off + n * esz].bitcast(dt)
        if len(shape) == 3:
            v = v.rearrange("p (a b) -> p a b", a=shape[1])
        elif len(shape) == 4:
            v = v.rearrange("p (a b c) -> p a b c", a=shape[1], b=shape[2])
        t = Tl(v, name)
        lo, hi = self.off, self.off + nb
        keep = []
        for (a, b, o) in self.live:
            if a < hi and lo < b:
                if o.r.lastw is not None:
                    t.r.readers.append(o.r.lastw)
                t.r.readers.extend(o.r.readers)
                if a >= lo and b <= hi:
                    continue
            keep.append((a, b, o))
        keep.append((lo, hi, t))
        self.live = keep
        self.off += nb
        return t

    def mark(self):
        return self.off

    def reset(self, m):
        self.off = m


def build(NSEQ=4, SEQ=2048, CAP=768, dbg=None, stop_after=None):
    nc = bass.Bass("TRN2", target_bir_lowering=False)
    NT = SEQ // 128
    NTOK = NSEQ * SEQ
    NTILES = NSEQ * NT
    NSLOT = 32 * CAP
    NBLK = CAP // 128
    dbg = dbg or {}
    dbg_outs = {}

    def din(name, shape, dt=F32):
        return nc.dram_tensor(name, list(shape), dt, kind="ExternalInput").ap()

    x_d = din("x", [NTOK, D])
    cT_d = din("cT", [128, 8, NSEQ])
    w_ada_d = din("w_ada", [D, 6 * D])
    b_ada_d = din("b_ada_rep", [NSEQ, 6 * D])
    g1T_d = din("g1T", [128, 8])
    w_in_d = din("w_in", [D, 1536])
    w_gate_d = din("w_gate", [D, 2048])
    b_gateT_d = din("b_gateT", [128, 16])
    lam_sm_d = din("lam_sm", [128, 3, 16])
    lam_c_d = din("lam_c", [128, 3, 4, 2, 128])
    Bc_d = din("Bc", [128, 2, 4, 2, 128])
    Cc_d = din("Cc", [128, 2, 16, 64])
    dT_d = din("dT", [128, 4])
    w_glu_d = din("w_glu", [512, 512])
    b_gluT_d = din("b_gluT", [128, 4])
    wsT_d = din("wsT", [128, 8, 128])
    lngT_d = din("lngT", [128, 4])
    lnbT_d = din("lnbT", [128, 4])
    bsT_d = din("bsT", [128, 4, 128])
    w_bra_d = din("w_bra", [512, D])
    w_brb_d = din("w_brb", [512, D])
    w_out_d = din("w_out", [D, D])
    g2_d = din("g2", [1, D])
    w_r_d = din("w_r", [128, 8, 36])
    b_r_d = din("b_r", [1, 36])
    w1_d = din("w1", [32, D, 512])
    w3_d = din("w3", [32, D, 512])
    w2_d = din("w2", [32, 512, D])
    gf_d = din("gf", [1, D])
    out_d = nc.dram_tensor("out", [NTOK, D], F32, kind="ExternalOutput").ap()
    mod_d = Tl(nc.dram_tensor("mod_d", [NSEQ, 6 * D], F32, kind="Internal").ap(), "mod_d")
    H_d = nc.dram_tensor("H_d", [NTOK, D], F32, kind="Internal").ap()
    X_d = nc.dram_tensor("X_d", [NSLOT, D], BF16, kind="Internal").ap()
    Y_d = nc.dram_tensor("Y_d", [NSLOT, D], F32, kind="Internal").ap()
    r_X = Res("X_d")
    r_Y = Res("Y_d")
    r_H = Res("H_d")

    S = Sched(nc)
    final_ops = []
    with ExitStack() as st:
        A = Arena(nc, st, 206 * 1024)
        ps_all = st.enter_context(nc.psum_tensor("ps_all", [128, 8, 512], F32))
        PSB = [Tl(ps_all[:, b, :], "psb%d" % b) for b in range(8)]

        def psv(b, shape, dt=F32):
            v = PSB[b].ap
            if dt == BF16:
                v = v.bitcast(BF16)
            n = 1
            for s in shape[1:]:
                n *= s
            v = v[0:shape[0], 0:n]
            if len(shape) == 3:
                v = v.rearrange("p (a b) -> p a b", a=shape[1])
            elif len(shape) == 4:
                v = v.rearrange("p (a b c) -> p a b c", a=shape[1], b=shape[2])
            return v

        def psv2(b, shape):
            v = ps_all[:, b:b + 2, :].rearrange("p a b -> p (a b)")
            n = 1
            for s in shape[1:]:
                n *= s
            v = v[0:shape[0], 0:n]
            if len(shape) == 3:
                v = v.rearrange("p (a b) -> p a b", a=shape[1])
            elif len(shape) == 4:
                v = v.rearrange("p (a b c) -> p a b c", a=shape[1], b=shape[2])
            return v

        def R(*ts):
            return [t.r if isinstance(t, Tl) else t for t in ts]

        def dma(eng, out, in_, reads, writes, key, **kw):
            return S.add(eng, lambda e: e.dma_start(out=out, in_=in_, **kw), R(*reads), R(*writes), dma_key=key)

        def tt(eng, out, in0, in1, op, reads, writes):
            return S.add(eng, lambda e: e.tensor_tensor(out=out, in0=in0, in1=in1, op=op), R(*reads), R(*writes))

        def ts(eng, out, in0, s1, s2, op0, op1, reads, writes, accum=None):
            if op1 is None:
                return S.add(eng, lambda e: e.tensor_scalar(out=out, in0=in0, scalar1=s1, scalar2=None, op0=op0), R(*reads), R(*writes))
            if accum is not None:
                return S.add(eng, lambda e: e.tensor_scalar(out=out, in0=in0, scalar1=s1, scalar2=s2, op0=op0, op1=op1, accum_out=accum), R(*reads), R(*writes))
            return S.add(eng, lambda e: e.tensor_scalar(out=out, in0=in0, scalar1=s1, scalar2=s2, op0=op0, op1=op1), R(*reads), R(*writes))

        def stt(out, in0, scalar, in1, op0, op1, reads, writes):
            return S.add("dve", lambda e: e.scalar_tensor_tensor(out=out, in0=in0, scalar=scalar, in1=in1, op0=op0, op1=op1), R(*reads), R(*writes))

        def act(out, in_, func, reads, writes, bias=None, scale=None, accum=None):
            kw = {}
            if bias is not None:
                kw["bias"] = bias
            if scale is not None:
                kw["scale"] = scale
            if accum is not None:
                kw["accum_out"] = accum
            return S.add("act", lambda e: e.activation(out=out, in_=in_, func=func, **kw), R(*reads), R(*writes))

        def cp(eng, out, in_, reads, writes):
            if eng == "act":
                return S.add("act", lambda e: e.copy(out=out, in_=in_), R(*reads), R(*writes))
            return S.add(eng, lambda e: e.tensor_copy(out=out, in_=in_), R(*reads), R(*writes))

        def mm(out, lhsT, rhs, start, stop, reads, writes):
            return S.add("pe", lambda e: e.matmul(out, lhsT=lhsT, rhs=rhs, start=start, stop=stop), R(*reads), R(*writes))

        def tr(out, in_, ident, reads, writes):
            return S.add("pe", lambda e: e.transpose(out=out, in_=in_, identity=ident), R(*reads), R(*writes))

        def memset(eng, ap, val, writes):
            return S.add(eng, lambda e: e.memset(ap, val), [], R(*writes))

        _regs = {}

        def breg(e, val):
            if val not in _regs:
                _regs[val] = e.to_reg(val)
            return _regs[val]

        def dump(name, t, ap=None):
            ap = t.ap if ap is None else ap
            shp = list(ap.shape)
            o = nc.dram_tensor("dbg_" + name, shp, ap.dtype, kind="ExternalOutput").ap()
            dbg_outs[name] = "dbg_" + name
            final_ops.append(dma("sp", o, ap, [t], [], "dbg_" + name))

        ident_f = A.alloc("ident_f", [128, 128], F32)
        ident_b = A.alloc("ident_b", [128, 128], BF16)
        tri_b = A.alloc("tri_b", [128, 128], BF16)
        ones_b = A.alloc("ones_b", [128, 128], BF16)
        memset("pool", ident_f.ap, 0.0, [ident_f])
        S.add("pool", lambda e: e.affine_select(out=ident_f.ap, in_=ident_f.ap, pattern=[[-1, 128]], compare_op=ALU.not_equal,
                                                  fill=1.0, base=0, channel_multiplier=1), R(ident_f), R(ident_f))
        cp("pool", ident_b.ap, ident_f.ap, [ident_f], [ident_b])
        memset("pool", ones_b.ap, 1.0, [ones_b])
        S.add("pool", lambda e: e.affine_select(out=tri_b.ap, in_=ones_b.ap, pattern=[[1, 128]], compare_op=ALU.is_gt,
                                                  fill=0.0, base=0, channel_multiplier=-1), R(ones_b), R(tri_b))

        wgt = A.alloc("wgt", [128, NTILES, 2], F32)
        sloti = A.alloc("sloti", [128, NTILES, 2], I32)
        base = A.alloc("base", [128, 32], F32)
        ecap = A.alloc("ecap", [128, 32], F32)
        memset("pool", base.ap, 0.0, [base])
        ecap_i = A.alloc("ecap_i", [128, 32], I32)
        S.add("pool", lambda e: e.iota(ecap_i.ap, pattern=[[CAP, 32]], base=0, channel_multiplier=0), [], R(ecap_i))
        cp("pool", ecap.ap, ecap_i.ap, [ecap_i], [ecap])
        A1T = A.alloc("A1T", [128, 8, NSEQ], F32)
        sh1T = A.alloc("sh1T", [128, 8, NSEQ], F32)
        eps_rms = A.alloc("eps_rms", [128, 1], F32)
        eps_ln = A.alloc("eps_ln", [128, 1], F32)
        memset("pool", eps_rms.ap, 1e-6, [eps_rms])
        memset("pool", eps_ln.ap, 1e-5, [eps_ln])

        mark_persist = A.mark()

        cact = A.alloc("cact", [128, 8, NSEQ], F32)
        dma("sp", cact.ap, cT_d, [], [cact], "cact")
        act(cact.ap, cact.ap, AF.Silu, [cact], [cact])
        modrow = A.alloc("modrow", [NSEQ, 6 * D], F32)
        bada = A.alloc("bada", [NSEQ, 6 * D], F32)
        dma("sp", bada.ap, b_ada_d, [], [bada], "bada")
        wa = [A.alloc("wa%d" % i, [128, 8, 512], F32) for i in range(2)]
        wa_view = w_ada_d.rearrange("(kc p) n -> p kc n", p=128)
        for cb in range(12):
            w = wa[cb % 2]
            dma("sp", w.ap, wa_view[:, :, cb * 512:(cb + 1) * 512], [], [w], "wa%d" % (cb % 2))
            pb = PSB[cb % 2]
            for kc in range(8):
                mm(pb.ap[0:NSEQ, :], cact.ap[:, kc, :], w.ap[:, kc, :], kc == 0, kc == 7, [cact, w], [pb])
            tt("dve", modrow.ap[:, cb * 512:(cb + 1) * 512], pb.ap[0:NSEQ, :], bada.ap[:, cb * 512:(cb + 1) * 512], ALU.add,
               [pb, bada], [modrow])
        dma("sp", mod_d.ap, modrow.ap, [modrow], [mod_d], "mod_d")
        sc1T = A.alloc("sc1T", [128, 8, NSEQ], F32)
        g1T = A.alloc("g1T", [128, 8], F32)
        dma("sp", g1T.ap, g1T_d, [], [g1T], "g1T")
        for b in range(NSEQ):
            S.add("sp", lambda e, b=b: e.dma_start(out=sh1T.ap[:, :, b], in_=mod_d.ap[b, 0:D].rearrange("(kc p) -> p kc", p=128),
                                                  allow_slow_non_contiguous=True), R(mod_d), R(sh1T), dma_key="sh1T")
            S.add("sp", lambda e, b=b: e.dma_start(out=sc1T.ap[:, :, b], in_=mod_d.ap[b, D:2 * D].rearrange("(kc p) -> p kc", p=128),
                                                  allow_slow_non_contiguous=True), R(mod_d), R(sc1T), dma_key="sc1T")
        for b in range(NSEQ):
            stt(A1T.ap[:, :, b], sc1T.ap[:, :, b], 1.0, g1T.ap, ALU.add, ALU.mult, [sc1T, g1T], [A1T])
        if "mod" in dbg:
            dump("modrow", modrow)
            dump("A1T", A1T)
        A.reset(mark_persist)
        S.barrier()
        if stop_after == "mod":
            S.finish(final_ops)
            S.emit(st)
            return nc, dbg_outs

        w_r = A.alloc("w_r", [128, 8, 36], F32)
        dma("sp", w_r.ap, w_r_d, [], [w_r], "w_r")
        b_r = A.alloc("b_r", [128, 36], F32)
        dma("sp", b_r.ap, b_r_d.partition_broadcast(128), [], [b_r], "b_r")
        b_gateT = A.alloc("b_gateT", [128, 16], F32)
        dma("sp", b_gateT.ap, b_gateT_d, [], [b_gateT], "b_gateT")
        b_gluT = A.alloc("b_gluT", [128, 4], F32)
        dma("sp", b_gluT.ap, b_gluT_d, [], [b_gluT], "b_gluT")
        dT = A.alloc("dT", [128, 4], F32)
        dma("sp", dT.ap, dT_d, [], [dT], "dT")
        lngT = A.alloc("lngT", [128, 4], F32)
        dma("sp", lngT.ap, lngT_d, [], [lngT], "lngT")
        lnbT = A.alloc("lnbT", [128, 4], F32)
        dma("sp", lnbT.ap, lnbT_d, [], [lnbT], "lnbT")

        tabC = A.alloc("tabC", [128, 16, 129], F32)
        tabD = A.alloc("tabD", [128, 16, 129], F32)
        rmag = A.alloc("rmag", [128, 16], F32)
        Bt = A.alloc("Bt", [128, 2, 8, 128], BF16)
        Ct = A.alloc("Ct", [128, 2, 16, 64], BF16)
        mark_setup = A.mark()

        def range_reduce(ph, tmpf, tmpi, n):
            ts("dve", tmpi, ph, 1.0 / TWO_PI, None, ALU.mult, None, [tabC], [tabC])
            cp("dve", tmpf, tmpi, [tabC], [tabC])
            stt(ph, tmpf, -TWO_PI, ph, ALU.mult, ALU.add, [tabC], [tabC])
            wrap(ph, tmpf)

        def wrap(ph, tmpf):
            ts("dve", tmpf, ph, math.pi, None, ALU.is_gt, None, [tabC], [tabC])
            stt(ph, tmpf, -TWO_PI, ph, ALU.mult, ALU.add, [tabC], [tabC])
            ts("dve", tmpf, ph, -math.pi, None, ALU.is_lt, None, [tabC], [tabC])
            stt(ph, tmpf, TWO_PI, ph, ALU.mult, ALU.add, [tabC], [tabC])

        def scr(name, shape, dt):
            t = A.alloc(name, shape, dt)
            t.r = tabC.r
            return t

        lam = scr("lam", [128, 3, 16], F32)
        dma("sp", lam.ap, lam_sm_d, [], [tabC], "lam")
        dt_s = scr("dt_s", [128, 16], F32)
        th_s = scr("th_s", [128, 16], F32)
        lre_s = scr("lre_s", [128, 16], F32)
        sv_i = scr("sv_i", [128, 129], I32)
        sv = scr("sv", [128, 129], F32)
        tmpf = scr("tmpf", [128, 16 * 129], F32)
        tmpi = scr("tmpi", [128, 16 * 129], I32)
        S.add("pool", lambda e: e.iota(sv_i.ap, pattern=[[1, 129]], base=0, channel_multiplier=0), [], R(tabC))
        cp("dve", sv.ap, sv_i.ap, [tabC], [tabC])
        ts("dve", lre_s.ap, lam.ap[:, 0, :], -1e-4, None, ALU.min, None, [tabC], [tabC])
        act(dt_s.ap, lam.ap[:, 2, :], AF.Exp, [tabC], [tabC])
        tt("dve", rmag.ap, lre_s.ap, dt_s.ap, ALU.mult, [tabC], [tabC, rmag])
        act(rmag.ap, rmag.ap, AF.Exp, [tabC, rmag], [tabC, rmag])
        tt("dve", th_s.ap, lam.ap[:, 1, :], dt_s.ap, ALU.mult, [tabC], [tabC])
        for j in range(16):
            ts("dve", tabD.ap[:, j, :], sv.ap, th_s.ap[:, j:j + 1], None, ALU.mult, None, [tabC], [tabC])
        phD = tabD.ap.rearrange("p a b -> p (a b)")
        phC = tabC.ap.rearrange("p a b -> p (a b)")
        range_reduce(phD, tmpf.ap, tmpi.ap, 16 * 129)
        ts("dve", phC, phD, math.pi / 2, None, ALU.add, None, [tabC], [tabC])
        wrap(phC, tmpf.ap)
        act(phD, phD, AF.Sin, [tabC], [tabC, tabD])
        act(phC, phC, AF.Sin, [tabC], [tabC, tabD])

        A.reset(mark_setup)
        lamc = scr("lamc", [128, 3, 1024], F32)
        dma("sp", lamc.ap, lam_c_d.rearrange("p a b c d -> p a (b c d)"), [], [tabC], "lamc")
        Bc = scr("Bc", [128, 2, 1024], F32)
        dma("sp", Bc.ap, Bc_d.rearrange("p a b c d -> p a (b c d)"), [], [tabC], "Bc")
        NQ = 1024
        zz = [scr("zz%d" % i, [128, NQ], F32) for i in range(10)]
        zi = scr("zzi", [128, NQ], I32)
        lre, dtc, mag, thc, cs, sn, den, nr, fre, fim = [z.ap for z in zz]
        ts("dve", lre, lamc.ap[:, 0, :], -1e-4, None, ALU.min, None, [tabC], [tabC])
        act(dtc, lamc.ap[:, 2, :], AF.Exp, [tabC], [tabC])
        tt("dve", mag, lre, dtc, ALU.mult, [tabC], [tabC])
        act(mag, mag, AF.Exp, [tabC], [tabC])
        tt("dve", thc, lamc.ap[:, 1, :], dtc, ALU.mult, [tabC], [tabC])
        cp("dve", sn, thc, [tabC], [tabC])
        range_reduce(sn, den, zi.ap, NQ)
        ts("dve", cs, sn, math.pi / 2, None, ALU.add, None, [tabC], [tabC])
        wrap(cs, den)
        act(sn, sn, AF.Sin, [tabC], [tabC])
        act(cs, cs, AF.Sin, [tabC], [tabC])
        tt("dve", cs, cs, mag, ALU.mult, [tabC], [tabC])
        tt("dve", sn, sn, mag, ALU.mult, [tabC], [tabC])
        tt("dve", den, lre, lre, ALU.mult, [tabC], [tabC])
        tt("dve", nr, lamc.ap[:, 1, :], lamc.ap[:, 1, :], ALU.mult, [tabC], [tabC])
        tt("dve", den, den, nr, ALU.add, [tabC], [tabC])
        S.add("dve", lambda e: e.reciprocal(out=den, in_=den), R(tabC), R(tabC))
        ts("dve", nr, cs, -1.0, None, ALU.add, None, [tabC], [tabC])
        tt("dve", fre, nr, lre, ALU.mult, [tabC], [tabC])
        tt("dve", fim, sn, lamc.ap[:, 1, :], ALU.mult, [tabC], [tabC])
        tt("dve", fre, fre, fim, ALU.add, [tabC], [tabC])
        tt("dve", fre, fre, den, ALU.mult, [tabC], [tabC])
        tt("dve", fim, sn, lre, ALU.mult, [tabC], [tabC])
        tt("dve", mag, nr, lamc.ap[:, 1, :], ALU.mult, [tabC], [tabC])
        tt("dve", fim, fim, mag, ALU.subtract, [tabC], [tabC])
        tt("dve", fim, fim, den, ALU.mult, [tabC], [tabC])
        Btf = Bt.ap.rearrange("p a b c -> p a (b c)")
        tt("dve", mag, fre, Bc.ap[:, 0, :], ALU.mult, [tabC], [tabC])
        tt("dve", thc, fim, Bc.ap[:, 1, :], ALU.mult, [tabC], [tabC])
        tt("dve", Btf[:, 0, :], mag, thc, ALU.subtract, [tabC], [tabC, Bt])
        tt("dve", mag, fre, Bc.ap[:, 1, :], ALU.mult, [tabC], [tabC])
        tt("dve", thc, fim, Bc.ap[:, 0, :], ALU.mult, [tabC], [tabC])
        tt("dve", Btf[:, 1, :], mag, thc, ALU.add, [tabC], [tabC, Bt])
        Ccf = scr("Ccf", [128, 2, 1024], F32)
        dma("sp", Ccf.ap, Cc_d.rearrange("p a b c -> p a (b c)"), [], [tabC], "Ccf")
        Ctf = Ct.ap.rearrange("p a b c -> p a (b c)")
        cp("dve", Ctf[:, 0, :], Ccf.ap[:, 0, :], [tabC], [tabC, Ct])
        ts("dve", Ctf[:, 1, :], Ccf.ap[:, 1, :], -1.0, None, ALU.mult, None, [tabC], [tabC, Ct])
        if "s5setup" in dbg:
            dump("tabC", tabC)
            dump("tabD", tabD)
            dump("rmag", rmag)
            dump("Bt", Bt)
            dump("fre", zz[8])
            dump("fim", zz[9])
        A.reset(mark_setup)

        wsT_b = A.alloc("wsT_b", [128, 8, 128], BF16)
        sgub = A.alloc("sgub", [128, 4, 128], F32)
        mark_sgu = A.mark()
        wsf = A.alloc("wsf", [128, 8, 128], F32)
        dma("sp", wsf.ap, wsT_d, [], [wsf], "wsf")
        for h in range(8):
            S.add("pool", lambda e, h=h: e.affine_select(out=wsf.ap[:, h, :], in_=wsf.ap[:, h, :], pattern=[[1, 128]],
                                                           compare_op=ALU.is_ge, fill=0.0, base=0, channel_multiplier=-1),
                  R(wsf), R(wsf))
        cp("pool", wsT_b.ap, wsf.ap, [wsf], [wsT_b])
        bsT = A.alloc("bsT", [128, 4, 128], F32)
        dma("sp", bsT.ap, bsT_d, [], [bsT], "bsT")
        pmix0 = psv(3, [128, 4, 128])
        for h in range(8):
            po = (h % 2) * 64
            mm(pmix0[po:po + 64, h // 2, :], ones_b.ap[:, 0:64], wsT_b.ap[:, h, :], True, True, [ones_b, wsT_b], [PSB[3]])
        for q in range(4):
            stt(sgub.ap[:, q, :], pmix0[:, q, :], lnbT.ap[:, q:q + 1], bsT.ap[:, q, :], ALU.mult, ALU.add,
                [PSB[3], lnbT, bsT], [sgub])
        if "sgusetup" in dbg:
            dump("sgub", sgub)
            dump("wsT_b", wsT_b)
        A.reset(mark_sgu)

        def wload(name, src_view, shape, key=None):
            t = A.alloc(name, shape, BF16)
            dma("pool", t.ap, src_view, [], [t], key or name)
            return t

        w_in_b = wload("w_in_b", w_in_d.rearrange("(kc p) n -> p kc n", p=128), [128, 8, 1536])
        w_gate_b = wload("w_gate_b", w_gate_d.rearrange("(kc p) n -> p kc n", p=128), [128, 8, 2048])
        w_glu_b = wload("w_glu_b", w_glu_d.rearrange("(kc p) n -> p kc n", p=128), [128, 4, 512])
        w_bra_b = wload("w_bra_b", w_bra_d.rearrange("(kc p) n -> p kc n", p=128), [128, 4, D])
        w_brb_b = wload("w_brb_b", w_brb_d.rearrange("(kc p) n -> p kc n", p=128), [128, 4, D])
        w_out_b = wload("w_out_b", w_out_d.rearrange("(kc p) n -> p kc n", p=128), [128, 8, D])
        def alias(name, shape, dt, of):
            t = Tl(None, name)
            n = 1
            for s_ in shape[1:]:
                n *= s_
            base_ = of.ap
            if len(base_.shape) == 3:
                base_ = base_.rearrange("p a b -> p (a b)")
            elif len(base_.shape) == 4:
                base_ = base_.rearrange("p a b c -> p (a b c)")
            if base_.dtype != dt:
                base_ = base_.bitcast(dt)
            v = base_[:, 0:n]
            if len(shape) == 3:
                v = v.rearrange("p (a b) -> p a b", a=shape[1])
            t.ap = v
            t.r = of.r
            return t

        xt = [A.alloc("xt%d" % i, [128, D], F32) for i in range(2)]
        xr = A.alloc("xr", [128, D], F32)
        xsb = A.alloc("xsb", [128, D], BF16)
        ssq = A.alloc("ssq", [128, 1], F32)
        rstd = A.alloc("rstd", [128, 1], F32)
        xnT = [A.alloc("xnT%d" % i, [128, 8, 128], BF16) for i in range(3)]
        uf = [A.alloc("uf%d" % i, [128, 4, 128], F32) for i in range(2)]
        uT = [A.alloc("uT%d" % i, [128, 4, 128], BF16) for i in range(2)]
        guT = A.alloc("guT", [128, 4, 128], F32)
        gv = A.alloc("gv", [128, 512], F32)
        vst = A.alloc("vst", [128, 6], F32)
        vmv = A.alloc("vmv", [128, 2], F32)
        vrs = A.alloc("vrs", [128, 1], F32)
        vhat = A.alloc("vhat", [128, 512], BF16)
        mixT = alias("mixT", [128, 4, 128], F32, gv)
        ybT = [A.alloc("ybT%d" % i, [128, 4, 128], BF16) for i in range(3)]
        xtil = A.alloc("xtil", [128, 4, 2, 128], F32)
        s5a = A.alloc("s5a", [128, 4, 128], F32)
        s5b = A.alloc("s5b", [128, 4, 128], F32)
        gsc = A.alloc("gsc", [128, 4, 2, 128], F32)
        gp = A.alloc("gp", [128, 16, 2], F32)
        carry = A.alloc("carry", [128, 16, 2], F32)
        c4 = [A.alloc("c4_%d" % i, [128, 16], F32) for i in range(4)]
        hT = A.alloc("hT", [128, 4, 2, 128], BF16)
        ypre = A.alloc("ypre", [128, 4, 128], F32)
        ygT = A.alloc("ygT", [128, 4, 128], BF16)
        sg = alias("sg", [128, 4, 128], F32, s5a)
        yaT = [A.alloc("yaT%d" % i, [128, 4, 128], BF16) for i in range(2)]
        gates = A.alloc("gates", [128, 16, 128], F32)
        xn2T = alias("xn2T", [128, 8, 128], F32, gates)
        mergedT = A.alloc("mergedT", [128, 8, 128], BF16)
        jk2 = alias("jk2", [128, D], BF16, mergedT)
        xn2 = A.alloc("xn2", [128, D], F32)
        gt1b = A.alloc("gt1b", [128, D], F32)
        A2b = A.alloc("A2b", [128, D], F32)
        sh2b = A.alloc("sh2b", [128, D], F32)
        ssq2 = A.alloc("ssq2", [128, 1], F32)
        rstd2 = A.alloc("rstd2", [128, 1], F32)
        lg = A.alloc("lg", [128, 36], F32)
        rt = {n: A.alloc("rt_" + n, [128, w_], F32) for n, w_ in
              [("gmax", 1), ("ngmax", 1), ("maskg", 4), ("eg", 4), ("sume", 1), ("pgs", 1), ("pen", 4), ("lem", 32),
               ("m1", 1), ("oh1", 32), ("lem2", 32), ("m2", 1), ("oh2", 32), ("dm", 1), ("e2", 1), ("p1", 1), ("p2", 1),
               ("rank", 32), ("slotv", 32), ("junk", 32), ("sl", 2), ("val", 32), ("vk", 2)]}
        ohb = A.alloc("ohb", [128, 32], BF16)

        def rms_rstd(src, ssq_t, rstd_t, junk_ap, junk_t):
            act(junk_ap, src.ap, AF.Square, [src], [junk_t, ssq_t], accum=ssq_t.ap)
            act(ssq_t.ap, ssq_t.ap, AF.Sqrt, [ssq_t, eps_rms], [ssq_t], bias=eps_rms.ap, scale=1.0 / D)
            S.add("dve", lambda e: e.reciprocal(out=rstd_t.ap, in_=ssq_t.ap), R(ssq_t), R(rstd_t))

        store_ops = []
        scat_ops = []
        c128 = tabC.ap[:, :, 128]
        d128 = tabD.ap[:, :, 128]

        def seqof(i):
            return i // NT

        def P1(i):
            b = seqof(i)
            X = xt[i % 2]
            XN = xnT[i % 3]
            dma("sp", X.ap, x_d[i * 128:(i + 1) * 128, :], [], [X], "xt%d" % (i % 2))
            rms_rstd(X, ssq, rstd, xsb.ap, xsb)
            act(xsb.ap, X.ap, AF.Copy, [X, rstd], [xsb], scale=rstd.ap[:, 0:1])
            pX = psv(0, [128, 8, 128], BF16)
            for kc in range(8):
                tr(pX[:, kc, :], xsb.ap[:, kc * 128:(kc + 1) * 128], ident_b.ap, [xsb, ident_b], [PSB[0]])
            for kc in range(8):
                act(XN.ap[:, kc, :], pX[:, kc, :], AF.Identity, [PSB[0], A1T, sh1T], [XN],
                    bias=sh1T.ap[:, kc, b:b + 1], scale=A1T.ap[:, kc, b:b + 1])
            if dbg.get("tile") == i:
                dump("xnT", XN)

        def P2(i):
            XN = xnT[i % 3]
            UF, UT = uf[i % 2], uT[i % 2]
            pZa = psv(1, [128, 4, 128])
            pZu = psv(0, [128, 4, 128])
            for m in range(4):
                for kc in range(8):
                    mm(pZa[:, m, :], w_in_b.ap[:, kc, m * 128:(m + 1) * 128], XN.ap[:, kc, :], kc == 0, kc == 7,
                       [w_in_b, XN], [PSB[1]])
            cp("act", UF.ap, pZa, [PSB[1]], [UF])
            cp("pool", UT.ap, UF.ap, [UF], [UT])
            for m in range(4):
                for kc in range(8):
                    mm(pZu[:, m, :], w_in_b.ap[:, kc, 512 + m * 128:512 + (m + 1) * 128], XN.ap[:, kc, :], kc == 0, kc == 7,
                       [w_in_b, XN], [PSB[0]])
            act(guT.ap, pZu, AF.Gelu_apprx_tanh, [PSB[0]], [guT])
            pV = psv(1, [128, 512])
            for kc in range(8):
                mm(pV, XN.ap[:, kc, :], w_in_b.ap[:, kc, 1024:1536], kc == 0, kc == 7, [w_in_b, XN], [PSB[1]])
            act(gv.ap, pV, AF.Gelu_apprx_tanh, [PSB[1]], [gv])
            if dbg.get("tile") == i:
                dump("uf", UF)

        def P3(i):
            YB = ybT[i % 3]
            S.add("dve", lambda e: e.bn_stats(out=vst.ap, in_=gv.ap), R(gv), R(vst))
            S.add("dve", lambda e: e.bn_aggr(out=vmv.ap, in_=vst.ap), R(vst), R(vmv))
            act(vrs.ap, vmv.ap[:, 1:2], AF.Sqrt, [vmv, eps_ln], [vrs], bias=eps_ln.ap, scale=1.0)
            S.add("dve", lambda e: e.reciprocal(out=vrs.ap, in_=vrs.ap), R(vrs), R(vrs))
            ts("dve", vhat.ap, gv.ap, vmv.ap[:, 0:1], vrs.ap[:, 0:1], ALU.subtract, ALU.mult, [gv, vmv, vrs], [vhat])
            pMix = psv(0, [128, 4, 128])
            for h in range(8):
                po = (h % 2) * 64
                mm(pMix[po:po + 64, h // 2, :], vhat.ap[:, h * 64:(h + 1) * 64], wsT_b.ap[:, h, :], True, True,
                   [vhat, wsT_b], [PSB[0]])
            for q in range(4):
                stt(mixT.ap[:, q, :], pMix[:, q, :], lngT.ap[:, q:q + 1], sgub.ap[:, q, :], ALU.mult, ALU.add,
                    [PSB[0], lngT, sgub], [mixT])
            tt("pool", YB.ap, guT.ap, mixT.ap, ALU.mult, [guT, mixT], [YB])
            if dbg.get("tile") == i:
                dump("ybT", YB)

        def QB(i, q):
            UT = uT[i % 2]
            if q == 0 and i % NT == 0:
                memset("dve", carry.ap, 0.0, [carry])
            pXs = psv2(2, [128, 4, 2, 128])
            for jj in range(4):
                ro = 64 * (jj // 2)
                for part in range(2):
                    mm(pXs[:, jj, part, :], Bt.ap[ro:ro + 64, part, 2 * q + jj % 2, :], UT.ap[ro:ro + 64, q, :], True, True,
                       [Bt, UT], [PSB[2], PSB[3]])

        def QD(i, q):
            pXs = psv2(2, [128, 4, 2, 128])
            tc_ = tabC.ap[:, 4 * q:4 * q + 4, 0:128]
            td_ = tabD.ap[:, 4 * q:4 * q + 4, 0:128]
            PB = [PSB[2], PSB[3]]
            tt("dve", s5a.ap, pXs[:, :, 0, :], tc_, ALU.mult, PB + [tabC], [s5a])
            tt("dve", s5b.ap, pXs[:, :, 1, :], td_, ALU.mult, PB + [tabD], [s5b])
            tt("dve", xtil.ap[:, :, 0, :], s5a.ap, s5b.ap, ALU.add, [s5a, s5b], [xtil])
            tt("dve", s5a.ap, pXs[:, :, 1, :], tc_, ALU.mult, PB + [tabC, xtil], [s5a])
            tt("dve", s5b.ap, pXs[:, :, 0, :], td_, ALU.mult, PB + [tabD, xtil], [s5b])
            tt("dve", xtil.ap[:, :, 1, :], s5a.ap, s5b.ap, ALU.subtract, [s5a, s5b], [xtil])
            for jj in range(4):
                j = 4 * q + jj
                for part in range(2):
                    S.add("dve", lambda e, jj=jj, j=j, part=part: e.tensor_tensor_scan(
                        out=gsc.ap[:, jj, part, :], data0=rmag.ap[:, j:j + 1].to_broadcast([128, 128]),
                        data1=xtil.ap[:, jj, part, :], initial=carry.ap[:, j, part:part + 1],
                        op0=ALU.mult, op1=ALU.add), R(rmag, xtil, carry), R(gsc))
            cp("dve", gp.ap[:, 4 * q:4 * q + 4, :], gsc.ap[:, :, :, 127], [gsc], [gp])
            tt("dve", s5a.ap, gsc.ap[:, :, 0, :], tc_, ALU.mult, [gsc, tabC], [s5a])
            tt("dve", s5b.ap, gsc.ap[:, :, 1, :], td_, ALU.mult, [gsc, tabD], [s5b])
            tt("dve", hT.ap[:, :, 0, :], s5a.ap, s5b.ap, ALU.subtract, [s5a, s5b], [hT])
            tt("dve", s5a.ap, gsc.ap[:, :, 1, :], tc_, ALU.mult, [gsc, tabC, hT], [s5a])
            tt("dve", s5b.ap, gsc.ap[:, :, 0, :], td_, ALU.mult, [gsc, tabD, hT], [s5b])
            tt("dve", hT.ap[:, :, 1, :], s5a.ap, s5b.ap, ALU.add, [s5a, s5b], [hT])
            if dbg.get("tile") == i and q == 0:
                dump("xtil0", xtil)
                dump("gsc0", gsc)

        def QC(i, q):
            UF = uf[i % 2]
            pYs = psv(4, [128, 4, 128])
            for jj in range(4):
                j = 4 * q + jj
                ro = 64 * (jj // 2)
                for part in range(2):
                    mm(pYs[ro:ro + 64, q, :], Ct.ap[:, part, j, :], hT.ap[:, jj, part, :],
                       jj % 2 == 0 and part == 0, jj % 2 == 1 and part == 1, [Ct, hT], [PSB[4]])
            stt(ypre.ap[:, q, :], UF.ap[:, q, :], dT.ap[:, q:q + 1], pYs[:, q, :], ALU.mult, ALU.add,
                [UF, dT, PSB[4]], [ypre])

        def Qcarry(i):
            tt("dve", c4[0].ap, gp.ap[:, :, 0], c128, ALU.mult, [gp, tabC], [c4[0]])
            tt("dve", c4[1].ap, gp.ap[:, :, 1], d128, ALU.mult, [gp, tabD], [c4[1]])
            tt("dve", c4[2].ap, gp.ap[:, :, 1], c128, ALU.mult, [gp, tabC], [c4[2]])
            tt("dve", c4[3].ap, gp.ap[:, :, 0], d128, ALU.mult, [gp, tabD], [c4[3]])
            tt("dve", carry.ap[:, :, 0], c4[0].ap, c4[1].ap, ALU.subtract, [c4[0], c4[1]], [carry])
            tt("dve", carry.ap[:, :, 1], c4[2].ap, c4[3].ap, ALU.add, [c4[2], c4[3]], [carry])

        def Qtail(i):
            YA = yaT[i % 2]
            if dbg.get("tile") == i:
                dump("ypre", ypre)
            act(ypre.ap, ypre.ap, AF.Gelu_apprx_tanh, [ypre], [ypre])
            cp("pool", ygT.ap, ypre.ap, [ypre], [ygT])
            pG = psv(4, [128, 4, 128])
            for m in range(4):
                for kc in range(4):
                    mm(pG[:, m, :], w_glu_b.ap[:, kc, m * 128:(m + 1) * 128], ygT.ap[:, kc, :], kc == 0, kc == 3,
                       [w_glu_b, ygT], [PSB[4]])
            for m in range(4):
                act(sg.ap[:, m, :], pG[:, m, :], AF.Sigmoid, [PSB[4], b_gluT], [sg], bias=b_gluT.ap[:, m:m + 1], scale=1.0)
            tt("pool", YA.ap, ypre.ap, sg.ap, ALU.mult, [ypre, sg], [YA])
            if dbg.get("tile") == i:
                dump("yaT", YA)

        def R0(i):
            b = seqof(i)
            if i % NT == 0:
                dma("sp", gt1b.ap, mod_d.ap[b:b + 1, 2 * D:3 * D].partition_broadcast(128), [mod_d], [gt1b], "gt1b")
                dma("sp", sh2b.ap, mod_d.ap[b:b + 1, 3 * D:4 * D].partition_broadcast(128), [mod_d], [sh2b], "sh2b")
                dma("sp", A2b.ap, mod_d.ap[b:b + 1, 4 * D:5 * D].partition_broadcast(128), [mod_d], [A2b], "A2b")
                dma("sp", xn2.ap, g2_d.partition_broadcast(128), [], [xn2], "g2tmp")
                stt(A2b.ap, A2b.ap, 1.0, xn2.ap, ALU.add, ALU.mult, [A2b, xn2], [A2b])
            dma("sp", xr.ap, x_d[i * 128:(i + 1) * 128, :], [], [xr], "xr")

        def R1(i, mg):
            XN = xnT[i % 3]
            bk = 5 if mg % 2 == 0 else 7
            pGt = psv(bk, [128, 4, 128])
            for mm_ in range(4):
                m = mg * 4 + mm_
                for kc in range(8):
                    mm(pGt[:, mm_, :], w_gate_b.ap[:, kc, m * 128:(m + 1) * 128], XN.ap[:, kc, :], kc == 0, kc == 7,
                       [w_gate_b, XN], [PSB[bk]])
            for mm_ in range(4):
                m = mg * 4 + mm_
                act(gates.ap[:, m, :], pGt[:, mm_, :], AF.Sigmoid, [PSB[bk], b_gateT], [gates],
                    bias=b_gateT.ap[:, m:m + 1], scale=1.0)

        def R2(i, half):
            YA, YB = yaT[i % 2], ybT[i % 3]
            pA = psv(5, [128, 4, 128])
            pB = psv(6, [128, 4, 128])
            for mm_ in range(4):
                m = half * 4 + mm_
                for kc in range(4):
                    mm(pA[:, mm_, :], w_bra_b.ap[:, kc, m * 128:(m + 1) * 128], YA.ap[:, kc, :], kc == 0, kc == 3,
                       [w_bra_b, YA], [PSB[5]])
            for mm_ in range(4):
                m = half * 4 + mm_
                for kc in range(4):
                    mm(pB[:, mm_, :], w_brb_b.ap[:, kc, m * 128:(m + 1) * 128], YB.ap[:, kc, :], kc == 0, kc == 3,
                       [w_brb_b, YB], [PSB[6]])
            ga = gates.ap[:, half * 4:half * 4 + 4, :]
            gb = gates.ap[:, 8 + half * 4:8 + half * 4 + 4, :]
            tt("dve", ga, ga, pA, ALU.mult, [gates, PSB[5]], [gates])
            tt("dve", gb, gb, pB, ALU.mult, [gates, PSB[6]], [gates])
            tt("pool", mergedT.ap[:, half * 4:half * 4 + 4, :], ga, gb, ALU.add, [gates], [mergedT])
            if dbg.get("tile") == i and half == 1:
                dump("mergedT", mergedT)

        def R3(i):
            for half in range(2):
                bk = 6 + half
                for kc in range(8):
                    mm(PSB[bk].ap, mergedT.ap[:, kc, :], w_out_b.ap[:, kc, half * 512:(half + 1) * 512], kc == 0, kc == 7,
                       [mergedT, w_out_b], [PSB[bk]])
                sl = slice(half * 512, (half + 1) * 512)
                tt("dve", xn2.ap[:, sl], PSB[bk].ap, gt1b.ap[:, sl], ALU.mult, [PSB[bk], gt1b], [xn2])
            tt("pool", xr.ap, xr.ap, xn2.ap, ALU.add, [xr, xn2], [xr])
            store_ops.append(dma("sp", H_d[i * 128:(i + 1) * 128, :], xr.ap, [xr], [r_H], "hst"))
            rms_rstd(xr, ssq2, rstd2, jk2.ap, jk2)
            stt(xn2.ap, xr.ap, rstd2.ap[:, 0:1], A2b.ap, ALU.mult, ALU.mult, [xr, rstd2, A2b], [xn2])
            tt("pool", xn2.ap, xn2.ap, sh2b.ap, ALU.add, [xn2, sh2b], [xn2])
            if dbg.get("tile") == i:
                dump("h", xr)
                dump("xn2", xn2)

        def R4(i):
            pX2 = psv2(6, [128, 8, 128])
            for kc in range(8):
                tr(pX2[:, kc, :], xn2.ap[:, kc * 128:(kc + 1) * 128], ident_f.ap, [xn2, ident_f], [PSB[6], PSB[7]])
            cp("act", xn2T.ap, pX2, [PSB[6], PSB[7]], [xn2T])
            pL = psv(5, [128, 36])
            for kc in range(8):
                mm(pL, xn2T.ap[:, kc, :], w_r.ap[:, kc, :], kc == 0, kc == 7, [xn2T, w_r], [PSB[5]])
            tt("dve", lg.ap, pL, b_r.ap, ALU.add, [PSB[5], b_r], [lg])
            r_ = rt
            BIG = 1.0e9
            S.add("dve", lambda e: e.tensor_reduce(out=r_["gmax"].ap, in_=lg.ap[:, 0:4], axis=AX.X, op=ALU.max), R(lg), R(r_["gmax"]))
            ts("dve", r_["maskg"].ap, lg.ap[:, 0:4], r_["gmax"].ap[:, 0:1], None, ALU.is_equal, None, [lg, r_["gmax"]], [r_["maskg"]])
            ts("dve", r_["ngmax"].ap, r_["gmax"].ap, -1.0, None, ALU.mult, None, [r_["gmax"]], [r_["ngmax"]])
            act(r_["eg"].ap, lg.ap[:, 0:4], AF.Exp, [lg, r_["ngmax"]], [r_["eg"], r_["sume"]], bias=r_["ngmax"].ap[:, 0:1], scale=1.0,
                accum=r_["sume"].ap)
            S.add("dve", lambda e: e.reciprocal(out=r_["pgs"].ap, in_=r_["sume"].ap), R(r_["sume"]), R(r_["pgs"]))
            ts("dve", r_["pen"].ap, r_["maskg"].ap, BIG, -BIG, ALU.mult, ALU.add, [r_["maskg"]], [r_["pen"]])
            for g in range(4):
                ts("dve", r_["lem"].ap[:, g * 8:(g + 1) * 8], lg.ap[:, 4 + g * 8:4 + (g + 1) * 8], r_["pen"].ap[:, g:g + 1], None,
                   ALU.add, None, [lg, r_["pen"]], [r_["lem"]])
            S.add("dve", lambda e: e.tensor_reduce(out=r_["m1"].ap, in_=r_["lem"].ap, axis=AX.X, op=ALU.max), R(r_["lem"]), R(r_["m1"]))
            ts("dve", r_["oh1"].ap, r_["lem"].ap, r_["m1"].ap[:, 0:1], None, ALU.is_equal, None, [r_["lem"], r_["m1"]], [r_["oh1"]])
            stt(r_["lem2"].ap, r_["oh1"].ap, -BIG, r_["lem"].ap, ALU.mult, ALU.add, [r_["oh1"], r_["lem"]], [r_["lem2"]])
            S.add("dve", lambda e: e.tensor_reduce(out=r_["m2"].ap, in_=r_["lem2"].ap, axis=AX.X, op=ALU.max), R(r_["lem2"]), R(r_["m2"]))
            ts("dve", r_["oh2"].ap, r_["lem2"].ap, r_["m2"].ap[:, 0:1], None, ALU.is_equal, None, [r_["lem2"], r_["m2"]], [r_["oh2"]])
            tt("dve", r_["dm"].ap, r_["m2"].ap, r_["m1"].ap, ALU.subtract, [r_["m1"], r_["m2"]], [r_["dm"]])
            act(r_["e2"].ap, r_["dm"].ap, AF.Exp, [r_["dm"]], [r_["e2"]])
            ts("dve", r_["p1"].ap, r_["e2"].ap, 1.0, None, ALU.add, None, [r_["e2"]], [r_["p1"]])
            S.add("dve", lambda e: e.reciprocal(out=r_["p1"].ap, in_=r_["p1"].ap), R(r_["p1"]), R(r_["p1"]))
            tt("dve", r_["p2"].ap, r_["e2"].ap, r_["p1"].ap, ALU.mult, [r_["e2"], r_["p1"]], [r_["p2"]])
            tt("dve", ohb.ap, r_["oh1"].ap, r_["oh2"].ap, ALU.add, [r_["oh1"], r_["oh2"]], [ohb])
            pR = psv(5, [128, 128])
            mm(pR[:, 64:96], tri_b.ap, ohb.ap, True, True, [tri_b, ohb], [PSB[5]])
            mm(pR[:, 96:128], ones_b.ap, ohb.ap, True, True, [ones_b, ohb], [PSB[5]])
            tt("dve", r_["rank"].ap, pR[:, 64:96], base.ap, ALU.add, [PSB[5], base], [r_["rank"]])
            tt("dve", base.ap, pR[:, 96:128], base.ap, ALU.add, [PSB[5], base, r_["rank"]], [base])
            ts("dve", r_["val"].ap, r_["rank"].ap, float(CAP), None, ALU.is_lt, None, [r_["rank"]], [r_["val"]])
            tt("dve", r_["slotv"].ap, r_["rank"].ap, ecap.ap, ALU.add, [r_["rank"], ecap], [r_["slotv"]])
            stt(r_["slotv"].ap, r_["val"].ap, -4.0e6, r_["slotv"].ap, ALU.mult, ALU.add, [r_["val"], r_["slotv"]], [r_["slotv"]])
            ts("dve", r_["slotv"].ap, r_["slotv"].ap, 4.0e6, None, ALU.add, None, [r_["slotv"]], [r_["slotv"]])
            for k, ohn in enumerate(("oh1", "oh2")):
                tt("dve", r_["junk"].ap, r_[ohn].ap, r_["slotv"].ap, ALU.mult, [r_[ohn], r_["slotv"]], [r_["junk"]])
                S.add("dve", lambda e, k=k: e.tensor_reduce(out=r_["sl"].ap[:, k:k + 1], in_=r_["junk"].ap, axis=AX.X, op=ALU.add),
                      R(r_["junk"]), R(r_["sl"]))
                tt("dve", r_["junk"].ap, r_[ohn].ap, r_["val"].ap, ALU.mult, [r_[ohn], r_["val"]], [r_["junk"]])
                S.add("dve", lambda e, k=k: e.tensor_reduce(out=r_["vk"].ap[:, k:k + 1], in_=r_["junk"].ap, axis=AX.X, op=ALU.add),
                      R(r_["junk"]), R(r_["vk"]))
            cp("dve", sloti.ap[:, i, :], r_["sl"].ap, [r_["sl"]], [sloti])
            stt(wgt.ap[:, i, 0:1], r_["p1"].ap, r_["pgs"].ap[:, 0:1], r_["vk"].ap[:, 0:1], ALU.mult, ALU.mult,
                [r_["p1"], r_["pgs"], r_["vk"]], [wgt])
            stt(wgt.ap[:, i, 1:2], r_["p2"].ap, r_["pgs"].ap[:, 0:1], r_["vk"].ap[:, 1:2], ALU.mult, ALU.mult,
                [r_["p2"], r_["pgs"], r_["vk"]], [wgt])
            if dbg.get("tile") == i:
                dump("lg", lg)
                dump("rank", r_["rank"])
            for k in range(2):
                scat_ops.append(S.add("pool", lambda e, i=i, k=k: e.indirect_dma_start(
                    out=X_d, out_offset=bass.IndirectOffsetOnAxis(ap=sloti.ap[:, i, k:k + 1], axis=0),
                    in_=xn2.ap, in_offset=None, bounds_check=breg(e, NSLOT - 1), oob_is_err=False),
                    R(xn2, sloti), [r_X], dma_key="scat"))

        for s_ in range(NTILES + 2):
            ip, iq, ir = s_, s_ - 1, s_ - 2
            hp = 0 <= ip < NTILES
            hq = 0 <= iq < NTILES
            hr = 0 <= ir < NTILES
            if hr:
                R0(ir)
            if hq:
                QB(iq, 0)
            if hr:
                R1(ir, 0)
                R1(ir, 1)
            if hp:
                P1(ip)
            if hq:
                QD(iq, 0)
            if hr:
                R1(ir, 2)
                R1(ir, 3)
                R2(ir, 0)
                R2(ir, 1)
            if hq:
                QB(iq, 1)
                QC(iq, 0)
            if hp:
                P2(ip)
            if hq:
                QD(iq, 1)
            if hr:
                R3(ir)
            if hq:
                QB(iq, 2)
                QC(iq, 1)
                QD(iq, 2)
            if hr:
                R4(ir)
            if hq:
                QB(iq, 3)
                QC(iq, 2)
            if hp:
                P3(ip)
            if hq:
                QD(iq, 3)
                QC(iq, 3)
                Qcarry(iq)
                Qtail(iq)
        if "route" in dbg:
            dump("wgt", wgt)
            dump("sloti", sloti)
        if stop_after == "A":
            S.finish(final_ops + store_ops + scat_ops)
            S.emit(st)
            return nc, dbg_outs

        S.barrier(dma_ops=[scat_ops[-1], store_ops[-1]])
        A.reset(mark_persist)
        w1b = [A.alloc("w1b%d" % i, [128, 8, 512], BF16) for i in range(2)]
        w3b = [A.alloc("w3b%d" % i, [128, 8, 512], BF16) for i in range(2)]
        w2b = [A.alloc("w2b%d" % i, [128, 4, D], BF16) for i in range(2)]
        Xblk = [A.alloc("Xblk%d" % i, [128, D], BF16) for i in range(3)]
        XT = [A.alloc("XT%d" % i, [128, 8, CAP], BF16) for i in range(2)]
        hidT = [A.alloc("hidT%d" % i, [128, 4, CAP], BF16) for i in range(2)]
        s1 = [A.alloc("s1_%d" % i, [128, 512], F32) for i in range(2)]
        Yblk = [A.alloc("Yblk%d" % i, [128, D], F32) for i in range(3)]
        NH = (CAP + 511) // 512
        HW_ = CAP // NH
        ystore = []
        cnt = {"x": 0, "y": 0, "h": 0}

        def W13(e_):
            sl_ = e_ % 2
            dma("pool", w1b[sl_].ap, w1_d[e_].rearrange("(kc p) n -> p kc n", p=128), [], [w1b[sl_]], "w1b%d" % sl_)
            dma("pool", w3b[sl_].ap, w3_d[e_].rearrange("(kc p) n -> p kc n", p=128), [], [w3b[sl_]], "w3b%d" % sl_)

        def W2(e_):
            sl_ = e_ % 2
            dma("pool", w2b[sl_].ap, w2_d[e_].rearrange("(kc p) n -> p kc n", p=128), [], [w2b[sl_]], "w2b%d" % sl_)

        def TX(e_):
            xt_ = XT[e_ % 2]
            for blk in range(NBLK):
                n_ = cnt["x"]
                cnt["x"] += 1
                xb_ = Xblk[n_ % 3]
                r0 = e_ * CAP + blk * 128
                dma("sp", xb_.ap, X_d[r0:r0 + 128, :], [r_X], [xb_], "xblk%d" % (n_ % 3))
                bk = n_ % 2
                pXT = psv(bk, [128, 8, 128], BF16)
                for kc in range(8):
                    tr(pXT[:, kc, :], xb_.ap[:, kc * 128:(kc + 1) * 128], ident_b.ap, [xb_, ident_b], [PSB[bk]])
                cp("act" if blk % 2 == 0 else "dve", xt_.ap[:, :, blk * 128:(blk + 1) * 128], pXT, [PSB[bk]], [xt_])

        def HH(e_):
            sl_ = e_ % 2
            xt_, hd_ = XT[e_ % 2], hidT[e_ % 2]
            for m in range(4):
                for nh in range(NH):
                    cs_ = slice(nh * HW_, (nh + 1) * HW_)
                    n_ = cnt["h"]
                    cnt["h"] += 1
                    b1 = 2 + n_ % 2
                    b3 = 4 + n_ % 2
                    p1_ = PSB[b1].ap[:, 0:HW_]
                    p3_ = PSB[b3].ap[:, 0:HW_]
                    for kc in range(8):
                        mm(p1_, w1b[sl_].ap[:, kc, m * 128:(m + 1) * 128], xt_.ap[:, kc, cs_], kc == 0, kc == 7,
                           [w1b[sl_], xt_], [PSB[b1]])
                    for kc in range(8):
                        mm(p3_, w3b[sl_].ap[:, kc, m * 128:(m + 1) * 128], xt_.ap[:, kc, cs_], kc == 0, kc == 7,
                           [w3b[sl_], xt_], [PSB[b3]])
                    s1_ = s1[n_ % 2]
                    act(s1_.ap[:, 0:HW_], p1_, AF.Silu, [PSB[b1]], [s1_])
                    tt("dve", hd_.ap[:, m, cs_], s1_.ap[:, 0:HW_], p3_, ALU.mult, [s1_, PSB[b3]], [hd_])

        def YY(e_):
            sl_ = e_ % 2
            hd_ = hidT[e_ % 2]
            for blk in range(NBLK):
                n_ = cnt["y"]
                cnt["y"] += 1
                yb_ = Yblk[n_ % 3]
                for half in range(2):
                    bk = 6 + half
                    for kc in range(4):
                        mm(PSB[bk].ap, hd_.ap[:, kc, blk * 128:(blk + 1) * 128], w2b[sl_].ap[:, kc, half * 512:(half + 1) * 512],
                           kc == 0, kc == 3, [hd_, w2b[sl_]], [PSB[bk]])
                    cp("act" if half == 0 else "dve", yb_.ap[:, half * 512:(half + 1) * 512], PSB[bk].ap, [PSB[bk]], [yb_])
                r0 = e_ * CAP + blk * 128
                ystore.append(dma("sp", Y_d[r0:r0 + 128, :], yb_.ap, [yb_], [r_Y], "yst%d" % (n_ % 3)))

        W13(0)
        W2(0)
        W13(1)
        W2(1)
        TX(0)
        for e_ in range(32):
            HH(e_)
            if e_ + 2 < 32:
                W13(e_ + 2)
            if e_ + 1 < 32:
                TX(e_ + 1)
            YY(e_)
            if e_ + 2 < 32:
                W2(e_ + 2)
        if stop_after == "B":
            S.finish(final_ops + ystore[-3:])
            S.emit(st)
            return nc, dbg_outs

        S.barrier(dma_ops=ystore[-3:])
        A.reset(mark_persist)
        NBC = 3
        Hc = [A.alloc("Hc%d" % i, [128, D], F32) for i in range(NBC)]
        Y0 = [A.alloc("Y0_%d" % i, [128, D], F32) for i in range(NBC)]
        Y1 = [A.alloc("Y1_%d" % i, [128, D], F32) for i in range(NBC)]
        acc = A.alloc("acc", [128, D], F32)
        ob = [A.alloc("ob%d" % i, [128, D], F32) for i in range(2)]
        gt2b = A.alloc("gt2b", [128, D], F32)
        gfb = A.alloc("gfb", [128, D], F32)
        jk = A.alloc("jk", [128, D], BF16)
        ssq3 = A.alloc("ssq3", [128, 1], F32)
        rstd3 = A.alloc("rstd3", [128, 1], F32)
        dma("sp", gfb.ap, gf_d.partition_broadcast(128), [], [gfb], "gfb")
        for s_ in range(NBC):
            memset("pool", Y0[s_].ap, 0.0, [Y0[s_]])
            memset("pool", Y1[s_].ap, 0.0, [Y1[s_]])

        def Cload(i):
            s_ = i % NBC
            dma("sp", Hc[s_].ap, H_d[i * 128:(i + 1) * 128, :], [r_H], [Hc[s_]], "hc%d" % s_)
            for k, Yk in enumerate((Y0[s_], Y1[s_])):
                S.add("pool", lambda e, i=i, k=k, Yk=Yk: e.indirect_dma_start(
                    out=Yk.ap, out_offset=None, in_=Y_d, in_offset=bass.IndirectOffsetOnAxis(ap=sloti.ap[:, i, k:k + 1], axis=0),
                    bounds_check=breg(e, NSLOT - 1), oob_is_err=False), R(r_Y, sloti), R(Yk), dma_key="gath%d_%d" % (k, s_))

        Cload(0)
        if NTILES > 1:
            Cload(1)
        for i in range(NTILES):
            b, tau = i // NT, i % NT
            s_ = i % NBC
            if tau == 0:
                dma("sp", gt2b.ap, mod_d.ap[b:b + 1, 5 * D:6 * D].partition_broadcast(128), [mod_d], [gt2b], "gt2b")
            if i + 2 < NTILES:
                Cload(i + 2)
            ts("dve", acc.ap, Y0[s_].ap, wgt.ap[:, i, 0:1], None, ALU.mult, None, [Y0[s_], wgt], [acc])
            stt(acc.ap, Y1[s_].ap, wgt.ap[:, i, 1:2], acc.ap, ALU.mult, ALU.add, [Y1[s_], wgt, acc], [acc])
            tt("dve", acc.ap, acc.ap, gt2b.ap, ALU.mult, [acc, gt2b], [acc])
            tt("dve", acc.ap, acc.ap, Hc[s_].ap, ALU.add, [acc, Hc[s_]], [acc])
            rms_rstd(acc, ssq3, rstd3, jk.ap, jk)
            stt(ob[i % 2].ap, acc.ap, rstd3.ap[:, 0:1], gfb.ap, ALU.mult, ALU.mult, [acc, rstd3, gfb], [ob[i % 2]])
            final_ops.append(dma("sp", out_d[i * 128:(i + 1) * 128, :], ob[i % 2].ap, [ob[i % 2]], [], "ost%d" % (i % 2)))
        S.finish(final_ops)
        S.emit(st)
    return nc, dbg_outs


def prep_shared(inp):
    f = np.float32
    g = {}
    L = 0
    g["w_ada"] = np.ascontiguousarray(inp["w_ada"][L], f)
    g["g1T"] = np.ascontiguousarray(inp["norm1_g"][L].reshape(8, 128).T, f)
    g["w_in"] = np.ascontiguousarray(inp["w_in"][L], f)
    g["w_gate"] = np.ascontiguousarray(inp["w_gate"][L], f)
    g["b_gateT"] = np.ascontiguousarray(inp["b_gate"][L].reshape(16, 128).T, f)
    a_re, a_im, ls = inp["ssm_a_re"][L], inp["ssm_a_im"][L], inp["ssm_log_step"][L]
    def sm(v):
        return v.reshape(16, 2, 64).transpose(1, 2, 0).reshape(128, 16)
    lsx = np.repeat(ls[:, None], 64, axis=1)
    g["lam_sm"] = np.ascontiguousarray(np.stack([sm(a_re), sm(a_im), sm(lsx)], axis=1), f)
    def cl(v):
        s = sm(v)
        o = np.zeros((128, 4, 2, 128), f)
        for p in range(128):
            for q in range(4):
                for jo in range(2):
                    o[p, q, jo, :] = s[:, 4 * q + 2 * (p // 64) + jo]
        return o
    g["lam_c"] = np.ascontiguousarray(np.stack([cl(a_re), cl(a_im), cl(lsx)], axis=1), f)
    Bc = np.zeros((128, 2, 4, 2, 128), f)
    for pi, Bsrc in enumerate((inp["ssm_b_re"][L], inp["ssm_b_im"][L])):
        for gi in range(32):
            j = gi // 2
            p0 = 32 * (j % 4) + 16 * (gi % 2)
            n0 = 64 * (gi % 2)
            Bc[p0:p0 + 16, pi, j // 4, j % 2, n0:n0 + 64] = Bsrc[gi].T
    g["Bc"] = Bc
    Cc = np.zeros((128, 2, 16, 64), f)
    for pi, Csrc in enumerate((inp["ssm_c_re"][L], inp["ssm_c_im"][L])):
        for gi in range(32):
            j = gi // 2
            n0 = 64 * (gi % 2)
            c0 = 32 * (j % 2) + 16 * (gi % 2)
            Cc[n0:n0 + 64, pi, j, c0:c0 + 16] = Csrc[gi].T
    g["Cc"] = Cc
    g["dT"] = np.ascontiguousarray(inp["ssm_d"][L].reshape(4, 128).T, f)
    g["w_glu"] = np.ascontiguousarray(inp["w_glu"][L], f)
    g["b_gluT"] = np.ascontiguousarray(inp["b_glu"][L].reshape(4, 128).T, f)
    g["wsT"] = np.ascontiguousarray(inp["sgu_w"][L].transpose(2, 0, 1), f)
    g["lngT"] = np.ascontiguousarray(inp["sgu_ln_g"][L].reshape(4, 128).T, f)
    g["lnbT"] = np.ascontiguousarray(inp["sgu_ln_b"][L].reshape(4, 128).T, f)
    bs = inp["sgu_b"][L]
    bsT = np.zeros((128, 4, 128), f)
    for q in range(4):
        bsT[0:64, q, :] = bs[2 * q][None, :]
        bsT[64:128, q, :] = bs[2 * q + 1][None, :]
    g["bsT"] = bsT
    g["w_bra"] = np.ascontiguousarray(inp["w_branch_a"][L], f)
    g["w_brb"] = np.ascontiguousarray(inp["w_branch_b"][L], f)
    g["w_out"] = np.ascontiguousarray(inp["w_out"][L], f)
    g["g2"] = np.ascontiguousarray(inp["norm2_g"][L].reshape(1, D), f)
    wr = np.concatenate([inp["w_router_group"][L], inp["w_router_expert"][L].transpose(1, 0, 2).reshape(D, 32)], axis=1)
    g["w_r"] = np.ascontiguousarray(wr.reshape(8, 128, 36).transpose(1, 0, 2), f)
    g["b_r"] = np.ascontiguousarray(np.concatenate([inp["b_router_group"][L], inp["b_router_expert"][L].reshape(32)]).reshape(1, 36), f)
    g["w1"] = np.ascontiguousarray(inp["w1"][L], f)
    g["w3"] = np.ascontiguousarray(inp["w3"][L], f)
    g["w2"] = np.ascontiguousarray(inp["w2"][L], f)
    g["gf"] = np.ascontiguousarray(inp["norm_f_g"].reshape(1, D), f)
    return g


def prep_core(inp, shared, b0, nseq):
    m = dict(shared)
    xs = inp["x"][b0:b0 + nseq]
    m["x"] = np.ascontiguousarray(xs.reshape(-1, D), np.float32)
    c = inp["c"][b0:b0 + nseq]
    m["cT"] = np.ascontiguousarray(c.reshape(nseq, 8, 128).transpose(2, 1, 0), np.float32)
    m["b_ada_rep"] = np.ascontiguousarray(np.repeat(inp["b_ada"][0][None, :], nseq, axis=0), np.float32)
    return m


_CACHE = {}


def kernel(**inputs):
    inp = {k: np.asarray(v) for k, v in inputs.items()}
    B, SEQ = inp["x"].shape[0], inp["x"].shape[1]
    nseq = B // N_CORES
    key = (nseq, SEQ)
    if key not in _CACHE:
        _CACHE[key] = build(NSEQ=nseq, SEQ=SEQ, CAP=768)[0]
    nc = _CACHE[key]
    shared = prep_shared(inp)
    in_maps = [prep_core(inp, shared, c * nseq, nseq) for c in range(N_CORES)]
    res = run_bass_kernel_spmd(nc, in_maps, core_ids=list(range(N_CORES)))
    outs = [np.asarray(r["out"]).reshape(nseq, SEQ, D) for r in res.results]
    return np.concatenate(outs, axis=0).astype(np.float32)
```

```python
import math
from contextlib import ExitStack
import numpy as np
import concourse.bass as bass
import concourse.mybir as mybir
from concourse.bass_utils import run_bass_kernel_spmd

F32 = mybir.dt.float32
BF16 = mybir.dt.bfloat16
I32 = mybir.dt.int32
U8 = mybir.dt.uint8
AF = mybir.ActivationFunctionType
ALU = mybir.AluOpType
AX = mybir.AxisListType

ENGS = ("pe", "act", "dve", "pool", "sp")
N_CORES = 8
D = 1024
TWO_PI = 2.0 * math.pi


class Res:
    __slots__ = ("name", "lastw", "readers")

    def __init__(self, name):
        self.name = name
        self.lastw = None
        self.readers = []


class Op:
    __slots__ = ("eng", "fn", "deps", "dma_key", "dma_val", "signal", "sig_idx")

    def __init__(self, eng, fn, dma_key):
        self.eng = eng
        self.fn = fn
        self.deps = []
        self.dma_key = dma_key
        self.dma_val = None
        self.signal = False
        self.sig_idx = None


class Sched:
    def __init__(self, nc):
        self.nc = nc
        self.ops = {e: [] for e in ENGS}
        self.dma_cnt = {}
        self.finals = []
        self.pending_barrier = {}

    def add(self, eng, fn, reads=(), writes=(), dma_key=None):
        op = Op(eng, fn, dma_key)
        deps = []
        for r in reads:
            if r.lastw is not None:
                deps.append(r.lastw)
        for w in writes:
            if w.lastw is not None:
                deps.append(w.lastw)
            deps.extend(w.readers)
        if eng in self.pending_barrier:
            deps.extend(self.pending_barrier.pop(eng))
        seen = set()
        for d in deps:
            if d is op or id(d) in seen:
                continue
            seen.add(id(d))
            if d.eng == "pe" and eng == "pe" and d.dma_key is None and dma_key is None:
                continue
            op.deps.append(d)
            if d.dma_key is None:
                d.signal = True
        if dma_key is not None:
            self.dma_cnt[dma_key] = self.dma_cnt.get(dma_key, 0) + 16
            op.dma_val = self.dma_cnt[dma_key]
        for r in reads:
            r.readers.append(op)
        for w in writes:
            w.lastw = op
            w.readers = []
        self.ops[eng].append(op)
        return op

    def barrier(self, dma_ops=()):
        lasts = [self.ops[e][-1] for e in ENGS if self.ops[e]]
        lasts = [o for o in lasts if o.dma_key is None] + list(dma_ops)
        for e in ENGS:
            self.pending_barrier.setdefault(e, []).extend(lasts)

    def finish(self, ops):
        self.finals.extend(ops)

    def emit(self, stack):
        nc = self.nc
        sems = {e: stack.enter_context(nc.semaphore("sem_" + e)) for e in ENGS}
        dsem = {k: stack.enter_context(nc.semaphore("dsem_%s" % (k,))) for k in self.dma_cnt}
        for e in ENGS:
            n = 0
            for op in self.ops[e]:
                if op.dma_key is None and op.signal:
                    n += 1
                    op.sig_idx = n
        block = stack.enter_context(nc.Block())
        engobj = {"pe": "tensor", "act": "scalar", "dve": "vector", "pool": "gpsimd", "sp": "sync"}
        finals = self.finals

        def body_for(e):
            def body(eng):
                seen = {}
                for op in self.ops[e]:
                    need = {}
                    for d in op.deps:
                        if d.dma_key is not None:
                            s, v, key = dsem[d.dma_key], d.dma_val, ("d", d.dma_key)
                        else:
                            s, v, key = sems[d.eng], d.sig_idx, ("e", d.eng)
                        if key not in need or need[key][1] < v:
                            need[key] = (s, v)
                    for key, (s, v) in need.items():
                        if seen.get(key, 0) >= v:
                            continue
                        seen[key] = v
                        eng.wait_ge(s, v)
                    inst = op.fn(eng)
                    if op.dma_key is not None:
                        inst.then_inc(dsem[op.dma_key], 16)
                    elif op.signal:
                        inst.then_inc(sems[e], 1)
                if e == "sp":
                    for d in finals:
                        eng.wait_ge(dsem[d.dma_key], d.dma_val)
            return body

        for e in ENGS:
            getattr(block, engobj[e])(body_for(e))


class Tl:
    __slots__ = ("ap", "r")

    def __init__(self, ap, name):
        self.ap = ap
        self.r = Res(name)


class Arena:
    def __init__(self, nc, stack, nbytes):
        self.buf = stack.enter_context(nc.sbuf_tensor("arena", [128, nbytes], U8))
        self.off = 0
        self.cap = nbytes
        self.live = []

    def alloc(self, name, shape, dt):
        esz = {F32: 4, BF16: 2, I32: 4}[dt]
        n = 1
        for s in shape[1:]:
            n *= s
        nb = (n * esz + 31) // 32 * 32
        assert self.off + nb <= self.cap, "SBUF arena overflow at %s: %d + %d > %d" % (name, self.off, nb, self.cap)
        v = self.buf[0:shape[0], self.off:self.off + n * esz].bitcast(dt)
        if len(shape) == 3:
            v = v.rearrange("p (a b) -> p a b", a=shape[1])
        elif len(shape) == 4:
            v = v.rearrange("p (a b c) -> p a b c", a=shape[1], b=shape[2])
        t = Tl(v, name)
        lo, hi = self.off, self.off + nb
        keep = []
        for (a, b, o) in self.live:
            if a < hi and lo < b:
                if o.r.lastw is not None:
                    t.r.readers.append(o.r.lastw)
                t.r.readers.extend(o.r.readers)
                if a >= lo and b <= hi:
                    continue
            keep.append((a, b, o))
        keep.append((lo, hi, t))
        self.live = keep
        self.off += nb
        return t

    def mark(self):
        return self.off

    def reset(self, m):
        self.off = m


def build(NSEQ=4, SEQ=2048, CAP=768, dbg=None, stop_after=None):
    nc = bass.Bass("TRN2", target_bir_lowering=False)
    NT = SEQ // 128
    NTOK = NSEQ * SEQ
    NTILES = NSEQ * NT
    NSLOT = 32 * CAP
    NBLK = CAP // 128
    dbg = dbg or {}
    dbg_outs = {}

    def din(name, shape, dt=F32):
        return nc.dram_tensor(name, list(shape), dt, kind="ExternalInput").ap()

    x_d = din("x", [NTOK, D])
    cT_d = din("cT", [128, 8, NSEQ])
    w_ada_d = din("w_ada", [D, 6 * D])
    b_ada_d = din("b_ada_rep", [NSEQ, 6 * D])
    g1T_d = din("g1T", [128, 8])
    w_in_d = din("w_in", [D, 1536])
    w_gate_d = din("w_gate", [D, 2048])
    b_gateT_d = din("b_gateT", [128, 16])
    lam_sm_d = din("lam_sm", [128, 3, 16])
    Bsm_d = din("Bsm", [128, 2, 16, 128])
    Csm_d = din("Csm", [128, 2, 16, 128])
    dT_d = din("dT", [128, 4])
    w_glu_d = din("w_glu", [512, 512])
    b_gluT_d = din("b_gluT", [128, 4])
    wsT_d = din("wsT", [128, 8, 128])
    lngT_d = din("lngT", [128, 4])
    lnbT_d = din("lnbT", [128, 4])
    bsT_d = din("bsT", [128, 4, 128])
    w_bra_d = din("w_bra", [512, D])
    w_brb_d = din("w_brb", [512, D])
    w_out_d = din("w_out", [D, D])
    g2_d = din("g2", [1, D])
    w_r_d = din("w_r", [128, 8, 36])
    b_r_d = din("b_r", [1, 36])
    w1_d = din("w1", [32, D, 512])
    w3_d = din("w3", [32, D, 512])
    w2_d = din("w2", [32, 512, D])
    gf_d = din("gf", [1, D])
    out_d = nc.dram_tensor("out", [NTOK, D], F32, kind="ExternalOutput").ap()
    mod_d = Tl(nc.dram_tensor("mod_d", [NSEQ, 6 * D], F32, kind="Internal").ap(), "mod_d")
    H_d = nc.dram_tensor("H_d", [NTOK, D], F32, kind="Internal").ap()
    X_d = nc.dram_tensor("X_d", [NSLOT, D], BF16, kind="Internal").ap()
    Y_d = nc.dram_tensor("Y_d", [NSLOT, D], F32, kind="Internal").ap()
    r_X = Res("X_d")
    r_Y = Res("Y_d")
    r_H = Res("H_d")

    S = Sched(nc)
    final_ops = []
    with ExitStack() as st:
        A = Arena(nc, st, 212800)
        ps_all = st.enter_context(nc.psum_tensor("ps_all", [128, 8, 512], F32))
        PSB = [Tl(ps_all[:, b, :], "psb%d" % b) for b in range(8)]

        def psv(b, shape, dt=F32):
            v = PSB[b].ap
            if dt == BF16:
                v = v.bitcast(BF16)
            n = 1
            for s in shape[1:]:
                n *= s
            v = v[0:shape[0], 0:n]
            if len(shape) == 3:
                v = v.rearrange("p (a b) -> p a b", a=shape[1])
            elif len(shape) == 4:
                v = v.rearrange("p (a b c) -> p a b c", a=shape[1], b=shape[2])
            return v

        def psv2(b, shape):
            v = ps_all[:, b:b + 2, :].rearrange("p a b -> p (a b)")
            n = 1
            for s in shape[1:]:
                n *= s
            v = v[0:shape[0], 0:n]
            if len(shape) == 3:
                v = v.rearrange("p (a b) -> p a b", a=shape[1])
            elif len(shape) == 4:
                v = v.rearrange("p (a b c) -> p a b c", a=shape[1], b=shape[2])
            return v

        def R(*ts):
            return [t.r if isinstance(t, Tl) else t for t in ts]

        def dma(eng, out, in_, reads, writes, key, **kw):
            return S.add(eng, lambda e: e.dma_start(out=out, in_=in_, **kw), R(*reads), R(*writes), dma_key=key)

        def tt(eng, out, in0, in1, op, reads, writes):
            return S.add(eng, lambda e: e.tensor_tensor(out=out, in0=in0, in1=in1, op=op), R(*reads), R(*writes))

        def ts(eng, out, in0, s1, s2, op0, op1, reads, writes, accum=None):
            if op1 is None:
                return S.add(eng, lambda e: e.tensor_scalar(out=out, in0=in0, scalar1=s1, scalar2=None, op0=op0), R(*reads), R(*writes))
            if accum is not None:
                return S.add(eng, lambda e: e.tensor_scalar(out=out, in0=in0, scalar1=s1, scalar2=s2, op0=op0, op1=op1, accum_out=accum), R(*reads), R(*writes))
            return S.add(eng, lambda e: e.tensor_scalar(out=out, in0=in0, scalar1=s1, scalar2=s2, op0=op0, op1=op1), R(*reads), R(*writes))

        def stt(out, in0, scalar, in1, op0, op1, reads, writes):
            return S.add("dve", lambda e: e.scalar_tensor_tensor(out=out, in0=in0, scalar=scalar, in1=in1, op0=op0, op1=op1), R(*reads), R(*writes))

        def act(out, in_, func, reads, writes, bias=None, scale=None, accum=None):
            kw = {}
            if bias is not None:
                kw["bias"] = bias
            if scale is not None:
                kw["scale"] = scale
            if accum is not None:
                kw["accum_out"] = accum
            return S.add("act", lambda e: e.activation(out=out, in_=in_, func=func, **kw), R(*reads), R(*writes))

        def cp(eng, out, in_, reads, writes):
            if eng == "act":
                return S.add("act", lambda e: e.copy(out=out, in_=in_), R(*reads), R(*writes))
            return S.add(eng, lambda e: e.tensor_copy(out=out, in_=in_), R(*reads), R(*writes))

        def mm(out, lhsT, rhs, start, stop, reads, writes):
            return S.add("pe", lambda e: e.matmul(out, lhsT=lhsT, rhs=rhs, start=start, stop=stop), R(*reads), R(*writes))

        def tr(out, in_, ident, reads, writes):
            return S.add("pe", lambda e: e.transpose(out=out, in_=in_, identity=ident), R(*reads), R(*writes))

        def memset(eng, ap, val, writes):
            return S.add(eng, lambda e: e.memset(ap, val), [], R(*writes))

        _regs = {}

        def breg(e, val):
            if val not in _regs:
                _regs[val] = e.to_reg(val)
            return _regs[val]

        def dump(name, t, ap=None):
            ap = t.ap if ap is None else ap
            shp = list(ap.shape)
            o = nc.dram_tensor("dbg_" + name, shp, ap.dtype, kind="ExternalOutput").ap()
            dbg_outs[name] = "dbg_" + name
            final_ops.append(dma("sp", o, ap, [t], [], "dbg_" + name))

        ident_f = A.alloc("ident_f", [128, 128], F32)
        ident_b = A.alloc("ident_b", [128, 128], BF16)
        tri_b = A.alloc("tri_b", [128, 128], BF16)
        ones_b = A.alloc("ones_b", [128, 128], BF16)
        memset("pool", ident_f.ap, 0.0, [ident_f])
        S.add("pool", lambda e: e.affine_select(out=ident_f.ap, in_=ident_f.ap, pattern=[[-1, 128]], compare_op=ALU.not_equal,
                                                  fill=1.0, base=0, channel_multiplier=1), R(ident_f), R(ident_f))
        cp("pool", ident_b.ap, ident_f.ap, [ident_f], [ident_b])
        memset("pool", ones_b.ap, 1.0, [ones_b])
        S.add("pool", lambda e: e.affine_select(out=tri_b.ap, in_=ones_b.ap, pattern=[[1, 128]], compare_op=ALU.is_gt,
                                                  fill=0.0, base=0, channel_multiplier=-1), R(ones_b), R(tri_b))

        wgt = A.alloc("wgt", [128, NTILES, 2], F32)
        sloti = A.alloc("sloti", [128, NTILES, 2], I32)
        base = A.alloc("base", [128, 32], F32)
        ecap = A.alloc("ecap", [128, 32], F32)
        memset("pool", base.ap, 0.0, [base])
        ecap_i = A.alloc("ecap_i", [128, 32], I32)
        S.add("pool", lambda e: e.iota(ecap_i.ap, pattern=[[CAP, 32]], base=0, channel_multiplier=0), [], R(ecap_i))
        cp("pool", ecap.ap, ecap_i.ap, [ecap_i], [ecap])
        A1T = A.alloc("A1T", [128, 8, NSEQ], F32)
        sh1T = A.alloc("sh1T", [128, 8, NSEQ], F32)
        eps_rms = A.alloc("eps_rms", [128, 1], F32)
        eps_ln = A.alloc("eps_ln", [128, 1], F32)
        memset("pool", eps_rms.ap, 1e-6, [eps_rms])
        memset("pool", eps_ln.ap, 1e-5, [eps_ln])

        mark_persist = A.mark()

        cact = A.alloc("cact", [128, 8, NSEQ], F32)
        dma("sp", cact.ap, cT_d, [], [cact], "cact")
        act(cact.ap, cact.ap, AF.Silu, [cact], [cact])
        modrow = A.alloc("modrow", [NSEQ, 6 * D], F32)
        bada = A.alloc("bada", [NSEQ, 6 * D], F32)
        dma("sp", bada.ap, b_ada_d, [], [bada], "bada")
        wa = [A.alloc("wa%d" % i, [128, 8, 512], F32) for i in range(2)]
        wa_view = w_ada_d.rearrange("(kc p) n -> p kc n", p=128)
        for cb in range(12):
            w = wa[cb % 2]
            dma("sp", w.ap, wa_view[:, :, cb * 512:(cb + 1) * 512], [], [w], "wa%d" % (cb % 2))
            pb = PSB[cb % 2]
            for kc in range(8):
                mm(pb.ap[0:NSEQ, :], cact.ap[:, kc, :], w.ap[:, kc, :], kc == 0, kc == 7, [cact, w], [pb])
            tt("dve", modrow.ap[:, cb * 512:(cb + 1) * 512], pb.ap[0:NSEQ, :], bada.ap[:, cb * 512:(cb + 1) * 512], ALU.add,
               [pb, bada], [modrow])
        dma("sp", mod_d.ap, modrow.ap, [modrow], [mod_d], "mod_d")
        sc1T = A.alloc("sc1T", [128, 8, NSEQ], F32)
        g1T = A.alloc("g1T", [128, 8], F32)
        dma("sp", g1T.ap, g1T_d, [], [g1T], "g1T")
        for b in range(NSEQ):
            S.add("sp", lambda e, b=b: e.dma_start(out=sh1T.ap[:, :, b], in_=mod_d.ap[b, 0:D].rearrange("(kc p) -> p kc", p=128),
                                                  allow_slow_non_contiguous=True), R(mod_d), R(sh1T), dma_key="sh1T")
            S.add("sp", lambda e, b=b: e.dma_start(out=sc1T.ap[:, :, b], in_=mod_d.ap[b, D:2 * D].rearrange("(kc p) -> p kc", p=128),
                                                  allow_slow_non_contiguous=True), R(mod_d), R(sc1T), dma_key="sc1T")
        for b in range(NSEQ):
            stt(A1T.ap[:, :, b], sc1T.ap[:, :, b], 1.0, g1T.ap, ALU.add, ALU.mult, [sc1T, g1T], [A1T])
        if "mod" in dbg:
            dump("modrow", modrow)
            dump("A1T", A1T)
        A.reset(mark_persist)
        S.barrier()
        if stop_after == "mod":
            S.finish(final_ops)
            S.emit(st)
            return nc, dbg_outs

        w_r = A.alloc("w_r", [128, 8, 36], F32)
        dma("sp", w_r.ap, w_r_d, [], [w_r], "w_r")
        b_r = A.alloc("b_r", [128, 36], F32)
        dma("sp", b_r.ap, b_r_d.partition_broadcast(128), [], [b_r], "b_r")
        b_gateT = A.alloc("b_gateT", [128, 16], F32)
        dma("sp", b_gateT.ap, b_gateT_d, [], [b_gateT], "b_gateT")
        b_gluT = A.alloc("b_gluT", [128, 4], F32)
        dma("sp", b_gluT.ap, b_gluT_d, [], [b_gluT], "b_gluT")
        dT = A.alloc("dT", [128, 4], F32)
        dma("sp", dT.ap, dT_d, [], [dT], "dT")
        lngT = A.alloc("lngT", [128, 4], F32)
        dma("sp", lngT.ap, lngT_d, [], [lngT], "lngT")
        lnbT = A.alloc("lnbT", [128, 4], F32)
        dma("sp", lnbT.ap, lnbT_d, [], [lnbT], "lnbT")

        LC = 4
        NCH = 128 // LC
        tabC4 = A.alloc("tabC4", [128, 16, NCH + 1], F32)
        tabD4 = A.alloc("tabD4", [128, 16, NCH + 1], F32)
        Rtab = A.alloc("Rtab", [128, 16, 2, NCH], F32)
        rmagL = A.alloc("rmagL", [128, 16], F32)
        Pt = A.alloc("Pt", [128, 2, 8 * LC, 128], BF16)
        Qs = A.alloc("Qs", [128, 2, 16 * LC, 64], BF16)
        Kb = A.alloc("Kb", [128, 4 * LC, 128], BF16)
        tabC = Tl(None, "s5scr")
        mark_setup = A.mark()

        def range_reduce(ph, tmpf, tmpi, n):
            ts("dve", tmpi, ph, 1.0 / TWO_PI, None, ALU.mult, None, [tabC], [tabC])
            cp("dve", tmpf, tmpi, [tabC], [tabC])
            stt(ph, tmpf, -TWO_PI, ph, ALU.mult, ALU.add, [tabC], [tabC])
            wrap(ph, tmpf)

        def wrap(ph, tmpf):
            ts("dve", tmpf, ph, math.pi, None, ALU.is_gt, None, [tabC], [tabC])
            stt(ph, tmpf, -TWO_PI, ph, ALU.mult, ALU.add, [tabC], [tabC])
            ts("dve", tmpf, ph, -math.pi, None, ALU.is_lt, None, [tabC], [tabC])
            stt(ph, tmpf, TWO_PI, ph, ALU.mult, ALU.add, [tabC], [tabC])

        def scr(name, shape, dt):
            t = A.alloc(name, shape, dt)
            t.r = tabC.r
            return t

        TC = [tabC]
        lam = scr("lam", [128, 3, 16], F32)
        dma("sp", lam.ap, lam_sm_d, [], TC, "lam")
        zs = {n: scr("z_" + n, [128, 16], F32) for n in ("dt", "th", "lre", "lrdt", "den", "nr", "fre", "fim", "t0", "t1")}
        sv_i = scr("sv_i", [128, 129], I32)
        sv = scr("sv", [128, 129], F32)
        tabCf = scr("tabCf", [128, 16, 129], F32)
        tabDf = scr("tabDf", [128, 16, 129], F32)
        tmpf = scr("tmpf", [128, 16 * 129], F32)
        tmpi = scr("tmpi", [128, 16 * 129], I32)
        aim = lam.ap[:, 1, :]
        S.add("pool", lambda e: e.iota(sv_i.ap, pattern=[[1, 129]], base=0, channel_multiplier=0), [], R(tabC))
        cp("dve", sv.ap, sv_i.ap, TC, TC)
        ts("dve", zs["lre"].ap, lam.ap[:, 0, :], -1e-4, None, ALU.min, None, TC, TC)
        act(zs["dt"].ap, lam.ap[:, 2, :], AF.Exp, TC, TC)
        tt("dve", zs["lrdt"].ap, zs["lre"].ap, zs["dt"].ap, ALU.mult, TC, TC)
        tt("dve", zs["th"].ap, aim, zs["dt"].ap, ALU.mult, TC, TC)
        for j in range(16):
            ts("dve", tabDf.ap[:, j, :], sv.ap, zs["th"].ap[:, j:j + 1], None, ALU.mult, None, TC, TC)
        phD = tabDf.ap.rearrange("p a b -> p (a b)")
        phC = tabCf.ap.rearrange("p a b -> p (a b)")
        range_reduce(phD, tmpf.ap, tmpi.ap, 16 * 129)
        ts("dve", phC, phD, math.pi / 2, None, ALU.add, None, TC, TC)
        wrap(phC, tmpf.ap)
        act(phD, phD, AF.Sin, TC, TC)
        act(phC, phC, AF.Sin, TC, TC)
        cp("dve", tabC4.ap, tabCf.ap[:, :, 0:129:LC], TC, TC + [tabC4])
        cp("dve", tabD4.ap, tabDf.ap[:, :, 0:129:LC], TC, TC + [tabD4])
        rk = scr("rk", [128, 16, LC + 1], F32)
        pwr = scr("pwr", [128, 16, LC + 1], F32)
        pwi = scr("pwi", [128, 16, LC + 1], F32)
        for k in range(LC + 1):
            act(rk.ap[:, :, k], zs["lrdt"].ap, AF.Exp, TC, TC, scale=float(k))
        tt("dve", pwr.ap, rk.ap, tabCf.ap[:, :, 0:LC + 1], ALU.mult, TC, TC)
        tt("dve", pwi.ap, rk.ap, tabDf.ap[:, :, 0:LC + 1], ALU.mult, TC, TC)
        cp("dve", rmagL.ap, rk.ap[:, :, LC], TC, TC + [rmagL])
        cp("dve", Rtab.ap.rearrange("p a b c -> p a (b c)"), rmagL.ap.unsqueeze(2).to_broadcast([128, 16, 2 * NCH]),
           TC + [rmagL], TC + [Rtab])
        memset("dve", Rtab.ap[:, :, :, 0], 0.0, TC + [Rtab])
        abr, abi = pwr.ap[:, :, 1], pwi.ap[:, :, 1]
        lre_ = zs["lre"].ap
        tt("dve", zs["den"].ap, lre_, lre_, ALU.mult, TC, TC)
        tt("dve", zs["t0"].ap, aim, aim, ALU.mult, TC, TC)
        tt("dve", zs["den"].ap, zs["den"].ap, zs["t0"].ap, ALU.add, TC, TC)
        S.add("dve", lambda e: e.reciprocal(out=zs["den"].ap, in_=zs["den"].ap), R(tabC), R(tabC))
        ts("dve", zs["nr"].ap, abr, -1.0, None, ALU.add, None, TC, TC)
        tt("dve", zs["t0"].ap, zs["nr"].ap, lre_, ALU.mult, TC, TC)
        tt("dve", zs["t1"].ap, abi, aim, ALU.mult, TC, TC)
        tt("dve", zs["t0"].ap, zs["t0"].ap, zs["t1"].ap, ALU.add, TC, TC)
        tt("dve", zs["fre"].ap, zs["t0"].ap, zs["den"].ap, ALU.mult, TC, TC)
        tt("dve", zs["t0"].ap, abi, lre_, ALU.mult, TC, TC)
        tt("dve", zs["t1"].ap, zs["nr"].ap, aim, ALU.mult, TC, TC)
        tt("dve", zs["t0"].ap, zs["t0"].ap, zs["t1"].ap, ALU.subtract, TC, TC)
        tt("dve", zs["fim"].ap, zs["t0"].ap, zs["den"].ap, ALU.mult, TC, TC)
        Bsm = scr("Bsm", [128, 2, 16, 128], F32)
        Csm = scr("Csm", [128, 2, 16, 128], F32)
        dma("sp", Bsm.ap, Bsm_d, [], TC, "Bsm")
        dma("sp", Csm.ap, Csm_d, [], TC, "Csm")
        Btr = scr("Btr", [128, 16, 128], F32)
        Bti = scr("Bti", [128, 16, 128], F32)
        Gr = scr("Gr", [128, 16, 128], F32)
        Gi = scr("Gi", [128, 16, 128], F32)
        w0 = scr("w0", [128, 16, 128], F32)
        w1_ = scr("w1_", [128, 16, 128], F32)
        Psr = scr("Psr", [128, 16, 128], BF16)
        Psi = scr("Psi", [128, 16, 128], BF16)
        Kf = scr("Kf", [128, 128], F32)

        def bc(v):
            return v.unsqueeze(2).to_broadcast([128, 16, 128])

        def cmul(o_re, o_im, a_re, a_im, s_re, s_im, neg_im=False):
            tt("dve", w0.ap, a_re, bc(s_re), ALU.mult, TC, TC)
            tt("dve", w1_.ap, a_im, bc(s_im), ALU.mult, TC, TC)
            tt("dve", o_re, w0.ap, w1_.ap, ALU.subtract, TC, TC)
            tt("dve", w0.ap, a_re, bc(s_im), ALU.mult, TC, TC)
            tt("dve", w1_.ap, a_im, bc(s_re), ALU.mult, TC, TC)
            if neg_im:
                stt(o_im, w0.ap, -1.0, w1_.ap, ALU.mult, ALU.subtract, TC, TC)
            else:
                tt("dve", o_im, w0.ap, w1_.ap, ALU.add, TC, TC)

        cmul(Btr.ap, Bti.ap, Bsm.ap[:, 0], Bsm.ap[:, 1], zs["fre"].ap, zs["fim"].ap)
        memset("dve", Kb.ap, 0.0, TC + [Kb])
        for k in range(LC + 1):
            cmul(Gr.ap, Gi.ap, Csm.ap[:, 0], Csm.ap[:, 1], pwr.ap[:, :, k], pwi.ap[:, :, k], neg_im=True)
            if k >= 1:
                for j in range(16):
                    c0 = 64 * ((j % 4) // 2)
                    cp("act", Qs.ap[:, 0, j * LC + k - 1, :], Gr.ap[:, j, c0:c0 + 64], TC, TC + [Qs])
                    cp("act", Qs.ap[:, 1, j * LC + k - 1, :], Gi.ap[:, j, c0:c0 + 64], TC, TC + [Qs])
            if k < LC:
                for q in range(4):
                    pK = PSB[2 + q % 2]
                    for jm in range(4):
                        j = 4 * q + jm
                        mm(pK.ap[:, 0:128], Btr.ap[:, j, :], Gr.ap[:, j, :], jm == 0, False, TC, [pK])
                        mm(pK.ap[:, 0:128], Bti.ap[:, j, :], Gi.ap[:, j, :], False, jm == 3, TC, [pK])
                    if k == 0:
                        stt(Kf.ap, ident_f.ap, dT.ap[:, q:q + 1], pK.ap[:, 0:128], ALU.mult, ALU.add, [pK, ident_f, dT] + TC, TC)
                        cp("dve", Kb.ap[:, q * LC + k, :], Kf.ap, TC, TC + [Kb])
                    else:
                        cp("dve", Kb.ap[:, q * LC + k, :], pK.ap[:, 0:128], [pK] + TC, TC + [Kb])
                s_ = LC - 1 - k
                cmul(Psr.ap, Psi.ap, Btr.ap, Bti.ap, pwr.ap[:, :, k], pwi.ap[:, :, k])
                for part, Ps_ in enumerate((Psr, Psi)):
                    for pr in range(2):
                        bk = (2 * part + pr) % 2
                        pT = psv(bk, [128, 8, 128], BF16)
                        for idx in range(8):
                            j = 4 * (idx // 2) + 2 * pr + idx % 2
                            tr(pT[64 * pr:64 * pr + 64, idx, :], Ps_.ap[:, j, 64 * pr:64 * pr + 64], ident_b.ap, TC + [ident_b], [PSB[bk]])
                        cp("act", Pt.ap[64 * pr:64 * pr + 64, part, s_::LC, :], pT[64 * pr:64 * pr + 64, :, :], [PSB[bk]] + TC, TC + [Pt])
        if "s5setup" in dbg:
            dump("tabC4", tabC4)
            dump("Kb", Kb)
            dump("Pt", Pt)
            dump("Qs", Qs)
        A.reset(mark_setup)

        wsT_b = A.alloc("wsT_b", [128, 8, 128], BF16)
        sgub = A.alloc("sgub", [128, 4, 128], F32)
        mark_sgu = A.mark()
        wsf = A.alloc("wsf", [128, 8, 128], F32)
        dma("sp", wsf.ap, wsT_d, [], [wsf], "wsf")
        for h in range(8):
            S.add("pool", lambda e, h=h: e.affine_select(out=wsf.ap[:, h, :], in_=wsf.ap[:, h, :], pattern=[[1, 128]],
                                                           compare_op=ALU.is_ge, fill=0.0, base=0, channel_multiplier=-1),
                  R(wsf), R(wsf))
        cp("pool", wsT_b.ap, wsf.ap, [wsf], [wsT_b])
        bsT = A.alloc("bsT", [128, 4, 128], F32)
        dma("sp", bsT.ap, bsT_d, [], [bsT], "bsT")
        pmix0 = psv(3, [128, 4, 128])
        for h in range(8):
            po = (h % 2) * 64
            mm(pmix0[po:po + 64, h // 2, :], ones_b.ap[:, 0:64], wsT_b.ap[:, h, :], True, True, [ones_b, wsT_b], [PSB[3]])
        for q in range(4):
            stt(sgub.ap[:, q, :], pmix0[:, q, :], lnbT.ap[:, q:q + 1], bsT.ap[:, q, :], ALU.mult, ALU.add,
                [PSB[3], lnbT, bsT], [sgub])
        if "sgusetup" in dbg:
            dump("sgub", sgub)
            dump("wsT_b", wsT_b)
        A.reset(mark_sgu)

        if stop_after == "setup":
            S.finish(final_ops)
            S.emit(st)
            return nc, dbg_outs

        def wload(name, src_view, shape, key=None):
            t = A.alloc(name, shape, BF16)
            dma("pool", t.ap, src_view, [], [t], key or name)
            return t

        w_in_b = wload("w_in_b", w_in_d.rearrange("(kc p) n -> p kc n", p=128), [128, 8, 1536])
        w_gate_b = wload("w_gate_b", w_gate_d.rearrange("(kc p) n -> p kc n", p=128), [128, 8, 2048])
        w_glu_b = wload("w_glu_b", w_glu_d.rearrange("(kc p) n -> p kc n", p=128), [128, 4, 512])
        w_bra_b = wload("w_bra_b", w_bra_d.rearrange("(kc p) n -> p kc n", p=128), [128, 4, D])
        w_brb_b = wload("w_brb_b", w_brb_d.rearrange("(kc p) n -> p kc n", p=128), [128, 4, D])
        w_out_b = wload("w_out_b", w_out_d.rearrange("(kc p) n -> p kc n", p=128), [128, 8, D])
        def alias(name, shape, dt, of, off=0):
            t = Tl(None, name)
            n = 1
            for s_ in shape[1:]:
                n *= s_
            base_ = of.ap
            if len(base_.shape) == 3:
                base_ = base_.rearrange("p a b -> p (a b)")
            elif len(base_.shape) == 4:
                base_ = base_.rearrange("p a b c -> p (a b c)")
            if base_.dtype != dt:
                base_ = base_.bitcast(dt)
            v = base_[:, off:off + n]
            if len(shape) == 3:
                v = v.rearrange("p (a b) -> p a b", a=shape[1])
            t.ap = v
            t.r = of.r
            return t

        xt0 = A.alloc("xt0", [128, D], F32)
        xr = A.alloc("xr", [128, D], F32)
        xsb = A.alloc("xsb", [128, D], BF16)
        ssq = A.alloc("ssq", [128, 1], F32)
        rstd = A.alloc("rstd", [128, 1], F32)
        xnT = [A.alloc("xnT%d" % i, [128, 8, 128], BF16) for i in range(3)]
        uT = [A.alloc("uT%d" % i, [128, 4, 128], BF16) for i in range(2)]
        guT = A.alloc("guT", [128, 4, 128], F32)
        gv = A.alloc("gv", [128, 512], F32)
        vst = A.alloc("vst", [128, 6], F32)
        vmv = A.alloc("vmv", [128, 2], F32)
        vrs = A.alloc("vrs", [128, 1], F32)
        vhat = A.alloc("vhat", [128, 512], BF16)
        mixT = alias("mixT", [128, 4, 128], F32, gv)
        ybT = [A.alloc("ybT%d" % i, [128, 4, 128], BF16) for i in range(3)]
        xtil = A.alloc("xtil", [128, 4, 2, NCH], F32)
        s5a = A.alloc("s5a", [128, 4, NCH], F32)
        s5b = A.alloc("s5b", [128, 4, NCH], F32)
        gsc = A.alloc("gsc", [128, 4, 2, NCH], F32)
        gp = A.alloc("gp", [128, 16, 2], F32)
        carry = A.alloc("carry", [128, 16, 2], F32)
        sm1 = A.alloc("sm1", [128, 16, 2], F32)
        cf2 = A.alloc("cf2", [128, 4, 2], F32)
        c4 = [A.alloc("c4_%d" % i, [128, 16], F32) for i in range(4)]
        Sprev = [A.alloc("Sprev%d" % i, [128, 4, 2, NCH], BF16) for i in range(2)]
        ypre = A.alloc("ypre", [128, 4, 128], F32)
        ygT = A.alloc("ygT", [128, 4, 128], BF16)
        sg = A.alloc("sg", [128, 4, 128], F32)
        yaT = [A.alloc("yaT%d" % i, [128, 4, 128], BF16) for i in range(2)]
        gates = A.alloc("gates", [128, 16, 128], F32)
        xn2T = alias("xn2T", [128, 8, 128], F32, gates, off=1024)
        xn2 = alias("xn2", [128, D], F32, gates, off=0)
        mergedT = A.alloc("mergedT", [128, 8, 128], BF16)
        jk2 = alias("jk2", [128, D], BF16, mergedT)
        gt1b = A.alloc("gt1b", [128, D], F32)
        A2b = A.alloc("A2b", [128, D], F32)
        sh2b = A.alloc("sh2b", [128, D], F32)
        ssq2 = A.alloc("ssq2", [128, 1], F32)
        rstd2 = A.alloc("rstd2", [128, 1], F32)
        lg = A.alloc("lg", [128, 36], F32)
        rt = {n: A.alloc("rt_" + n, [128, w_], F32) for n, w_ in
              [("gmax", 1), ("ngmax", 1), ("maskg", 4), ("eg", 4), ("sume", 1), ("pgs", 1), ("pen", 4), ("lem", 32),
               ("m1", 1), ("oh1", 32), ("lem2", 32), ("m2", 1), ("oh2", 32), ("dm", 1), ("e2", 1), ("p1", 1), ("p2", 1),
               ("rank", 32), ("slotv", 32), ("junk", 32), ("sl", 2), ("val", 32), ("vk", 2)]}
        ohb = A.alloc("ohb", [128, 32], BF16)

        def rms_rstd(src, ssq_t, rstd_t, junk_ap, junk_t):
            act(junk_ap, src.ap, AF.Square, [src], [junk_t, ssq_t], accum=ssq_t.ap)
            act(ssq_t.ap, ssq_t.ap, AF.Sqrt, [ssq_t, eps_rms], [ssq_t], bias=eps_rms.ap, scale=1.0 / D)
            S.add("dve", lambda e: e.reciprocal(out=rstd_t.ap, in_=ssq_t.ap), R(ssq_t), R(rstd_t))

        store_ops = []
        scat_ops = []
        c128 = tabC4.ap[:, :, NCH]
        d128 = tabD4.ap[:, :, NCH]

        def seqof(i):
            return i // NT

        def P1(i):
            b = seqof(i)
            X = xt0
            XN = xnT[i % 3]
            if i == 0:
                dma("sp", X.ap, x_d[0:128, :], [], [X], "xt0")
            rms_rstd(X, ssq, rstd, xsb.ap, xsb)
            act(xsb.ap, X.ap, AF.Copy, [X, rstd], [xsb], scale=rstd.ap[:, 0:1])
            if i + 1 < NTILES:
                dma("sp", X.ap, x_d[(i + 1) * 128:(i + 2) * 128, :], [], [X], "xt0")
            pX = psv(0, [128, 8, 128], BF16)
            for kc in range(8):
                tr(pX[:, kc, :], xsb.ap[:, kc * 128:(kc + 1) * 128], ident_b.ap, [xsb, ident_b], [PSB[0]])
            for kc in range(8):
                act(XN.ap[:, kc, :], pX[:, kc, :], AF.Identity, [PSB[0], A1T, sh1T], [XN],
                    bias=sh1T.ap[:, kc, b:b + 1], scale=A1T.ap[:, kc, b:b + 1])
            if dbg.get("tile") == i:
                dump("xnT", XN)

        def P2(i):
            XN = xnT[i % 3]
            UT = uT[i % 2]
            pZa = psv(1, [128, 4, 128])
            pZu = psv(0, [128, 4, 128])
            for m in range(4):
                for kc in range(8):
                    mm(pZa[:, m, :], w_in_b.ap[:, kc, m * 128:(m + 1) * 128], XN.ap[:, kc, :], kc == 0, kc == 7,
                       [w_in_b, XN], [PSB[1]])
            cp("act", UT.ap, pZa, [PSB[1]], [UT])
            for m in range(4):
                for kc in range(8):
                    mm(pZu[:, m, :], w_in_b.ap[:, kc, 512 + m * 128:512 + (m + 1) * 128], XN.ap[:, kc, :], kc == 0, kc == 7,
                       [w_in_b, XN], [PSB[0]])
            act(guT.ap, pZu, AF.Gelu_apprx_tanh, [PSB[0]], [guT])
            pV = psv(1, [128, 512])
            for kc in range(8):
                mm(pV, XN.ap[:, kc, :], w_in_b.ap[:, kc, 1024:1536], kc == 0, kc == 7, [w_in_b, XN], [PSB[1]])
            act(gv.ap, pV, AF.Gelu_apprx_tanh, [PSB[1]], [gv])
            if dbg.get("tile") == i:
                dump("uT", UT)

        def P3(i):
            YB = ybT[i % 3]
            S.add("dve", lambda e: e.bn_stats(out=vst.ap, in_=gv.ap), R(gv), R(vst))
            S.add("dve", lambda e: e.bn_aggr(out=vmv.ap, in_=vst.ap), R(vst), R(vmv))
            act(vrs.ap, vmv.ap[:, 1:2], AF.Sqrt, [vmv, eps_ln], [vrs], bias=eps_ln.ap, scale=1.0)
            S.add("dve", lambda e: e.reciprocal(out=vrs.ap, in_=vrs.ap), R(vrs), R(vrs))
            ts("dve", vhat.ap, gv.ap, vmv.ap[:, 0:1], vrs.ap[:, 0:1], ALU.subtract, ALU.mult, [gv, vmv, vrs], [vhat])
            pMix = psv(0, [128, 4, 128])
            for h in range(8):
                po = (h % 2) * 64
                mm(pMix[po:po + 64, h // 2, :], vhat.ap[:, h * 64:(h + 1) * 64], wsT_b.ap[:, h, :], True, True,
                   [vhat, wsT_b], [PSB[0]])
            for q in range(4):
                stt(mixT.ap[:, q, :], pMix[:, q, :], lngT.ap[:, q:q + 1], sgub.ap[:, q, :], ALU.mult, ALU.add,
                    [PSB[0], lngT, sgub], [mixT])
            tt("pool", YB.ap, guT.ap, mixT.ap, ALU.mult, [guT, mixT], [YB])
            if dbg.get("tile") == i:
                dump("ybT", YB)

        def pS5(q):
            o = (q % 2) * 4 * NCH
            return ps_all[:, 2:4, o:o + 4 * NCH].rearrange("p k (a b c) -> p k a b c", a=2, b=2)

        def Qpre(i):
            if i % NT == 0:
                memset("dve", carry.ap, 0.0, [carry])
            cT_, dT_ = tabC4.ap[:, :, 1], tabD4.ap[:, :, 1]
            tt("dve", c4[0].ap, carry.ap[:, :, 0], cT_, ALU.mult, [carry, tabC4], [c4[0]])
            tt("dve", c4[1].ap, carry.ap[:, :, 1], dT_, ALU.mult, [carry, tabD4], [c4[1]])
            tt("dve", c4[2].ap, carry.ap[:, :, 1], cT_, ALU.mult, [carry, tabC4], [c4[2]])
            tt("dve", c4[3].ap, carry.ap[:, :, 0], dT_, ALU.mult, [carry, tabD4], [c4[3]])
            tt("dve", sm1.ap[:, :, 0], c4[0].ap, c4[1].ap, ALU.add, [c4[0], c4[1]], [sm1])
            tt("dve", sm1.ap[:, :, 1], c4[2].ap, c4[3].ap, ALU.subtract, [c4[2], c4[3]], [sm1])

        def QB(i, q):
            UT = uT[i % 2]
            pS = pS5(q)
            for jj in range(4):
                ro = 64 * (jj // 2)
                for part in range(2):
                    for sx in range(LC):
                        mm(pS[:, jj // 2, jj % 2, part, :], Pt.ap[ro:ro + 64, part, (2 * q + jj % 2) * LC + sx, :],
                           UT.ap[ro:ro + 64, q, sx::LC], sx == 0, sx == LC - 1, [Pt, UT], [PSB[2 + jj // 2]])

        def QD(i, q):
            pS = pS5(q)
            SP = Sprev[q % 2]
            tc_ = tabC4.ap[:, 4 * q:4 * q + 4, 0:NCH]
            td_ = tabD4.ap[:, 4 * q:4 * q + 4, 0:NCH]
            tc4 = tc_.rearrange("p (k a) c -> p k a c", k=2)
            td4 = td_.rearrange("p (k a) c -> p k a c", k=2)
            a4 = s5a.ap.rearrange("p (k a) c -> p k a c", k=2)
            b4 = s5b.ap.rearrange("p (k a) c -> p k a c", k=2)
            PB = [PSB[2], PSB[3]]
            tt("dve", a4, pS[:, :, :, 0, :], tc4, ALU.mult, PB + [tabC4], [s5a])
            tt("dve", b4, pS[:, :, :, 1, :], td4, ALU.mult, PB + [tabD4], [s5b])
            tt("dve", xtil.ap[:, :, 0, :], s5a.ap, s5b.ap, ALU.add, [s5a, s5b], [xtil])
            tt("dve", a4, pS[:, :, :, 1, :], tc4, ALU.mult, PB + [tabC4, xtil], [s5a])
            tt("dve", b4, pS[:, :, :, 0, :], td4, ALU.mult, PB + [tabD4, xtil], [s5b])
            tt("dve", xtil.ap[:, :, 1, :], s5a.ap, s5b.ap, ALU.subtract, [s5a, s5b], [xtil])
            tt("dve", cf2.ap, carry.ap[:, 4 * q:4 * q + 4, :], rmagL.ap[:, 4 * q:4 * q + 4].unsqueeze(2).to_broadcast([128, 4, 2]),
               ALU.mult, [carry, rmagL], [cf2])
            tt("dve", xtil.ap[:, :, :, 0], xtil.ap[:, :, :, 0], cf2.ap, ALU.add, [xtil, cf2], [xtil])
            S.add("dve", lambda e: e.tensor_tensor_scan(
                out=gsc.ap.rearrange("p a b c -> p (a b c)"),
                data0=Rtab.ap[:, 4 * q:4 * q + 4, :, :].rearrange("p a b c -> p (a b c)"),
                data1=xtil.ap.rearrange("p a b c -> p (a b c)"), initial=0.0, op0=ALU.mult, op1=ALU.add),
                R(Rtab, xtil), R(gsc))
            cp("dve", gp.ap[:, 4 * q:4 * q + 4, :], gsc.ap[:, :, :, NCH - 1], [gsc], [gp])
            n1 = NCH - 1
            tt("dve", s5a.ap[:, :, 0:n1], gsc.ap[:, :, 0, 0:n1], tc_[:, :, 0:n1], ALU.mult, [gsc, tabC4], [s5a])
            tt("dve", s5b.ap[:, :, 0:n1], gsc.ap[:, :, 1, 0:n1], td_[:, :, 0:n1], ALU.mult, [gsc, tabD4], [s5b])
            tt("dve", SP.ap[:, :, 0, 1:NCH], s5a.ap[:, :, 0:n1], s5b.ap[:, :, 0:n1], ALU.subtract, [s5a, s5b], [SP])
            tt("dve", s5a.ap[:, :, 0:n1], gsc.ap[:, :, 1, 0:n1], tc_[:, :, 0:n1], ALU.mult, [gsc, tabC4, SP], [s5a])
            tt("dve", s5b.ap[:, :, 0:n1], gsc.ap[:, :, 0, 0:n1], td_[:, :, 0:n1], ALU.mult, [gsc, tabD4, SP], [s5b])
            tt("dve", SP.ap[:, :, 1, 1:NCH], s5a.ap[:, :, 0:n1], s5b.ap[:, :, 0:n1], ALU.add, [s5a, s5b], [SP])
            cp("dve", SP.ap[:, :, :, 0], sm1.ap[:, 4 * q:4 * q + 4, :], [sm1], [SP])
            if dbg.get("tile") == i and q == 0:
                dump("gsc0", gsc)

        def QC(i, q):
            UT = uT[i % 2]
            SP = Sprev[q % 2]
            pY = psv(4, [128, 4, LC, NCH])
            for sp_ in range(LC):
                first = True
                for sx in range(sp_ + 1):
                    mm(pY[:, q, sp_, :], Kb.ap[:, q * LC + (sp_ - sx), :], UT.ap[:, q, sx::LC], first, False, [Kb, UT], [PSB[4]])
                    first = False
                for jj in range(4):
                    j = 4 * q + jj
                    ro = 64 * (jj // 2)
                    for part in range(2):
                        mm(pY[ro:ro + 64, q, sp_, :], Qs.ap[:, part, j * LC + sp_, :], SP.ap[:, jj, part, :],
                           False, jj == 3 and part == 1, [Qs, SP], [PSB[4]])

        def Qcarry(i):
            tt("dve", c4[0].ap, gp.ap[:, :, 0], c128, ALU.mult, [gp, tabC4], [c4[0]])
            tt("dve", c4[1].ap, gp.ap[:, :, 1], d128, ALU.mult, [gp, tabD4], [c4[1]])
            tt("dve", c4[2].ap, gp.ap[:, :, 1], c128, ALU.mult, [gp, tabC4], [c4[2]])
            tt("dve", c4[3].ap, gp.ap[:, :, 0], d128, ALU.mult, [gp, tabD4], [c4[3]])
            tt("dve", carry.ap[:, :, 0], c4[0].ap, c4[1].ap, ALU.subtract, [c4[0], c4[1]], [carry])
            tt("dve", carry.ap[:, :, 1], c4[2].ap, c4[3].ap, ALU.add, [c4[2], c4[3]], [carry])

        def Qtail(i):
            YA = yaT[i % 2]
            pY = psv(4, [128, 4, LC, NCH])
            for q in range(4):
                act(ypre.ap[:, q, :].rearrange("p (c s) -> p s c", s=LC), pY[:, q, :, :], AF.Gelu_apprx_tanh, [PSB[4]], [ypre])
            if dbg.get("tile") == i:
                dump("ypre", ypre)
            cp("pool", ygT.ap, ypre.ap, [ypre], [ygT])
            pG = psv(4, [128, 4, 128])
            for m in range(4):
                for kc in range(4):
                    mm(pG[:, m, :], w_glu_b.ap[:, kc, m * 128:(m + 1) * 128], ygT.ap[:, kc, :], kc == 0, kc == 3,
                       [w_glu_b, ygT], [PSB[4]])
            for m in range(4):
                act(sg.ap[:, m, :], pG[:, m, :], AF.Sigmoid, [PSB[4], b_gluT], [sg], bias=b_gluT.ap[:, m:m + 1], scale=1.0)
            tt("pool", YA.ap, ypre.ap, sg.ap, ALU.mult, [ypre, sg], [YA])
            if dbg.get("tile") == i:
                dump("yaT", YA)

        def R0(i):
            b = seqof(i)
            if i % NT == 0:
                dma("sp", gt1b.ap, mod_d.ap[b:b + 1, 2 * D:3 * D].partition_broadcast(128), [mod_d], [gt1b], "gt1b")
                dma("sp", sh2b.ap, mod_d.ap[b:b + 1, 3 * D:4 * D].partition_broadcast(128), [mod_d], [sh2b], "sh2b")
                dma("sp", A2b.ap, mod_d.ap[b:b + 1, 4 * D:5 * D].partition_broadcast(128), [mod_d], [A2b], "A2b")
                dma("sp", xn2.ap, g2_d.partition_broadcast(128), [], [xn2], "g2tmp")
                stt(A2b.ap, A2b.ap, 1.0, xn2.ap, ALU.add, ALU.mult, [A2b, xn2], [A2b])
            dma("sp", xr.ap, x_d[i * 128:(i + 1) * 128, :], [], [xr], "xr")

        def R1(i, mg):
            XN = xnT[i % 3]
            bk = 5 if mg % 2 == 0 else 7
            pGt = psv(bk, [128, 4, 128])
            for mm_ in range(4):
                m = mg * 4 + mm_
                for kc in range(8):
                    mm(pGt[:, mm_, :], w_gate_b.ap[:, kc, m * 128:(m + 1) * 128], XN.ap[:, kc, :], kc == 0, kc == 7,
                       [w_gate_b, XN], [PSB[bk]])
            for mm_ in range(4):
                m = mg * 4 + mm_
                act(gates.ap[:, m, :], pGt[:, mm_, :], AF.Sigmoid, [PSB[bk], b_gateT], [gates],
                    bias=b_gateT.ap[:, m:m + 1], scale=1.0)

        def R2(i, half):
            YA, YB = yaT[i % 2], ybT[i % 3]
            pA = psv(5, [128, 4, 128])
            pB = psv(6, [128, 4, 128])
            for mm_ in range(4):
                m = half * 4 + mm_
                for kc in range(4):
                    mm(pA[:, mm_, :], w_bra_b.ap[:, kc, m * 128:(m + 1) * 128], YA.ap[:, kc, :], kc == 0, kc == 3,
                       [w_bra_b, YA], [PSB[5]])
            for mm_ in range(4):
                m = half * 4 + mm_
                for kc in range(4):
                    mm(pB[:, mm_, :], w_brb_b.ap[:, kc, m * 128:(m + 1) * 128], YB.ap[:, kc, :], kc == 0, kc == 3,
                       [w_brb_b, YB], [PSB[6]])
            ga = gates.ap[:, half * 4:half * 4 + 4, :]
            gb = gates.ap[:, 8 + half * 4:8 + half * 4 + 4, :]
            tt("dve", ga, ga, pA, ALU.mult, [gates, PSB[5]], [gates])
            tt("dve", gb, gb, pB, ALU.mult, [gates, PSB[6]], [gates])
            tt("pool", mergedT.ap[:, half * 4:half * 4 + 4, :], ga, gb, ALU.add, [gates], [mergedT])
            if dbg.get("tile") == i and half == 1:
                dump("mergedT", mergedT)

        def R3(i):
            for half in range(2):
                bk = 6 + half
                for kc in range(8):
                    mm(PSB[bk].ap, mergedT.ap[:, kc, :], w_out_b.ap[:, kc, half * 512:(half + 1) * 512], kc == 0, kc == 7,
                       [mergedT, w_out_b], [PSB[bk]])
                sl = slice(half * 512, (half + 1) * 512)
                tt("dve", xn2.ap[:, sl], PSB[bk].ap, gt1b.ap[:, sl], ALU.mult, [PSB[bk], gt1b], [xn2])
            tt("pool", xr.ap, xr.ap, xn2.ap, ALU.add, [xr, xn2], [xr])
            store_ops.append(dma("sp", H_d[i * 128:(i + 1) * 128, :], xr.ap, [xr], [r_H], "hst"))
            rms_rstd(xr, ssq2, rstd2, jk2.ap, jk2)
            stt(xn2.ap, xr.ap, rstd2.ap[:, 0:1], A2b.ap, ALU.mult, ALU.mult, [xr, rstd2, A2b], [xn2])
            tt("pool", xn2.ap, xn2.ap, sh2b.ap, ALU.add, [xn2, sh2b], [xn2])
            if dbg.get("tile") == i:
                dump("h", xr)
                dump("xn2", xn2)

        def R4(i):
            pX2 = psv2(6, [128, 8, 128])
            for kc in range(8):
                tr(pX2[:, kc, :], xn2.ap[:, kc * 128:(kc + 1) * 128], ident_f.ap, [xn2, ident_f], [PSB[6], PSB[7]])
            cp("act", xn2T.ap, pX2, [PSB[6], PSB[7]], [xn2T])
            pL = psv(5, [128, 36])
            for kc in range(8):
                mm(pL, xn2T.ap[:, kc, :], w_r.ap[:, kc, :], kc == 0, kc == 7, [xn2T, w_r], [PSB[5]])
            tt("dve", lg.ap, pL, b_r.ap, ALU.add, [PSB[5], b_r], [lg])
            r_ = rt
            BIG = 1.0e9
            S.add("dve", lambda e: e.tensor_reduce(out=r_["gmax"].ap, in_=lg.ap[:, 0:4], axis=AX.X, op=ALU.max), R(lg), R(r_["gmax"]))
            ts("dve", r_["maskg"].ap, lg.ap[:, 0:4], r_["gmax"].ap[:, 0:1], None, ALU.is_equal, None, [lg, r_["gmax"]], [r_["maskg"]])
            ts("dve", r_["ngmax"].ap, r_["gmax"].ap, -1.0, None, ALU.mult, None, [r_["gmax"]], [r_["ngmax"]])
            act(r_["eg"].ap, lg.ap[:, 0:4], AF.Exp, [lg, r_["ngmax"]], [r_["eg"], r_["sume"]], bias=r_["ngmax"].ap[:, 0:1], scale=1.0,
                accum=r_["sume"].ap)
            S.add("dve", lambda e: e.reciprocal(out=r_["pgs"].ap, in_=r_["sume"].ap), R(r_["sume"]), R(r_["pgs"]))
            ts("dve", r_["pen"].ap, r_["maskg"].ap, BIG, -BIG, ALU.mult, ALU.add, [r_["maskg"]], [r_["pen"]])
            for g in range(4):
                ts("dve", r_["lem"].ap[:, g * 8:(g + 1) * 8], lg.ap[:, 4 + g * 8:4 + (g + 1) * 8], r_["pen"].ap[:, g:g + 1], None,
                   ALU.add, None, [lg, r_["pen"]], [r_["lem"]])
            S.add("dve", lambda e: e.tensor_reduce(out=r_["m1"].ap, in_=r_["lem"].ap, axis=AX.X, op=ALU.max), R(r_["lem"]), R(r_["m1"]))
            ts("dve", r_["oh1"].ap, r_["lem"].ap, r_["m1"].ap[:, 0:1], None, ALU.is_equal, None, [r_["lem"], r_["m1"]], [r_["oh1"]])
            stt(r_["lem2"].ap, r_["oh1"].ap, -BIG, r_["lem"].ap, ALU.mult, ALU.add, [r_["oh1"], r_["lem"]], [r_["lem2"]])
            S.add("dve", lambda e: e.tensor_reduce(out=r_["m2"].ap, in_=r_["lem2"].ap, axis=AX.X, op=ALU.max), R(r_["lem2"]), R(r_["m2"]))
            ts("dve", r_["oh2"].ap, r_["lem2"].ap, r_["m2"].ap[:, 0:1], None, ALU.is_equal, None, [r_["lem2"], r_["m2"]], [r_["oh2"]])
            tt("dve", r_["dm"].ap, r_["m2"].ap, r_["m1"].ap, ALU.subtract, [r_["m1"], r_["m2"]], [r_["dm"]])
            act(r_["e2"].ap, r_["dm"].ap, AF.Exp, [r_["dm"]], [r_["e2"]])
            ts("dve", r_["p1"].ap, r_["e2"].ap, 1.0, None, ALU.add, None, [r_["e2"]], [r_["p1"]])
            S.add("dve", lambda e: e.reciprocal(out=r_["p1"].ap, in_=r_["p1"].ap), R(r_["p1"]), R(r_["p1"]))
            tt("dve", r_["p2"].ap, r_["e2"].ap, r_["p1"].ap, ALU.mult, [r_["e2"], r_["p1"]], [r_["p2"]])
            tt("dve", ohb.ap, r_["oh1"].ap, r_["oh2"].ap, ALU.add, [r_["oh1"], r_["oh2"]], [ohb])
            pR = psv(5, [128, 128])
            mm(pR[:, 64:96], tri_b.ap, ohb.ap, True, True, [tri_b, ohb], [PSB[5]])
            mm(pR[:, 96:128], ones_b.ap, ohb.ap, True, True, [ones_b, ohb], [PSB[5]])
            tt("dve", r_["rank"].ap, pR[:, 64:96], base.ap, ALU.add, [PSB[5], base], [r_["rank"]])
            tt("dve", base.ap, pR[:, 96:128], base.ap, ALU.add, [PSB[5], base, r_["rank"]], [base])
            ts("dve", r_["val"].ap, r_["rank"].ap, float(CAP), None, ALU.is_lt, None, [r_["rank"]], [r_["val"]])
            tt("dve", r_["slotv"].ap, r_["rank"].ap, ecap.ap, ALU.add, [r_["rank"], ecap], [r_["slotv"]])
            stt(r_["slotv"].ap, r_["val"].ap, -4.0e6, r_["slotv"].ap, ALU.mult, ALU.add, [r_["val"], r_["slotv"]], [r_["slotv"]])
            ts("dve", r_["slotv"].ap, r_["slotv"].ap, 4.0e6, None, ALU.add, None, [r_["slotv"]], [r_["slotv"]])
            for k, ohn in enumerate(("oh1", "oh2")):
                tt("dve", r_["junk"].ap, r_[ohn].ap, r_["slotv"].ap, ALU.mult, [r_[ohn], r_["slotv"]], [r_["junk"]])
                S.add("dve", lambda e, k=k: e.tensor_reduce(out=r_["sl"].ap[:, k:k + 1], in_=r_["junk"].ap, axis=AX.X, op=ALU.add),
                      R(r_["junk"]), R(r_["sl"]))
                tt("dve", r_["junk"].ap, r_[ohn].ap, r_["val"].ap, ALU.mult, [r_[ohn], r_["val"]], [r_["junk"]])
                S.add("dve", lambda e, k=k: e.tensor_reduce(out=r_["vk"].ap[:, k:k + 1], in_=r_["junk"].ap, axis=AX.X, op=ALU.add),
                      R(r_["junk"]), R(r_["vk"]))
            cp("dve", sloti.ap[:, i, :], r_["sl"].ap, [r_["sl"]], [sloti])
            stt(wgt.ap[:, i, 0:1], r_["p1"].ap, r_["pgs"].ap[:, 0:1], r_["vk"].ap[:, 0:1], ALU.mult, ALU.mult,
                [r_["p1"], r_["pgs"], r_["vk"]], [wgt])
            stt(wgt.ap[:, i, 1:2], r_["p2"].ap, r_["pgs"].ap[:, 0:1], r_["vk"].ap[:, 1:2], ALU.mult, ALU.mult,
                [r_["p2"], r_["pgs"], r_["vk"]], [wgt])
            if dbg.get("tile") == i:
                dump("lg", lg)
                dump("rank", r_["rank"])
            for k in range(2):
                scat_ops.append(S.add("pool", lambda e, i=i, k=k: e.indirect_dma_start(
                    out=X_d, out_offset=bass.IndirectOffsetOnAxis(ap=sloti.ap[:, i, k:k + 1], axis=0),
                    in_=xn2.ap, in_offset=None, bounds_check=breg(e, NSLOT - 1), oob_is_err=False),
                    R(xn2, sloti), [r_X], dma_key="scat"))

        import os as _os
        _skip = set(_os.environ.get('QSKIP', '').split(','))
        _w = lambda f, n: (lambda *a: None) if n in _skip else f
        Qpre, QB, QD, QC, Qcarry, Qtail = _w(Qpre, 'pre'), _w(QB, 'B'), _w(QD, 'D'), _w(QC, 'C'), _w(Qcarry, 'carry'), _w(Qtail, 'tail')
        for s_ in range(NTILES + 2):
            ip, iq, ir = s_, s_ - 1, s_ - 2
            hp = 0 <= ip < NTILES
            hq = 0 <= iq < NTILES
            hr = 0 <= ir < NTILES
            if hr:
                R0(ir)
            if hq:
                Qpre(iq)
                QB(iq, 0)
            if hr:
                R1(ir, 0)
                R1(ir, 1)
            if hp:
                P1(ip)
            if hq:
                QD(iq, 0)
            if hr:
                R1(ir, 2)
                R1(ir, 3)
                R2(ir, 0)
                R2(ir, 1)
            if hq:
                QB(iq, 1)
                QC(iq, 0)
            if hp:
                P2(ip)
            if hq:
                QD(iq, 1)
            if hr:
                R3(ir)
            if hq:
                QB(iq, 2)
                QC(iq, 1)
                QD(iq, 2)
            if hr:
                R4(ir)
            if hq:
                QB(iq, 3)
                QC(iq, 2)
            if hp:
                P3(ip)
            if hq:
                QD(iq, 3)
                QC(iq, 3)
                Qcarry(iq)
                Qtail(iq)
        if "route" in dbg:
            dump("wgt", wgt)
            dump("sloti", sloti)
        if stop_after == "A":
            S.finish(final_ops + store_ops + scat_ops)
            S.emit(st)
            return nc, dbg_outs

        S.barrier(dma_ops=[scat_ops[-1], store_ops[-1]])
        A.reset(mark_persist)
        w1b = [A.alloc("w1b%d" % i, [128, 8, 512], BF16) for i in range(2)]
        w3b = [A.alloc("w3b%d" % i, [128, 8, 512], BF16) for i in range(2)]
        w2b = [A.alloc("w2b%d" % i, [128, 4, D], BF16) for i in range(2)]
        Xblk = [A.alloc("Xblk%d" % i, [128, D], BF16) for i in range(3)]
        XT = [A.alloc("XT%d" % i, [128, 8, CAP], BF16) for i in range(2)]
        hidT = [A.alloc("hidT%d" % i, [128, 4, CAP], BF16) for i in range(2)]
        s1 = [A.alloc("s1_%d" % i, [128, 512], F32) for i in range(2)]
        Yblk = [A.alloc("Yblk%d" % i, [128, D], F32) for i in range(3)]
        NH = (CAP + 511) // 512
        HW_ = CAP // NH
        ystore = []
        cnt = {"x": 0, "y": 0, "h": 0}

        def W13(e_):
            sl_ = e_ % 2
            dma("pool", w1b[sl_].ap, w1_d[e_].rearrange("(kc p) n -> p kc n", p=128), [], [w1b[sl_]], "w1b%d" % sl_)
            dma("pool", w3b[sl_].ap, w3_d[e_].rearrange("(kc p) n -> p kc n", p=128), [], [w3b[sl_]], "w3b%d" % sl_)

        def W2(e_):
            sl_ = e_ % 2
            dma("pool", w2b[sl_].ap, w2_d[e_].rearrange("(kc p) n -> p kc n", p=128), [], [w2b[sl_]], "w2b%d" % sl_)

        def TX(e_):
            xt_ = XT[e_ % 2]
            for blk in range(NBLK):
                n_ = cnt["x"]
                cnt["x"] += 1
                xb_ = Xblk[n_ % 3]
                r0 = e_ * CAP + blk * 128
                dma("sp", xb_.ap, X_d[r0:r0 + 128, :], [r_X], [xb_], "xblk%d" % (n_ % 3))
                bk = n_ % 2
                pXT = psv(bk, [128, 8, 128], BF16)
                for kc in range(8):
                    tr(pXT[:, kc, :], xb_.ap[:, kc * 128:(kc + 1) * 128], ident_b.ap, [xb_, ident_b], [PSB[bk]])
                cp("act" if blk % 2 == 0 else "dve", xt_.ap[:, :, blk * 128:(blk + 1) * 128], pXT, [PSB[bk]], [xt_])

        def HH(e_):
            sl_ = e_ % 2
            xt_, hd_ = XT[e_ % 2], hidT[e_ % 2]
            for m in range(4):
                for nh in range(NH):
                    cs_ = slice(nh * HW_, (nh + 1) * HW_)
                    n_ = cnt["h"]
                    cnt["h"] += 1
                    b1 = 2 + n_ % 2
                    b3 = 4 + n_ % 2
                    p1_ = PSB[b1].ap[:, 0:HW_]
                    p3_ = PSB[b3].ap[:, 0:HW_]
                    for kc in range(8):
                        mm(p1_, w1b[sl_].ap[:, kc, m * 128:(m + 1) * 128], xt_.ap[:, kc, cs_], kc == 0, kc == 7,
                           [w1b[sl_], xt_], [PSB[b1]])
                    for kc in range(8):
                        mm(p3_, w3b[sl_].ap[:, kc, m * 128:(m + 1) * 128], xt_.ap[:, kc, cs_], kc == 0, kc == 7,
                           [w3b[sl_], xt_], [PSB[b3]])
                    s1_ = s1[n_ % 2]
                    act(s1_.ap[:, 0:HW_], p1_, AF.Silu, [PSB[b1]], [s1_])
                    tt("dve", hd_.ap[:, m, cs_], s1_.ap[:, 0:HW_], p3_, ALU.mult, [s1_, PSB[b3]], [hd_])

        def YY(e_):
            sl_ = e_ % 2
            hd_ = hidT[e_ % 2]
            for blk in range(NBLK):
                n_ = cnt["y"]
                cnt["y"] += 1
                yb_ = Yblk[n_ % 3]
                for half in range(2):
                    bk = 6 + half
                    for kc in range(4):
                        mm(PSB[bk].ap, hd_.ap[:, kc, blk * 128:(blk + 1) * 128], w2b[sl_].ap[:, kc, half * 512:(half + 1) * 512],
                           kc == 0, kc == 3, [hd_, w2b[sl_]], [PSB[bk]])
                    cp("act" if half == 0 else "dve", yb_.ap[:, half * 512:(half + 1) * 512], PSB[bk].ap, [PSB[bk]], [yb_])
                r0 = e_ * CAP + blk * 128
                ystore.append(dma("sp", Y_d[r0:r0 + 128, :], yb_.ap, [yb_], [r_Y], "yst%d" % (n_ % 3)))

        W13(0)
        W2(0)
        W13(1)
        W2(1)
        TX(0)
        for e_ in range(32):
            HH(e_)
            if e_ + 2 < 32:
                W13(e_ + 2)
            if e_ + 1 < 32:
                TX(e_ + 1)
            YY(e_)
            if e_ + 2 < 32:
                W2(e_ + 2)
        if stop_after == "B":
            S.finish(final_ops + ystore[-3:])
            S.emit(st)
            return nc, dbg_outs

        S.barrier(dma_ops=ystore[-3:])
        A.reset(mark_persist)
        NBC = 3
        Hc = [A.alloc("Hc%d" % i, [128, D], F32) for i in range(NBC)]
        Y0 = [A.alloc("Y0_%d" % i, [128, D], F32) for i in range(NBC)]
        Y1 = [A.alloc("Y1_%d" % i, [128, D], F32) for i in range(NBC)]
        acc = A.alloc("acc", [128, D], F32)
        ob = [A.alloc("ob%d" % i, [128, D], F32) for i in range(2)]
        gt2b = A.alloc("gt2b", [128, D], F32)
        gfb = A.alloc("gfb", [128, D], F32)
        jk = A.alloc("jk", [128, D], BF16)
        ssq3 = A.alloc("ssq3", [128, 1], F32)
        rstd3 = A.alloc("rstd3", [128, 1], F32)
        dma("sp", gfb.ap, gf_d.partition_broadcast(128), [], [gfb], "gfb")
        for s_ in range(NBC):
            memset("pool", Y0[s_].ap, 0.0, [Y0[s_]])
            memset("pool", Y1[s_].ap, 0.0, [Y1[s_]])

        def Cload(i):
            s_ = i % NBC
            dma("sp", Hc[s_].ap, H_d[i * 128:(i + 1) * 128, :], [r_H], [Hc[s_]], "hc%d" % s_)
            for k, Yk in enumerate((Y0[s_], Y1[s_])):
                S.add("pool", lambda e, i=i, k=k, Yk=Yk: e.indirect_dma_start(
                    out=Yk.ap, out_offset=None, in_=Y_d, in_offset=bass.IndirectOffsetOnAxis(ap=sloti.ap[:, i, k:k + 1], axis=0),
                    bounds_check=breg(e, NSLOT - 1), oob_is_err=False), R(r_Y, sloti), R(Yk), dma_key="gath%d_%d" % (k, s_))

        Cload(0)
        if NTILES > 1:
            Cload(1)
        for i in range(NTILES):
            b, tau = i // NT, i % NT
            s_ = i % NBC
            if tau == 0:
                dma("sp", gt2b.ap, mod_d.ap[b:b + 1, 5 * D:6 * D].partition_broadcast(128), [mod_d], [gt2b], "gt2b")
            if i + 2 < NTILES:
                Cload(i + 2)
            ts("dve", acc.ap, Y0[s_].ap, wgt.ap[:, i, 0:1], None, ALU.mult, None, [Y0[s_], wgt], [acc])
            stt(acc.ap, Y1[s_].ap, wgt.ap[:, i, 1:2], acc.ap, ALU.mult, ALU.add, [Y1[s_], wgt, acc], [acc])
            tt("dve", acc.ap, acc.ap, gt2b.ap, ALU.mult, [acc, gt2b], [acc])
            tt("dve", acc.ap, acc.ap, Hc[s_].ap, ALU.add, [acc, Hc[s_]], [acc])
            rms_rstd(acc, ssq3, rstd3, jk.ap, jk)
            stt(ob[i % 2].ap, acc.ap, rstd3.ap[:, 0:1], gfb.ap, ALU.mult, ALU.mult, [acc, rstd3, gfb], [ob[i % 2]])
            final_ops.append(dma("sp", out_d[i * 128:(i + 1) * 128, :], ob[i % 2].ap, [ob[i % 2]], [], "ost%d" % (i % 2)))
        S.finish(final_ops)
        S.emit(st)
    return nc, dbg_outs


def prep_shared(inp):
    f = np.float32
    g = {}
    L = 0
    g["w_ada"] = np.ascontiguousarray(inp["w_ada"][L], f)
    g["g1T"] = np.ascontiguousarray(inp["norm1_g"][L].reshape(8, 128).T, f)
    g["w_in"] = np.ascontiguousarray(inp["w_in"][L], f)
    g["w_gate"] = np.ascontiguousarray(inp["w_gate"][L], f)
    g["b_gateT"] = np.ascontiguousarray(inp["b_gate"][L].reshape(16, 128).T, f)
    a_re, a_im, ls = inp["ssm_a_re"][L], inp["ssm_a_im"][L], inp["ssm_log_step"][L]
    def sm(v):
        return v.reshape(16, 2, 64).transpose(1, 2, 0).reshape(128, 16)
    lsx = np.repeat(ls[:, None], 64, axis=1)
    g["lam_sm"] = np.ascontiguousarray(np.stack([sm(a_re), sm(a_im), sm(lsx)], axis=1), f)
    Bsm = np.zeros((128, 2, 16, 128), f)
    for pi, Bsrc in enumerate((inp["ssm_b_re"][L], inp["ssm_b_im"][L])):
        for gi in range(32):
            j = gi // 2
            n0 = 64 * (gi % 2)
            c0 = 32 * (j % 4) + 16 * (gi % 2)
            Bsm[n0:n0 + 64, pi, j, c0:c0 + 16] = Bsrc[gi]
    g["Bsm"] = Bsm
    Csm = np.zeros((128, 2, 16, 128), f)
    for pi, Csrc in enumerate((inp["ssm_c_re"][L], inp["ssm_c_im"][L])):
        for gi in range(32):
            j = gi // 2
            n0 = 64 * (gi % 2)
            c0 = 32 * (j % 4) + 16 * (gi % 2)
            Csm[n0:n0 + 64, pi, j, c0:c0 + 16] = Csrc[gi].T
    g["Csm"] = Csm
    g["dT"] = np.ascontiguousarray(inp["ssm_d"][L].reshape(4, 128).T, f)
    g["w_glu"] = np.ascontiguousarray(inp["w_glu"][L], f)
    g["b_gluT"] = np.ascontiguousarray(inp["b_glu"][L].reshape(4, 128).T, f)
    g["wsT"] = np.ascontiguousarray(inp["sgu_w"][L].transpose(2, 0, 1), f)
    g["lngT"] = np.ascontiguousarray(inp["sgu_ln_g"][L].reshape(4, 128).T, f)
    g["lnbT"] = np.ascontiguousarray(inp["sgu_ln_b"][L].reshape(4, 128).T, f)
    bs = inp["sgu_b"][L]
    bsT = np.zeros((128, 4, 128), f)
    for q in range(4):
        bsT[0:64, q, :] = bs[2 * q][None, :]
        bsT[64:128, q, :] = bs[2 * q + 1][None, :]
    g["bsT"] = bsT
    g["w_bra"] = np.ascontiguousarray(inp["w_branch_a"][L], f)
    g["w_brb"] = np.ascontiguousarray(inp["w_branch_b"][L], f)
    g["w_out"] = np.ascontiguousarray(inp["w_out"][L], f)
    g["g2"] = np.ascontiguousarray(inp["norm2_g"][L].reshape(1, D), f)
    wr = np.concatenate([inp["w_router_group"][L], inp["w_router_expert"][L].transpose(1, 0, 2).reshape(D, 32)], axis=1)
    g["w_r"] = np.ascontiguousarray(wr.reshape(8, 128, 36).transpose(1, 0, 2), f)
    g["b_r"] = np.ascontiguousarray(np.concatenate([inp["b_router_group"][L], inp["b_router_expert"][L].reshape(32)]).reshape(1, 36), f)
    g["w1"] = np.ascontiguousarray(inp["w1"][L], f)
    g["w3"] = np.ascontiguousarray(inp["w3"][L], f)
    g["w2"] = np.ascontiguousarray(inp["w2"][L], f)
    g["gf"] = np.ascontiguousarray(inp["norm_f_g"].reshape(1, D), f)
    return g


def prep_core(inp, shared, b0, nseq):
    m = dict(shared)
    xs = inp["x"][b0:b0 + nseq]
    m["x"] = np.ascontiguousarray(xs.reshape(-1, D), np.float32)
    c = inp["c"][b0:b0 + nseq]
    m["cT"] = np.ascontiguousarray(c.reshape(nseq, 8, 128).transpose(2, 1, 0), np.float32)
    m["b_ada_rep"] = np.ascontiguousarray(np.repeat(inp["b_ada"][0][None, :], nseq, axis=0), np.float32)
    return m


_CACHE = {}


def kernel(**inputs):
    inp = {k: np.asarray(v) for k, v in inputs.items()}
    B, SEQ = inp["x"].shape[0], inp["x"].shape[1]
    nseq = B // N_CORES
    key = (nseq, SEQ)
    if key not in _CACHE:
        _CACHE[key] = build(NSEQ=nseq, SEQ=SEQ, CAP=768)[0]
    nc = _CACHE[key]
    shared = prep_shared(inp)
    in_maps = [prep_core(inp, shared, c * nseq, nseq) for c in range(N_CORES)]
    res = run_bass_kernel_spmd(nc, in_maps, core_ids=list(range(N_CORES)))
    outs = [np.asarray(r["out"]).reshape(nseq, SEQ, D) for r in res.results]
    return np.concatenate(outs, axis=0).astype(np.float32)
```

```python
import math
from contextlib import ExitStack
import numpy as np
import concourse.bass as bass
import concourse.mybir as mybir
from concourse.bass_utils import run_bass_kernel_spmd

F32 = mybir.dt.float32
BF16 = mybir.dt.bfloat16
I32 = mybir.dt.int32
U8 = mybir.dt.uint8
AF = mybir.ActivationFunctionType
ALU = mybir.AluOpType
AX = mybir.AxisListType

ENGS = ("pe", "act", "dve", "pool", "sp")
N_CORES = 8
D = 1024
TWO_PI = 2.0 * math.pi
RMS_EPS = 1e-6
LN_EPS = 1e-5


class Res:
    __slots__ = ("name", "lastw", "readers")

    def __init__(self, name):
        self.name = name
        self.lastw = None
        self.readers = []


class Op:
    __slots__ = ("eng", "fn", "deps", "dma_key", "dma_val", "signal", "sig_idx")

    def __init__(self, eng, fn, dma_key):
        self.eng = eng
        self.fn = fn
        self.deps = []
        self.dma_key = dma_key
        self.dma_val = None
        self.signal = False
        self.sig_idx = None


class Sched:
    def __init__(self, nc):
        self.nc = nc
        self.ops = {e: [] for e in ENGS}
        self.dma_cnt = {}
        self.finals = []
        self.pending_barrier = {}

    def add(self, eng, fn, reads=(), writes=(), dma_key=None):
        op = Op(eng, fn, dma_key)
        deps = []
        for r in reads:
            if r.lastw is not None:
                deps.append(r.lastw)
        for w in writes:
            if w.lastw is not None:
                deps.append(w.lastw)
            deps.extend(w.readers)
        if eng in self.pending_barrier:
            deps.extend(self.pending_barrier.pop(eng))
        seen = set()
        for d in deps:
            if d is op or id(d) in seen:
                continue
            seen.add(id(d))
            if d.eng == "pe" and eng == "pe" and d.dma_key is None and dma_key is None:
                continue
            op.deps.append(d)
            if d.dma_key is None:
                d.signal = True
        if dma_key is not None:
            self.dma_cnt[dma_key] = self.dma_cnt.get(dma_key, 0) + 16
            op.dma_val = self.dma_cnt[dma_key]
        for r in reads:
            r.readers.append(op)
        for w in writes:
            w.lastw = op
            w.readers = []
        self.ops[eng].append(op)
        return op

    def barrier(self, dma_ops=()):
        lasts = [self.ops[e][-1] for e in ENGS if self.ops[e]]
        lasts = [o for o in lasts if o.dma_key is None] + list(dma_ops)
        for e in ENGS:
            self.pending_barrier.setdefault(e, []).extend(lasts)

    def finish(self, ops):
        self.finals.extend(ops)

    def emit(self, stack):
        nc = self.nc
        sems = {e: stack.enter_context(nc.semaphore("sem_" + e)) for e in ENGS}
        dsem = {k: stack.enter_context(nc.semaphore("dsem_%s" % (k,))) for k in self.dma_cnt}
        for e in ENGS:
            n = 0
            for op in self.ops[e]:
                if op.dma_key is None and op.signal:
                    n += 1
                    op.sig_idx = n
        block = stack.enter_context(nc.Block())
        engobj = {"pe": "tensor", "act": "scalar", "dve": "vector", "pool": "gpsimd", "sp": "sync"}
        finals = self.finals

        def body_for(e):
            def body(eng):
                seen = {}
                for op in self.ops[e]:
                    need = {}
                    for d in op.deps:
                        if d.dma_key is not None:
                            s, v, key = dsem[d.dma_key], d.dma_val, ("d", d.dma_key)
                        else:
                            s, v, key = sems[d.eng], d.sig_idx, ("e", d.eng)
                        if key not in need or need[key][1] < v:
                            need[key] = (s, v)
                    for key, (s, v) in need.items():
                        if seen.get(key, 0) >= v:
                            continue
                        seen[key] = v
                        eng.wait_ge(s, v)
                    inst = op.fn(eng)
                    if op.dma_key is not None:
                        inst.then_inc(dsem[op.dma_key], 16)
                    elif op.signal:
                        inst.then_inc(sems[e], 1)
                if e == "sp":
                    for d in finals:
                        eng.wait_ge(dsem[d.dma_key], d.dma_val)
            return body

        for e in ENGS:
            getattr(block, engobj[e])(body_for(e))


class Tl:
    __slots__ = ("ap", "r")

    def __init__(self, ap, name):
        self.ap = ap
        self.r = Res(name)


class Arena:
    def __init__(self, nc, stack, nbytes):
        self.buf = stack.enter_context(nc.sbuf_tensor("arena", [128, nbytes], U8))
        self.off = 0
        self.cap = nbytes
        self.live = []

    def alloc(self, name, shape, dt):
        esz = {F32: 4, BF16: 2, I32: 4}[dt]
        n = 1
        for s in shape[1:]:
            n *= s
        nb = (n * esz + 31) // 32 * 32
        assert self.off + nb <= self.cap, "SBUF arena overflow at %s: %d + %d > %d" % (name, self.off, nb, self.cap)
        v = self.buf[0:shape[0], self.off:self.off + n * esz].bitcast(dt)
        if len(shape) == 3:
            v = v.rearrange("p (a b) -> p a b", a=shape[1])
        elif len(shape) == 4:
            v = v.rearrange("p (a b c) -> p a b c", a=shape[1], b=shape[2])
        t = Tl(v, name)
        lo, hi = self.off, self.off + nb
        keep = []
        for (a, b, o) in self.live:
            if a < hi and lo < b:
                if o.r.lastw is not None:
                    t.r.readers.append(o.r.lastw)
                t.r.readers.extend(o.r.readers)
                if a >= lo and b <= hi:
                    continue
            keep.append((a, b, o))
        keep.append((lo, hi, t))
        self.live = keep
        self.off += nb
        return t

    def mark(self):
        return self.off

    def reset(self, m):
        self.off = m


def build(NSEQ=4, SEQ=2048, CAP=768, dbg=None, stop_after=None):
    nc = bass.Bass("TRN2", target_bir_lowering=False)
    NT = SEQ // 128
    NTOK = NSEQ * SEQ
    NTILES = NSEQ * NT
    NSLOT = 32 * CAP
    NBLK = CAP // 128
    dbg = dbg or {}
    dbg_outs = {}

    def din(name, shape, dt=F32):
        return nc.dram_tensor(name, list(shape), dt, kind="ExternalInput").ap()

    x_d = din("x", [NTOK, D])
    cT_d = din("cT", [128, 8, NSEQ])
    w_ada_d = din("w_ada", [D, 6 * D])
    b_ada_d = din("b_ada_rep", [NSEQ, 6 * D])
    g1T_d = din("g1T", [128, 8])
    w_in_d = din("w_in", [D, 1536])
    w_gate_d = din("w_gate", [D, 2048])
    b_gateT_d = din("b_gateT", [128, 16])
    lam_sm_d = din("lam_sm", [128, 3, 16])
    Bsm_d = din("Bsm", [128, 2, 16, 128])
    Csm_d = din("Csm", [128, 2, 16, 128])
    dT_d = din("dT", [128, 4])
    w_glu_d = din("w_glu", [512, 512])
    b_gluT_d = din("b_gluT", [128, 4])
    wsT_d = din("wsT", [128, 8, 128])
    lngT_d = din("lngT", [128, 4])
    lnbT_d = din("lnbT", [128, 4])
    bsT_d = din("bsT", [128, 4, 128])
    w_bra_d = din("w_bra", [512, D])
    w_brb_d = din("w_brb", [512, D])
    w_out_d = din("w_out", [D, D])
    g2_d = din("g2", [1, D])
    w_r_d = din("w_r", [128, 8, 36])
    b_r_d = din("b_r", [1, 36])
    w1_d = din("w1", [32, D, 512])
    w3_d = din("w3", [32, D, 512])
    w2_d = din("w2", [32, 512, D])
    gf_d = din("gf", [1, D])
    out_d = nc.dram_tensor("out", [NTOK, D], F32, kind="ExternalOutput").ap()
    mod_d = Tl(nc.dram_tensor("mod_d", [NSEQ, 6 * D], F32, kind="Internal").ap(), "mod_d")
    H_d = nc.dram_tensor("H_d", [NTOK, D], F32, kind="Internal").ap()
    X_d = nc.dram_tensor("X_d", [NSLOT, D], BF16, kind="Internal").ap()
    Y_d = nc.dram_tensor("Y_d", [NSLOT, D], F32, kind="Internal").ap()
    r_X = Res("X_d")
    r_Y = Res("Y_d")
    r_H = Res("H_d")

    S = Sched(nc)
    final_ops = []
    with ExitStack() as st:
        A = Arena(nc, st, 212800)
        ps_all = st.enter_context(nc.psum_tensor("ps_all", [128, 8, 512], F32))
        PSB = [Tl(ps_all[:, b, :], "psb%d" % b) for b in range(8)]

        def psv(b, shape, dt=F32):
            v = PSB[b].ap
            if dt == BF16:
                v = v.bitcast(BF16)
            n = 1
            for s in shape[1:]:
                n *= s
            v = v[0:shape[0], 0:n]
            if len(shape) == 3:
                v = v.rearrange("p (a b) -> p a b", a=shape[1])
            elif len(shape) == 4:
                v = v.rearrange("p (a b c) -> p a b c", a=shape[1], b=shape[2])
            return v

        def psv2(b, shape):
            v = ps_all[:, b:b + 2, :].rearrange("p a b -> p (a b)")
            n = 1
            for s in shape[1:]:
                n *= s
            v = v[0:shape[0], 0:n]
            if len(shape) == 3:
                v = v.rearrange("p (a b) -> p a b", a=shape[1])
            elif len(shape) == 4:
                v = v.rearrange("p (a b c) -> p a b c", a=shape[1], b=shape[2])
            return v

        def R(*ts):
            return [t.r if isinstance(t, Tl) else t for t in ts]

        def dma(eng, out, in_, reads, writes, key, **kw):
            return S.add(eng, lambda e: e.dma_start(out=out, in_=in_, **kw), R(*reads), R(*writes), dma_key=key)

        def tt(eng, out, in0, in1, op, reads, writes):
            return S.add(eng, lambda e: e.tensor_tensor(out=out, in0=in0, in1=in1, op=op), R(*reads), R(*writes))

        def ts(eng, out, in0, s1, s2, op0, op1, reads, writes, accum=None):
            if op1 is None:
                return S.add(eng, lambda e: e.tensor_scalar(out=out, in0=in0, scalar1=s1, scalar2=None, op0=op0), R(*reads), R(*writes))
            if accum is not None:
                return S.add(eng, lambda e: e.tensor_scalar(out=out, in0=in0, scalar1=s1, scalar2=s2, op0=op0, op1=op1, accum_out=accum), R(*reads), R(*writes))
            return S.add(eng, lambda e: e.tensor_scalar(out=out, in0=in0, scalar1=s1, scalar2=s2, op0=op0, op1=op1), R(*reads), R(*writes))

        def stt(out, in0, scalar, in1, op0, op1, reads, writes):
            return S.add("dve", lambda e: e.scalar_tensor_tensor(out=out, in0=in0, scalar=scalar, in1=in1, op0=op0, op1=op1), R(*reads), R(*writes))

        def act(out, in_, func, reads, writes, bias=None, scale=None, accum=None):
            kw = {}
            if bias is not None:
                kw["bias"] = bias
            if scale is not None:
                kw["scale"] = scale
            if accum is not None:
                kw["accum_out"] = accum
            return S.add("act", lambda e: e.activation(out=out, in_=in_, func=func, **kw), R(*reads), R(*writes))

        def cp(eng, out, in_, reads, writes):
            if eng == "act":
                return S.add("act", lambda e: e.copy(out=out, in_=in_), R(*reads), R(*writes))
            return S.add(eng, lambda e: e.tensor_copy(out=out, in_=in_), R(*reads), R(*writes))

        def mm(out, lhsT, rhs, start, stop, reads, writes):
            return S.add("pe", lambda e: e.matmul(out, lhsT=lhsT, rhs=rhs, start=start, stop=stop), R(*reads), R(*writes))

        def tr(out, in_, ident, reads, writes):
            return S.add("pe", lambda e: e.transpose(out=out, in_=in_, identity=ident), R(*reads), R(*writes))

        def memset(eng, ap, val, writes):
            return S.add(eng, lambda e: e.memset(ap, val), [], R(*writes))

        _regs = {}

        def breg(e, val):
            if val not in _regs:
                _regs[val] = e.to_reg(val)
            return _regs[val]

        def dump(name, t, ap=None):
            ap = t.ap if ap is None else ap
            shp = list(ap.shape)
            o = nc.dram_tensor("dbg_" + name, shp, ap.dtype, kind="ExternalOutput").ap()
            dbg_outs[name] = "dbg_" + name
            final_ops.append(dma("sp", o, ap, [t], [], "dbg_" + name))

        ident_f = A.alloc("ident_f", [128, 128], F32)
        ident_b = A.alloc("ident_b", [128, 128], BF16)
        tri_b = A.alloc("tri_b", [128, 128], BF16)
        ones_b = A.alloc("ones_b", [128, 128], BF16)
        memset("pool", ident_f.ap, 0.0, [ident_f])
        S.add("pool", lambda e: e.affine_select(out=ident_f.ap, in_=ident_f.ap, pattern=[[-1, 128]], compare_op=ALU.not_equal,
                                                  fill=1.0, base=0, channel_multiplier=1), R(ident_f), R(ident_f))
        cp("pool", ident_b.ap, ident_f.ap, [ident_f], [ident_b])
        memset("pool", ones_b.ap, 1.0, [ones_b])
        S.add("pool", lambda e: e.affine_select(out=tri_b.ap, in_=ones_b.ap, pattern=[[1, 128]], compare_op=ALU.is_gt,
                                                  fill=0.0, base=0, channel_multiplier=-1), R(ones_b), R(tri_b))

        wgt = A.alloc("wgt", [128, NTILES, 2], F32)
        sloti = A.alloc("sloti", [128, NTILES, 2], I32)
        base = A.alloc("base", [128, 32], F32)
        ecap = A.alloc("ecap", [128, 32], F32)
        memset("pool", base.ap, 0.0, [base])
        ecap_i = A.alloc("ecap_i", [128, 32], I32)
        S.add("pool", lambda e: e.iota(ecap_i.ap, pattern=[[CAP, 32]], base=0, channel_multiplier=0), [], R(ecap_i))
        cp("pool", ecap.ap, ecap_i.ap, [ecap_i], [ecap])
        A1T = A.alloc("A1T", [128, 8, NSEQ], F32)
        sh1T = A.alloc("sh1T", [128, 8, NSEQ], F32)
        eps_rms = A.alloc("eps_rms", [128, 1], F32)
        eps_ln = A.alloc("eps_ln", [128, 1], F32)
        memset("pool", eps_rms.ap, 1e-6, [eps_rms])
        memset("pool", eps_ln.ap, 1e-5, [eps_ln])
        mhalf = A.alloc("mhalf", [128, 1], F32)
        memset("pool", mhalf.ap, -0.5, [mhalf])

        mark_persist = A.mark()

        cact = A.alloc("cact", [128, 8, NSEQ], F32)
        dma("sp", cact.ap, cT_d, [], [cact], "cact")
        act(cact.ap, cact.ap, AF.Silu, [cact], [cact])
        modrow = A.alloc("modrow", [NSEQ, 6 * D], F32)
        bada = A.alloc("bada", [NSEQ, 6 * D], F32)
        dma("sp", bada.ap, b_ada_d, [], [bada], "bada")
        wa = [A.alloc("wa%d" % i, [128, 8, 512], F32) for i in range(2)]
        wa_view = w_ada_d.rearrange("(kc p) n -> p kc n", p=128)
        for cb in range(12):
            w = wa[cb % 2]
            dma("sp", w.ap, wa_view[:, :, cb * 512:(cb + 1) * 512], [], [w], "wa%d" % (cb % 2))
            pb = PSB[cb % 2]
            for kc in range(8):
                mm(pb.ap[0:NSEQ, :], cact.ap[:, kc, :], w.ap[:, kc, :], kc == 0, kc == 7, [cact, w], [pb])
            tt("dve", modrow.ap[:, cb * 512:(cb + 1) * 512], pb.ap[0:NSEQ, :], bada.ap[:, cb * 512:(cb + 1) * 512], ALU.add,
               [pb, bada], [modrow])
        dma("sp", mod_d.ap, modrow.ap, [modrow], [mod_d], "mod_d")
        sc1T = A.alloc("sc1T", [128, 8, NSEQ], F32)
        g1T = A.alloc("g1T", [128, 8], F32)
        dma("sp", g1T.ap, g1T_d, [], [g1T], "g1T")
        for b in range(NSEQ):
            S.add("sp", lambda e, b=b: e.dma_start(out=sh1T.ap[:, :, b], in_=mod_d.ap[b, 0:D].rearrange("(kc p) -> p kc", p=128),
                                                  allow_slow_non_contiguous=True), R(mod_d), R(sh1T), dma_key="sh1T")
            S.add("sp", lambda e, b=b: e.dma_start(out=sc1T.ap[:, :, b], in_=mod_d.ap[b, D:2 * D].rearrange("(kc p) -> p kc", p=128),
                                                  allow_slow_non_contiguous=True), R(mod_d), R(sc1T), dma_key="sc1T")
        for b in range(NSEQ):
            stt(A1T.ap[:, :, b], sc1T.ap[:, :, b], 1.0, g1T.ap, ALU.add, ALU.mult, [sc1T, g1T], [A1T])
        if "mod" in dbg:
            dump("modrow", modrow)
            dump("A1T", A1T)
        A.reset(mark_persist)
        S.barrier()
        if stop_after == "mod":
            S.finish(final_ops)
            S.emit(st)
            return nc, dbg_outs

        w_r = A.alloc("w_r", [128, 8, 36], F32)
        dma("sp", w_r.ap, w_r_d, [], [w_r], "w_r")
        b_r = A.alloc("b_r", [128, 36], F32)
        dma("sp", b_r.ap, b_r_d.partition_broadcast(128), [], [b_r], "b_r")
        b_gateT = A.alloc("b_gateT", [128, 16], F32)
        dma("sp", b_gateT.ap, b_gateT_d, [], [b_gateT], "b_gateT")
        ts("pool", b_gateT.ap, b_gateT.ap, 0.5, 1.0, ALU.mult, ALU.mult, [b_gateT], [b_gateT])
        b_gluT = A.alloc("b_gluT", [128, 4], F32)
        dma("sp", b_gluT.ap, b_gluT_d, [], [b_gluT], "b_gluT")
        ts("pool", b_gluT.ap, b_gluT.ap, 0.5, 1.0, ALU.mult, ALU.mult, [b_gluT], [b_gluT])
        dT = A.alloc("dT", [128, 4], F32)
        dma("sp", dT.ap, dT_d, [], [dT], "dT")
        lngT = A.alloc("lngT", [128, 4], F32)
        dma("sp", lngT.ap, lngT_d, [], [lngT], "lngT")
        lnbT = A.alloc("lnbT", [128, 4], F32)
        dma("sp", lnbT.ap, lnbT_d, [], [lnbT], "lnbT")

        LC = 4
        NCH = 128 // LC
        tabC4 = A.alloc("tabC4", [128, 16, NCH + 1], F32)
        tabD4 = A.alloc("tabD4", [128, 16, NCH + 1], F32)
        Rtab = A.alloc("Rtab", [128, 16, 2, NCH], F32)
        rmagL = A.alloc("rmagL", [128, 16], F32)
        Pt = A.alloc("Pt", [128, 2, 8 * LC, 128], BF16)
        Qs = A.alloc("Qs", [128, 2, 16 * LC, 64], BF16)
        Kb = A.alloc("Kb", [128, 4 * LC, 128], BF16)
        tabC = Tl(None, "s5scr")
        mark_setup = A.mark()

        def range_reduce(ph, tmpf, tmpi, n):
            ts("dve", tmpi, ph, 1.0 / TWO_PI, None, ALU.mult, None, [tabC], [tabC])
            cp("dve", tmpf, tmpi, [tabC], [tabC])
            stt(ph, tmpf, -TWO_PI, ph, ALU.mult, ALU.add, [tabC], [tabC])
            wrap(ph, tmpf)

        def wrap(ph, tmpf):
            ts("dve", tmpf, ph, math.pi, None, ALU.is_gt, None, [tabC], [tabC])
            stt(ph, tmpf, -TWO_PI, ph, ALU.mult, ALU.add, [tabC], [tabC])
            ts("dve", tmpf, ph, -math.pi, None, ALU.is_lt, None, [tabC], [tabC])
            stt(ph, tmpf, TWO_PI, ph, ALU.mult, ALU.add, [tabC], [tabC])

        def scr(name, shape, dt):
            t = A.alloc(name, shape, dt)
            t.r = tabC.r
            return t

        TC = [tabC]
        lam = scr("lam", [128, 3, 16], F32)
        dma("sp", lam.ap, lam_sm_d, [], TC, "lam")
        zs = {n: scr("z_" + n, [128, 16], F32) for n in ("dt", "th", "lre", "lrdt", "den", "nr", "fre", "fim", "t0", "t1")}
        sv_i = scr("sv_i", [128, 129], I32)
        sv = scr("sv", [128, 129], F32)
        tabCf = scr("tabCf", [128, 16, 129], F32)
        tabDf = scr("tabDf", [128, 16, 129], F32)
        tmpf = scr("tmpf", [128, 16 * 129], F32)
        tmpi = scr("tmpi", [128, 16 * 129], I32)
        aim = lam.ap[:, 1, :]
        S.add("pool", lambda e: e.iota(sv_i.ap, pattern=[[1, 129]], base=0, channel_multiplier=0), [], R(tabC))
        cp("dve", sv.ap, sv_i.ap, TC, TC)
        ts("dve", zs["lre"].ap, lam.ap[:, 0, :], -1e-4, None, ALU.min, None, TC, TC)
        act(zs["dt"].ap, lam.ap[:, 2, :], AF.Exp, TC, TC)
        tt("dve", zs["lrdt"].ap, zs["lre"].ap, zs["dt"].ap, ALU.mult, TC, TC)
        tt("dve", zs["th"].ap, aim, zs["dt"].ap, ALU.mult, TC, TC)
        for j in range(16):
            ts("dve", tabDf.ap[:, j, :], sv.ap, zs["th"].ap[:, j:j + 1], None, ALU.mult, None, TC, TC)
        phD = tabDf.ap.rearrange("p a b -> p (a b)")
        phC = tabCf.ap.rearrange("p a b -> p (a b)")
        range_reduce(phD, tmpf.ap, tmpi.ap, 16 * 129)
        ts("dve", phC, phD, math.pi / 2, None, ALU.add, None, TC, TC)
        wrap(phC, tmpf.ap)
        act(phD, phD, AF.Sin, TC, TC)
        act(phC, phC, AF.Sin, TC, TC)
        cp("dve", tabC4.ap, tabCf.ap[:, :, 0:129:LC], TC, TC + [tabC4])
        cp("dve", tabD4.ap, tabDf.ap[:, :, 0:129:LC], TC, TC + [tabD4])
        rk = scr("rk", [128, 16, LC + 1], F32)
        pwr = scr("pwr", [128, 16, LC + 1], F32)
        pwi = scr("pwi", [128, 16, LC + 1], F32)
        for k in range(LC + 1):
            act(rk.ap[:, :, k], zs["lrdt"].ap, AF.Exp, TC, TC, scale=float(k))
        tt("dve", pwr.ap, rk.ap, tabCf.ap[:, :, 0:LC + 1], ALU.mult, TC, TC)
        tt("dve", pwi.ap, rk.ap, tabDf.ap[:, :, 0:LC + 1], ALU.mult, TC, TC)
        cp("dve", rmagL.ap, rk.ap[:, :, LC], TC, TC + [rmagL])
        cp("dve", Rtab.ap.rearrange("p a b c -> p a (b c)"), rmagL.ap.unsqueeze(2).to_broadcast([128, 16, 2 * NCH]),
           TC + [rmagL], TC + [Rtab])
        memset("dve", Rtab.ap[:, :, :, 0], 0.0, TC + [Rtab])
        abr, abi = pwr.ap[:, :, 1], pwi.ap[:, :, 1]
        lre_ = zs["lre"].ap
        tt("dve", zs["den"].ap, lre_, lre_, ALU.mult, TC, TC)
        tt("dve", zs["t0"].ap, aim, aim, ALU.mult, TC, TC)
        tt("dve", zs["den"].ap, zs["den"].ap, zs["t0"].ap, ALU.add, TC, TC)
        S.add("dve", lambda e: e.reciprocal(out=zs["den"].ap, in_=zs["den"].ap), R(tabC), R(tabC))
        ts("dve", zs["nr"].ap, abr, -1.0, None, ALU.add, None, TC, TC)
        tt("dve", zs["t0"].ap, zs["nr"].ap, lre_, ALU.mult, TC, TC)
        tt("dve", zs["t1"].ap, abi, aim, ALU.mult, TC, TC)
        tt("dve", zs["t0"].ap, zs["t0"].ap, zs["t1"].ap, ALU.add, TC, TC)
        tt("dve", zs["fre"].ap, zs["t0"].ap, zs["den"].ap, ALU.mult, TC, TC)
        tt("dve", zs["t0"].ap, abi, lre_, ALU.mult, TC, TC)
        tt("dve", zs["t1"].ap, zs["nr"].ap, aim, ALU.mult, TC, TC)
        tt("dve", zs["t0"].ap, zs["t0"].ap, zs["t1"].ap, ALU.subtract, TC, TC)
        tt("dve", zs["fim"].ap, zs["t0"].ap, zs["den"].ap, ALU.mult, TC, TC)
        Bsm = scr("Bsm", [128, 2, 16, 128], F32)
        Csm = scr("Csm", [128, 2, 16, 128], F32)
        dma("sp", Bsm.ap, Bsm_d, [], TC, "Bsm")
        dma("sp", Csm.ap, Csm_d, [], TC, "Csm")
        Btr = scr("Btr", [128, 16, 128], F32)
        Bti = scr("Bti", [128, 16, 128], F32)
        Gr = scr("Gr", [128, 16, 128], F32)
        Gi = scr("Gi", [128, 16, 128], F32)
        w0 = scr("w0", [128, 16, 128], F32)
        w1_ = scr("w1_", [128, 16, 128], F32)
        Psr = scr("Psr", [128, 16, 128], BF16)
        Psi = scr("Psi", [128, 16, 128], BF16)
        Kf = scr("Kf", [128, 128], F32)

        def bc(v):
            return v.unsqueeze(2).to_broadcast([128, 16, 128])

        def cmul(o_re, o_im, a_re, a_im, s_re, s_im, neg_im=False):
            tt("dve", w0.ap, a_re, bc(s_re), ALU.mult, TC, TC)
            tt("dve", w1_.ap, a_im, bc(s_im), ALU.mult, TC, TC)
            tt("dve", o_re, w0.ap, w1_.ap, ALU.subtract, TC, TC)
            tt("dve", w0.ap, a_re, bc(s_im), ALU.mult, TC, TC)
            tt("dve", w1_.ap, a_im, bc(s_re), ALU.mult, TC, TC)
            if neg_im:
                stt(o_im, w0.ap, -1.0, w1_.ap, ALU.mult, ALU.subtract, TC, TC)
            else:
                tt("dve", o_im, w0.ap, w1_.ap, ALU.add, TC, TC)

        cmul(Btr.ap, Bti.ap, Bsm.ap[:, 0], Bsm.ap[:, 1], zs["fre"].ap, zs["fim"].ap)
        memset("dve", Kb.ap, 0.0, TC + [Kb])
        for k in range(LC + 1):
            cmul(Gr.ap, Gi.ap, Csm.ap[:, 0], Csm.ap[:, 1], pwr.ap[:, :, k], pwi.ap[:, :, k], neg_im=True)
            if k >= 1:
                for j in range(16):
                    c0 = 64 * ((j % 4) // 2)
                    cp("act", Qs.ap[:, 0, j * LC + k - 1, :], Gr.ap[:, j, c0:c0 + 64], TC, TC + [Qs])
                    cp("act", Qs.ap[:, 1, j * LC + k - 1, :], Gi.ap[:, j, c0:c0 + 64], TC, TC + [Qs])
            if k < LC:
                for q in range(4):
                    pK = PSB[2 + q % 2]
                    for jm in range(4):
                        j = 4 * q + jm
                        mm(pK.ap[:, 0:128], Btr.ap[:, j, :], Gr.ap[:, j, :], jm == 0, False, TC, [pK])
                        mm(pK.ap[:, 0:128], Bti.ap[:, j, :], Gi.ap[:, j, :], False, jm == 3, TC, [pK])
                    if k == 0:
                        stt(Kf.ap, ident_f.ap, dT.ap[:, q:q + 1], pK.ap[:, 0:128], ALU.mult, ALU.add, [pK, ident_f, dT] + TC, TC)
                        cp("dve", Kb.ap[:, q * LC + k, :], Kf.ap, TC, TC + [Kb])
                    else:
                        cp("dve", Kb.ap[:, q * LC + k, :], pK.ap[:, 0:128], [pK] + TC, TC + [Kb])
                s_ = LC - 1 - k
                cmul(Psr.ap, Psi.ap, Btr.ap, Bti.ap, pwr.ap[:, :, k], pwi.ap[:, :, k])
                for part, Ps_ in enumerate((Psr, Psi)):
                    for pr in range(2):
                        bk = (2 * part + pr) % 2
                        pT = psv(bk, [128, 8, 128], BF16)
                        for idx in range(8):
                            j = 4 * (idx // 2) + 2 * pr + idx % 2
                            tr(pT[64 * pr:64 * pr + 64, idx, :], Ps_.ap[:, j, 64 * pr:64 * pr + 64], ident_b.ap, TC + [ident_b], [PSB[bk]])
                        cp("act", Pt.ap[64 * pr:64 * pr + 64, part, s_::LC, :], pT[64 * pr:64 * pr + 64, :, :], [PSB[bk]] + TC, TC + [Pt])
        if "s5setup" in dbg:
            dump("tabC4", tabC4)
            dump("Kb", Kb)
            dump("Pt", Pt)
            dump("Qs", Qs)
        A.reset(mark_setup)

        wsT_b = A.alloc("wsT_b", [128, 8, 128], BF16)
        sgub = A.alloc("sgub", [128, 4, 128], F32)
        mark_sgu = A.mark()
        wsf = A.alloc("wsf", [128, 8, 128], F32)
        dma("sp", wsf.ap, wsT_d, [], [wsf], "wsf")
        for h in range(8):
            S.add("pool", lambda e, h=h: e.affine_select(out=wsf.ap[:, h, :], in_=wsf.ap[:, h, :], pattern=[[1, 128]],
                                                           compare_op=ALU.is_ge, fill=0.0, base=0, channel_multiplier=-1),
                  R(wsf), R(wsf))
        cp("pool", wsT_b.ap, wsf.ap, [wsf], [wsT_b])
        bsT = A.alloc("bsT", [128, 4, 128], F32)
        dma("sp", bsT.ap, bsT_d, [], [bsT], "bsT")
        pmix0 = psv(3, [128, 4, 128])
        for h in range(8):
            po = (h % 2) * 64
            mm(pmix0[po:po + 64, h // 2, :], ones_b.ap[:, 0:64], wsT_b.ap[:, h, :], True, True, [ones_b, wsT_b], [PSB[3]])
        for q in range(4):
            stt(sgub.ap[:, q, :], pmix0[:, q, :], lnbT.ap[:, q:q + 1], bsT.ap[:, q, :], ALU.mult, ALU.add,
                [PSB[3], lnbT, bsT], [sgub])
        if "sgusetup" in dbg:
            dump("sgub", sgub)
            dump("wsT_b", wsT_b)
        A.reset(mark_sgu)

        if stop_after == "setup":
            S.finish(final_ops)
            S.emit(st)
            return nc, dbg_outs

        def wload(name, src_view, shape, key=None):
            t = A.alloc(name, shape, BF16)
            dma("pool", t.ap, src_view, [], [t], key or name)
            return t

        w_in_b = wload("w_in_b", w_in_d.rearrange("(kc p) n -> p kc n", p=128), [128, 8, 1536])
        w_gate_b = wload("w_gate_b", w_gate_d.rearrange("(kc p) n -> p kc n", p=128), [128, 8, 2048])
        w_glu_b = wload("w_glu_b", w_glu_d.rearrange("(kc p) n -> p kc n", p=128), [128, 4, 512])
        w_bra_b = wload("w_bra_b", w_bra_d.rearrange("(kc p) n -> p kc n", p=128), [128, 4, D])
        ts("pool", w_bra_b.ap, w_bra_b.ap, 0.5, 1.0, ALU.mult, ALU.mult, [w_bra_b], [w_bra_b])
        w_brb_b = wload("w_brb_b", w_brb_d.rearrange("(kc p) n -> p kc n", p=128), [128, 4, D])
        w_out_b = wload("w_out_b", w_out_d.rearrange("(kc p) n -> p kc n", p=128), [128, 8, D])
        def alias(name, shape, dt, of, off=0):
            t = Tl(None, name)
            n = 1
            for s_ in shape[1:]:
                n *= s_
            base_ = of.ap
            if len(base_.shape) == 3:
                base_ = base_.rearrange("p a b -> p (a b)")
            elif len(base_.shape) == 4:
                base_ = base_.rearrange("p a b c -> p (a b c)")
            if base_.dtype != dt:
                base_ = base_.bitcast(dt)
            v = base_[:, off:off + n]
            if len(shape) == 3:
                v = v.rearrange("p (a b) -> p a b", a=shape[1])
            t.ap = v
            t.r = of.r
            return t

        xt0 = A.alloc("xt0", [128, D], F32)
        xr = A.alloc("xr", [128, D], F32)
        xsb = A.alloc("xsb", [128, D], BF16)
        ssq = A.alloc("ssq", [128, 1], F32)
        rstd = A.alloc("rstd", [128, 1], F32)
        xnT = [A.alloc("xnT%d" % i, [128, 8, 128], BF16) for i in range(3)]
        uT = [A.alloc("uT%d" % i, [128, 4, 128], BF16) for i in range(2)]
        guT = A.alloc("guT", [128, 4, 128], F32)
        gv = A.alloc("gv", [128, 512], F32)
        vst = A.alloc("vst", [128, 6], F32)
        vmv = A.alloc("vmv", [128, 2], F32)
        vrs = A.alloc("vrs", [128, 1], F32)
        vhat = A.alloc("vhat", [128, 512], BF16)
        mixT = alias("mixT", [128, 4, 128], F32, gv)
        ybT = [A.alloc("ybT%d" % i, [128, 4, 128], BF16) for i in range(3)]
        xtil = A.alloc("xtil", [128, 4, 2, NCH], F32)
        s5a = A.alloc("s5a", [128, 4, NCH], F32)
        s5b = A.alloc("s5b", [128, 4, NCH], F32)
        gsc = A.alloc("gsc", [128, 4, 2, NCH], F32)
        gp = A.alloc("gp", [128, 16, 2], F32)
        carry = A.alloc("carry", [128, 16, 2], F32)
        sm1 = A.alloc("sm1", [128, 16, 2], F32)
        cf2 = A.alloc("cf2", [128, 4, 2], F32)
        c4 = [A.alloc("c4_%d" % i, [128, 16], F32) for i in range(4)]
        Sprev = [A.alloc("Sprev%d" % i, [128, 4, 2, NCH], BF16) for i in range(2)]
        ygT = A.alloc("ygT", [128, 4, 128], BF16)
        sg = A.alloc("sg", [128, 4, 128], F32)
        yaT = [A.alloc("yaT%d" % i, [128, 4, 128], BF16) for i in range(2)]
        gates = A.alloc("gates", [128, 16, 128], F32)
        xn2T = alias("xn2T", [128, 8, 128], F32, gates, off=1024)
        xn2 = alias("xn2", [128, D], F32, gates, off=0)
        mergedT = A.alloc("mergedT", [128, 8, 128], BF16)
        jk2 = alias("jk2", [128, D], BF16, mergedT)
        gt1b = A.alloc("gt1b", [128, D], F32)
        A2b = A.alloc("A2b", [128, D], F32)
        sh2b = A.alloc("sh2b", [128, D], F32)
        ssq2 = A.alloc("ssq2", [128, 1], F32)
        rstd2 = A.alloc("rstd2", [128, 1], F32)
        lg = A.alloc("lg", [128, 36], F32)
        rt = {n: A.alloc("rt_" + n, [128, w_], F32) for n, w_ in
              [("gmax", 1), ("ngmax", 1), ("maskg", 4), ("eg", 4), ("sume", 1), ("pgs", 1), ("pen", 4), ("lem", 32),
               ("m1", 1), ("oh1", 32), ("lem2", 32), ("m2", 1), ("oh2", 32), ("dm", 1), ("e2", 1), ("p1", 1), ("p2", 1),
               ("rank", 32), ("slotv", 32), ("junk", 32), ("sl", 2), ("val", 32), ("vk", 2)]}
        ohb = A.alloc("ohb", [128, 32], BF16)

        def rms_rstd(src, ssq_t, rstd_t, junk_ap, junk_t):
            act(junk_ap, src.ap, AF.Square, [src], [junk_t, ssq_t], accum=ssq_t.ap)
            ts("pool", ssq_t.ap, ssq_t.ap, 1.0 / D, RMS_EPS, ALU.mult, ALU.add, [ssq_t], [ssq_t])
            tt("pool", rstd_t.ap, ssq_t.ap, mhalf.ap, ALU.pow, [ssq_t, mhalf], [rstd_t])

        store_ops = []
        scat_ops = []
        c128 = tabC4.ap[:, :, NCH]
        d128 = tabD4.ap[:, :, NCH]

        def seqof(i):
            return i // NT

        def P1(i):
            b = seqof(i)
            X = xt0
            XN = xnT[i % 3]
            if i == 0:
                dma("sp", X.ap, x_d[0:128, :], [], [X], "xt0")
            rms_rstd(X, ssq, rstd, xsb.ap, xsb)
            act(xsb.ap, X.ap, AF.Copy, [X, rstd], [xsb], scale=rstd.ap[:, 0:1])
            if i + 1 < NTILES:
                dma("sp", X.ap, x_d[(i + 1) * 128:(i + 2) * 128, :], [], [X], "xt0")
            pX = psv(0, [128, 8, 128], BF16)
            for kc in range(8):
                tr(pX[:, kc, :], xsb.ap[:, kc * 128:(kc + 1) * 128], ident_b.ap, [xsb, ident_b], [PSB[0]])
            for kc in range(8):
                act(XN.ap[:, kc, :], pX[:, kc, :], AF.Identity, [PSB[0], A1T, sh1T], [XN],
                    bias=sh1T.ap[:, kc, b:b + 1], scale=A1T.ap[:, kc, b:b + 1])
            if dbg.get("tile") == i:
                dump("xnT", XN)

        def P2(i):
            XN = xnT[i % 3]
            UT = uT[i % 2]
            pZa = psv(1, [128, 4, 128])
            pZu = psv(0, [128, 4, 128])
            for m in range(4):
                for kc in range(8):
                    mm(pZa[:, m, :], w_in_b.ap[:, kc, m * 128:(m + 1) * 128], XN.ap[:, kc, :], kc == 0, kc == 7,
                       [w_in_b, XN], [PSB[1]])
            cp("act", UT.ap, pZa, [PSB[1]], [UT])
            for m in range(4):
                for kc in range(8):
                    mm(pZu[:, m, :], w_in_b.ap[:, kc, 512 + m * 128:512 + (m + 1) * 128], XN.ap[:, kc, :], kc == 0, kc == 7,
                       [w_in_b, XN], [PSB[0]])
            act(guT.ap, pZu, AF.Gelu_apprx_tanh, [PSB[0]], [guT])
            pV = psv(1, [128, 512])
            for kc in range(8):
                mm(pV, XN.ap[:, kc, :], w_in_b.ap[:, kc, 1024:1536], kc == 0, kc == 7, [w_in_b, XN], [PSB[1]])
            act(gv.ap, pV, AF.Gelu_apprx_tanh, [PSB[1]], [gv])
            if dbg.get("tile") == i:
                dump("uT", UT)

        def P3(i):
            YB = ybT[i % 3]
            S.add("dve", lambda e: e.bn_stats(out=vst.ap, in_=gv.ap), R(gv), R(vst))
            S.add("dve", lambda e: e.bn_aggr(out=vmv.ap, in_=vst.ap), R(vst), R(vmv))
            ts("pool", vrs.ap, vmv.ap[:, 1:2], 1.0, LN_EPS, ALU.mult, ALU.add, [vmv], [vrs])
            tt("pool", vrs.ap, vrs.ap, mhalf.ap, ALU.pow, [vrs, mhalf], [vrs])
            ts("dve", vhat.ap, gv.ap, vmv.ap[:, 0:1], vrs.ap[:, 0:1], ALU.subtract, ALU.mult, [gv, vmv, vrs], [vhat])
            pMix = psv(0, [128, 4, 128])
            for h in range(8):
                po = (h % 2) * 64
                mm(pMix[po:po + 64, h // 2, :], vhat.ap[:, h * 64:(h + 1) * 64], wsT_b.ap[:, h, :], True, True,
                   [vhat, wsT_b], [PSB[0]])
            for q in range(4):
                stt(mixT.ap[:, q, :], pMix[:, q, :], lngT.ap[:, q:q + 1], sgub.ap[:, q, :], ALU.mult, ALU.add,
                    [PSB[0], lngT, sgub], [mixT])
            tt("pool", YB.ap, guT.ap, mixT.ap, ALU.mult, [guT, mixT], [YB])
            if dbg.get("tile") == i:
                dump("ybT", YB)

        def pS5(q):
            o = (q % 2) * 4 * NCH
            return ps_all[:, 2:4, o:o + 4 * NCH].rearrange("p k (a b c) -> p k a b c", a=2, b=2)

        def Qpre(i):
            if i % NT == 0:
                memset("dve", carry.ap, 0.0, [carry])
            cT_, dT_ = tabC4.ap[:, :, 1], tabD4.ap[:, :, 1]
            tt("dve", c4[0].ap, carry.ap[:, :, 0], cT_, ALU.mult, [carry, tabC4], [c4[0]])
            tt("dve", c4[1].ap, carry.ap[:, :, 1], dT_, ALU.mult, [carry, tabD4], [c4[1]])
            tt("dve", c4[2].ap, carry.ap[:, :, 1], cT_, ALU.mult, [carry, tabC4], [c4[2]])
            tt("dve", c4[3].ap, carry.ap[:, :, 0], dT_, ALU.mult, [carry, tabD4], [c4[3]])
            tt("dve", sm1.ap[:, :, 0], c4[0].ap, c4[1].ap, ALU.add, [c4[0], c4[1]], [sm1])
            tt("dve", sm1.ap[:, :, 1], c4[2].ap, c4[3].ap, ALU.subtract, [c4[2], c4[3]], [sm1])

        def QB(i, q):
            UT = uT[i % 2]
            pS = pS5(q)
            for jj in range(4):
                ro = 64 * (jj // 2)
                for part in range(2):
                    for sx in range(LC):
                        mm(pS[:, jj // 2, jj % 2, part, :], Pt.ap[ro:ro + 64, part, (2 * q + jj % 2) * LC + sx, :],
                           UT.ap[ro:ro + 64, q, sx::LC], sx == 0, sx == LC - 1, [Pt, UT], [PSB[2 + jj // 2]])

        def QD(i, q):
            pS = pS5(q)
            SP = Sprev[q % 2]
            tc_ = tabC4.ap[:, 4 * q:4 * q + 4, 0:NCH]
            td_ = tabD4.ap[:, 4 * q:4 * q + 4, 0:NCH]
            tc4 = tc_.rearrange("p (k a) c -> p k a c", k=2)
            td4 = td_.rearrange("p (k a) c -> p k a c", k=2)
            a4 = s5a.ap.rearrange("p (k a) c -> p k a c", k=2)
            b4 = s5b.ap.rearrange("p (k a) c -> p k a c", k=2)
            PB = [PSB[2], PSB[3]]
            tt("dve", a4, pS[:, :, :, 0, :], tc4, ALU.mult, PB + [tabC4], [s5a])
            tt("dve", b4, pS[:, :, :, 1, :], td4, ALU.mult, PB + [tabD4], [s5b])
            tt("dve", xtil.ap[:, :, 0, :], s5a.ap, s5b.ap, ALU.add, [s5a, s5b], [xtil])
            tt("dve", a4, pS[:, :, :, 1, :], tc4, ALU.mult, PB + [tabC4, xtil], [s5a])
            tt("dve", b4, pS[:, :, :, 0, :], td4, ALU.mult, PB + [tabD4, xtil], [s5b])
            tt("dve", xtil.ap[:, :, 1, :], s5a.ap, s5b.ap, ALU.subtract, [s5a, s5b], [xtil])
            tt("dve", cf2.ap, carry.ap[:, 4 * q:4 * q + 4, :], rmagL.ap[:, 4 * q:4 * q + 4].unsqueeze(2).to_broadcast([128, 4, 2]),
               ALU.mult, [carry, rmagL], [cf2])
            tt("dve", xtil.ap[:, :, :, 0], xtil.ap[:, :, :, 0], cf2.ap, ALU.add, [xtil, cf2], [xtil])
            S.add("dve", lambda e: e.tensor_tensor_scan(
                out=gsc.ap.rearrange("p a b c -> p (a b c)"),
                data0=Rtab.ap[:, 4 * q:4 * q + 4, :, :].rearrange("p a b c -> p (a b c)"),
                data1=xtil.ap.rearrange("p a b c -> p (a b c)"), initial=0.0, op0=ALU.mult, op1=ALU.add),
                R(Rtab, xtil), R(gsc))
            cp("dve", gp.ap[:, 4 * q:4 * q + 4, :], gsc.ap[:, :, :, NCH - 1], [gsc], [gp])
            n1 = NCH - 1
            tt("dve", s5a.ap[:, :, 0:n1], gsc.ap[:, :, 0, 0:n1], tc_[:, :, 0:n1], ALU.mult, [gsc, tabC4], [s5a])
            tt("dve", s5b.ap[:, :, 0:n1], gsc.ap[:, :, 1, 0:n1], td_[:, :, 0:n1], ALU.mult, [gsc, tabD4], [s5b])
            tt("dve", SP.ap[:, :, 0, 1:NCH], s5a.ap[:, :, 0:n1], s5b.ap[:, :, 0:n1], ALU.subtract, [s5a, s5b], [SP])
            tt("dve", s5a.ap[:, :, 0:n1], gsc.ap[:, :, 1, 0:n1], tc_[:, :, 0:n1], ALU.mult, [gsc, tabC4, SP], [s5a])
            tt("dve", s5b.ap[:, :, 0:n1], gsc.ap[:, :, 0, 0:n1], td_[:, :, 0:n1], ALU.mult, [gsc, tabD4, SP], [s5b])
            tt("dve", SP.ap[:, :, 1, 1:NCH], s5a.ap[:, :, 0:n1], s5b.ap[:, :, 0:n1], ALU.add, [s5a, s5b], [SP])
            cp("dve", SP.ap[:, :, :, 0], sm1.ap[:, 4 * q:4 * q + 4, :], [sm1], [SP])
            if dbg.get("tile") == i and q == 0:
                dump("gsc0", gsc)

        def QC(i, q):
            UT = uT[i % 2]
            SP = Sprev[q % 2]
            pY = psv(4, [128, 4, LC, NCH])
            for sp_ in range(LC):
                first = True
                for sx in range(sp_ + 1):
                    mm(pY[:, q, sp_, :], Kb.ap[:, q * LC + (sp_ - sx), :], UT.ap[:, q, sx::LC], first, False, [Kb, UT], [PSB[4]])
                    first = False
                for jj in range(4):
                    j = 4 * q + jj
                    ro = 64 * (jj // 2)
                    for part in range(2):
                        mm(pY[ro:ro + 64, q, sp_, :], Qs.ap[:, part, j * LC + sp_, :], SP.ap[:, jj, part, :],
                           False, jj == 3 and part == 1, [Qs, SP], [PSB[4]])

        def Qcarry(i):
            tt("dve", c4[0].ap, gp.ap[:, :, 0], c128, ALU.mult, [gp, tabC4], [c4[0]])
            tt("dve", c4[1].ap, gp.ap[:, :, 1], d128, ALU.mult, [gp, tabD4], [c4[1]])
            tt("dve", c4[2].ap, gp.ap[:, :, 1], c128, ALU.mult, [gp, tabC4], [c4[2]])
            tt("dve", c4[3].ap, gp.ap[:, :, 0], d128, ALU.mult, [gp, tabD4], [c4[3]])
            tt("dve", carry.ap[:, :, 0], c4[0].ap, c4[1].ap, ALU.subtract, [c4[0], c4[1]], [carry])
            tt("dve", carry.ap[:, :, 1], c4[2].ap, c4[3].ap, ALU.add, [c4[2], c4[3]], [carry])

        def Qtail(i):
            YA = yaT[i % 2]
            pY = psv(4, [128, 4, LC, NCH])
            for q in range(4):
                act(ygT.ap[:, q, :].rearrange("p (c s) -> p s c", s=LC), pY[:, q, :, :], AF.Gelu_apprx_tanh, [PSB[4]], [ygT])
            if dbg.get("tile") == i:
                dump("ypre", ygT)
            pG = psv(4, [128, 4, 128])
            for m in range(4):
                for kc in range(4):
                    mm(pG[:, m, :], w_glu_b.ap[:, kc, m * 128:(m + 1) * 128], ygT.ap[:, kc, :], kc == 0, kc == 3,
                       [w_glu_b, ygT], [PSB[4]])
            for m in range(4):
                act(sg.ap[:, m, :], pG[:, m, :], AF.Tanh, [PSB[4], b_gluT], [sg], bias=b_gluT.ap[:, m:m + 1], scale=0.5)
            stt(YA.ap, sg.ap, 1.0, ygT.ap, ALU.add, ALU.mult, [sg, ygT], [YA])
            if dbg.get("tile") == i:
                dump("yaT", YA)

        def R0(i):
            b = seqof(i)
            if i % NT == 0:
                dma("sp", gt1b.ap, mod_d.ap[b:b + 1, 2 * D:3 * D].partition_broadcast(128), [mod_d], [gt1b], "gt1b")
                ts("dve", gt1b.ap, gt1b.ap, 0.5, None, ALU.mult, None, [gt1b], [gt1b])
                dma("sp", sh2b.ap, mod_d.ap[b:b + 1, 3 * D:4 * D].partition_broadcast(128), [mod_d], [sh2b], "sh2b")
                dma("sp", A2b.ap, mod_d.ap[b:b + 1, 4 * D:5 * D].partition_broadcast(128), [mod_d], [A2b], "A2b")
                dma("sp", xn2.ap, g2_d.partition_broadcast(128), [], [xn2], "g2tmp")
                stt(A2b.ap, A2b.ap, 1.0, xn2.ap, ALU.add, ALU.mult, [A2b, xn2], [A2b])
            dma("sp", xr.ap, x_d[i * 128:(i + 1) * 128, :], [], [xr], "xr")

        def R1(i, mg):
            XN = xnT[i % 3]
            bk = 5 if mg % 2 == 0 else 7
            pGt = psv(bk, [128, 4, 128])
            for mm_ in range(4):
                m = mg * 4 + mm_
                for kc in range(8):
                    mm(pGt[:, mm_, :], w_gate_b.ap[:, kc, m * 128:(m + 1) * 128], XN.ap[:, kc, :], kc == 0, kc == 7,
                       [w_gate_b, XN], [PSB[bk]])
            for mm_ in range(4):
                m = mg * 4 + mm_
                act(gates.ap[:, m, :], pGt[:, mm_, :], AF.Tanh, [PSB[bk], b_gateT], [gates],
                    bias=b_gateT.ap[:, m:m + 1], scale=0.5)

        def R2(i, half):
            YA, YB = yaT[i % 2], ybT[i % 3]
            pA = psv(5, [128, 4, 128])
            pB = psv(6, [128, 4, 128])
            for mm_ in range(4):
                m = half * 4 + mm_
                for kc in range(4):
                    mm(pA[:, mm_, :], w_bra_b.ap[:, kc, m * 128:(m + 1) * 128], YA.ap[:, kc, :], kc == 0, kc == 3,
                       [w_bra_b, YA], [PSB[5]])
            for mm_ in range(4):
                m = half * 4 + mm_
                for kc in range(4):
                    mm(pB[:, mm_, :], w_brb_b.ap[:, kc, m * 128:(m + 1) * 128], YB.ap[:, kc, :], kc == 0, kc == 3,
                       [w_brb_b, YB], [PSB[6]])
            ga = gates.ap[:, half * 4:half * 4 + 4, :]
            gb = gates.ap[:, 8 + half * 4:8 + half * 4 + 4, :]
            stt(ga, ga, 1.0, pA, ALU.add, ALU.mult, [gates, PSB[5]], [gates])
            stt(gb, gb, 1.0, pB, ALU.add, ALU.mult, [gates, PSB[6]], [gates])
            tt("dve", mergedT.ap[:, half * 4:half * 4 + 4, :], ga, gb, ALU.add, [gates], [mergedT])
            if dbg.get("tile") == i and half == 1:
                dump("mergedT", mergedT)

        def R3(i):
            for half in range(2):
                bk = 6 + half
                for kc in range(8):
                    mm(PSB[bk].ap, mergedT.ap[:, kc, :], w_out_b.ap[:, kc, half * 512:(half + 1) * 512], kc == 0, kc == 7,
                       [mergedT, w_out_b], [PSB[bk]])
                sl = slice(half * 512, (half + 1) * 512)
                tt("dve", xn2.ap[:, sl], PSB[bk].ap, gt1b.ap[:, sl], ALU.mult, [PSB[bk], gt1b], [xn2])
            tt("dve", xr.ap, xr.ap, xn2.ap, ALU.add, [xr, xn2], [xr])
            store_ops.append(dma("sp", H_d[i * 128:(i + 1) * 128, :], xr.ap, [xr], [r_H], "hst"))
            rms_rstd(xr, ssq2, rstd2, jk2.ap, jk2)
            stt(xn2.ap, xr.ap, rstd2.ap[:, 0:1], A2b.ap, ALU.mult, ALU.mult, [xr, rstd2, A2b], [xn2])
            tt("dve", xn2.ap, xn2.ap, sh2b.ap, ALU.add, [xn2, sh2b], [xn2])
            if dbg.get("tile") == i:
                dump("h", xr)
                dump("xn2", xn2)

        def R4(i):
            pX2 = psv2(6, [128, 8, 128])
            for kc in range(8):
                tr(pX2[:, kc, :], xn2.ap[:, kc * 128:(kc + 1) * 128], ident_f.ap, [xn2, ident_f], [PSB[6], PSB[7]])
            cp("act", xn2T.ap, pX2, [PSB[6], PSB[7]], [xn2T])
            pL = psv(5, [128, 36])
            for kc in range(8):
                mm(pL, xn2T.ap[:, kc, :], w_r.ap[:, kc, :], kc == 0, kc == 7, [xn2T, w_r], [PSB[5]])
            tt("dve", lg.ap, pL, b_r.ap, ALU.add, [PSB[5], b_r], [lg])
            r_ = rt
            BIG = 1.0e9
            S.add("dve", lambda e: e.tensor_reduce(out=r_["gmax"].ap, in_=lg.ap[:, 0:4], axis=AX.X, op=ALU.max), R(lg), R(r_["gmax"]))
            ts("dve", r_["maskg"].ap, lg.ap[:, 0:4], r_["gmax"].ap[:, 0:1], None, ALU.is_equal, None, [lg, r_["gmax"]], [r_["maskg"]])
            ts("dve", r_["ngmax"].ap, r_["gmax"].ap, -0.5, None, ALU.mult, None, [r_["gmax"]], [r_["ngmax"]])
            act(r_["eg"].ap, lg.ap[:, 0:4], AF.Tanh, [lg, r_["ngmax"]], [r_["eg"]], bias=r_["ngmax"].ap[:, 0:1], scale=0.5)
            ts("dve", r_["pen"].ap, r_["eg"].ap, -1.0, 1.0, ALU.mult, ALU.add, [r_["eg"]], [r_["pen"]])
            S.add("dve", lambda e: e.reciprocal(out=r_["pen"].ap, in_=r_["pen"].ap), R(r_["pen"]), R(r_["pen"]))
            stt(r_["eg"].ap, r_["eg"].ap, 1.0, r_["pen"].ap, ALU.add, ALU.mult, [r_["eg"], r_["pen"]], [r_["eg"]])
            S.add("dve", lambda e: e.tensor_reduce(out=r_["sume"].ap, in_=r_["eg"].ap, axis=AX.X, op=ALU.add), R(r_["eg"]), R(r_["sume"]))
            S.add("dve", lambda e: e.reciprocal(out=r_["pgs"].ap, in_=r_["sume"].ap), R(r_["sume"]), R(r_["pgs"]))
            ts("dve", r_["pen"].ap, r_["maskg"].ap, BIG, -BIG, ALU.mult, ALU.add, [r_["maskg"]], [r_["pen"]])
            for g in range(4):
                ts("dve", r_["lem"].ap[:, g * 8:(g + 1) * 8], lg.ap[:, 4 + g * 8:4 + (g + 1) * 8], r_["pen"].ap[:, g:g + 1], None,
                   ALU.add, None, [lg, r_["pen"]], [r_["lem"]])
            S.add("dve", lambda e: e.tensor_reduce(out=r_["m1"].ap, in_=r_["lem"].ap, axis=AX.X, op=ALU.max), R(r_["lem"]), R(r_["m1"]))
            ts("dve", r_["oh1"].ap, r_["lem"].ap, r_["m1"].ap[:, 0:1], None, ALU.is_equal, None, [r_["lem"], r_["m1"]], [r_["oh1"]])
            stt(r_["lem2"].ap, r_["oh1"].ap, -BIG, r_["lem"].ap, ALU.mult, ALU.add, [r_["oh1"], r_["lem"]], [r_["lem2"]])
            S.add("dve", lambda e: e.tensor_reduce(out=r_["m2"].ap, in_=r_["lem2"].ap, axis=AX.X, op=ALU.max), R(r_["lem2"]), R(r_["m2"]))
            ts("dve", r_["oh2"].ap, r_["lem2"].ap, r_["m2"].ap[:, 0:1], None, ALU.is_equal, None, [r_["lem2"], r_["m2"]], [r_["oh2"]])
            tt("dve", r_["dm"].ap, r_["m1"].ap, r_["m2"].ap, ALU.subtract, [r_["m1"], r_["m2"]], [r_["dm"]])
            act(r_["e2"].ap, r_["dm"].ap, AF.Tanh, [r_["dm"]], [r_["e2"]], scale=0.5)
            ts("dve", r_["p1"].ap, r_["e2"].ap, 0.5, 0.5, ALU.mult, ALU.add, [r_["e2"]], [r_["p1"]])
            ts("dve", r_["p2"].ap, r_["e2"].ap, -0.5, 0.5, ALU.mult, ALU.add, [r_["e2"]], [r_["p2"]])
            tt("dve", ohb.ap, r_["oh1"].ap, r_["oh2"].ap, ALU.add, [r_["oh1"], r_["oh2"]], [ohb])
            pR = psv(5, [128, 128])
            mm(pR[:, 64:96], tri_b.ap, ohb.ap, True, True, [tri_b, ohb], [PSB[5]])
            mm(pR[:, 96:128], ones_b.ap, ohb.ap, True, True, [ones_b, ohb], [PSB[5]])
            tt("dve", r_["rank"].ap, pR[:, 64:96], base.ap, ALU.add, [PSB[5], base], [r_["rank"]])
            tt("dve", base.ap, pR[:, 96:128], base.ap, ALU.add, [PSB[5], base, r_["rank"]], [base])
            ts("dve", r_["val"].ap, r_["rank"].ap, float(CAP), None, ALU.is_lt, None, [r_["rank"]], [r_["val"]])
            tt("dve", r_["slotv"].ap, r_["rank"].ap, ecap.ap, ALU.add, [r_["rank"], ecap], [r_["slotv"]])
            stt(r_["slotv"].ap, r_["val"].ap, -4.0e6, r_["slotv"].ap, ALU.mult, ALU.add, [r_["val"], r_["slotv"]], [r_["slotv"]])
            ts("dve", r_["slotv"].ap, r_["slotv"].ap, 4.0e6, None, ALU.add, None, [r_["slotv"]], [r_["slotv"]])
            for k, ohn in enumerate(("oh1", "oh2")):
                tt("dve", r_["junk"].ap, r_[ohn].ap, r_["slotv"].ap, ALU.mult, [r_[ohn], r_["slotv"]], [r_["junk"]])
                S.add("dve", lambda e, k=k: e.tensor_reduce(out=r_["sl"].ap[:, k:k + 1], in_=r_["junk"].ap, axis=AX.X, op=ALU.add),
                      R(r_["junk"]), R(r_["sl"]))
                tt("dve", r_["junk"].ap, r_[ohn].ap, r_["val"].ap, ALU.mult, [r_[ohn], r_["val"]], [r_["junk"]])
                S.add("dve", lambda e, k=k: e.tensor_reduce(out=r_["vk"].ap[:, k:k + 1], in_=r_["junk"].ap, axis=AX.X, op=ALU.add),
                      R(r_["junk"]), R(r_["vk"]))
            cp("dve", sloti.ap[:, i, :], r_["sl"].ap, [r_["sl"]], [sloti])
            stt(wgt.ap[:, i, 0:1], r_["p1"].ap, r_["pgs"].ap[:, 0:1], r_["vk"].ap[:, 0:1], ALU.mult, ALU.mult,
                [r_["p1"], r_["pgs"], r_["vk"]], [wgt])
            stt(wgt.ap[:, i, 1:2], r_["p2"].ap, r_["pgs"].ap[:, 0:1], r_["vk"].ap[:, 1:2], ALU.mult, ALU.mult,
                [r_["p2"], r_["pgs"], r_["vk"]], [wgt])
            if dbg.get("tile") == i:
                dump("lg", lg)
                dump("rank", r_["rank"])
            for k in range(2):
                scat_ops.append(S.add("pool", lambda e, i=i, k=k: e.indirect_dma_start(
                    out=X_d, out_offset=bass.IndirectOffsetOnAxis(ap=sloti.ap[:, i, k:k + 1], axis=0),
                    in_=xn2.ap, in_offset=None, bounds_check=breg(e, NSLOT - 1), oob_is_err=False),
                    R(xn2, sloti), [r_X], dma_key="scat"))

        import os as _os
        _skip = set(_os.environ.get('QSKIP', '').split(','))
        _w = lambda f, n: (lambda *a: None) if n in _skip else f
        Qpre, QB, QD, QC, Qcarry, Qtail = _w(Qpre, 'pre'), _w(QB, 'B'), _w(QD, 'D'), _w(QC, 'C'), _w(Qcarry, 'carry'), _w(Qtail, 'tail')
        for s_ in range(NTILES + 2):
            ip, iq, ir = s_, s_ - 1, s_ - 2
            hp = 0 <= ip < NTILES
            hq = 0 <= iq < NTILES
            hr = 0 <= ir < NTILES
            if hr:
                R0(ir)
            if hq:
                Qpre(iq)
                QB(iq, 0)
            if hr:
                R1(ir, 0)
                R1(ir, 1)
            if hp:
                P1(ip)
            if hq:
                QD(iq, 0)
            if hr:
                R1(ir, 2)
                R1(ir, 3)
                R2(ir, 0)
                R2(ir, 1)
            if hq:
                QB(iq, 1)
                QC(iq, 0)
            if hp:
                P2(ip)
            if hq:
                QD(iq, 1)
            if hr:
                R3(ir)
            if hq:
                QB(iq, 2)
                QC(iq, 1)
                QD(iq, 2)
            if hr:
                R4(ir)
            if hq:
                QB(iq, 3)
                QC(iq, 2)
            if hp:
                P3(ip)
            if hq:
                QD(iq, 3)
                QC(iq, 3)
                Qcarry(iq)
                Qtail(iq)
        if "route" in dbg:
            dump("wgt", wgt)
            dump("sloti", sloti)
        if stop_after == "A":
            S.finish(final_ops + store_ops + scat_ops)
            S.emit(st)
            return nc, dbg_outs

        S.barrier(dma_ops=[scat_ops[-1], store_ops[-1]])
        A.reset(mark_persist)
        w1b = [A.alloc("w1b%d" % i, [128, 8, 512], BF16) for i in range(2)]
        w3b = [A.alloc("w3b%d" % i, [128, 8, 512], BF16) for i in range(2)]
        w2b = [A.alloc("w2b%d" % i, [128, 4, D], BF16) for i in range(2)]
        Xblk = [A.alloc("Xblk%d" % i, [128, D], BF16) for i in range(3)]
        XT = [A.alloc("XT%d" % i, [128, 8, CAP], BF16) for i in range(2)]
        hidT = [A.alloc("hidT%d" % i, [128, 4, CAP], BF16) for i in range(2)]
        s1 = [A.alloc("s1_%d" % i, [128, 512], F32) for i in range(2)]
        Yblk = [A.alloc("Yblk%d" % i, [128, D], F32) for i in range(3)]
        NH = (CAP + 511) // 512
        HW_ = CAP // NH
        ystore = []
        cnt = {"x": 0, "y": 0, "h": 0}

        def W13(e_):
            sl_ = e_ % 2
            dma("pool", w1b[sl_].ap, w1_d[e_].rearrange("(kc p) n -> p kc n", p=128), [], [w1b[sl_]], "w1b%d" % sl_)
            dma("pool", w3b[sl_].ap, w3_d[e_].rearrange("(kc p) n -> p kc n", p=128), [], [w3b[sl_]], "w3b%d" % sl_)

        def W2(e_):
            sl_ = e_ % 2
            dma("pool", w2b[sl_].ap, w2_d[e_].rearrange("(kc p) n -> p kc n", p=128), [], [w2b[sl_]], "w2b%d" % sl_)

        def TX(e_):
            xt_ = XT[e_ % 2]
            for blk in range(NBLK):
                n_ = cnt["x"]
                cnt["x"] += 1
                xb_ = Xblk[n_ % 3]
                r0 = e_ * CAP + blk * 128
                dma("sp", xb_.ap, X_d[r0:r0 + 128, :], [r_X], [xb_], "xblk%d" % (n_ % 3))
                bk = n_ % 2
                pXT = psv(bk, [128, 8, 128], BF16)
                for kc in range(8):
                    tr(pXT[:, kc, :], xb_.ap[:, kc * 128:(kc + 1) * 128], ident_b.ap, [xb_, ident_b], [PSB[bk]])
                cp("act" if blk % 2 == 0 else "dve", xt_.ap[:, :, blk * 128:(blk + 1) * 128], pXT, [PSB[bk]], [xt_])

        def HH(e_):
            sl_ = e_ % 2
            xt_, hd_ = XT[e_ % 2], hidT[e_ % 2]
            for m in range(4):
                for nh in range(NH):
                    cs_ = slice(nh * HW_, (nh + 1) * HW_)
                    n_ = cnt["h"]
                    cnt["h"] += 1
                    b1 = 2 + n_ % 2
                    b3 = 4 + n_ % 2
                    p1_ = PSB[b1].ap[:, 0:HW_]
                    p3_ = PSB[b3].ap[:, 0:HW_]
                    for kc in range(8):
                        mm(p1_, w1b[sl_].ap[:, kc, m * 128:(m + 1) * 128], xt_.ap[:, kc, cs_], kc == 0, kc == 7,
                           [w1b[sl_], xt_], [PSB[b1]])
                    for kc in range(8):
                        mm(p3_, w3b[sl_].ap[:, kc, m * 128:(m + 1) * 128], xt_.ap[:, kc, cs_], kc == 0, kc == 7,
                           [w3b[sl_], xt_], [PSB[b3]])
                    s1_ = s1[n_ % 2]
                    act(s1_.ap[:, 0:HW_], p1_, AF.Silu, [PSB[b1]], [s1_])
                    tt("dve", hd_.ap[:, m, cs_], s1_.ap[:, 0:HW_], p3_, ALU.mult, [s1_, PSB[b3]], [hd_])

        def YY(e_):
            sl_ = e_ % 2
            hd_ = hidT[e_ % 2]
            for blk in range(NBLK):
                n_ = cnt["y"]
                cnt["y"] += 1
                yb_ = Yblk[n_ % 3]
                for half in range(2):
                    bk = 6 + half
                    for kc in range(4):
                        mm(PSB[bk].ap, hd_.ap[:, kc, blk * 128:(blk + 1) * 128], w2b[sl_].ap[:, kc, half * 512:(half + 1) * 512],
                           kc == 0, kc == 3, [hd_, w2b[sl_]], [PSB[bk]])
                    cp("act" if half == 0 else "dve", yb_.ap[:, half * 512:(half + 1) * 512], PSB[bk].ap, [PSB[bk]], [yb_])
                r0 = e_ * CAP + blk * 128
                ystore.append(dma("sp", Y_d[r0:r0 + 128, :], yb_.ap, [yb_], [r_Y], "yst%d" % (n_ % 3)))

        W13(0)
        W2(0)
        W13(1)
        W2(1)
        TX(0)
        for e_ in range(32):
            HH(e_)
            if e_ + 2 < 32:
                W13(e_ + 2)
            if e_ + 1 < 32:
                TX(e_ + 1)
            YY(e_)
            if e_ + 2 < 32:
                W2(e_ + 2)
        if stop_after == "B":
            S.finish(final_ops + ystore[-3:])
            S.emit(st)
            return nc, dbg_outs

        S.barrier(dma_ops=ystore[-3:])
        A.reset(mark_persist)
        NBC = 3
        Hc = [A.alloc("Hc%d" % i, [128, D], F32) for i in range(NBC)]
        Y0 = [A.alloc("Y0_%d" % i, [128, D], F32) for i in range(NBC)]
        Y1 = [A.alloc("Y1_%d" % i, [128, D], F32) for i in range(NBC)]
        acc = A.alloc("acc", [128, D], F32)
        ob = [A.alloc("ob%d" % i, [128, D], F32) for i in range(2)]
        gt2b = A.alloc("gt2b", [128, D], F32)
        gfb = A.alloc("gfb", [128, D], F32)
        jk = A.alloc("jk", [128, D], BF16)
        ssq3 = A.alloc("ssq3", [128, 1], F32)
        rstd3 = A.alloc("rstd3", [128, 1], F32)
        dma("sp", gfb.ap, gf_d.partition_broadcast(128), [], [gfb], "gfb")
        for s_ in range(NBC):
            memset("pool", Y0[s_].ap, 0.0, [Y0[s_]])
            memset("pool", Y1[s_].ap, 0.0, [Y1[s_]])

        def Cload(i):
            s_ = i % NBC
            dma("sp", Hc[s_].ap, H_d[i * 128:(i + 1) * 128, :], [r_H], [Hc[s_]], "hc%d" % s_)
            for k, Yk in enumerate((Y0[s_], Y1[s_])):
                S.add("pool", lambda e, i=i, k=k, Yk=Yk: e.indirect_dma_start(
                    out=Yk.ap, out_offset=None, in_=Y_d, in_offset=bass.IndirectOffsetOnAxis(ap=sloti.ap[:, i, k:k + 1], axis=0),
                    bounds_check=breg(e, NSLOT - 1), oob_is_err=False), R(r_Y, sloti), R(Yk), dma_key="gath%d_%d" % (k, s_))

        Cload(0)
        if NTILES > 1:
            Cload(1)
        for i in range(NTILES):
            b, tau = i // NT, i % NT
            s_ = i % NBC
            if tau == 0:
                dma("sp", gt2b.ap, mod_d.ap[b:b + 1, 5 * D:6 * D].partition_broadcast(128), [mod_d], [gt2b], "gt2b")
            if i + 2 < NTILES:
                Cload(i + 2)
            ts("dve", acc.ap, Y0[s_].ap, wgt.ap[:, i, 0:1], None, ALU.mult, None, [Y0[s_], wgt], [acc])
            stt(acc.ap, Y1[s_].ap, wgt.ap[:, i, 1:2], acc.ap, ALU.mult, ALU.add, [Y1[s_], wgt, acc], [acc])
            tt("dve", acc.ap, acc.ap, gt2b.ap, ALU.mult, [acc, gt2b], [acc])
            tt("dve", acc.ap, acc.ap, Hc[s_].ap, ALU.add, [acc, Hc[s_]], [acc])
            rms_rstd(acc, ssq3, rstd3, jk.ap, jk)
            stt(ob[i % 2].ap, acc.ap, rstd3.ap[:, 0:1], gfb.ap, ALU.mult, ALU.mult, [acc, rstd3, gfb], [ob[i % 2]])
            final_ops.append(dma("sp", out_d[i * 128:(i + 1) * 128, :], ob[i % 2].ap, [ob[i % 2]], [], "ost%d" % (i % 2)))
        S.finish(final_ops)
        S.emit(st)
    return nc, dbg_outs


def prep_shared(inp):
    f = np.float32
    g = {}
    L = 0
    g["w_ada"] = np.ascontiguousarray(inp["w_ada"][L], f)
    g["g1T"] = np.ascontiguousarray(inp["norm1_g"][L].reshape(8, 128).T, f)
    g["w_in"] = np.ascontiguousarray(inp["w_in"][L], f)
    g["w_gate"] = np.ascontiguousarray(inp["w_gate"][L], f)
    g["b_gateT"] = np.ascontiguousarray(inp["b_gate"][L].reshape(16, 128).T, f)
    a_re, a_im, ls = inp["ssm_a_re"][L], inp["ssm_a_im"][L], inp["ssm_log_step"][L]
    def sm(v):
        return v.reshape(16, 2, 64).transpose(1, 2, 0).reshape(128, 16)
    lsx = np.repeat(ls[:, None], 64, axis=1)
    g["lam_sm"] = np.ascontiguousarray(np.stack([sm(a_re), sm(a_im), sm(lsx)], axis=1), f)
    Bsm = np.zeros((128, 2, 16, 128), f)
    for pi, Bsrc in enumerate((inp["ssm_b_re"][L], inp["ssm_b_im"][L])):
        for gi in range(32):
            j = gi // 2
            n0 = 64 * (gi % 2)
            c0 = 32 * (j % 4) + 16 * (gi % 2)
            Bsm[n0:n0 + 64, pi, j, c0:c0 + 16] = Bsrc[gi]
    g["Bsm"] = Bsm
    Csm = np.zeros((128, 2, 16, 128), f)
    for pi, Csrc in enumerate((inp["ssm_c_re"][L], inp["ssm_c_im"][L])):
        for gi in range(32):
            j = gi // 2
            n0 = 64 * (gi % 2)
            c0 = 32 * (j % 4) + 16 * (gi % 2)
            Csm[n0:n0 + 64, pi, j, c0:c0 + 16] = Csrc[gi].T
    g["Csm"] = Csm
    g["dT"] = np.ascontiguousarray(inp["ssm_d"][L].reshape(4, 128).T, f)
    g["w_glu"] = np.ascontiguousarray(inp["w_glu"][L], f)
    g["b_gluT"] = np.ascontiguousarray(inp["b_glu"][L].reshape(4, 128).T, f)
    g["wsT"] = np.ascontiguousarray(inp["sgu_w"][L].transpose(2, 0, 1), f)
    g["lngT"] = np.ascontiguousarray(inp["sgu_ln_g"][L].reshape(4, 128).T, f)
    g["lnbT"] = np.ascontiguousarray(inp["sgu_ln_b"][L].reshape(4, 128).T, f)
    bs = inp["sgu_b"][L]
    bsT = np.zeros((128, 4, 128), f)
    for q in range(4):
        bsT[0:64, q, :] = bs[2 * q][None, :]
        bsT[64:128, q, :] = bs[2 * q + 1][None, :]
    g["bsT"] = bsT
    g["w_bra"] = np.ascontiguousarray(inp["w_branch_a"][L], f)
    g["w_brb"] = np.ascontiguousarray(inp["w_branch_b"][L], f)
    g["w_out"] = np.ascontiguousarray(inp["w_out"][L], f)
    g["g2"] = np.ascontiguousarray(inp["norm2_g"][L].reshape(1, D), f)
    wr = np.concatenate([inp["w_router_group"][L], inp["w_router_expert"][L].transpose(1, 0, 2).reshape(D, 32)], axis=1)
    g["w_r"] = np.ascontiguousarray(wr.reshape(8, 128, 36).transpose(1, 0, 2), f)
    g["b_r"] = np.ascontiguousarray(np.concatenate([inp["b_router_group"][L], inp["b_router_expert"][L].reshape(32)]).reshape(1, 36), f)
    g["w1"] = np.ascontiguousarray(inp["w1"][L], f)
    g["w3"] = np.ascontiguousarray(inp["w3"][L], f)
    g["w2"] = np.ascontiguousarray(inp["w2"][L], f)
    g["gf"] = np.ascontiguousarray(inp["norm_f_g"].reshape(1, D), f)
    return g


def prep_core(inp, shared, b0, nseq):
    m = dict(shared)
    xs = inp["x"][b0:b0 + nseq]
    m["x"] = np.ascontiguousarray(xs.reshape(-1, D), np.float32)
    c = inp["c"][b0:b0 + nseq]
    m["cT"] = np.ascontiguousarray(c.reshape(nseq, 8, 128).transpose(2, 1, 0), np.float32)
    m["b_ada_rep"] = np.ascontiguousarray(np.repeat(inp["b_ada"][0][None, :], nseq, axis=0), np.float32)
    return m


_CACHE = {}


def kernel(**inputs):
    inp = {k: np.asarray(v) for k, v in inputs.items()}
    B, SEQ = inp["x"].shape[0], inp["x"].shape[1]
    nseq = B // N_CORES
    key = (nseq, SEQ)
    if key not in _CACHE:
        _CACHE[key] = build(NSEQ=nseq, SEQ=SEQ, CAP=768)[0]
    nc = _CACHE[key]
    shared = prep_shared(inp)
    in_maps = [prep_core(inp, shared, c * nseq, nseq) for c in range(N_CORES)]
    res = run_bass_kernel_spmd(nc, in_maps, core_ids=list(range(N_CORES)))
    outs = [np.asarray(r["out"]).reshape(nseq, SEQ, D) for r in res.results]
    return np.concatenate(outs, axis=0).astype(np.float32)
```

```python
import math
from contextlib import ExitStack
import numpy as np
import concourse.bass as bass
import concourse.mybir as mybir
from concourse.bass_utils import run_bass_kernel_spmd

F32 = mybir.dt.float32
BF16 = mybir.dt.bfloat16
I32 = mybir.dt.int32
U8 = mybir.dt.uint8
AF = mybir.ActivationFunctionType
ALU = mybir.AluOpType
AX = mybir.AxisListType

ENGS = ("pe", "act", "dve", "pool", "sp")
N_CORES = 8
D = 1024
TWO_PI = 2.0 * math.pi
RMS_EPS = 1e-6
LN_EPS = 1e-5


class Res:
    __slots__ = ("name", "lastw", "readers")

    def __init__(self, name):
        self.name = name
        self.lastw = None
        self.readers = []


class Op:
    __slots__ = ("eng", "fn", "deps", "sdeps", "dma_key", "dma_val", "signal", "sig_idx", "cost", "lat", "idx", "succ", "nin", "fin")

    def __init__(self, eng, fn, dma_key):
        self.eng = eng
        self.fn = fn
        self.deps = []
        self.sdeps = []
        self.dma_key = dma_key
        self.dma_val = None
        self.signal = False
        self.sig_idx = None
        self.cost = 0.2
        self.lat = 0.0


class Sched:
    def __init__(self, nc):
        self.nc = nc
        self.ops = {e: [] for e in ENGS}
        self.all = []
        self.finals = []
        self.pending_barrier = {}
        import os as _o
        self.reorder = _o.environ.get("NOREORDER") is None

    def add(self, eng, fn, reads=(), writes=(), dma_key=None, cost=None, lat=0.0):
        op = Op(eng, fn, dma_key)
        if cost is not None:
            op.cost = cost
        op.lat = lat
        deps = []
        for r in reads:
            if r.lastw is not None:
                deps.append(r.lastw)
        for w in writes:
            if w.lastw is not None:
                deps.append(w.lastw)
            deps.extend(w.readers)
        if eng in self.pending_barrier:
            deps.extend(self.pending_barrier.pop(eng))
        seen = set()
        for d in deps:
            if d is op or id(d) in seen:
                continue
            seen.add(id(d))
            if d.eng == "pe" and eng == "pe" and d.dma_key is None and dma_key is None:
                op.sdeps.append(d)
                continue
            op.deps.append(d)
        for r in reads:
            r.readers.append(op)
        for w in writes:
            w.lastw = op
            w.readers = []
        op.idx = len(self.all)
        self.all.append(op)
        self.ops[eng].append(op)
        return op

    def barrier(self, dma_ops=()):
        lasts = [self.ops[e][-1] for e in ENGS if self.ops[e]]
        lasts = [o for o in lasts if o.dma_key is None] + list(dma_ops)
        for e in ENGS:
            self.pending_barrier.setdefault(e, []).extend(lasts)

    def finish(self, ops):
        self.finals.extend(ops)

    def schedule(self):
        import heapq
        ops = self.all
        for o in ops:
            o.succ = []
            o.nin = 0
        for o in ops:
            for d in o.deps + o.sdeps:
                d.succ.append(o)
                o.nin += 1
        SYNC = 0.25
        wait_h = {e: [] for e in ENGS}
        t_eng = {e: 0.0 for e in ENGS}
        order = {e: [] for e in ENGS}
        ready_at = {}
        for o in ops:
            if o.nin == 0:
                heapq.heappush(wait_h[o.eng], (0.0, o.idx))
        placed = 0
        n = len(ops)
        while placed < n:
            best = None
            for e in ENGS:
                h = wait_h[e]
                if not h:
                    continue
                te = t_eng[e]
                if h[0][0] <= te:
                    cand_idx = min(i for (r, i) in h if r <= te)
                    st = te
                else:
                    st, cand_idx = h[0]
                if best is None or (st, cand_idx) < (best[0], best[1]):
                    best = (st, cand_idx, e)
            st, ci, e = best
            h = wait_h[e]
            for k, (r, i) in enumerate(h):
                if i == ci:
                    h[k] = h[-1]
                    h.pop()
                    break
            heapq.heapify(h)
            o = ops[ci]
            t_eng[e] = st + o.cost
            o.fin = st + o.cost + o.lat
            order[e].append(o)
            placed += 1
            for sct in o.succ:
                sct.nin -= 1
                ra = max(ready_at.get(sct.idx, 0.0), o.fin + (SYNC if o.eng != sct.eng or o.dma_key is not None else 0.0))
                ready_at[sct.idx] = ra
                if sct.nin == 0:
                    heapq.heappush(wait_h[sct.eng], (ra, sct.idx))
        self.ops = order
        self.sim_time = max(t_eng.values())

    def emit(self, stack):
        nc = self.nc
        if self.reorder:
            self.schedule()
        for e in ENGS:
            for op in self.ops[e]:
                for d in op.deps:
                    if d.dma_key is None:
                        d.signal = True
        dma_cnt = {}
        for e in ENGS:
            for op in self.ops[e]:
                if op.dma_key is not None:
                    dma_cnt[op.dma_key] = dma_cnt.get(op.dma_key, 0) + 16
                    op.dma_val = dma_cnt[op.dma_key]
        sems = {e: stack.enter_context(nc.semaphore("sem_" + e)) for e in ENGS}
        dsem = {k: stack.enter_context(nc.semaphore("dsem_%s" % (k,))) for k in dma_cnt}
        for e in ENGS:
            n = 0
            for op in self.ops[e]:
                if op.dma_key is None and op.signal:
                    n += 1
                    op.sig_idx = n
        block = stack.enter_context(nc.Block())
        engobj = {"pe": "tensor", "act": "scalar", "dve": "vector", "pool": "gpsimd", "sp": "sync"}
        finals = self.finals

        def body_for(e):
            def body(eng):
                seen = {}
                for op in self.ops[e]:
                    need = {}
                    for d in op.deps:
                        if d.dma_key is not None:
                            s, v, key = dsem[d.dma_key], d.dma_val, ("d", d.dma_key)
                        else:
                            s, v, key = sems[d.eng], d.sig_idx, ("e", d.eng)
                        if key not in need or need[key][1] < v:
                            need[key] = (s, v)
                    for key, (s, v) in need.items():
                        if seen.get(key, 0) >= v:
                            continue
                        seen[key] = v
                        eng.wait_ge(s, v)
                    inst = op.fn(eng)
                    if op.dma_key is not None:
                        inst.then_inc(dsem[op.dma_key], 16)
                    elif op.signal:
                        inst.then_inc(sems[e], 1)
                if e == "sp":
                    for d in finals:
                        eng.wait_ge(dsem[d.dma_key], d.dma_val)
            return body

        for e in ENGS:
            getattr(block, engobj[e])(body_for(e))


class Tl:
    __slots__ = ("ap", "r")

    def __init__(self, ap, name):
        self.ap = ap
        self.r = Res(name)


class Arena:
    def __init__(self, nc, stack, nbytes):
        self.buf = stack.enter_context(nc.sbuf_tensor("arena", [128, nbytes], U8))
        self.off = 0
        self.cap = nbytes
        self.live = []

    def alloc(self, name, shape, dt):
        esz = {F32: 4, BF16: 2, I32: 4}[dt]
        n = 1
        for s in shape[1:]:
            n *= s
        nb = (n * esz + 31) // 32 * 32
        assert self.off + nb <= self.cap, "SBUF arena overflow at %s: %d + %d > %d" % (name, self.off, nb, self.cap)
        v = self.buf[0:shape[0], self.off:self.off + n * esz].bitcast(dt)
        if len(shape) == 3:
            v = v.rearrange("p (a b) -> p a b", a=shape[1])
        elif len(shape) == 4:
            v = v.rearrange("p (a b c) -> p a b c", a=shape[1], b=shape[2])
        t = Tl(v, name)
        lo, hi = self.off, self.off + nb
        keep = []
        for (a, b, o) in self.live:
            if a < hi and lo < b:
                if o.r.lastw is not None:
                    t.r.readers.append(o.r.lastw)
                t.r.readers.extend(o.r.readers)
                if a >= lo and b <= hi:
                    continue
            keep.append((a, b, o))
        keep.append((lo, hi, t))
        self.live = keep
        self.off += nb
        return t

    def mark(self):
        return self.off

    def reset(self, m):
        self.off = m


def build(NSEQ=4, SEQ=2048, CAP=768, dbg=None, stop_after=None):
    nc = bass.Bass("TRN2", target_bir_lowering=False)
    NT = SEQ // 128
    NTOK = NSEQ * SEQ
    NTILES = NSEQ * NT
    NSLOT = 32 * CAP
    NBLK = CAP // 128
    dbg = dbg or {}
    dbg_outs = {}

    def din(name, shape, dt=F32):
        return nc.dram_tensor(name, list(shape), dt, kind="ExternalInput").ap()

    x_d = din("x", [NTOK, D])
    cT_d = din("cT", [128, 8, NSEQ])
    w_ada_d = din("w_ada", [D, 6 * D])
    b_ada_d = din("b_ada_rep", [NSEQ, 6 * D])
    g1T_d = din("g1T", [128, 8])
    w_in_d = din("w_in", [D, 1536])
    w_gate_d = din("w_gate", [D, 2048])
    b_gateT_d = din("b_gateT", [128, 16])
    lam_sm_d = din("lam_sm", [128, 3, 16])
    Bsm_d = din("Bsm", [128, 2, 16, 128])
    Csm_d = din("Csm", [128, 2, 16, 128])
    dT_d = din("dT", [128, 4])
    w_glu_d = din("w_glu", [512, 512])
    b_gluT_d = din("b_gluT", [128, 4])
    wsT_d = din("wsT", [128, 8, 128])
    lngT_d = din("lngT", [128, 4])
    lnbT_d = din("lnbT", [128, 4])
    bsT_d = din("bsT", [128, 4, 128])
    w_bra_d = din("w_bra", [512, D])
    w_brb_d = din("w_brb", [512, D])
    w_out_d = din("w_out", [D, D])
    g2_d = din("g2", [1, D])
    w_r_d = din("w_r", [128, 8, 36])
    b_r_d = din("b_r", [1, 36])
    w1_d = din("w1", [32, D, 512])
    w3_d = din("w3", [32, D, 512])
    w2_d = din("w2", [32, 512, D])
    gf_d = din("gf", [1, D])
    out_d = nc.dram_tensor("out", [NTOK, D], F32, kind="ExternalOutput").ap()
    mod_d = Tl(nc.dram_tensor("mod_d", [NSEQ, 6 * D], F32, kind="Internal").ap(), "mod_d")
    H_d = nc.dram_tensor("H_d", [NTOK, D], F32, kind="Internal").ap()
    X_d = nc.dram_tensor("X_d", [NSLOT, D], BF16, kind="Internal").ap()
    Y_d = nc.dram_tensor("Y_d", [NSLOT, D], F32, kind="Internal").ap()
    r_X = Res("X_d")
    r_Y = Res("Y_d")
    r_H = Res("H_d")

    S = Sched(nc)
    final_ops = []
    with ExitStack() as st:
        A = Arena(nc, st, 212800)
        ps_all = st.enter_context(nc.psum_tensor("ps_all", [128, 8, 512], F32))
        PSB = [Tl(ps_all[:, b, :], "psb%d" % b) for b in range(8)]

        def psv(b, shape, dt=F32):
            v = PSB[b].ap
            if dt == BF16:
                v = v.bitcast(BF16)
            n = 1
            for s in shape[1:]:
                n *= s
            v = v[0:shape[0], 0:n]
            if len(shape) == 3:
                v = v.rearrange("p (a b) -> p a b", a=shape[1])
            elif len(shape) == 4:
                v = v.rearrange("p (a b c) -> p a b c", a=shape[1], b=shape[2])
            return v

        def psv2(b, shape):
            v = ps_all[:, b:b + 2, :].rearrange("p a b -> p (a b)")
            n = 1
            for s in shape[1:]:
                n *= s
            v = v[0:shape[0], 0:n]
            if len(shape) == 3:
                v = v.rearrange("p (a b) -> p a b", a=shape[1])
            elif len(shape) == 4:
                v = v.rearrange("p (a b c) -> p a b c", a=shape[1], b=shape[2])
            return v

        def R(*ts):
            return [t.r if isinstance(t, Tl) else t for t in ts]

        def fsz(ap):
            n = 1
            for s_ in ap.shape[1:]:
                n *= s_
            return n

        def isz(ap):
            return 2 if ap.dtype == BF16 else 4

        def ecost(eng, n):
            if eng == "dve":
                return 0.1 + n / 900.0
            if eng == "pool":
                return 0.25 + n / 430.0
            return 0.2 + n / 1150.0

        def dma(eng, out, in_, reads, writes, key, **kw):
            nb = out.shape[0] * fsz(out) * isz(out)
            return S.add(eng, lambda e: e.dma_start(out=out, in_=in_, **kw), R(*reads), R(*writes), dma_key=key,
                         cost=(1.0 if eng == "pool" else 0.07), lat=2.0 + nb / 180e3)

        def tt(eng, out, in0, in1, op, reads, writes):
            c = 0.45 if op == ALU.pow else ecost(eng, fsz(out))
            return S.add(eng, lambda e: e.tensor_tensor(out=out, in0=in0, in1=in1, op=op), R(*reads), R(*writes), cost=c)

        def ts(eng, out, in0, s1, s2, op0, op1, reads, writes, accum=None):
            c = ecost(eng, fsz(out))
            if op1 is None:
                return S.add(eng, lambda e: e.tensor_scalar(out=out, in0=in0, scalar1=s1, scalar2=None, op0=op0), R(*reads), R(*writes), cost=c)
            if accum is not None:
                return S.add(eng, lambda e: e.tensor_scalar(out=out, in0=in0, scalar1=s1, scalar2=s2, op0=op0, op1=op1, accum_out=accum), R(*reads), R(*writes), cost=c)
            return S.add(eng, lambda e: e.tensor_scalar(out=out, in0=in0, scalar1=s1, scalar2=s2, op0=op0, op1=op1), R(*reads), R(*writes), cost=c)

        def stt(out, in0, scalar, in1, op0, op1, reads, writes):
            return S.add("dve", lambda e: e.scalar_tensor_tensor(out=out, in0=in0, scalar=scalar, in1=in1, op0=op0, op1=op1), R(*reads), R(*writes),
                         cost=ecost("dve", fsz(out)))

        def act(out, in_, func, reads, writes, bias=None, scale=None, accum=None):
            kw = {}
            if bias is not None:
                kw["bias"] = bias
            if scale is not None:
                kw["scale"] = scale
            if accum is not None:
                kw["accum_out"] = accum
            return S.add("act", lambda e: e.activation(out=out, in_=in_, func=func, **kw), R(*reads), R(*writes),
                         cost=ecost("act", fsz(out)) + (0.1 if accum is not None else 0.0))

        def cp(eng, out, in_, reads, writes):
            c = ecost(eng, fsz(out))
            if eng == "act":
                return S.add("act", lambda e: e.copy(out=out, in_=in_), R(*reads), R(*writes), cost=c)
            return S.add(eng, lambda e: e.tensor_copy(out=out, in_=in_), R(*reads), R(*writes), cost=c)

        def mm(out, lhsT, rhs, start, stop, reads, writes):
            c = 0.064 + fsz(rhs) / 2400.0 * (4.0 if lhsT.dtype == F32 else 1.0)
            return S.add("pe", lambda e: e.matmul(out, lhsT=lhsT, rhs=rhs, start=start, stop=stop), R(*reads), R(*writes), cost=c)

        def tr(out, in_, ident, reads, writes):
            c = 0.064 + 128 / 2400.0 * (2.0 if in_.dtype == F32 else 1.0)
            return S.add("pe", lambda e: e.transpose(out=out, in_=in_, identity=ident), R(*reads), R(*writes), cost=c)

        def memset(eng, ap, val, writes):
            return S.add(eng, lambda e: e.memset(ap, val), [], R(*writes))

        _regs = {}

        def breg(e, val):
            if val not in _regs:
                _regs[val] = e.to_reg(val)
            return _regs[val]

        def dump(name, t, ap=None):
            ap = t.ap if ap is None else ap
            shp = list(ap.shape)
            o = nc.dram_tensor("dbg_" + name, shp, ap.dtype, kind="ExternalOutput").ap()
            dbg_outs[name] = "dbg_" + name
            final_ops.append(dma("sp", o, ap, [t], [], "dbg_" + name))

        ident_f = A.alloc("ident_f", [128, 128], F32)
        ident_b = A.alloc("ident_b", [128, 128], BF16)
        tri_b = A.alloc("tri_b", [128, 128], BF16)
        ones_b = A.alloc("ones_b", [128, 128], BF16)
        memset("pool", ident_f.ap, 0.0, [ident_f])
        S.add("pool", lambda e: e.affine_select(out=ident_f.ap, in_=ident_f.ap, pattern=[[-1, 128]], compare_op=ALU.not_equal,
                                                  fill=1.0, base=0, channel_multiplier=1), R(ident_f), R(ident_f))
        cp("pool", ident_b.ap, ident_f.ap, [ident_f], [ident_b])
        memset("pool", ones_b.ap, 1.0, [ones_b])
        S.add("pool", lambda e: e.affine_select(out=tri_b.ap, in_=ones_b.ap, pattern=[[1, 128]], compare_op=ALU.is_gt,
                                                  fill=0.0, base=0, channel_multiplier=-1), R(ones_b), R(tri_b))

        wgt = A.alloc("wgt", [128, NTILES, 2], F32)
        sloti = A.alloc("sloti", [128, NTILES, 2], I32)
        base = A.alloc("base", [128, 32], F32)
        ecap = A.alloc("ecap", [128, 32], F32)
        memset("pool", base.ap, 0.0, [base])
        ecap_i = A.alloc("ecap_i", [128, 32], I32)
        S.add("pool", lambda e: e.iota(ecap_i.ap, pattern=[[CAP, 32]], base=0, channel_multiplier=0), [], R(ecap_i))
        cp("pool", ecap.ap, ecap_i.ap, [ecap_i], [ecap])
        A1T = A.alloc("A1T", [128, 8, NSEQ], F32)
        sh1T = A.alloc("sh1T", [128, 8, NSEQ], F32)
        eps_rms = A.alloc("eps_rms", [128, 1], F32)
        eps_ln = A.alloc("eps_ln", [128, 1], F32)
        memset("pool", eps_rms.ap, 1e-6, [eps_rms])
        memset("pool", eps_ln.ap, 1e-5, [eps_ln])
        mhalf = A.alloc("mhalf", [128, 1], F32)
        memset("pool", mhalf.ap, -0.5, [mhalf])

        mark_persist = A.mark()

        cact = A.alloc("cact", [128, 8, NSEQ], F32)
        dma("sp", cact.ap, cT_d, [], [cact], "cact")
        act(cact.ap, cact.ap, AF.Silu, [cact], [cact])
        modrow = A.alloc("modrow", [NSEQ, 6 * D], F32)
        bada = A.alloc("bada", [NSEQ, 6 * D], F32)
        dma("sp", bada.ap, b_ada_d, [], [bada], "bada")
        wa = [A.alloc("wa%d" % i, [128, 8, 512], F32) for i in range(2)]
        wa_view = w_ada_d.rearrange("(kc p) n -> p kc n", p=128)
        for cb in range(12):
            w = wa[cb % 2]
            dma("sp", w.ap, wa_view[:, :, cb * 512:(cb + 1) * 512], [], [w], "wa%d" % (cb % 2))
            pb = PSB[cb % 2]
            for kc in range(8):
                mm(pb.ap[0:NSEQ, :], cact.ap[:, kc, :], w.ap[:, kc, :], kc == 0, kc == 7, [cact, w], [pb])
            tt("dve", modrow.ap[:, cb * 512:(cb + 1) * 512], pb.ap[0:NSEQ, :], bada.ap[:, cb * 512:(cb + 1) * 512], ALU.add,
               [pb, bada], [modrow])
        dma("sp", mod_d.ap, modrow.ap, [modrow], [mod_d], "mod_d")
        sc1T = A.alloc("sc1T", [128, 8, NSEQ], F32)
        g1T = A.alloc("g1T", [128, 8], F32)
        dma("sp", g1T.ap, g1T_d, [], [g1T], "g1T")
        for b in range(NSEQ):
            S.add("sp", lambda e, b=b: e.dma_start(out=sh1T.ap[:, :, b], in_=mod_d.ap[b, 0:D].rearrange("(kc p) -> p kc", p=128),
                                                  allow_slow_non_contiguous=True), R(mod_d), R(sh1T), dma_key="sh1T")
            S.add("sp", lambda e, b=b: e.dma_start(out=sc1T.ap[:, :, b], in_=mod_d.ap[b, D:2 * D].rearrange("(kc p) -> p kc", p=128),
                                                  allow_slow_non_contiguous=True), R(mod_d), R(sc1T), dma_key="sc1T")
        for b in range(NSEQ):
            stt(A1T.ap[:, :, b], sc1T.ap[:, :, b], 1.0, g1T.ap, ALU.add, ALU.mult, [sc1T, g1T], [A1T])
        if "mod" in dbg:
            dump("modrow", modrow)
            dump("A1T", A1T)
        A.reset(mark_persist)
        S.barrier()
        if stop_after == "mod":
            S.finish(final_ops)
            S.emit(st)
            return nc, dbg_outs

        w_r = A.alloc("w_r", [128, 8, 36], F32)
        dma("sp", w_r.ap, w_r_d, [], [w_r], "w_r")
        b_r = A.alloc("b_r", [128, 36], F32)
        dma("sp", b_r.ap, b_r_d.partition_broadcast(128), [], [b_r], "b_r")
        b_gateT = A.alloc("b_gateT", [128, 16], F32)
        dma("sp", b_gateT.ap, b_gateT_d, [], [b_gateT], "b_gateT")
        ts("pool", b_gateT.ap, b_gateT.ap, 0.5, 1.0, ALU.mult, ALU.mult, [b_gateT], [b_gateT])
        b_gluT = A.alloc("b_gluT", [128, 4], F32)
        dma("sp", b_gluT.ap, b_gluT_d, [], [b_gluT], "b_gluT")
        ts("pool", b_gluT.ap, b_gluT.ap, 0.5, 1.0, ALU.mult, ALU.mult, [b_gluT], [b_gluT])
        dT = A.alloc("dT", [128, 4], F32)
        dma("sp", dT.ap, dT_d, [], [dT], "dT")
        lngT = A.alloc("lngT", [128, 4], F32)
        dma("sp", lngT.ap, lngT_d, [], [lngT], "lngT")
        lnbT = A.alloc("lnbT", [128, 4], F32)
        dma("sp", lnbT.ap, lnbT_d, [], [lnbT], "lnbT")

        LC = 4
        NCH = 128 // LC
        tabC4 = A.alloc("tabC4", [128, 16, NCH + 1], F32)
        tabD4 = A.alloc("tabD4", [128, 16, NCH + 1], F32)
        Rtab = A.alloc("Rtab", [128, 16, 2, NCH], F32)
        rmagL = A.alloc("rmagL", [128, 16], F32)
        Pt = A.alloc("Pt", [128, 2, 8 * LC, 128], BF16)
        Qs = A.alloc("Qs", [128, 2, 16 * LC, 64], BF16)
        Kb = A.alloc("Kb", [128, 4 * LC, 128], BF16)
        tabC = Tl(None, "s5scr")
        mark_setup = A.mark()

        def range_reduce(ph, tmpf, tmpi, n):
            ts("dve", tmpi, ph, 1.0 / TWO_PI, None, ALU.mult, None, [tabC], [tabC])
            cp("dve", tmpf, tmpi, [tabC], [tabC])
            stt(ph, tmpf, -TWO_PI, ph, ALU.mult, ALU.add, [tabC], [tabC])
            wrap(ph, tmpf)

        def wrap(ph, tmpf):
            ts("dve", tmpf, ph, math.pi, None, ALU.is_gt, None, [tabC], [tabC])
            stt(ph, tmpf, -TWO_PI, ph, ALU.mult, ALU.add, [tabC], [tabC])
            ts("dve", tmpf, ph, -math.pi, None, ALU.is_lt, None, [tabC], [tabC])
            stt(ph, tmpf, TWO_PI, ph, ALU.mult, ALU.add, [tabC], [tabC])

        def scr(name, shape, dt):
            t = A.alloc(name, shape, dt)
            tabC.r.readers.extend(t.r.readers)
            t.r = tabC.r
            for k_, (a_, b_, o_) in enumerate(A.live):
                if o_ is t:
                    A.live[k_] = (a_, b_, t)
            return t

        TC = [tabC]
        lam = scr("lam", [128, 3, 16], F32)
        dma("sp", lam.ap, lam_sm_d, [], TC, "lam")
        zs = {n: scr("z_" + n, [128, 16], F32) for n in ("dt", "th", "lre", "lrdt", "den", "nr", "fre", "fim", "t0", "t1")}
        sv_i = scr("sv_i", [128, 129], I32)
        sv = scr("sv", [128, 129], F32)
        tabCf = scr("tabCf", [128, 16, 129], F32)
        tabDf = scr("tabDf", [128, 16, 129], F32)
        tmpf = scr("tmpf", [128, 16 * 129], F32)
        tmpi = scr("tmpi", [128, 16 * 129], I32)
        aim = lam.ap[:, 1, :]
        S.add("pool", lambda e: e.iota(sv_i.ap, pattern=[[1, 129]], base=0, channel_multiplier=0), [], R(tabC))
        cp("dve", sv.ap, sv_i.ap, TC, TC)
        ts("dve", zs["lre"].ap, lam.ap[:, 0, :], -1e-4, None, ALU.min, None, TC, TC)
        act(zs["dt"].ap, lam.ap[:, 2, :], AF.Exp, TC, TC)
        tt("dve", zs["lrdt"].ap, zs["lre"].ap, zs["dt"].ap, ALU.mult, TC, TC)
        tt("dve", zs["th"].ap, aim, zs["dt"].ap, ALU.mult, TC, TC)
        for j in range(16):
            ts("dve", tabDf.ap[:, j, :], sv.ap, zs["th"].ap[:, j:j + 1], None, ALU.mult, None, TC, TC)
        phD = tabDf.ap.rearrange("p a b -> p (a b)")
        phC = tabCf.ap.rearrange("p a b -> p (a b)")
        range_reduce(phD, tmpf.ap, tmpi.ap, 16 * 129)
        ts("dve", phC, phD, math.pi / 2, None, ALU.add, None, TC, TC)
        wrap(phC, tmpf.ap)
        act(phD, phD, AF.Sin, TC, TC)
        act(phC, phC, AF.Sin, TC, TC)
        cp("dve", tabC4.ap, tabCf.ap[:, :, 0:129:LC], TC, TC + [tabC4])
        cp("dve", tabD4.ap, tabDf.ap[:, :, 0:129:LC], TC, TC + [tabD4])
        rk = scr("rk", [128, 16, LC + 1], F32)
        pwr = scr("pwr", [128, 16, LC + 1], F32)
        pwi = scr("pwi", [128, 16, LC + 1], F32)
        for k in range(LC + 1):
            act(rk.ap[:, :, k], zs["lrdt"].ap, AF.Exp, TC, TC, scale=float(k))
        tt("dve", pwr.ap, rk.ap, tabCf.ap[:, :, 0:LC + 1], ALU.mult, TC, TC)
        tt("dve", pwi.ap, rk.ap, tabDf.ap[:, :, 0:LC + 1], ALU.mult, TC, TC)
        cp("dve", rmagL.ap, rk.ap[:, :, LC], TC, TC + [rmagL])
        cp("dve", Rtab.ap.rearrange("p a b c -> p a (b c)"), rmagL.ap.unsqueeze(2).to_broadcast([128, 16, 2 * NCH]),
           TC + [rmagL], TC + [Rtab])
        memset("dve", Rtab.ap[:, :, :, 0], 0.0, TC + [Rtab])
        abr, abi = pwr.ap[:, :, 1], pwi.ap[:, :, 1]
        lre_ = zs["lre"].ap
        tt("dve", zs["den"].ap, lre_, lre_, ALU.mult, TC, TC)
        tt("dve", zs["t0"].ap, aim, aim, ALU.mult, TC, TC)
        tt("dve", zs["den"].ap, zs["den"].ap, zs["t0"].ap, ALU.add, TC, TC)
        S.add("dve", lambda e: e.reciprocal(out=zs["den"].ap, in_=zs["den"].ap), R(tabC), R(tabC))
        ts("dve", zs["nr"].ap, abr, -1.0, None, ALU.add, None, TC, TC)
        tt("dve", zs["t0"].ap, zs["nr"].ap, lre_, ALU.mult, TC, TC)
        tt("dve", zs["t1"].ap, abi, aim, ALU.mult, TC, TC)
        tt("dve", zs["t0"].ap, zs["t0"].ap, zs["t1"].ap, ALU.add, TC, TC)
        tt("dve", zs["fre"].ap, zs["t0"].ap, zs["den"].ap, ALU.mult, TC, TC)
        tt("dve", zs["t0"].ap, abi, lre_, ALU.mult, TC, TC)
        tt("dve", zs["t1"].ap, zs["nr"].ap, aim, ALU.mult, TC, TC)
        tt("dve", zs["t0"].ap, zs["t0"].ap, zs["t1"].ap, ALU.subtract, TC, TC)
        tt("dve", zs["fim"].ap, zs["t0"].ap, zs["den"].ap, ALU.mult, TC, TC)
        Bsm = scr("Bsm", [128, 2, 16, 128], F32)
        Csm = scr("Csm", [128, 2, 16, 128], F32)
        dma("sp", Bsm.ap, Bsm_d, [], TC, "Bsm")
        dma("sp", Csm.ap, Csm_d, [], TC, "Csm")
        Btr = scr("Btr", [128, 16, 128], F32)
        Bti = scr("Bti", [128, 16, 128], F32)
        Gr = scr("Gr", [128, 16, 128], F32)
        Gi = scr("Gi", [128, 16, 128], F32)
        w0 = scr("w0", [128, 16, 128], F32)
        w1_ = scr("w1_", [128, 16, 128], F32)
        Psr = scr("Psr", [128, 16, 128], BF16)
        Psi = scr("Psi", [128, 16, 128], BF16)
        Kf = scr("Kf", [128, 128], F32)

        def bc(v):
            return v.unsqueeze(2).to_broadcast([128, 16, 128])

        def cmul(o_re, o_im, a_re, a_im, s_re, s_im, neg_im=False):
            tt("dve", w0.ap, a_re, bc(s_re), ALU.mult, TC, TC)
            tt("dve", w1_.ap, a_im, bc(s_im), ALU.mult, TC, TC)
            tt("dve", o_re, w0.ap, w1_.ap, ALU.subtract, TC, TC)
            tt("dve", w0.ap, a_re, bc(s_im), ALU.mult, TC, TC)
            tt("dve", w1_.ap, a_im, bc(s_re), ALU.mult, TC, TC)
            if neg_im:
                stt(o_im, w0.ap, -1.0, w1_.ap, ALU.mult, ALU.subtract, TC, TC)
            else:
                tt("dve", o_im, w0.ap, w1_.ap, ALU.add, TC, TC)

        cmul(Btr.ap, Bti.ap, Bsm.ap[:, 0], Bsm.ap[:, 1], zs["fre"].ap, zs["fim"].ap)
        memset("dve", Kb.ap, 0.0, TC + [Kb])
        for k in range(LC + 1):
            cmul(Gr.ap, Gi.ap, Csm.ap[:, 0], Csm.ap[:, 1], pwr.ap[:, :, k], pwi.ap[:, :, k], neg_im=True)
            if k >= 1:
                for j in range(16):
                    c0 = 64 * ((j % 4) // 2)
                    cp("act", Qs.ap[:, 0, j * LC + k - 1, :], Gr.ap[:, j, c0:c0 + 64], TC, TC + [Qs])
                    cp("act", Qs.ap[:, 1, j * LC + k - 1, :], Gi.ap[:, j, c0:c0 + 64], TC, TC + [Qs])
            if k < LC:
                for q in range(4):
                    pK = PSB[2 + q % 2]
                    for jm in range(4):
                        j = 4 * q + jm
                        mm(pK.ap[:, 0:128], Btr.ap[:, j, :], Gr.ap[:, j, :], jm == 0, False, TC, [pK])
                        mm(pK.ap[:, 0:128], Bti.ap[:, j, :], Gi.ap[:, j, :], False, jm == 3, TC, [pK])
                    if k == 0:
                        stt(Kf.ap, ident_f.ap, dT.ap[:, q:q + 1], pK.ap[:, 0:128], ALU.mult, ALU.add, [pK, ident_f, dT] + TC, TC)
                        cp("dve", Kb.ap[:, q * LC + k, :], Kf.ap, TC, TC + [Kb])
                    else:
                        cp("dve", Kb.ap[:, q * LC + k, :], pK.ap[:, 0:128], [pK] + TC, TC + [Kb])
                s_ = LC - 1 - k
                cmul(Psr.ap, Psi.ap, Btr.ap, Bti.ap, pwr.ap[:, :, k], pwi.ap[:, :, k])
                for part, Ps_ in enumerate((Psr, Psi)):
                    for pr in range(2):
                        bk = (2 * part + pr) % 2
                        pT = psv(bk, [128, 8, 128], BF16)
                        for idx in range(8):
                            j = 4 * (idx // 2) + 2 * pr + idx % 2
                            tr(pT[64 * pr:64 * pr + 64, idx, :], Ps_.ap[:, j, 64 * pr:64 * pr + 64], ident_b.ap, TC + [ident_b], [PSB[bk]])
                        cp("act", Pt.ap[64 * pr:64 * pr + 64, part, s_::LC, :], pT[64 * pr:64 * pr + 64, :, :], [PSB[bk]] + TC, TC + [Pt])
        if "s5setup" in dbg:
            dump("tabC4", tabC4)
            dump("Kb", Kb)
            dump("Pt", Pt)
            dump("Qs", Qs)
        A.reset(mark_setup)

        wsT_b = A.alloc("wsT_b", [128, 8, 128], BF16)
        sgub = A.alloc("sgub", [128, 4, 128], F32)
        mark_sgu = A.mark()
        wsf = A.alloc("wsf", [128, 8, 128], F32)
        dma("sp", wsf.ap, wsT_d, [], [wsf], "wsf")
        for h in range(8):
            S.add("pool", lambda e, h=h: e.affine_select(out=wsf.ap[:, h, :], in_=wsf.ap[:, h, :], pattern=[[1, 128]],
                                                           compare_op=ALU.is_ge, fill=0.0, base=0, channel_multiplier=-1),
                  R(wsf), R(wsf))
        cp("pool", wsT_b.ap, wsf.ap, [wsf], [wsT_b])
        bsT = A.alloc("bsT", [128, 4, 128], F32)
        dma("sp", bsT.ap, bsT_d, [], [bsT], "bsT")
        pmix0 = psv(3, [128, 4, 128])
        for h in range(8):
            po = (h % 2) * 64
            mm(pmix0[po:po + 64, h // 2, :], ones_b.ap[:, 0:64], wsT_b.ap[:, h, :], True, True, [ones_b, wsT_b], [PSB[3]])
        for q in range(4):
            stt(sgub.ap[:, q, :], pmix0[:, q, :], lnbT.ap[:, q:q + 1], bsT.ap[:, q, :], ALU.mult, ALU.add,
                [PSB[3], lnbT, bsT], [sgub])
        if "sgusetup" in dbg:
            dump("sgub", sgub)
            dump("wsT_b", wsT_b)
        A.reset(mark_sgu)

        if stop_after == "setup":
            S.finish(final_ops)
            S.emit(st)
            return nc, dbg_outs

        def wload(name, src_view, shape, key=None):
            t = A.alloc(name, shape, BF16)
            dma("pool", t.ap, src_view, [], [t], key or name)
            return t

        w_in_b = wload("w_in_b", w_in_d.rearrange("(kc p) n -> p kc n", p=128), [128, 8, 1536])
        w_gate_b = wload("w_gate_b", w_gate_d.rearrange("(kc p) n -> p kc n", p=128), [128, 8, 2048])
        w_glu_b = wload("w_glu_b", w_glu_d.rearrange("(kc p) n -> p kc n", p=128), [128, 4, 512])
        w_bra_b = wload("w_bra_b", w_bra_d.rearrange("(kc p) n -> p kc n", p=128), [128, 4, D])
        ts("pool", w_bra_b.ap, w_bra_b.ap, 0.5, 1.0, ALU.mult, ALU.mult, [w_bra_b], [w_bra_b])
        w_brb_b = wload("w_brb_b", w_brb_d.rearrange("(kc p) n -> p kc n", p=128), [128, 4, D])
        w_out_b = wload("w_out_b", w_out_d.rearrange("(kc p) n -> p kc n", p=128), [128, 8, D])
        def alias(name, shape, dt, of, off=0):
            t = Tl(None, name)
            n = 1
            for s_ in shape[1:]:
                n *= s_
            base_ = of.ap
            if len(base_.shape) == 3:
                base_ = base_.rearrange("p a b -> p (a b)")
            elif len(base_.shape) == 4:
                base_ = base_.rearrange("p a b c -> p (a b c)")
            if base_.dtype != dt:
                base_ = base_.bitcast(dt)
            v = base_[:, off:off + n]
            if len(shape) == 3:
                v = v.rearrange("p (a b) -> p a b", a=shape[1])
            t.ap = v
            t.r = of.r
            return t

        xt0 = A.alloc("xt0", [128, D], F32)
        xr = A.alloc("xr", [128, D], F32)
        xsb = A.alloc("xsb", [128, D], BF16)
        ssq = A.alloc("ssq", [128, 1], F32)
        rstd = A.alloc("rstd", [128, 1], F32)
        xnT = [A.alloc("xnT%d" % i, [128, 8, 128], BF16) for i in range(3)]
        uT = [A.alloc("uT%d" % i, [128, 4, 128], BF16) for i in range(2)]
        guT = A.alloc("guT", [128, 4, 128], F32)
        gv = A.alloc("gv", [128, 512], F32)
        vst = A.alloc("vst", [128, 6], F32)
        vmv = A.alloc("vmv", [128, 2], F32)
        vrs = A.alloc("vrs", [128, 1], F32)
        vhat = A.alloc("vhat", [128, 512], BF16)
        mixT = alias("mixT", [128, 4, 128], F32, gv)
        ybT = [A.alloc("ybT%d" % i, [128, 4, 128], BF16) for i in range(3)]
        xtil = A.alloc("xtil", [128, 4, 2, NCH], F32)
        s5a = A.alloc("s5a", [128, 4, NCH], F32)
        s5b = A.alloc("s5b", [128, 4, NCH], F32)
        gsc = A.alloc("gsc", [128, 4, 2, NCH], F32)
        gp = A.alloc("gp", [128, 16, 2], F32)
        carry = A.alloc("carry", [128, 16, 2], F32)
        sm1 = A.alloc("sm1", [128, 16, 2], F32)
        cf2 = A.alloc("cf2", [128, 4, 2], F32)
        c4 = [A.alloc("c4_%d" % i, [128, 16], F32) for i in range(4)]
        Sprev = [A.alloc("Sprev%d" % i, [128, 4, 2, NCH], BF16) for i in range(2)]
        ygT = A.alloc("ygT", [128, 4, 128], BF16)
        sg = A.alloc("sg", [128, 4, 128], F32)
        yaT = [A.alloc("yaT%d" % i, [128, 4, 128], BF16) for i in range(2)]
        gates = A.alloc("gates", [128, 16, 128], F32)
        xn2T = alias("xn2T", [128, 8, 128], F32, gates, off=1024)
        xn2 = alias("xn2", [128, D], F32, gates, off=0)
        mergedT = A.alloc("mergedT", [128, 8, 128], BF16)
        jk2 = alias("jk2", [128, D], BF16, mergedT)
        gt1b = A.alloc("gt1b", [128, D], F32)
        A2b = A.alloc("A2b", [128, D], F32)
        sh2b = A.alloc("sh2b", [128, D], F32)
        ssq2 = A.alloc("ssq2", [128, 1], F32)
        rstd2 = A.alloc("rstd2", [128, 1], F32)
        lg = A.alloc("lg", [128, 36], F32)
        rt = {n: A.alloc("rt_" + n, [128, w_], F32) for n, w_ in
              [("gmax", 1), ("ngmax", 1), ("maskg", 4), ("eg", 4), ("sume", 1), ("pgs", 1), ("pen", 4), ("lem", 32),
               ("m1", 1), ("oh1", 32), ("lem2", 32), ("m2", 1), ("oh2", 32), ("dm", 1), ("e2", 1), ("p1", 1), ("p2", 1),
               ("rank", 32), ("slotv", 32), ("junk", 32), ("sl", 2), ("val", 32), ("vk", 2)]}
        ohb = A.alloc("ohb", [128, 32], BF16)

        def rms_rstd(src, ssq_t, rstd_t, junk_ap, junk_t):
            act(junk_ap, src.ap, AF.Square, [src], [junk_t, ssq_t], accum=ssq_t.ap)
            ts("pool", ssq_t.ap, ssq_t.ap, 1.0 / D, RMS_EPS, ALU.mult, ALU.add, [ssq_t], [ssq_t])
            tt("pool", rstd_t.ap, ssq_t.ap, mhalf.ap, ALU.pow, [ssq_t, mhalf], [rstd_t])

        store_ops = []
        scat_ops = []
        c128 = tabC4.ap[:, :, NCH]
        d128 = tabD4.ap[:, :, NCH]

        def seqof(i):
            return i // NT

        def P1(i):
            b = seqof(i)
            X = xt0
            XN = xnT[i % 3]
            if i == 0:
                dma("sp", X.ap, x_d[0:128, :], [], [X], "xt0")
            rms_rstd(X, ssq, rstd, xsb.ap, xsb)
            act(xsb.ap, X.ap, AF.Copy, [X, rstd], [xsb], scale=rstd.ap[:, 0:1])
            if i + 1 < NTILES:
                dma("sp", X.ap, x_d[(i + 1) * 128:(i + 2) * 128, :], [], [X], "xt0")
            pX = psv(0, [128, 8, 128], BF16)
            for kc in range(8):
                tr(pX[:, kc, :], xsb.ap[:, kc * 128:(kc + 1) * 128], ident_b.ap, [xsb, ident_b], [PSB[0]])
            for kc in range(8):
                act(XN.ap[:, kc, :], pX[:, kc, :], AF.Identity, [PSB[0], A1T, sh1T], [XN],
                    bias=sh1T.ap[:, kc, b:b + 1], scale=A1T.ap[:, kc, b:b + 1])
            if dbg.get("tile") == i:
                dump("xnT", XN)

        def P2(i):
            XN = xnT[i % 3]
            UT = uT[i % 2]
            pZa = psv(1, [128, 4, 128])
            pZu = psv(0, [128, 4, 128])
            for m in range(4):
                for kc in range(8):
                    mm(pZa[:, m, :], w_in_b.ap[:, kc, m * 128:(m + 1) * 128], XN.ap[:, kc, :], kc == 0, kc == 7,
                       [w_in_b, XN], [PSB[1]])
            cp("act", UT.ap, pZa, [PSB[1]], [UT])
            for m in range(4):
                for kc in range(8):
                    mm(pZu[:, m, :], w_in_b.ap[:, kc, 512 + m * 128:512 + (m + 1) * 128], XN.ap[:, kc, :], kc == 0, kc == 7,
                       [w_in_b, XN], [PSB[0]])
            act(guT.ap, pZu, AF.Gelu_apprx_tanh, [PSB[0]], [guT])
            pV = psv(1, [128, 512])
            for kc in range(8):
                mm(pV, XN.ap[:, kc, :], w_in_b.ap[:, kc, 1024:1536], kc == 0, kc == 7, [w_in_b, XN], [PSB[1]])
            act(gv.ap, pV, AF.Gelu_apprx_tanh, [PSB[1]], [gv])
            if dbg.get("tile") == i:
                dump("uT", UT)

        def P3(i):
            YB = ybT[i % 3]
            S.add("dve", lambda e: e.bn_stats(out=vst.ap, in_=gv.ap), R(gv), R(vst))
            S.add("dve", lambda e: e.bn_aggr(out=vmv.ap, in_=vst.ap), R(vst), R(vmv))
            ts("pool", vrs.ap, vmv.ap[:, 1:2], 1.0, LN_EPS, ALU.mult, ALU.add, [vmv], [vrs])
            tt("pool", vrs.ap, vrs.ap, mhalf.ap, ALU.pow, [vrs, mhalf], [vrs])
            ts("dve", vhat.ap, gv.ap, vmv.ap[:, 0:1], vrs.ap[:, 0:1], ALU.subtract, ALU.mult, [gv, vmv, vrs], [vhat])
            pMix = psv(0, [128, 4, 128])
            for h in range(8):
                po = (h % 2) * 64
                mm(pMix[po:po + 64, h // 2, :], vhat.ap[:, h * 64:(h + 1) * 64], wsT_b.ap[:, h, :], True, True,
                   [vhat, wsT_b], [PSB[0]])
            for q in range(4):
                stt(mixT.ap[:, q, :], pMix[:, q, :], lngT.ap[:, q:q + 1], sgub.ap[:, q, :], ALU.mult, ALU.add,
                    [PSB[0], lngT, sgub], [mixT])
            tt("pool", YB.ap, guT.ap, mixT.ap, ALU.mult, [guT, mixT], [YB])
            if dbg.get("tile") == i:
                dump("ybT", YB)

        def pS5(q):
            o = (q % 2) * 4 * NCH
            return ps_all[:, 2:4, o:o + 4 * NCH].rearrange("p k (a b c) -> p k a b c", a=2, b=2)

        def Qpre(i):
            if i % NT == 0:
                memset("dve", carry.ap, 0.0, [carry])
            cT_, dT_ = tabC4.ap[:, :, 1], tabD4.ap[:, :, 1]
            tt("dve", c4[0].ap, carry.ap[:, :, 0], cT_, ALU.mult, [carry, tabC4], [c4[0]])
            tt("dve", c4[1].ap, carry.ap[:, :, 1], dT_, ALU.mult, [carry, tabD4], [c4[1]])
            tt("dve", c4[2].ap, carry.ap[:, :, 1], cT_, ALU.mult, [carry, tabC4], [c4[2]])
            tt("dve", c4[3].ap, carry.ap[:, :, 0], dT_, ALU.mult, [carry, tabD4], [c4[3]])
            tt("dve", sm1.ap[:, :, 0], c4[0].ap, c4[1].ap, ALU.add, [c4[0], c4[1]], [sm1])
            tt("dve", sm1.ap[:, :, 1], c4[2].ap, c4[3].ap, ALU.subtract, [c4[2], c4[3]], [sm1])

        def QB(i, q):
            UT = uT[i % 2]
            pS = pS5(q)
            for jj in range(4):
                ro = 64 * (jj // 2)
                for part in range(2):
                    for sx in range(LC):
                        mm(pS[:, jj // 2, jj % 2, part, :], Pt.ap[ro:ro + 64, part, (2 * q + jj % 2) * LC + sx, :],
                           UT.ap[ro:ro + 64, q, sx::LC], sx == 0, sx == LC - 1, [Pt, UT], [PSB[2 + jj // 2]])

        def QD(i, q):
            pS = pS5(q)
            SP = Sprev[q % 2]
            tc_ = tabC4.ap[:, 4 * q:4 * q + 4, 0:NCH]
            td_ = tabD4.ap[:, 4 * q:4 * q + 4, 0:NCH]
            tc4 = tc_.rearrange("p (k a) c -> p k a c", k=2)
            td4 = td_.rearrange("p (k a) c -> p k a c", k=2)
            a4 = s5a.ap.rearrange("p (k a) c -> p k a c", k=2)
            b4 = s5b.ap.rearrange("p (k a) c -> p k a c", k=2)
            PB = [PSB[2], PSB[3]]
            tt("dve", a4, pS[:, :, :, 0, :], tc4, ALU.mult, PB + [tabC4], [s5a])
            tt("dve", b4, pS[:, :, :, 1, :], td4, ALU.mult, PB + [tabD4], [s5b])
            tt("dve", xtil.ap[:, :, 0, :], s5a.ap, s5b.ap, ALU.add, [s5a, s5b], [xtil])
            tt("dve", a4, pS[:, :, :, 1, :], tc4, ALU.mult, PB + [tabC4, xtil], [s5a])
            tt("dve", b4, pS[:, :, :, 0, :], td4, ALU.mult, PB + [tabD4, xtil], [s5b])
            tt("dve", xtil.ap[:, :, 1, :], s5a.ap, s5b.ap, ALU.subtract, [s5a, s5b], [xtil])
            tt("dve", cf2.ap, carry.ap[:, 4 * q:4 * q + 4, :], rmagL.ap[:, 4 * q:4 * q + 4].unsqueeze(2).to_broadcast([128, 4, 2]),
               ALU.mult, [carry, rmagL], [cf2])
            tt("dve", xtil.ap[:, :, :, 0], xtil.ap[:, :, :, 0], cf2.ap, ALU.add, [xtil, cf2], [xtil])
            S.add("dve", lambda e: e.tensor_tensor_scan(
                out=gsc.ap.rearrange("p a b c -> p (a b c)"),
                data0=Rtab.ap[:, 4 * q:4 * q + 4, :, :].rearrange("p a b c -> p (a b c)"),
                data1=xtil.ap.rearrange("p a b c -> p (a b c)"), initial=0.0, op0=ALU.mult, op1=ALU.add),
                R(Rtab, xtil), R(gsc), cost=0.25 + 8 * NCH / 500.0)
            cp("dve", gp.ap[:, 4 * q:4 * q + 4, :], gsc.ap[:, :, :, NCH - 1], [gsc], [gp])
            n1 = NCH - 1
            tt("dve", s5a.ap[:, :, 0:n1], gsc.ap[:, :, 0, 0:n1], tc_[:, :, 0:n1], ALU.mult, [gsc, tabC4], [s5a])
            tt("dve", s5b.ap[:, :, 0:n1], gsc.ap[:, :, 1, 0:n1], td_[:, :, 0:n1], ALU.mult, [gsc, tabD4], [s5b])
            tt("dve", SP.ap[:, :, 0, 1:NCH], s5a.ap[:, :, 0:n1], s5b.ap[:, :, 0:n1], ALU.subtract, [s5a, s5b], [SP])
            tt("dve", s5a.ap[:, :, 0:n1], gsc.ap[:, :, 1, 0:n1], tc_[:, :, 0:n1], ALU.mult, [gsc, tabC4, SP], [s5a])
            tt("dve", s5b.ap[:, :, 0:n1], gsc.ap[:, :, 0, 0:n1], td_[:, :, 0:n1], ALU.mult, [gsc, tabD4, SP], [s5b])
            tt("dve", SP.ap[:, :, 1, 1:NCH], s5a.ap[:, :, 0:n1], s5b.ap[:, :, 0:n1], ALU.add, [s5a, s5b], [SP])
            cp("dve", SP.ap[:, :, :, 0], sm1.ap[:, 4 * q:4 * q + 4, :], [sm1], [SP])
            if dbg.get("tile") == i and q == 0:
                dump("gsc0", gsc)

        def QC(i, q):
            UT = uT[i % 2]
            SP = Sprev[q % 2]
            pY = psv(4, [128, 4, LC, NCH])
            for sp_ in range(LC):
                first = True
                for sx in range(sp_ + 1):
                    mm(pY[:, q, sp_, :], Kb.ap[:, q * LC + (sp_ - sx), :], UT.ap[:, q, sx::LC], first, False, [Kb, UT], [PSB[4]])
                    first = False
                for jj in range(4):
                    j = 4 * q + jj
                    ro = 64 * (jj // 2)
                    for part in range(2):
                        mm(pY[ro:ro + 64, q, sp_, :], Qs.ap[:, part, j * LC + sp_, :], SP.ap[:, jj, part, :],
                           False, jj == 3 and part == 1, [Qs, SP], [PSB[4]])

        def Qcarry(i):
            tt("dve", c4[0].ap, gp.ap[:, :, 0], c128, ALU.mult, [gp, tabC4], [c4[0]])
            tt("dve", c4[1].ap, gp.ap[:, :, 1], d128, ALU.mult, [gp, tabD4], [c4[1]])
            tt("dve", c4[2].ap, gp.ap[:, :, 1], c128, ALU.mult, [gp, tabC4], [c4[2]])
            tt("dve", c4[3].ap, gp.ap[:, :, 0], d128, ALU.mult, [gp, tabD4], [c4[3]])
            tt("dve", carry.ap[:, :, 0], c4[0].ap, c4[1].ap, ALU.subtract, [c4[0], c4[1]], [carry])
            tt("dve", carry.ap[:, :, 1], c4[2].ap, c4[3].ap, ALU.add, [c4[2], c4[3]], [carry])

        def Qtail(i):
            YA = yaT[i % 2]
            pY = psv(4, [128, 4, LC, NCH])
            for q in range(4):
                act(ygT.ap[:, q, :].rearrange("p (c s) -> p s c", s=LC), pY[:, q, :, :], AF.Gelu_apprx_tanh, [PSB[4]], [ygT])
            if dbg.get("tile") == i:
                dump("ypre", ygT)
            pG = psv(4, [128, 4, 128])
            for m in range(4):
                for kc in range(4):
                    mm(pG[:, m, :], w_glu_b.ap[:, kc, m * 128:(m + 1) * 128], ygT.ap[:, kc, :], kc == 0, kc == 3,
                       [w_glu_b, ygT], [PSB[4]])
            for m in range(4):
                act(sg.ap[:, m, :], pG[:, m, :], AF.Tanh, [PSB[4], b_gluT], [sg], bias=b_gluT.ap[:, m:m + 1], scale=0.5)
            stt(YA.ap, sg.ap, 1.0, ygT.ap, ALU.add, ALU.mult, [sg, ygT], [YA])
            if dbg.get("tile") == i:
                dump("yaT", YA)

        def R0(i):
            b = seqof(i)
            if i % NT == 0:
                dma("sp", gt1b.ap, mod_d.ap[b:b + 1, 2 * D:3 * D].partition_broadcast(128), [mod_d], [gt1b], "gt1b")
                ts("dve", gt1b.ap, gt1b.ap, 0.5, None, ALU.mult, None, [gt1b], [gt1b])
                dma("sp", sh2b.ap, mod_d.ap[b:b + 1, 3 * D:4 * D].partition_broadcast(128), [mod_d], [sh2b], "sh2b")
                dma("sp", A2b.ap, mod_d.ap[b:b + 1, 4 * D:5 * D].partition_broadcast(128), [mod_d], [A2b], "A2b")
                dma("sp", xn2.ap, g2_d.partition_broadcast(128), [], [xn2], "g2tmp")
                stt(A2b.ap, A2b.ap, 1.0, xn2.ap, ALU.add, ALU.mult, [A2b, xn2], [A2b])
            dma("sp", xr.ap, x_d[i * 128:(i + 1) * 128, :], [], [xr], "xr")

        def R1(i, mg):
            XN = xnT[i % 3]
            bk = 5 if mg % 2 == 0 else 7
            pGt = psv(bk, [128, 4, 128])
            for mm_ in range(4):
                m = mg * 4 + mm_
                for kc in range(8):
                    mm(pGt[:, mm_, :], w_gate_b.ap[:, kc, m * 128:(m + 1) * 128], XN.ap[:, kc, :], kc == 0, kc == 7,
                       [w_gate_b, XN], [PSB[bk]])
            for mm_ in range(4):
                m = mg * 4 + mm_
                act(gates.ap[:, m, :], pGt[:, mm_, :], AF.Tanh, [PSB[bk], b_gateT], [gates],
                    bias=b_gateT.ap[:, m:m + 1], scale=0.5)

        def R2(i, half):
            YA, YB = yaT[i % 2], ybT[i % 3]
            pA = psv(5, [128, 4, 128])
            pB = psv(6, [128, 4, 128])
            for mm_ in range(4):
                m = half * 4 + mm_
                for kc in range(4):
                    mm(pA[:, mm_, :], w_bra_b.ap[:, kc, m * 128:(m + 1) * 128], YA.ap[:, kc, :], kc == 0, kc == 3,
                       [w_bra_b, YA], [PSB[5]])
            for mm_ in range(4):
                m = half * 4 + mm_
                for kc in range(4):
                    mm(pB[:, mm_, :], w_brb_b.ap[:, kc, m * 128:(m + 1) * 128], YB.ap[:, kc, :], kc == 0, kc == 3,
                       [w_brb_b, YB], [PSB[6]])
            ga = gates.ap[:, half * 4:half * 4 + 4, :]
            gb = gates.ap[:, 8 + half * 4:8 + half * 4 + 4, :]
            stt(ga, ga, 1.0, pA, ALU.add, ALU.mult, [gates, PSB[5]], [gates])
            stt(gb, gb, 1.0, pB, ALU.add, ALU.mult, [gates, PSB[6]], [gates])
            tt("dve", mergedT.ap[:, half * 4:half * 4 + 4, :], ga, gb, ALU.add, [gates], [mergedT])
            if dbg.get("tile") == i and half == 1:
                dump("mergedT", mergedT)

        def R3(i):
            for half in range(2):
                bk = 6 + half
                for kc in range(8):
                    mm(PSB[bk].ap, mergedT.ap[:, kc, :], w_out_b.ap[:, kc, half * 512:(half + 1) * 512], kc == 0, kc == 7,
                       [mergedT, w_out_b], [PSB[bk]])
                sl = slice(half * 512, (half + 1) * 512)
                tt("dve", xn2.ap[:, sl], PSB[bk].ap, gt1b.ap[:, sl], ALU.mult, [PSB[bk], gt1b], [xn2])
            tt("dve", xr.ap, xr.ap, xn2.ap, ALU.add, [xr, xn2], [xr])
            store_ops.append(dma("sp", H_d[i * 128:(i + 1) * 128, :], xr.ap, [xr], [r_H], "hst"))
            rms_rstd(xr, ssq2, rstd2, jk2.ap, jk2)
            stt(xn2.ap, xr.ap, rstd2.ap[:, 0:1], A2b.ap, ALU.mult, ALU.mult, [xr, rstd2, A2b], [xn2])
            tt("dve", xn2.ap, xn2.ap, sh2b.ap, ALU.add, [xn2, sh2b], [xn2])
            if dbg.get("tile") == i:
                dump("h", xr)
                dump("xn2", xn2)

        def R4(i):
            pX2 = psv2(6, [128, 8, 128])
            for kc in range(8):
                tr(pX2[:, kc, :], xn2.ap[:, kc * 128:(kc + 1) * 128], ident_f.ap, [xn2, ident_f], [PSB[6], PSB[7]])
            cp("act", xn2T.ap, pX2, [PSB[6], PSB[7]], [xn2T])
            pL = psv(5, [128, 36])
            for kc in range(8):
                mm(pL, xn2T.ap[:, kc, :], w_r.ap[:, kc, :], kc == 0, kc == 7, [xn2T, w_r], [PSB[5]])
            tt("dve", lg.ap, pL, b_r.ap, ALU.add, [PSB[5], b_r], [lg])
            r_ = rt
            BIG = 1.0e9
            S.add("dve", lambda e: e.tensor_reduce(out=r_["gmax"].ap, in_=lg.ap[:, 0:4], axis=AX.X, op=ALU.max), R(lg), R(r_["gmax"]))
            ts("dve", r_["maskg"].ap, lg.ap[:, 0:4], r_["gmax"].ap[:, 0:1], None, ALU.is_equal, None, [lg, r_["gmax"]], [r_["maskg"]])
            ts("dve", r_["ngmax"].ap, r_["gmax"].ap, -0.5, None, ALU.mult, None, [r_["gmax"]], [r_["ngmax"]])
            act(r_["eg"].ap, lg.ap[:, 0:4], AF.Tanh, [lg, r_["ngmax"]], [r_["eg"]], bias=r_["ngmax"].ap[:, 0:1], scale=0.5)
            ts("dve", r_["pen"].ap, r_["eg"].ap, -1.0, 1.0, ALU.mult, ALU.add, [r_["eg"]], [r_["pen"]])
            S.add("dve", lambda e: e.reciprocal(out=r_["pen"].ap, in_=r_["pen"].ap), R(r_["pen"]), R(r_["pen"]))
            stt(r_["eg"].ap, r_["eg"].ap, 1.0, r_["pen"].ap, ALU.add, ALU.mult, [r_["eg"], r_["pen"]], [r_["eg"]])
            S.add("dve", lambda e: e.tensor_reduce(out=r_["sume"].ap, in_=r_["eg"].ap, axis=AX.X, op=ALU.add), R(r_["eg"]), R(r_["sume"]))
            S.add("dve", lambda e: e.reciprocal(out=r_["pgs"].ap, in_=r_["sume"].ap), R(r_["sume"]), R(r_["pgs"]))
            ts("dve", r_["pen"].ap, r_["maskg"].ap, BIG, -BIG, ALU.mult, ALU.add, [r_["maskg"]], [r_["pen"]])
            for g in range(4):
                ts("dve", r_["lem"].ap[:, g * 8:(g + 1) * 8], lg.ap[:, 4 + g * 8:4 + (g + 1) * 8], r_["pen"].ap[:, g:g + 1], None,
                   ALU.add, None, [lg, r_["pen"]], [r_["lem"]])
            S.add("dve", lambda e: e.tensor_reduce(out=r_["m1"].ap, in_=r_["lem"].ap, axis=AX.X, op=ALU.max), R(r_["lem"]), R(r_["m1"]))
            ts("dve", r_["oh1"].ap, r_["lem"].ap, r_["m1"].ap[:, 0:1], None, ALU.is_equal, None, [r_["lem"], r_["m1"]], [r_["oh1"]])
            stt(r_["lem2"].ap, r_["oh1"].ap, -BIG, r_["lem"].ap, ALU.mult, ALU.add, [r_["oh1"], r_["lem"]], [r_["lem2"]])
            S.add("dve", lambda e: e.tensor_reduce(out=r_["m2"].ap, in_=r_["lem2"].ap, axis=AX.X, op=ALU.max), R(r_["lem2"]), R(r_["m2"]))
            ts("dve", r_["oh2"].ap, r_["lem2"].ap, r_["m2"].ap[:, 0:1], None, ALU.is_equal, None, [r_["lem2"], r_["m2"]], [r_["oh2"]])
            tt("dve", r_["dm"].ap, r_["m1"].ap, r_["m2"].ap, ALU.subtract, [r_["m1"], r_["m2"]], [r_["dm"]])
            act(r_["e2"].ap, r_["dm"].ap, AF.Tanh, [r_["dm"]], [r_["e2"]], scale=0.5)
            ts("dve", r_["p1"].ap, r_["e2"].ap, 0.5, 0.5, ALU.mult, ALU.add, [r_["e2"]], [r_["p1"]])
            ts("dve", r_["p2"].ap, r_["e2"].ap, -0.5, 0.5, ALU.mult, ALU.add, [r_["e2"]], [r_["p2"]])
            tt("dve", ohb.ap, r_["oh1"].ap, r_["oh2"].ap, ALU.add, [r_["oh1"], r_["oh2"]], [ohb])
            pR = psv(5, [128, 128])
            mm(pR[:, 64:96], tri_b.ap, ohb.ap, True, True, [tri_b, ohb], [PSB[5]])
            mm(pR[:, 96:128], ones_b.ap, ohb.ap, True, True, [ones_b, ohb], [PSB[5]])
            tt("dve", r_["rank"].ap, pR[:, 64:96], base.ap, ALU.add, [PSB[5], base], [r_["rank"]])
            tt("dve", base.ap, pR[:, 96:128], base.ap, ALU.add, [PSB[5], base, r_["rank"]], [base])
            ts("dve", r_["val"].ap, r_["rank"].ap, float(CAP), None, ALU.is_lt, None, [r_["rank"]], [r_["val"]])
            tt("dve", r_["slotv"].ap, r_["rank"].ap, ecap.ap, ALU.add, [r_["rank"], ecap], [r_["slotv"]])
            stt(r_["slotv"].ap, r_["val"].ap, -4.0e6, r_["slotv"].ap, ALU.mult, ALU.add, [r_["val"], r_["slotv"]], [r_["slotv"]])
            ts("dve", r_["slotv"].ap, r_["slotv"].ap, 4.0e6, None, ALU.add, None, [r_["slotv"]], [r_["slotv"]])
            for k, ohn in enumerate(("oh1", "oh2")):
                tt("dve", r_["junk"].ap, r_[ohn].ap, r_["slotv"].ap, ALU.mult, [r_[ohn], r_["slotv"]], [r_["junk"]])
                S.add("dve", lambda e, k=k: e.tensor_reduce(out=r_["sl"].ap[:, k:k + 1], in_=r_["junk"].ap, axis=AX.X, op=ALU.add),
                      R(r_["junk"]), R(r_["sl"]))
                tt("dve", r_["junk"].ap, r_[ohn].ap, r_["val"].ap, ALU.mult, [r_[ohn], r_["val"]], [r_["junk"]])
                S.add("dve", lambda e, k=k: e.tensor_reduce(out=r_["vk"].ap[:, k:k + 1], in_=r_["junk"].ap, axis=AX.X, op=ALU.add),
                      R(r_["junk"]), R(r_["vk"]))
            cp("dve", sloti.ap[:, i, :], r_["sl"].ap, [r_["sl"]], [sloti])
            stt(wgt.ap[:, i, 0:1], r_["p1"].ap, r_["pgs"].ap[:, 0:1], r_["vk"].ap[:, 0:1], ALU.mult, ALU.mult,
                [r_["p1"], r_["pgs"], r_["vk"]], [wgt])
            stt(wgt.ap[:, i, 1:2], r_["p2"].ap, r_["pgs"].ap[:, 0:1], r_["vk"].ap[:, 1:2], ALU.mult, ALU.mult,
                [r_["p2"], r_["pgs"], r_["vk"]], [wgt])
            if dbg.get("tile") == i:
                dump("lg", lg)
                dump("rank", r_["rank"])
            for k in range(2):
                scat_ops.append(S.add("pool", lambda e, i=i, k=k: e.indirect_dma_start(
                    out=X_d, out_offset=bass.IndirectOffsetOnAxis(ap=sloti.ap[:, i, k:k + 1], axis=0),
                    in_=xn2.ap, in_offset=None, bounds_check=breg(e, NSLOT - 1), oob_is_err=False),
                    R(xn2, sloti), [r_X], dma_key="scat", cost=1.2, lat=4.0))

        import os as _os
        _skip = set(_os.environ.get('QSKIP', '').split(','))
        _w = lambda f, n: (lambda *a: None) if n in _skip else f
        Qpre, QB, QD, QC, Qcarry, Qtail = _w(Qpre, 'pre'), _w(QB, 'B'), _w(QD, 'D'), _w(QC, 'C'), _w(Qcarry, 'carry'), _w(Qtail, 'tail')
        for s_ in range(NTILES + 2):
            ip, iq, ir = s_, s_ - 1, s_ - 2
            hp = 0 <= ip < NTILES
            hq = 0 <= iq < NTILES
            hr = 0 <= ir < NTILES
            if hr:
                R0(ir)
            if hq:
                Qpre(iq)
                QB(iq, 0)
            if hr:
                R1(ir, 0)
                R1(ir, 1)
            if hp:
                P1(ip)
            if hq:
                QD(iq, 0)
            if hr:
                R1(ir, 2)
                R1(ir, 3)
                R2(ir, 0)
                R2(ir, 1)
            if hq:
                QB(iq, 1)
                QC(iq, 0)
            if hp:
                P2(ip)
            if hq:
                QD(iq, 1)
            if hr:
                R3(ir)
            if hq:
                QB(iq, 2)
                QC(iq, 1)
                QD(iq, 2)
            if hr:
                R4(ir)
            if hq:
                QB(iq, 3)
                QC(iq, 2)
            if hp:
                P3(ip)
            if hq:
                QD(iq, 3)
                QC(iq, 3)
                Qcarry(iq)
                Qtail(iq)
        if "route" in dbg:
            dump("wgt", wgt)
            dump("sloti", sloti)
        if stop_after == "A":
            S.finish(final_ops + store_ops + scat_ops)
            S.emit(st)
            return nc, dbg_outs

        S.barrier(dma_ops=[scat_ops[-1], store_ops[-1]])
        A.reset(mark_persist)
        w1b = [A.alloc("w1b%d" % i, [128, 8, 512], BF16) for i in range(2)]
        w3b = [A.alloc("w3b%d" % i, [128, 8, 512], BF16) for i in range(2)]
        w2b = [A.alloc("w2b%d" % i, [128, 4, D], BF16) for i in range(2)]
        Xblk = [A.alloc("Xblk%d" % i, [128, D], BF16) for i in range(3)]
        XT = [A.alloc("XT%d" % i, [128, 8, CAP], BF16) for i in range(2)]
        hidT = [A.alloc("hidT%d" % i, [128, 4, CAP], BF16) for i in range(2)]
        s1 = [A.alloc("s1_%d" % i, [128, 512], F32) for i in range(2)]
        Yblk = [A.alloc("Yblk%d" % i, [128, D], F32) for i in range(3)]
        NH = (CAP + 511) // 512
        HW_ = CAP // NH
        ystore = []
        cnt = {"x": 0, "y": 0, "h": 0}

        def W13(e_):
            sl_ = e_ % 2
            dma("pool", w1b[sl_].ap, w1_d[e_].rearrange("(kc p) n -> p kc n", p=128), [], [w1b[sl_]], "w1b%d" % sl_)
            dma("pool", w3b[sl_].ap, w3_d[e_].rearrange("(kc p) n -> p kc n", p=128), [], [w3b[sl_]], "w3b%d" % sl_)

        def W2(e_):
            sl_ = e_ % 2
            dma("pool", w2b[sl_].ap, w2_d[e_].rearrange("(kc p) n -> p kc n", p=128), [], [w2b[sl_]], "w2b%d" % sl_)

        def TX(e_):
            xt_ = XT[e_ % 2]
            for blk in range(NBLK):
                n_ = cnt["x"]
                cnt["x"] += 1
                xb_ = Xblk[n_ % 3]
                r0 = e_ * CAP + blk * 128
                dma("sp", xb_.ap, X_d[r0:r0 + 128, :], [r_X], [xb_], "xblk%d" % (n_ % 3))
                bk = n_ % 2
                pXT = psv(bk, [128, 8, 128], BF16)
                for kc in range(8):
                    tr(pXT[:, kc, :], xb_.ap[:, kc * 128:(kc + 1) * 128], ident_b.ap, [xb_, ident_b], [PSB[bk]])
                cp("act" if blk % 2 == 0 else "dve", xt_.ap[:, :, blk * 128:(blk + 1) * 128], pXT, [PSB[bk]], [xt_])

        def HH(e_):
            sl_ = e_ % 2
            xt_, hd_ = XT[e_ % 2], hidT[e_ % 2]
            for m in range(4):
                for nh in range(NH):
                    cs_ = slice(nh * HW_, (nh + 1) * HW_)
                    n_ = cnt["h"]
                    cnt["h"] += 1
                    b1 = 2 + n_ % 2
                    b3 = 4 + n_ % 2
                    p1_ = PSB[b1].ap[:, 0:HW_]
                    p3_ = PSB[b3].ap[:, 0:HW_]
                    for kc in range(8):
                        mm(p1_, w1b[sl_].ap[:, kc, m * 128:(m + 1) * 128], xt_.ap[:, kc, cs_], kc == 0, kc == 7,
                           [w1b[sl_], xt_], [PSB[b1]])
                    for kc in range(8):
                        mm(p3_, w3b[sl_].ap[:, kc, m * 128:(m + 1) * 128], xt_.ap[:, kc, cs_], kc == 0, kc == 7,
                           [w3b[sl_], xt_], [PSB[b3]])
                    s1_ = s1[n_ % 2]
                    act(s1_.ap[:, 0:HW_], p1_, AF.Silu, [PSB[b1]], [s1_])
                    tt("dve", hd_.ap[:, m, cs_], s1_.ap[:, 0:HW_], p3_, ALU.mult, [s1_, PSB[b3]], [hd_])

        def YY(e_):
            sl_ = e_ % 2
            hd_ = hidT[e_ % 2]
            for blk in range(NBLK):
                n_ = cnt["y"]
                cnt["y"] += 1
                yb_ = Yblk[n_ % 3]
                for half in range(2):
                    bk = 6 + half
                    for kc in range(4):
                        mm(PSB[bk].ap, hd_.ap[:, kc, blk * 128:(blk + 1) * 128], w2b[sl_].ap[:, kc, half * 512:(half + 1) * 512],
                           kc == 0, kc == 3, [hd_, w2b[sl_]], [PSB[bk]])
                    cp("act" if half == 0 else "dve", yb_.ap[:, half * 512:(half + 1) * 512], PSB[bk].ap, [PSB[bk]], [yb_])
                r0 = e_ * CAP + blk * 128
                ystore.append(dma("sp", Y_d[r0:r0 + 128, :], yb_.ap, [yb_], [r_Y], "yst%d" % (n_ % 3)))

        W13(0)
        W2(0)
        W13(1)
        W2(1)
        TX(0)
        for e_ in range(32):
            HH(e_)
            if e_ + 2 < 32:
                W13(e_ + 2)
            if e_ + 1 < 32:
                TX(e_ + 1)
            YY(e_)
            if e_ + 2 < 32:
                W2(e_ + 2)
        if stop_after == "B":
            S.finish(final_ops + ystore[-3:])
            S.emit(st)
            return nc, dbg_outs

        S.barrier(dma_ops=ystore[-3:])
        A.reset(mark_persist)
        NBC = 3
        Hc = [A.alloc("Hc%d" % i, [128, D], F32) for i in range(NBC)]
        Y0 = [A.alloc("Y0_%d" % i, [128, D], F32) for i in range(NBC)]
        Y1 = [A.alloc("Y1_%d" % i, [128, D], F32) for i in range(NBC)]
        acc = A.alloc("acc", [128, D], F32)
        ob = [A.alloc("ob%d" % i, [128, D], F32) for i in range(2)]
        gt2b = A.alloc("gt2b", [128, D], F32)
        gfb = A.alloc("gfb", [128, D], F32)
        jk = A.alloc("jk", [128, D], BF16)
        ssq3 = A.alloc("ssq3", [128, 1], F32)
        rstd3 = A.alloc("rstd3", [128, 1], F32)
        dma("sp", gfb.ap, gf_d.partition_broadcast(128), [], [gfb], "gfb")
        for s_ in range(NBC):
            memset("pool", Y0[s_].ap, 0.0, [Y0[s_]])
            memset("pool", Y1[s_].ap, 0.0, [Y1[s_]])

        def Cload(i):
            s_ = i % NBC
            dma("sp", Hc[s_].ap, H_d[i * 128:(i + 1) * 128, :], [r_H], [Hc[s_]], "hc%d" % s_)
            for k, Yk in enumerate((Y0[s_], Y1[s_])):
                S.add("pool", lambda e, i=i, k=k, Yk=Yk: e.indirect_dma_start(
                    out=Yk.ap, out_offset=None, in_=Y_d, in_offset=bass.IndirectOffsetOnAxis(ap=sloti.ap[:, i, k:k + 1], axis=0),
                    bounds_check=breg(e, NSLOT - 1), oob_is_err=False), R(r_Y, sloti), R(Yk), dma_key="gath%d_%d" % (k, s_),
                    cost=1.2, lat=5.0)

        Cload(0)
        if NTILES > 1:
            Cload(1)
        for i in range(NTILES):
            b, tau = i // NT, i % NT
            s_ = i % NBC
            if tau == 0:
                dma("sp", gt2b.ap, mod_d.ap[b:b + 1, 5 * D:6 * D].partition_broadcast(128), [mod_d], [gt2b], "gt2b")
            if i + 2 < NTILES:
                Cload(i + 2)
            ts("dve", acc.ap, Y0[s_].ap, wgt.ap[:, i, 0:1], None, ALU.mult, None, [Y0[s_], wgt], [acc])
            stt(acc.ap, Y1[s_].ap, wgt.ap[:, i, 1:2], acc.ap, ALU.mult, ALU.add, [Y1[s_], wgt, acc], [acc])
            tt("dve", acc.ap, acc.ap, gt2b.ap, ALU.mult, [acc, gt2b], [acc])
            tt("dve", acc.ap, acc.ap, Hc[s_].ap, ALU.add, [acc, Hc[s_]], [acc])
            rms_rstd(acc, ssq3, rstd3, jk.ap, jk)
            stt(ob[i % 2].ap, acc.ap, rstd3.ap[:, 0:1], gfb.ap, ALU.mult, ALU.mult, [acc, rstd3, gfb], [ob[i % 2]])
            final_ops.append(dma("sp", out_d[i * 128:(i + 1) * 128, :], ob[i % 2].ap, [ob[i % 2]], [], "ost%d" % (i % 2)))
        S.finish(final_ops)
        S.emit(st)
    return nc, dbg_outs


def prep_shared(inp):
    f = np.float32
    g = {}
    L = 0
    g["w_ada"] = np.ascontiguousarray(inp["w_ada"][L], f)
    g["g1T"] = np.ascontiguousarray(inp["norm1_g"][L].reshape(8, 128).T, f)
    g["w_in"] = np.ascontiguousarray(inp["w_in"][L], f)
    g["w_gate"] = np.ascontiguousarray(inp["w_gate"][L], f)
    g["b_gateT"] = np.ascontiguousarray(inp["b_gate"][L].reshape(16, 128).T, f)
    a_re, a_im, ls = inp["ssm_a_re"][L], inp["ssm_a_im"][L], inp["ssm_log_step"][L]
    def sm(v):
        return v.reshape(16, 2, 64).transpose(1, 2, 0).reshape(128, 16)
    lsx = np.repeat(ls[:, None], 64, axis=1)
    g["lam_sm"] = np.ascontiguousarray(np.stack([sm(a_re), sm(a_im), sm(lsx)], axis=1), f)
    Bsm = np.zeros((128, 2, 16, 128), f)
    for pi, Bsrc in enumerate((inp["ssm_b_re"][L], inp["ssm_b_im"][L])):
        for gi in range(32):
            j = gi // 2
            n0 = 64 * (gi % 2)
            c0 = 32 * (j % 4) + 16 * (gi % 2)
            Bsm[n0:n0 + 64, pi, j, c0:c0 + 16] = Bsrc[gi]
    g["Bsm"] = Bsm
    Csm = np.zeros((128, 2, 16, 128), f)
    for pi, Csrc in enumerate((inp["ssm_c_re"][L], inp["ssm_c_im"][L])):
        for gi in range(32):
            j = gi // 2
            n0 = 64 * (gi % 2)
            c0 = 32 * (j % 4) + 16 * (gi % 2)
            Csm[n0:n0 + 64, pi, j, c0:c0 + 16] = Csrc[gi].T
    g["Csm"] = Csm
    g["dT"] = np.ascontiguousarray(inp["ssm_d"][L].reshape(4, 128).T, f)
    g["w_glu"] = np.ascontiguousarray(inp["w_glu"][L], f)
    g["b_gluT"] = np.ascontiguousarray(inp["b_glu"][L].reshape(4, 128).T, f)
    g["wsT"] = np.ascontiguousarray(inp["sgu_w"][L].transpose(2, 0, 1), f)
    g["lngT"] = np.ascontiguousarray(inp["sgu_ln_g"][L].reshape(4, 128).T, f)
    g["lnbT"] = np.ascontiguousarray(inp["sgu_ln_b"][L].reshape(4, 128).T, f)
    bs = inp["sgu_b"][L]
    bsT = np.zeros((128, 4, 128), f)
    for q in range(4):
        bsT[0:64, q, :] = bs[2 * q][None, :]
        bsT[64:128, q, :] = bs[2 * q + 1][None, :]
    g["bsT"] = bsT
    g["w_bra"] = np.ascontiguousarray(inp["w_branch_a"][L], f)
    g["w_brb"] = np.ascontiguousarray(inp["w_branch_b"][L], f)
    g["w_out"] = np.ascontiguousarray(inp["w_out"][L], f)
    g["g2"] = np.ascontiguousarray(inp["norm2_g"][L].reshape(1, D), f)
    wr = np.concatenate([inp["w_router_group"][L], inp["w_router_expert"][L].transpose(1, 0, 2).reshape(D, 32)], axis=1)
    g["w_r"] = np.ascontiguousarray(wr.reshape(8, 128, 36).transpose(1, 0, 2), f)
    g["b_r"] = np.ascontiguousarray(np.concatenate([inp["b_router_group"][L], inp["b_router_expert"][L].reshape(32)]).reshape(1, 36), f)
    g["w1"] = np.ascontiguousarray(inp["w1"][L], f)
    g["w3"] = np.ascontiguousarray(inp["w3"][L], f)
    g["w2"] = np.ascontiguousarray(inp["w2"][L], f)
    g["gf"] = np.ascontiguousarray(inp["norm_f_g"].reshape(1, D), f)
    return g


def prep_core(inp, shared, b0, nseq):
    m = dict(shared)
    xs = inp["x"][b0:b0 + nseq]
    m["x"] = np.ascontiguousarray(xs.reshape(-1, D), np.float32)
    c = inp["c"][b0:b0 + nseq]
    m["cT"] = np.ascontiguousarray(c.reshape(nseq, 8, 128).transpose(2, 1, 0), np.float32)
    m["b_ada_rep"] = np.ascontiguousarray(np.repeat(inp["b_ada"][0][None, :], nseq, axis=0), np.float32)
    return m


_CACHE = {}


def kernel(**inputs):
    inp = {k: np.asarray(v) for k, v in inputs.items()}
    B, SEQ = inp["x"].shape[0], inp["x"].shape[1]
    nseq = B // N_CORES
    key = (nseq, SEQ)
    if key not in _CACHE:
        _CACHE[key] = build(NSEQ=nseq, SEQ=SEQ, CAP=768)[0]
    nc = _CACHE[key]
    shared = prep_shared(inp)
    in_maps = [prep_core(inp, shared, c * nseq, nseq) for c in range(N_CORES)]
    res = run_bass_kernel_spmd(nc, in_maps, core_ids=list(range(N_CORES)))
    outs = [np.asarray(r["out"]).reshape(nseq, SEQ, D) for r in res.results]
    return np.concatenate(outs, axis=0).astype(np.float32)
```

```python
import math
from contextlib import ExitStack
import numpy as np
import concourse.bass as bass
import concourse.mybir as mybir
from concourse.bass_utils import run_bass_kernel_spmd

F32 = mybir.dt.float32
BF16 = mybir.dt.bfloat16
I32 = mybir.dt.int32
U8 = mybir.dt.uint8
AF = mybir.ActivationFunctionType
ALU = mybir.AluOpType
AX = mybir.AxisListType

ENGS = ("pe", "act", "dve", "pool", "sp")
N_CORES = 8
D = 1024
TWO_PI = 2.0 * math.pi
RMS_EPS = 1e-6
LN_EPS = 1e-5


class Res:
    __slots__ = ("name", "lastw", "readers")

    def __init__(self, name):
        self.name = name
        self.lastw = None
        self.readers = []


class Op:
    __slots__ = ("eng", "fn", "deps", "sdeps", "dma_key", "dma_val", "signal", "sig_idx", "cost", "lat", "idx", "succ", "nin", "fin")

    def __init__(self, eng, fn, dma_key):
        self.eng = eng
        self.fn = fn
        self.deps = []
        self.sdeps = []
        self.dma_key = dma_key
        self.dma_val = None
        self.signal = False
        self.sig_idx = None
        self.cost = 0.2
        self.lat = 0.0


class Sched:
    def __init__(self, nc):
        self.nc = nc
        self.ops = {e: [] for e in ENGS}
        self.all = []
        self.finals = []
        self.pending_barrier = {}
        import os as _o
        self.reorder = _o.environ.get("NOREORDER") is None

    def add(self, eng, fn, reads=(), writes=(), dma_key=None, cost=None, lat=0.0):
        op = Op(eng, fn, dma_key)
        if cost is not None:
            op.cost = cost
        op.lat = lat
        deps = []
        for r in reads:
            if r.lastw is not None:
                deps.append(r.lastw)
        for w in writes:
            if w.lastw is not None:
                deps.append(w.lastw)
            deps.extend(w.readers)
        if eng in self.pending_barrier:
            deps.extend(self.pending_barrier.pop(eng))
        seen = set()
        for d in deps:
            if d is op or id(d) in seen:
                continue
            seen.add(id(d))
            if d.eng == "pe" and eng == "pe" and d.dma_key is None and dma_key is None:
                op.sdeps.append(d)
                continue
            op.deps.append(d)
        for r in reads:
            r.readers.append(op)
        for w in writes:
            w.lastw = op
            w.readers = []
        op.idx = len(self.all)
        self.all.append(op)
        self.ops[eng].append(op)
        return op

    def barrier(self, dma_ops=()):
        lasts = [self.ops[e][-1] for e in ENGS if self.ops[e]]
        lasts = [o for o in lasts if o.dma_key is None] + list(dma_ops)
        for e in ENGS:
            self.pending_barrier.setdefault(e, []).extend(lasts)

    def finish(self, ops):
        self.finals.extend(ops)

    def schedule(self):
        import heapq
        ops = self.all
        for o in ops:
            o.succ = []
            o.nin = 0
        for o in ops:
            for d in o.deps + o.sdeps:
                d.succ.append(o)
                o.nin += 1
        import os as _o2
        SYNC = float(_o2.environ.get("SYNC", "1.0"))
        wait_h = {e: [] for e in ENGS}
        t_eng = {e: 0.0 for e in ENGS}
        order = {e: [] for e in ENGS}
        ready_at = {}
        for o in ops:
            if o.nin == 0:
                heapq.heappush(wait_h[o.eng], (0.0, o.idx))
        placed = 0
        n = len(ops)
        while placed < n:
            best = None
            for e in ENGS:
                h = wait_h[e]
                if not h:
                    continue
                te = t_eng[e]
                if h[0][0] <= te:
                    cand_idx = min(i for (r, i) in h if r <= te)
                    st = te
                else:
                    st, cand_idx = h[0]
                if best is None or (st, cand_idx) < (best[0], best[1]):
                    best = (st, cand_idx, e)
            st, ci, e = best
            h = wait_h[e]
            for k, (r, i) in enumerate(h):
                if i == ci:
                    h[k] = h[-1]
                    h.pop()
                    break
            heapq.heapify(h)
            o = ops[ci]
            t_eng[e] = st + o.cost
            o.fin = st + o.cost + o.lat
            order[e].append(o)
            placed += 1
            for sct in o.succ:
                sct.nin -= 1
                ra = max(ready_at.get(sct.idx, 0.0), o.fin + (SYNC if o.eng != sct.eng or o.dma_key is not None else 0.0))
                ready_at[sct.idx] = ra
                if sct.nin == 0:
                    heapq.heappush(wait_h[sct.eng], (ra, sct.idx))
        self.ops = order
        self.sim_time = max(t_eng.values())

    def emit(self, stack):
        nc = self.nc
        if self.reorder:
            self.schedule()
        for e in ENGS:
            for op in self.ops[e]:
                for d in op.deps:
                    if d.dma_key is None:
                        d.signal = True
        dma_cnt = {}
        for e in ENGS:
            for op in self.ops[e]:
                if op.dma_key is not None:
                    dma_cnt[op.dma_key] = dma_cnt.get(op.dma_key, 0) + 16
                    op.dma_val = dma_cnt[op.dma_key]
        sems = {e: stack.enter_context(nc.semaphore("sem_" + e)) for e in ENGS}
        dsem = {k: stack.enter_context(nc.semaphore("dsem_%s" % (k,))) for k in dma_cnt}
        for e in ENGS:
            n = 0
            for op in self.ops[e]:
                if op.dma_key is None and op.signal:
                    n += 1
                    op.sig_idx = n
        block = stack.enter_context(nc.Block())
        engobj = {"pe": "tensor", "act": "scalar", "dve": "vector", "pool": "gpsimd", "sp": "sync"}
        finals = self.finals

        def body_for(e):
            def body(eng):
                seen = {}
                for op in self.ops[e]:
                    need = {}
                    for d in op.deps:
                        if d.dma_key is not None:
                            s, v, key = dsem[d.dma_key], d.dma_val, ("d", d.dma_key)
                        else:
                            s, v, key = sems[d.eng], d.sig_idx, ("e", d.eng)
                        if key not in need or need[key][1] < v:
                            need[key] = (s, v)
                    for key, (s, v) in need.items():
                        if seen.get(key, 0) >= v:
                            continue
                        seen[key] = v
                        eng.wait_ge(s, v)
                    inst = op.fn(eng)
                    if op.dma_key is not None:
                        inst.then_inc(dsem[op.dma_key], 16)
                    elif op.signal:
                        inst.then_inc(sems[e], 1)
                if e == "sp":
                    for d in finals:
                        eng.wait_ge(dsem[d.dma_key], d.dma_val)
            return body

        for e in ENGS:
            getattr(block, engobj[e])(body_for(e))


class Tl:
    __slots__ = ("ap", "r")

    def __init__(self, ap, name):
        self.ap = ap
        self.r = Res(name)


class Arena:
    def __init__(self, nc, stack, nbytes):
        self.buf = stack.enter_context(nc.sbuf_tensor("arena", [128, nbytes], U8))
        self.off = 0
        self.cap = nbytes
        self.live = []

    def alloc(self, name, shape, dt):
        esz = {F32: 4, BF16: 2, I32: 4}[dt]
        n = 1
        for s in shape[1:]:
            n *= s
        nb = (n * esz + 31) // 32 * 32
        assert self.off + nb <= self.cap, "SBUF arena overflow at %s: %d + %d > %d" % (name, self.off, nb, self.cap)
        v = self.buf[0:shape[0], self.off:self.off + n * esz].bitcast(dt)
        if len(shape) == 3:
            v = v.rearrange("p (a b) -> p a b", a=shape[1])
        elif len(shape) == 4:
            v = v.rearrange("p (a b c) -> p a b c", a=shape[1], b=shape[2])
        t = Tl(v, name)
        lo, hi = self.off, self.off + nb
        keep = []
        for (a, b, o) in self.live:
            if a < hi and lo < b:
                if o.r.lastw is not None:
                    t.r.readers.append(o.r.lastw)
                t.r.readers.extend(o.r.readers)
                if a >= lo and b <= hi:
                    continue
            keep.append((a, b, o))
        keep.append((lo, hi, t))
        self.live = keep
        self.off += nb
        return t

    def mark(self):
        return self.off

    def reset(self, m):
        self.off = m


def build(NSEQ=4, SEQ=2048, CAP=768, dbg=None, stop_after=None):
    nc = bass.Bass("TRN2", target_bir_lowering=False)
    NT = SEQ // 128
    NTOK = NSEQ * SEQ
    NTILES = NSEQ * NT
    NSLOT = 32 * CAP
    NBLK = CAP // 128
    dbg = dbg or {}
    dbg_outs = {}

    def din(name, shape, dt=F32):
        return nc.dram_tensor(name, list(shape), dt, kind="ExternalInput").ap()

    x_d = din("x", [NTOK, D])
    cT_d = din("cT", [128, 8, NSEQ])
    w_ada_d = din("w_ada", [D, 6 * D])
    b_ada_d = din("b_ada_rep", [NSEQ, 6 * D])
    g1T_d = din("g1T", [128, 8])
    w_in_d = din("w_in", [D, 1536])
    w_gate_d = din("w_gate", [D, 2048])
    b_gateT_d = din("b_gateT", [128, 16])
    lam_sm_d = din("lam_sm", [128, 3, 16])
    Bsm_d = din("Bsm", [128, 2, 16, 128])
    Csm_d = din("Csm", [128, 2, 16, 128])
    dT_d = din("dT", [128, 4])
    w_glu_d = din("w_glu", [512, 512])
    b_gluT_d = din("b_gluT", [128, 4])
    wsT_d = din("wsT", [128, 8, 128])
    lngT_d = din("lngT", [128, 4])
    lnbT_d = din("lnbT", [128, 4])
    bsT_d = din("bsT", [128, 4, 128])
    w_bra_d = din("w_bra", [512, D])
    w_brb_d = din("w_brb", [512, D])
    w_out_d = din("w_out", [D, D])
    g2_d = din("g2", [1, D])
    w_r_d = din("w_r", [128, 8, 36])
    b_r_d = din("b_r", [1, 36])
    w1_d = din("w1", [32, D, 512])
    w3_d = din("w3", [32, D, 512])
    w2_d = din("w2", [32, 512, D])
    gf_d = din("gf", [1, D])
    out_d = nc.dram_tensor("out", [NTOK, D], F32, kind="ExternalOutput").ap()
    mod_d = Tl(nc.dram_tensor("mod_d", [NSEQ, 6 * D], F32, kind="Internal").ap(), "mod_d")
    H_d = nc.dram_tensor("H_d", [NTOK, D], F32, kind="Internal").ap()
    X_d = nc.dram_tensor("X_d", [NSLOT, D], BF16, kind="Internal").ap()
    Y_d = nc.dram_tensor("Y_d", [NSLOT, D], F32, kind="Internal").ap()
    r_X = Res("X_d")
    r_Y = Res("Y_d")
    r_H = Res("H_d")

    S = Sched(nc)
    final_ops = []
    with ExitStack() as st:
        A = Arena(nc, st, 212800)
        ps_all = st.enter_context(nc.psum_tensor("ps_all", [128, 8, 512], F32))
        PSB = [Tl(ps_all[:, b, :], "psb%d" % b) for b in range(8)]

        def psv(b, shape, dt=F32):
            v = PSB[b].ap
            if dt == BF16:
                v = v.bitcast(BF16)
            n = 1
            for s in shape[1:]:
                n *= s
            v = v[0:shape[0], 0:n]
            if len(shape) == 3:
                v = v.rearrange("p (a b) -> p a b", a=shape[1])
            elif len(shape) == 4:
                v = v.rearrange("p (a b c) -> p a b c", a=shape[1], b=shape[2])
            return v

        def psv2(b, shape):
            v = ps_all[:, b:b + 2, :].rearrange("p a b -> p (a b)")
            n = 1
            for s in shape[1:]:
                n *= s
            v = v[0:shape[0], 0:n]
            if len(shape) == 3:
                v = v.rearrange("p (a b) -> p a b", a=shape[1])
            elif len(shape) == 4:
                v = v.rearrange("p (a b c) -> p a b c", a=shape[1], b=shape[2])
            return v

        def R(*ts):
            return [t.r if isinstance(t, Tl) else t for t in ts]

        def fsz(ap):
            n = 1
            for s_ in ap.shape[1:]:
                n *= s_
            return n

        def isz(ap):
            return 2 if ap.dtype == BF16 else 4

        def ecost(eng, n):
            if eng == "dve":
                return 0.1 + n / 900.0
            if eng == "pool":
                return 0.25 + n / 430.0
            return 0.2 + n / 1150.0

        def dma(eng, out, in_, reads, writes, key, **kw):
            nb = out.shape[0] * fsz(out) * isz(out)
            return S.add(eng, lambda e: e.dma_start(out=out, in_=in_, **kw), R(*reads), R(*writes), dma_key=key,
                         cost=(1.0 if eng == "pool" else 0.07), lat=2.0 + nb / 180e3)

        def tt(eng, out, in0, in1, op, reads, writes):
            c = 0.45 if op == ALU.pow else ecost(eng, fsz(out))
            return S.add(eng, lambda e: e.tensor_tensor(out=out, in0=in0, in1=in1, op=op), R(*reads), R(*writes), cost=c)

        def ts(eng, out, in0, s1, s2, op0, op1, reads, writes, accum=None):
            c = ecost(eng, fsz(out))
            if op1 is None:
                return S.add(eng, lambda e: e.tensor_scalar(out=out, in0=in0, scalar1=s1, scalar2=None, op0=op0), R(*reads), R(*writes), cost=c)
            if accum is not None:
                return S.add(eng, lambda e: e.tensor_scalar(out=out, in0=in0, scalar1=s1, scalar2=s2, op0=op0, op1=op1, accum_out=accum), R(*reads), R(*writes), cost=c)
            return S.add(eng, lambda e: e.tensor_scalar(out=out, in0=in0, scalar1=s1, scalar2=s2, op0=op0, op1=op1), R(*reads), R(*writes), cost=c)

        def stt(out, in0, scalar, in1, op0, op1, reads, writes):
            return S.add("dve", lambda e: e.scalar_tensor_tensor(out=out, in0=in0, scalar=scalar, in1=in1, op0=op0, op1=op1), R(*reads), R(*writes),
                         cost=ecost("dve", fsz(out)))

        def act(out, in_, func, reads, writes, bias=None, scale=None, accum=None):
            kw = {}
            if bias is not None:
                kw["bias"] = bias
            if scale is not None:
                kw["scale"] = scale
            if accum is not None:
                kw["accum_out"] = accum
            return S.add("act", lambda e: e.activation(out=out, in_=in_, func=func, **kw), R(*reads), R(*writes),
                         cost=ecost("act", fsz(out)) + (0.1 if accum is not None else 0.0))

        def cp(eng, out, in_, reads, writes):
            c = ecost(eng, fsz(out))
            if eng == "act":
                return S.add("act", lambda e: e.copy(out=out, in_=in_), R(*reads), R(*writes), cost=c)
            return S.add(eng, lambda e: e.tensor_copy(out=out, in_=in_), R(*reads), R(*writes), cost=c)

        def mm(out, lhsT, rhs, start, stop, reads, writes):
            c = 0.03 + fsz(rhs) / 2400.0 * (4.0 if lhsT.dtype == F32 else 1.0)
            return S.add("pe", lambda e: e.matmul(out, lhsT=lhsT, rhs=rhs, start=start, stop=stop), R(*reads), R(*writes), cost=c)

        def tr(out, in_, ident, reads, writes):
            c = 0.03 + 128 / 2400.0 * (2.0 if in_.dtype == F32 else 1.0)
            return S.add("pe", lambda e: e.transpose(out=out, in_=in_, identity=ident), R(*reads), R(*writes), cost=c)

        def memset(eng, ap, val, writes):
            return S.add(eng, lambda e: e.memset(ap, val), [], R(*writes))

        _regs = {}

        def breg(e, val):
            if val not in _regs:
                _regs[val] = e.to_reg(val)
            return _regs[val]

        def dump(name, t, ap=None):
            ap = t.ap if ap is None else ap
            shp = list(ap.shape)
            o = nc.dram_tensor("dbg_" + name, shp, ap.dtype, kind="ExternalOutput").ap()
            dbg_outs[name] = "dbg_" + name
            final_ops.append(dma("sp", o, ap, [t], [], "dbg_" + name))

        ident_f = A.alloc("ident_f", [128, 128], F32)
        ident_b = A.alloc("ident_b", [128, 128], BF16)
        tri_b = A.alloc("tri_b", [128, 128], BF16)
        ones_b = A.alloc("ones_b", [128, 128], BF16)
        memset("pool", ident_f.ap, 0.0, [ident_f])
        S.add("pool", lambda e: e.affine_select(out=ident_f.ap, in_=ident_f.ap, pattern=[[-1, 128]], compare_op=ALU.not_equal,
                                                  fill=1.0, base=0, channel_multiplier=1), R(ident_f), R(ident_f))
        cp("pool", ident_b.ap, ident_f.ap, [ident_f], [ident_b])
        memset("pool", ones_b.ap, 1.0, [ones_b])
        S.add("pool", lambda e: e.affine_select(out=tri_b.ap, in_=ones_b.ap, pattern=[[1, 128]], compare_op=ALU.is_gt,
                                                  fill=0.0, base=0, channel_multiplier=-1), R(ones_b), R(tri_b))

        wgt = A.alloc("wgt", [128, NTILES, 2], F32)
        sloti = A.alloc("sloti", [128, NTILES, 2], I32)
        base = A.alloc("base", [128, 32], F32)
        ecap = A.alloc("ecap", [128, 32], F32)
        memset("pool", base.ap, 0.0, [base])
        ecap_i = A.alloc("ecap_i", [128, 32], I32)
        S.add("pool", lambda e: e.iota(ecap_i.ap, pattern=[[CAP, 32]], base=0, channel_multiplier=0), [], R(ecap_i))
        cp("pool", ecap.ap, ecap_i.ap, [ecap_i], [ecap])
        A1T = A.alloc("A1T", [128, 8, NSEQ], F32)
        sh1T = A.alloc("sh1T", [128, 8, NSEQ], F32)
        eps_rms = A.alloc("eps_rms", [128, 1], F32)
        eps_ln = A.alloc("eps_ln", [128, 1], F32)
        memset("pool", eps_rms.ap, 1e-6, [eps_rms])
        memset("pool", eps_ln.ap, 1e-5, [eps_ln])
        mhalf = A.alloc("mhalf", [128, 1], F32)
        memset("pool", mhalf.ap, -0.5, [mhalf])

        mark_persist = A.mark()

        cact = A.alloc("cact", [128, 8, NSEQ], F32)
        dma("sp", cact.ap, cT_d, [], [cact], "cact")
        act(cact.ap, cact.ap, AF.Silu, [cact], [cact])
        modrow = A.alloc("modrow", [NSEQ, 6 * D], F32)
        bada = A.alloc("bada", [NSEQ, 6 * D], F32)
        dma("sp", bada.ap, b_ada_d, [], [bada], "bada")
        wa = [A.alloc("wa%d" % i, [128, 8, 512], F32) for i in range(2)]
        wa_view = w_ada_d.rearrange("(kc p) n -> p kc n", p=128)
        for cb in range(12):
            w = wa[cb % 2]
            dma("sp", w.ap, wa_view[:, :, cb * 512:(cb + 1) * 512], [], [w], "wa%d" % (cb % 2))
            pb = PSB[cb % 2]
            for kc in range(8):
                mm(pb.ap[0:NSEQ, :], cact.ap[:, kc, :], w.ap[:, kc, :], kc == 0, kc == 7, [cact, w], [pb])
            tt("dve", modrow.ap[:, cb * 512:(cb + 1) * 512], pb.ap[0:NSEQ, :], bada.ap[:, cb * 512:(cb + 1) * 512], ALU.add,
               [pb, bada], [modrow])
        dma("sp", mod_d.ap, modrow.ap, [modrow], [mod_d], "mod_d")
        sc1T = A.alloc("sc1T", [128, 8, NSEQ], F32)
        g1T = A.alloc("g1T", [128, 8], F32)
        dma("sp", g1T.ap, g1T_d, [], [g1T], "g1T")
        for b in range(NSEQ):
            S.add("sp", lambda e, b=b: e.dma_start(out=sh1T.ap[:, :, b], in_=mod_d.ap[b, 0:D].rearrange("(kc p) -> p kc", p=128),
                                                  allow_slow_non_contiguous=True), R(mod_d), R(sh1T), dma_key="sh1T")
            S.add("sp", lambda e, b=b: e.dma_start(out=sc1T.ap[:, :, b], in_=mod_d.ap[b, D:2 * D].rearrange("(kc p) -> p kc", p=128),
                                                  allow_slow_non_contiguous=True), R(mod_d), R(sc1T), dma_key="sc1T")
        for b in range(NSEQ):
            stt(A1T.ap[:, :, b], sc1T.ap[:, :, b], 1.0, g1T.ap, ALU.add, ALU.mult, [sc1T, g1T], [A1T])
        if "mod" in dbg:
            dump("modrow", modrow)
            dump("A1T", A1T)
        A.reset(mark_persist)
        S.barrier()
        if stop_after == "mod":
            S.finish(final_ops)
            S.emit(st)
            return nc, dbg_outs

        w_r = A.alloc("w_r", [128, 8, 36], F32)
        dma("sp", w_r.ap, w_r_d, [], [w_r], "w_r")
        b_r = A.alloc("b_r", [128, 36], F32)
        dma("sp", b_r.ap, b_r_d.partition_broadcast(128), [], [b_r], "b_r")
        b_gateT = A.alloc("b_gateT", [128, 16], F32)
        dma("sp", b_gateT.ap, b_gateT_d, [], [b_gateT], "b_gateT")
        ts("pool", b_gateT.ap, b_gateT.ap, 0.5, 1.0, ALU.mult, ALU.mult, [b_gateT], [b_gateT])
        b_gluT = A.alloc("b_gluT", [128, 4], F32)
        dma("sp", b_gluT.ap, b_gluT_d, [], [b_gluT], "b_gluT")
        ts("pool", b_gluT.ap, b_gluT.ap, 0.5, 1.0, ALU.mult, ALU.mult, [b_gluT], [b_gluT])
        dT = A.alloc("dT", [128, 4], F32)
        dma("sp", dT.ap, dT_d, [], [dT], "dT")
        lngT = A.alloc("lngT", [128, 4], F32)
        dma("sp", lngT.ap, lngT_d, [], [lngT], "lngT")
        lnbT = A.alloc("lnbT", [128, 4], F32)
        dma("sp", lnbT.ap, lnbT_d, [], [lnbT], "lnbT")

        LC = 4
        NCH = 128 // LC
        tabC4 = A.alloc("tabC4", [128, 16, NCH + 1], F32)
        tabD4 = A.alloc("tabD4", [128, 16, NCH + 1], F32)
        Rtab = A.alloc("Rtab", [128, 16, 2, NCH], F32)
        rmagL = A.alloc("rmagL", [128, 16], F32)
        Pt = A.alloc("Pt", [128, 2, 8 * LC, 128], BF16)
        Qs = A.alloc("Qs", [128, 2, 16 * LC, 64], BF16)
        Kb = A.alloc("Kb", [128, 4 * LC, 128], BF16)
        tabC = Tl(None, "s5scr")
        mark_setup = A.mark()

        def range_reduce(ph, tmpf, tmpi, n):
            ts("dve", tmpi, ph, 1.0 / TWO_PI, None, ALU.mult, None, [tabC], [tabC])
            cp("dve", tmpf, tmpi, [tabC], [tabC])
            stt(ph, tmpf, -TWO_PI, ph, ALU.mult, ALU.add, [tabC], [tabC])
            wrap(ph, tmpf)

        def wrap(ph, tmpf):
            ts("dve", tmpf, ph, math.pi, None, ALU.is_gt, None, [tabC], [tabC])
            stt(ph, tmpf, -TWO_PI, ph, ALU.mult, ALU.add, [tabC], [tabC])
            ts("dve", tmpf, ph, -math.pi, None, ALU.is_lt, None, [tabC], [tabC])
            stt(ph, tmpf, TWO_PI, ph, ALU.mult, ALU.add, [tabC], [tabC])

        def scr(name, shape, dt):
            t = A.alloc(name, shape, dt)
            tabC.r.readers.extend(t.r.readers)
            t.r = tabC.r
            for k_, (a_, b_, o_) in enumerate(A.live):
                if o_ is t:
                    A.live[k_] = (a_, b_, t)
            return t

        TC = [tabC]
        lam = scr("lam", [128, 3, 16], F32)
        dma("sp", lam.ap, lam_sm_d, [], TC, "lam")
        zs = {n: scr("z_" + n, [128, 16], F32) for n in ("dt", "th", "lre", "lrdt", "den", "nr", "fre", "fim", "t0", "t1")}
        sv_i = scr("sv_i", [128, 129], I32)
        sv = scr("sv", [128, 129], F32)
        tabCf = scr("tabCf", [128, 16, 129], F32)
        tabDf = scr("tabDf", [128, 16, 129], F32)
        tmpf = scr("tmpf", [128, 16 * 129], F32)
        tmpi = scr("tmpi", [128, 16 * 129], I32)
        aim = lam.ap[:, 1, :]
        S.add("pool", lambda e: e.iota(sv_i.ap, pattern=[[1, 129]], base=0, channel_multiplier=0), [], R(tabC))
        cp("dve", sv.ap, sv_i.ap, TC, TC)
        ts("dve", zs["lre"].ap, lam.ap[:, 0, :], -1e-4, None, ALU.min, None, TC, TC)
        act(zs["dt"].ap, lam.ap[:, 2, :], AF.Exp, TC, TC)
        tt("dve", zs["lrdt"].ap, zs["lre"].ap, zs["dt"].ap, ALU.mult, TC, TC)
        tt("dve", zs["th"].ap, aim, zs["dt"].ap, ALU.mult, TC, TC)
        for j in range(16):
            ts("dve", tabDf.ap[:, j, :], sv.ap, zs["th"].ap[:, j:j + 1], None, ALU.mult, None, TC, TC)
        phD = tabDf.ap.rearrange("p a b -> p (a b)")
        phC = tabCf.ap.rearrange("p a b -> p (a b)")
        range_reduce(phD, tmpf.ap, tmpi.ap, 16 * 129)
        ts("dve", phC, phD, math.pi / 2, None, ALU.add, None, TC, TC)
        wrap(phC, tmpf.ap)
        act(phD, phD, AF.Sin, TC, TC)
        act(phC, phC, AF.Sin, TC, TC)
        cp("dve", tabC4.ap, tabCf.ap[:, :, 0:129:LC], TC, TC + [tabC4])
        cp("dve", tabD4.ap, tabDf.ap[:, :, 0:129:LC], TC, TC + [tabD4])
        rk = scr("rk", [128, 16, LC + 1], F32)
        pwr = scr("pwr", [128, 16, LC + 1], F32)
        pwi = scr("pwi", [128, 16, LC + 1], F32)
        for k in range(LC + 1):
            act(rk.ap[:, :, k], zs["lrdt"].ap, AF.Exp, TC, TC, scale=float(k))
        tt("dve", pwr.ap, rk.ap, tabCf.ap[:, :, 0:LC + 1], ALU.mult, TC, TC)
        tt("dve", pwi.ap, rk.ap, tabDf.ap[:, :, 0:LC + 1], ALU.mult, TC, TC)
        cp("dve", rmagL.ap, rk.ap[:, :, LC], TC, TC + [rmagL])
        cp("dve", Rtab.ap.rearrange("p a b c -> p a (b c)"), rmagL.ap.unsqueeze(2).to_broadcast([128, 16, 2 * NCH]),
           TC + [rmagL], TC + [Rtab])
        memset("dve", Rtab.ap[:, :, :, 0], 0.0, TC + [Rtab])
        abr, abi = pwr.ap[:, :, 1], pwi.ap[:, :, 1]
        lre_ = zs["lre"].ap
        tt("dve", zs["den"].ap, lre_, lre_, ALU.mult, TC, TC)
        tt("dve", zs["t0"].ap, aim, aim, ALU.mult, TC, TC)
        tt("dve", zs["den"].ap, zs["den"].ap, zs["t0"].ap, ALU.add, TC, TC)
        S.add("dve", lambda e: e.reciprocal(out=zs["den"].ap, in_=zs["den"].ap), R(tabC), R(tabC))
        ts("dve", zs["nr"].ap, abr, -1.0, None, ALU.add, None, TC, TC)
        tt("dve", zs["t0"].ap, zs["nr"].ap, lre_, ALU.mult, TC, TC)
        tt("dve", zs["t1"].ap, abi, aim, ALU.mult, TC, TC)
        tt("dve", zs["t0"].ap, zs["t0"].ap, zs["t1"].ap, ALU.add, TC, TC)
        tt("dve", zs["fre"].ap, zs["t0"].ap, zs["den"].ap, ALU.mult, TC, TC)
        tt("dve", zs["t0"].ap, abi, lre_, ALU.mult, TC, TC)
        tt("dve", zs["t1"].ap, zs["nr"].ap, aim, ALU.mult, TC, TC)
        tt("dve", zs["t0"].ap, zs["t0"].ap, zs["t1"].ap, ALU.subtract, TC, TC)
        tt("dve", zs["fim"].ap, zs["t0"].ap, zs["den"].ap, ALU.mult, TC, TC)
        Bsm = scr("Bsm", [128, 2, 16, 128], F32)
        Csm = scr("Csm", [128, 2, 16, 128], F32)
        dma("sp", Bsm.ap, Bsm_d, [], TC, "Bsm")
        dma("sp", Csm.ap, Csm_d, [], TC, "Csm")
        Btr = scr("Btr", [128, 16, 128], F32)
        Bti = scr("Bti", [128, 16, 128], F32)
        Gr = scr("Gr", [128, 16, 128], F32)
        Gi = scr("Gi", [128, 16, 128], F32)
        w0 = scr("w0", [128, 16, 128], F32)
        w1_ = scr("w1_", [128, 16, 128], F32)
        Psr = scr("Psr", [128, 16, 128], BF16)
        Psi = scr("Psi", [128, 16, 128], BF16)
        Kf = scr("Kf", [128, 128], F32)

        def bc(v):
            return v.unsqueeze(2).to_broadcast([128, 16, 128])

        def cmul(o_re, o_im, a_re, a_im, s_re, s_im, neg_im=False):
            tt("dve", w0.ap, a_re, bc(s_re), ALU.mult, TC, TC)
            tt("dve", w1_.ap, a_im, bc(s_im), ALU.mult, TC, TC)
            tt("dve", o_re, w0.ap, w1_.ap, ALU.subtract, TC, TC)
            tt("dve", w0.ap, a_re, bc(s_im), ALU.mult, TC, TC)
            tt("dve", w1_.ap, a_im, bc(s_re), ALU.mult, TC, TC)
            if neg_im:
                stt(o_im, w0.ap, -1.0, w1_.ap, ALU.mult, ALU.subtract, TC, TC)
            else:
                tt("dve", o_im, w0.ap, w1_.ap, ALU.add, TC, TC)

        cmul(Btr.ap, Bti.ap, Bsm.ap[:, 0], Bsm.ap[:, 1], zs["fre"].ap, zs["fim"].ap)
        memset("dve", Kb.ap, 0.0, TC + [Kb])
        for k in range(LC + 1):
            cmul(Gr.ap, Gi.ap, Csm.ap[:, 0], Csm.ap[:, 1], pwr.ap[:, :, k], pwi.ap[:, :, k], neg_im=True)
            if k >= 1:
                for j in range(16):
                    c0 = 64 * ((j % 4) // 2)
                    cp("act", Qs.ap[:, 0, j * LC + k - 1, :], Gr.ap[:, j, c0:c0 + 64], TC, TC + [Qs])
                    cp("act", Qs.ap[:, 1, j * LC + k - 1, :], Gi.ap[:, j, c0:c0 + 64], TC, TC + [Qs])
            if k < LC:
                for q in range(4):
                    pK = PSB[2 + q % 2]
                    for jm in range(4):
                        j = 4 * q + jm
                        mm(pK.ap[:, 0:128], Btr.ap[:, j, :], Gr.ap[:, j, :], jm == 0, False, TC, [pK])
                        mm(pK.ap[:, 0:128], Bti.ap[:, j, :], Gi.ap[:, j, :], False, jm == 3, TC, [pK])
                    if k == 0:
                        stt(Kf.ap, ident_f.ap, dT.ap[:, q:q + 1], pK.ap[:, 0:128], ALU.mult, ALU.add, [pK, ident_f, dT] + TC, TC)
                        cp("dve", Kb.ap[:, q * LC + k, :], Kf.ap, TC, TC + [Kb])
                    else:
                        cp("dve", Kb.ap[:, q * LC + k, :], pK.ap[:, 0:128], [pK] + TC, TC + [Kb])
                s_ = LC - 1 - k
                cmul(Psr.ap, Psi.ap, Btr.ap, Bti.ap, pwr.ap[:, :, k], pwi.ap[:, :, k])
                for part, Ps_ in enumerate((Psr, Psi)):
                    for pr in range(2):
                        bk = (2 * part + pr) % 2
                        pT = psv(bk, [128, 8, 128], BF16)
                        for idx in range(8):
                            j = 4 * (idx // 2) + 2 * pr + idx % 2
                            tr(pT[64 * pr:64 * pr + 64, idx, :], Ps_.ap[:, j, 64 * pr:64 * pr + 64], ident_b.ap, TC + [ident_b], [PSB[bk]])
                        cp("act", Pt.ap[64 * pr:64 * pr + 64, part, s_::LC, :], pT[64 * pr:64 * pr + 64, :, :], [PSB[bk]] + TC, TC + [Pt])
        if "s5setup" in dbg:
            dump("tabC4", tabC4)
            dump("Kb", Kb)
            dump("Pt", Pt)
            dump("Qs", Qs)
        A.reset(mark_setup)

        wsT_b = A.alloc("wsT_b", [128, 8, 128], BF16)
        sgub = A.alloc("sgub", [128, 4, 128], F32)
        mark_sgu = A.mark()
        wsf = A.alloc("wsf", [128, 8, 128], F32)
        dma("sp", wsf.ap, wsT_d, [], [wsf], "wsf")
        for h in range(8):
            S.add("pool", lambda e, h=h: e.affine_select(out=wsf.ap[:, h, :], in_=wsf.ap[:, h, :], pattern=[[1, 128]],
                                                           compare_op=ALU.is_ge, fill=0.0, base=0, channel_multiplier=-1),
                  R(wsf), R(wsf))
        cp("pool", wsT_b.ap, wsf.ap, [wsf], [wsT_b])
        bsT = A.alloc("bsT", [128, 4, 128], F32)
        dma("sp", bsT.ap, bsT_d, [], [bsT], "bsT")
        pmix0 = psv(3, [128, 4, 128])
        for h in range(8):
            po = (h % 2) * 64
            mm(pmix0[po:po + 64, h // 2, :], ones_b.ap[:, 0:64], wsT_b.ap[:, h, :], True, True, [ones_b, wsT_b], [PSB[3]])
        for q in range(4):
            stt(sgub.ap[:, q, :], pmix0[:, q, :], lnbT.ap[:, q:q + 1], bsT.ap[:, q, :], ALU.mult, ALU.add,
                [PSB[3], lnbT, bsT], [sgub])
        if "sgusetup" in dbg:
            dump("sgub", sgub)
            dump("wsT_b", wsT_b)
        A.reset(mark_sgu)

        if stop_after == "setup":
            S.finish(final_ops)
            S.emit(st)
            return nc, dbg_outs

        def wload(name, src_view, shape, key=None):
            t = A.alloc(name, shape, BF16)
            dma("pool", t.ap, src_view, [], [t], key or name)
            return t

        w_in_b = wload("w_in_b", w_in_d.rearrange("(kc p) n -> p kc n", p=128), [128, 8, 1536])
        w_gate_b = wload("w_gate_b", w_gate_d.rearrange("(kc p) n -> p kc n", p=128), [128, 8, 2048])
        w_glu_b = wload("w_glu_b", w_glu_d.rearrange("(kc p) n -> p kc n", p=128), [128, 4, 512])
        w_bra_b = wload("w_bra_b", w_bra_d.rearrange("(kc p) n -> p kc n", p=128), [128, 4, D])
        ts("pool", w_bra_b.ap, w_bra_b.ap, 0.5, 1.0, ALU.mult, ALU.mult, [w_bra_b], [w_bra_b])
        w_brb_b = wload("w_brb_b", w_brb_d.rearrange("(kc p) n -> p kc n", p=128), [128, 4, D])
        w_out_b = wload("w_out_b", w_out_d.rearrange("(kc p) n -> p kc n", p=128), [128, 8, D])
        def alias(name, shape, dt, of, off=0):
            t = Tl(None, name)
            n = 1
            for s_ in shape[1:]:
                n *= s_
            base_ = of.ap
            if len(base_.shape) == 3:
                base_ = base_.rearrange("p a b -> p (a b)")
            elif len(base_.shape) == 4:
                base_ = base_.rearrange("p a b c -> p (a b c)")
            if base_.dtype != dt:
                base_ = base_.bitcast(dt)
            v = base_[:, off:off + n]
            if len(shape) == 3:
                v = v.rearrange("p (a b) -> p a b", a=shape[1])
            t.ap = v
            t.r = of.r
            return t

        xt0 = A.alloc("xt0", [128, D], F32)
        xr = A.alloc("xr", [128, D], F32)
        xsb = A.alloc("xsb", [128, D], BF16)
        ssq = A.alloc("ssq", [128, 1], F32)
        rstd = A.alloc("rstd", [128, 1], F32)
        xnT = [A.alloc("xnT%d" % i, [128, 8, 128], BF16) for i in range(3)]
        uT = [A.alloc("uT%d" % i, [128, 4, 128], BF16) for i in range(2)]
        guT = A.alloc("guT", [128, 4, 128], F32)
        gv = A.alloc("gv", [128, 512], F32)
        vst = A.alloc("vst", [128, 6], F32)
        vmv = A.alloc("vmv", [128, 2], F32)
        vrs = A.alloc("vrs", [128, 1], F32)
        vhat = A.alloc("vhat", [128, 512], BF16)
        mixT = alias("mixT", [128, 4, 128], F32, gv)
        ybT = [A.alloc("ybT%d" % i, [128, 4, 128], BF16) for i in range(3)]
        xtil = A.alloc("xtil", [128, 4, 2, NCH], F32)
        s5a = A.alloc("s5a", [128, 4, NCH], F32)
        s5b = A.alloc("s5b", [128, 4, NCH], F32)
        gsc = A.alloc("gsc", [128, 4, 2, NCH], F32)
        gp = A.alloc("gp", [128, 16, 2], F32)
        carry = A.alloc("carry", [128, 16, 2], F32)
        sm1 = A.alloc("sm1", [128, 16, 2], F32)
        cf2 = A.alloc("cf2", [128, 4, 2], F32)
        c4 = [A.alloc("c4_%d" % i, [128, 16], F32) for i in range(4)]
        Sprev = [A.alloc("Sprev%d" % i, [128, 4, 2, NCH], BF16) for i in range(2)]
        ygT = A.alloc("ygT", [128, 4, 128], BF16)
        sg = A.alloc("sg", [128, 4, 128], F32)
        yaT = [A.alloc("yaT%d" % i, [128, 4, 128], BF16) for i in range(2)]
        gates = A.alloc("gates", [128, 16, 128], F32)
        xn2T = alias("xn2T", [128, 8, 128], F32, gates, off=1024)
        xn2 = alias("xn2", [128, D], F32, gates, off=0)
        mergedT = A.alloc("mergedT", [128, 8, 128], BF16)
        jk2 = alias("jk2", [128, D], BF16, mergedT)
        gt1b = A.alloc("gt1b", [128, D], F32)
        A2b = A.alloc("A2b", [128, D], F32)
        sh2b = A.alloc("sh2b", [128, D], F32)
        ssq2 = A.alloc("ssq2", [128, 1], F32)
        rstd2 = A.alloc("rstd2", [128, 1], F32)
        lg = A.alloc("lg", [128, 36], F32)
        rt = {n: A.alloc("rt_" + n, [128, w_], F32) for n, w_ in
              [("gmax", 1), ("ngmax", 1), ("maskg", 4), ("eg", 4), ("sume", 1), ("pgs", 1), ("pen", 4), ("lem", 32),
               ("m1", 1), ("oh1", 32), ("lem2", 32), ("m2", 1), ("oh2", 32), ("dm", 1), ("e2", 1), ("p1", 1), ("p2", 1),
               ("rank", 32), ("slotv", 32), ("junk", 32), ("sl", 2), ("val", 32), ("vk", 2)]}
        ohb = A.alloc("ohb", [128, 32], BF16)

        def rms_rstd(src, ssq_t, rstd_t, junk_ap, junk_t):
            act(junk_ap, src.ap, AF.Square, [src], [junk_t, ssq_t], accum=ssq_t.ap)
            ts("pool", ssq_t.ap, ssq_t.ap, 1.0 / D, RMS_EPS, ALU.mult, ALU.add, [ssq_t], [ssq_t])
            tt("pool", rstd_t.ap, ssq_t.ap, mhalf.ap, ALU.pow, [ssq_t, mhalf], [rstd_t])

        store_ops = []
        scat_ops = []
        c128 = tabC4.ap[:, :, NCH]
        d128 = tabD4.ap[:, :, NCH]

        def seqof(i):
            return i // NT

        def P1(i):
            b = seqof(i)
            X = xt0
            XN = xnT[i % 3]
            if i == 0:
                dma("sp", X.ap, x_d[0:128, :], [], [X], "xt0")
            rms_rstd(X, ssq, rstd, xsb.ap, xsb)
            act(xsb.ap, X.ap, AF.Copy, [X, rstd], [xsb], scale=rstd.ap[:, 0:1])
            if i + 1 < NTILES:
                dma("sp", X.ap, x_d[(i + 1) * 128:(i + 2) * 128, :], [], [X], "xt0")
            pX = psv(0, [128, 8, 128], BF16)
            for kc in range(8):
                tr(pX[:, kc, :], xsb.ap[:, kc * 128:(kc + 1) * 128], ident_b.ap, [xsb, ident_b], [PSB[0]])
            for kc in range(8):
                act(XN.ap[:, kc, :], pX[:, kc, :], AF.Identity, [PSB[0], A1T, sh1T], [XN],
                    bias=sh1T.ap[:, kc, b:b + 1], scale=A1T.ap[:, kc, b:b + 1])
            if dbg.get("tile") == i:
                dump("xnT", XN)

        def P2(i):
            XN = xnT[i % 3]
            UT = uT[i % 2]
            pZa = psv(1, [128, 4, 128])
            pZu = psv(0, [128, 4, 128])
            for m in range(4):
                for kc in range(8):
                    mm(pZa[:, m, :], w_in_b.ap[:, kc, m * 128:(m + 1) * 128], XN.ap[:, kc, :], kc == 0, kc == 7,
                       [w_in_b, XN], [PSB[1]])
            cp("act", UT.ap, pZa, [PSB[1]], [UT])
            for m in range(4):
                for kc in range(8):
                    mm(pZu[:, m, :], w_in_b.ap[:, kc, 512 + m * 128:512 + (m + 1) * 128], XN.ap[:, kc, :], kc == 0, kc == 7,
                       [w_in_b, XN], [PSB[0]])
            act(guT.ap, pZu, AF.Gelu_apprx_tanh, [PSB[0]], [guT])
            pV = psv(1, [128, 512])
            for kc in range(8):
                mm(pV, XN.ap[:, kc, :], w_in_b.ap[:, kc, 1024:1536], kc == 0, kc == 7, [w_in_b, XN], [PSB[1]])
            act(gv.ap, pV, AF.Gelu_apprx_tanh, [PSB[1]], [gv])
            if dbg.get("tile") == i:
                dump("uT", UT)

        def P3(i):
            YB = ybT[i % 3]
            S.add("dve", lambda e: e.bn_stats(out=vst.ap, in_=gv.ap), R(gv), R(vst))
            S.add("dve", lambda e: e.bn_aggr(out=vmv.ap, in_=vst.ap), R(vst), R(vmv))
            ts("pool", vrs.ap, vmv.ap[:, 1:2], 1.0, LN_EPS, ALU.mult, ALU.add, [vmv], [vrs])
            tt("pool", vrs.ap, vrs.ap, mhalf.ap, ALU.pow, [vrs, mhalf], [vrs])
            ts("dve", vhat.ap, gv.ap, vmv.ap[:, 0:1], vrs.ap[:, 0:1], ALU.subtract, ALU.mult, [gv, vmv, vrs], [vhat])
            pMix = psv(0, [128, 4, 128])
            for h in range(8):
                po = (h % 2) * 64
                mm(pMix[po:po + 64, h // 2, :], vhat.ap[:, h * 64:(h + 1) * 64], wsT_b.ap[:, h, :], True, True,
                   [vhat, wsT_b], [PSB[0]])
            for q in range(4):
                stt(mixT.ap[:, q, :], pMix[:, q, :], lngT.ap[:, q:q + 1], sgub.ap[:, q, :], ALU.mult, ALU.add,
                    [PSB[0], lngT, sgub], [mixT])
            tt("pool", YB.ap, guT.ap, mixT.ap, ALU.mult, [guT, mixT], [YB])
            if dbg.get("tile") == i:
                dump("ybT", YB)

        def pS5(q):
            o = (q % 2) * 4 * NCH
            return ps_all[:, 2:4, o:o + 4 * NCH].rearrange("p k (a b c) -> p k a b c", a=2, b=2)

        def Qpre(i):
            if i % NT == 0:
                memset("dve", carry.ap, 0.0, [carry])
            cT_, dT_ = tabC4.ap[:, :, 1], tabD4.ap[:, :, 1]
            tt("dve", c4[0].ap, carry.ap[:, :, 0], cT_, ALU.mult, [carry, tabC4], [c4[0]])
            tt("dve", c4[1].ap, carry.ap[:, :, 1], dT_, ALU.mult, [carry, tabD4], [c4[1]])
            tt("dve", c4[2].ap, carry.ap[:, :, 1], cT_, ALU.mult, [carry, tabC4], [c4[2]])
            tt("dve", c4[3].ap, carry.ap[:, :, 0], dT_, ALU.mult, [carry, tabD4], [c4[3]])
            tt("dve", sm1.ap[:, :, 0], c4[0].ap, c4[1].ap, ALU.add, [c4[0], c4[1]], [sm1])
            tt("dve", sm1.ap[:, :, 1], c4[2].ap, c4[3].ap, ALU.subtract, [c4[2], c4[3]], [sm1])

        def QB(i, q):
            UT = uT[i % 2]
            pS = pS5(q)
            for jj in range(4):
                ro = 64 * (jj // 2)
                for part in range(2):
                    for sx in range(LC):
                        mm(pS[:, jj // 2, jj % 2, part, :], Pt.ap[ro:ro + 64, part, (2 * q + jj % 2) * LC + sx, :],
                           UT.ap[ro:ro + 64, q, sx::LC], sx == 0, sx == LC - 1, [Pt, UT], [PSB[2 + jj // 2]])

        def QD(i, q):
            pS = pS5(q)
            SP = Sprev[q % 2]
            tc_ = tabC4.ap[:, 4 * q:4 * q + 4, 0:NCH]
            td_ = tabD4.ap[:, 4 * q:4 * q + 4, 0:NCH]
            tc4 = tc_.rearrange("p (k a) c -> p k a c", k=2)
            td4 = td_.rearrange("p (k a) c -> p k a c", k=2)
            a4 = s5a.ap.rearrange("p (k a) c -> p k a c", k=2)
            b4 = s5b.ap.rearrange("p (k a) c -> p k a c", k=2)
            PB = [PSB[2], PSB[3]]
            tt("dve", a4, pS[:, :, :, 0, :], tc4, ALU.mult, PB + [tabC4], [s5a])
            tt("dve", b4, pS[:, :, :, 1, :], td4, ALU.mult, PB + [tabD4], [s5b])
            tt("dve", xtil.ap[:, :, 0, :], s5a.ap, s5b.ap, ALU.add, [s5a, s5b], [xtil])
            tt("dve", a4, pS[:, :, :, 1, :], tc4, ALU.mult, PB + [tabC4, xtil], [s5a])
            tt("dve", b4, pS[:, :, :, 0, :], td4, ALU.mult, PB + [tabD4, xtil], [s5b])
            tt("dve", xtil.ap[:, :, 1, :], s5a.ap, s5b.ap, ALU.subtract, [s5a, s5b], [xtil])
            tt("dve", cf2.ap, carry.ap[:, 4 * q:4 * q + 4, :], rmagL.ap[:, 4 * q:4 * q + 4].unsqueeze(2).to_broadcast([128, 4, 2]),
               ALU.mult, [carry, rmagL], [cf2])
            tt("dve", xtil.ap[:, :, :, 0], xtil.ap[:, :, :, 0], cf2.ap, ALU.add, [xtil, cf2], [xtil])
            S.add("dve", lambda e: e.tensor_tensor_scan(
                out=gsc.ap.rearrange("p a b c -> p (a b c)"),
                data0=Rtab.ap[:, 4 * q:4 * q + 4, :, :].rearrange("p a b c -> p (a b c)"),
                data1=xtil.ap.rearrange("p a b c -> p (a b c)"), initial=0.0, op0=ALU.mult, op1=ALU.add),
                R(Rtab, xtil), R(gsc), cost=0.25 + 8 * NCH / 500.0)
            cp("dve", gp.ap[:, 4 * q:4 * q + 4, :], gsc.ap[:, :, :, NCH - 1], [gsc], [gp])
            n1 = NCH - 1
            tt("dve", s5a.ap[:, :, 0:n1], gsc.ap[:, :, 0, 0:n1], tc_[:, :, 0:n1], ALU.mult, [gsc, tabC4], [s5a])
            tt("dve", s5b.ap[:, :, 0:n1], gsc.ap[:, :, 1, 0:n1], td_[:, :, 0:n1], ALU.mult, [gsc, tabD4], [s5b])
            tt("dve", SP.ap[:, :, 0, 1:NCH], s5a.ap[:, :, 0:n1], s5b.ap[:, :, 0:n1], ALU.subtract, [s5a, s5b], [SP])
            tt("dve", s5a.ap[:, :, 0:n1], gsc.ap[:, :, 1, 0:n1], tc_[:, :, 0:n1], ALU.mult, [gsc, tabC4, SP], [s5a])
            tt("dve", s5b.ap[:, :, 0:n1], gsc.ap[:, :, 0, 0:n1], td_[:, :, 0:n1], ALU.mult, [gsc, tabD4, SP], [s5b])
            tt("dve", SP.ap[:, :, 1, 1:NCH], s5a.ap[:, :, 0:n1], s5b.ap[:, :, 0:n1], ALU.add, [s5a, s5b], [SP])
            cp("dve", SP.ap[:, :, :, 0], sm1.ap[:, 4 * q:4 * q + 4, :], [sm1], [SP])
            if dbg.get("tile") == i and q == 0:
                dump("gsc0", gsc)

        def QC(i, q):
            UT = uT[i % 2]
            SP = Sprev[q % 2]
            pY = psv(4, [128, 4, LC, NCH])
            for sp_ in range(LC):
                first = True
                for sx in range(sp_ + 1):
                    mm(pY[:, q, sp_, :], Kb.ap[:, q * LC + (sp_ - sx), :], UT.ap[:, q, sx::LC], first, False, [Kb, UT], [PSB[4]])
                    first = False
                for jj in range(4):
                    j = 4 * q + jj
                    ro = 64 * (jj // 2)
                    for part in range(2):
                        mm(pY[ro:ro + 64, q, sp_, :], Qs.ap[:, part, j * LC + sp_, :], SP.ap[:, jj, part, :],
                           False, jj == 3 and part == 1, [Qs, SP], [PSB[4]])

        def Qcarry(i):
            tt("dve", c4[0].ap, gp.ap[:, :, 0], c128, ALU.mult, [gp, tabC4], [c4[0]])
            tt("dve", c4[1].ap, gp.ap[:, :, 1], d128, ALU.mult, [gp, tabD4], [c4[1]])
            tt("dve", c4[2].ap, gp.ap[:, :, 1], c128, ALU.mult, [gp, tabC4], [c4[2]])
            tt("dve", c4[3].ap, gp.ap[:, :, 0], d128, ALU.mult, [gp, tabD4], [c4[3]])
            tt("dve", carry.ap[:, :, 0], c4[0].ap, c4[1].ap, ALU.subtract, [c4[0], c4[1]], [carry])
            tt("dve", carry.ap[:, :, 1], c4[2].ap, c4[3].ap, ALU.add, [c4[2], c4[3]], [carry])

        def Qtail(i):
            YA = yaT[i % 2]
            pY = psv(4, [128, 4, LC, NCH])
            for q in range(4):
                act(ygT.ap[:, q, :].rearrange("p (c s) -> p s c", s=LC), pY[:, q, :, :], AF.Gelu_apprx_tanh, [PSB[4]], [ygT])
            if dbg.get("tile") == i:
                dump("ypre", ygT)
            pG = psv(4, [128, 4, 128])
            for m in range(4):
                for kc in range(4):
                    mm(pG[:, m, :], w_glu_b.ap[:, kc, m * 128:(m + 1) * 128], ygT.ap[:, kc, :], kc == 0, kc == 3,
                       [w_glu_b, ygT], [PSB[4]])
            for m in range(4):
                act(sg.ap[:, m, :], pG[:, m, :], AF.Tanh, [PSB[4], b_gluT], [sg], bias=b_gluT.ap[:, m:m + 1], scale=0.5)
            stt(YA.ap, sg.ap, 1.0, ygT.ap, ALU.add, ALU.mult, [sg, ygT], [YA])
            if dbg.get("tile") == i:
                dump("yaT", YA)

        def R0(i):
            b = seqof(i)
            if i % NT == 0:
                dma("sp", gt1b.ap, mod_d.ap[b:b + 1, 2 * D:3 * D].partition_broadcast(128), [mod_d], [gt1b], "gt1b")
                ts("dve", gt1b.ap, gt1b.ap, 0.5, None, ALU.mult, None, [gt1b], [gt1b])
                dma("sp", sh2b.ap, mod_d.ap[b:b + 1, 3 * D:4 * D].partition_broadcast(128), [mod_d], [sh2b], "sh2b")
                dma("sp", A2b.ap, mod_d.ap[b:b + 1, 4 * D:5 * D].partition_broadcast(128), [mod_d], [A2b], "A2b")
                dma("sp", xn2.ap, g2_d.partition_broadcast(128), [], [xn2], "g2tmp")
                stt(A2b.ap, A2b.ap, 1.0, xn2.ap, ALU.add, ALU.mult, [A2b, xn2], [A2b])
            dma("sp", xr.ap, x_d[i * 128:(i + 1) * 128, :], [], [xr], "xr")

        def R1(i, mg):
            XN = xnT[i % 3]
            bk = 5 if mg % 2 == 0 else 7
            pGt = psv(bk, [128, 4, 128])
            for mm_ in range(4):
                m = mg * 4 + mm_
                for kc in range(8):
                    mm(pGt[:, mm_, :], w_gate_b.ap[:, kc, m * 128:(m + 1) * 128], XN.ap[:, kc, :], kc == 0, kc == 7,
                       [w_gate_b, XN], [PSB[bk]])
            for mm_ in range(4):
                m = mg * 4 + mm_
                act(gates.ap[:, m, :], pGt[:, mm_, :], AF.Tanh, [PSB[bk], b_gateT], [gates],
                    bias=b_gateT.ap[:, m:m + 1], scale=0.5)

        def R2(i, half):
            YA, YB = yaT[i % 2], ybT[i % 3]
            pA = psv(5, [128, 4, 128])
            pB = psv(6, [128, 4, 128])
            for mm_ in range(4):
                m = half * 4 + mm_
                for kc in range(4):
                    mm(pA[:, mm_, :], w_bra_b.ap[:, kc, m * 128:(m + 1) * 128], YA.ap[:, kc, :], kc == 0, kc == 3,
                       [w_bra_b, YA], [PSB[5]])
            for mm_ in range(4):
                m = half * 4 + mm_
                for kc in range(4):
                    mm(pB[:, mm_, :], w_brb_b.ap[:, kc, m * 128:(m + 1) * 128], YB.ap[:, kc, :], kc == 0, kc == 3,
                       [w_brb_b, YB], [PSB[6]])
            ga = gates.ap[:, half * 4:half * 4 + 4, :]
            gb = gates.ap[:, 8 + half * 4:8 + half * 4 + 4, :]
            stt(ga, ga, 1.0, pA, ALU.add, ALU.mult, [gates, PSB[5]], [gates])
            stt(gb, gb, 1.0, pB, ALU.add, ALU.mult, [gates, PSB[6]], [gates])
            tt("dve", mergedT.ap[:, half * 4:half * 4 + 4, :], ga, gb, ALU.add, [gates], [mergedT])
            if dbg.get("tile") == i and half == 1:
                dump("mergedT", mergedT)

        def R3(i):
            for half in range(2):
                bk = 6 + half
                for kc in range(8):
                    mm(PSB[bk].ap, mergedT.ap[:, kc, :], w_out_b.ap[:, kc, half * 512:(half + 1) * 512], kc == 0, kc == 7,
                       [mergedT, w_out_b], [PSB[bk]])
                sl = slice(half * 512, (half + 1) * 512)
                tt("dve", xn2.ap[:, sl], PSB[bk].ap, gt1b.ap[:, sl], ALU.mult, [PSB[bk], gt1b], [xn2])
            tt("dve", xr.ap, xr.ap, xn2.ap, ALU.add, [xr, xn2], [xr])
            store_ops.append(dma("sp", H_d[i * 128:(i + 1) * 128, :], xr.ap, [xr], [r_H], "hst"))
            rms_rstd(xr, ssq2, rstd2, jk2.ap, jk2)
            stt(xn2.ap, xr.ap, rstd2.ap[:, 0:1], A2b.ap, ALU.mult, ALU.mult, [xr, rstd2, A2b], [xn2])
            tt("dve", xn2.ap, xn2.ap, sh2b.ap, ALU.add, [xn2, sh2b], [xn2])
            if dbg.get("tile") == i:
                dump("h", xr)
                dump("xn2", xn2)

        def R4(i):
            pX2 = psv2(6, [128, 8, 128])
            for kc in range(8):
                tr(pX2[:, kc, :], xn2.ap[:, kc * 128:(kc + 1) * 128], ident_f.ap, [xn2, ident_f], [PSB[6], PSB[7]])
            cp("act", xn2T.ap, pX2, [PSB[6], PSB[7]], [xn2T])
            pL = psv(5, [128, 36])
            for kc in range(8):
                mm(pL, xn2T.ap[:, kc, :], w_r.ap[:, kc, :], kc == 0, kc == 7, [xn2T, w_r], [PSB[5]])
            tt("dve", lg.ap, pL, b_r.ap, ALU.add, [PSB[5], b_r], [lg])
            r_ = rt
            BIG = 1.0e9
            S.add("dve", lambda e: e.tensor_reduce(out=r_["gmax"].ap, in_=lg.ap[:, 0:4], axis=AX.X, op=ALU.max), R(lg), R(r_["gmax"]))
            ts("dve", r_["maskg"].ap, lg.ap[:, 0:4], r_["gmax"].ap[:, 0:1], None, ALU.is_equal, None, [lg, r_["gmax"]], [r_["maskg"]])
            ts("dve", r_["ngmax"].ap, r_["gmax"].ap, -0.5, None, ALU.mult, None, [r_["gmax"]], [r_["ngmax"]])
            act(r_["eg"].ap, lg.ap[:, 0:4], AF.Tanh, [lg, r_["ngmax"]], [r_["eg"]], bias=r_["ngmax"].ap[:, 0:1], scale=0.5)
            ts("dve", r_["pen"].ap, r_["eg"].ap, -1.0, 1.0, ALU.mult, ALU.add, [r_["eg"]], [r_["pen"]])
            S.add("dve", lambda e: e.reciprocal(out=r_["pen"].ap, in_=r_["pen"].ap), R(r_["pen"]), R(r_["pen"]))
            stt(r_["eg"].ap, r_["eg"].ap, 1.0, r_["pen"].ap, ALU.add, ALU.mult, [r_["eg"], r_["pen"]], [r_["eg"]])
            S.add("dve", lambda e: e.tensor_reduce(out=r_["sume"].ap, in_=r_["eg"].ap, axis=AX.X, op=ALU.add), R(r_["eg"]), R(r_["sume"]))
            S.add("dve", lambda e: e.reciprocal(out=r_["pgs"].ap, in_=r_["sume"].ap), R(r_["sume"]), R(r_["pgs"]))
            ts("dve", r_["pen"].ap, r_["maskg"].ap, BIG, -BIG, ALU.mult, ALU.add, [r_["maskg"]], [r_["pen"]])
            for g in range(4):
                ts("dve", r_["lem"].ap[:, g * 8:(g + 1) * 8], lg.ap[:, 4 + g * 8:4 + (g + 1) * 8], r_["pen"].ap[:, g:g + 1], None,
                   ALU.add, None, [lg, r_["pen"]], [r_["lem"]])
            S.add("dve", lambda e: e.tensor_reduce(out=r_["m1"].ap, in_=r_["lem"].ap, axis=AX.X, op=ALU.max), R(r_["lem"]), R(r_["m1"]))
            ts("dve", r_["oh1"].ap, r_["lem"].ap, r_["m1"].ap[:, 0:1], None, ALU.is_equal, None, [r_["lem"], r_["m1"]], [r_["oh1"]])
            stt(r_["lem2"].ap, r_["oh1"].ap, -BIG, r_["lem"].ap, ALU.mult, ALU.add, [r_["oh1"], r_["lem"]], [r_["lem2"]])
            S.add("dve", lambda e: e.tensor_reduce(out=r_["m2"].ap, in_=r_["lem2"].ap, axis=AX.X, op=ALU.max), R(r_["lem2"]), R(r_["m2"]))
            ts("dve", r_["oh2"].ap, r_["lem2"].ap, r_["m2"].ap[:, 0:1], None, ALU.is_equal, None, [r_["lem2"], r_["m2"]], [r_["oh2"]])
            tt("dve", r_["dm"].ap, r_["m1"].ap, r_["m2"].ap, ALU.subtract, [r_["m1"], r_["m2"]], [r_["dm"]])
            act(r_["e2"].ap, r_["dm"].ap, AF.Tanh, [r_["dm"]], [r_["e2"]], scale=0.5)
            ts("dve", r_["p1"].ap, r_["e2"].ap, 0.5, 0.5, ALU.mult, ALU.add, [r_["e2"]], [r_["p1"]])
            ts("dve", r_["p2"].ap, r_["e2"].ap, -0.5, 0.5, ALU.mult, ALU.add, [r_["e2"]], [r_["p2"]])
            tt("dve", ohb.ap, r_["oh1"].ap, r_["oh2"].ap, ALU.add, [r_["oh1"], r_["oh2"]], [ohb])
            pR = psv(5, [128, 128])
            mm(pR[:, 64:96], tri_b.ap, ohb.ap, True, True, [tri_b, ohb], [PSB[5]])
            mm(pR[:, 96:128], ones_b.ap, ohb.ap, True, True, [ones_b, ohb], [PSB[5]])
            tt("dve", r_["rank"].ap, pR[:, 64:96], base.ap, ALU.add, [PSB[5], base], [r_["rank"]])
            tt("dve", base.ap, pR[:, 96:128], base.ap, ALU.add, [PSB[5], base, r_["rank"]], [base])
            ts("dve", r_["val"].ap, r_["rank"].ap, float(CAP), None, ALU.is_lt, None, [r_["rank"]], [r_["val"]])
            tt("dve", r_["slotv"].ap, r_["rank"].ap, ecap.ap, ALU.add, [r_["rank"], ecap], [r_["slotv"]])
            stt(r_["slotv"].ap, r_["val"].ap, -4.0e6, r_["slotv"].ap, ALU.mult, ALU.add, [r_["val"], r_["slotv"]], [r_["slotv"]])
            ts("dve", r_["slotv"].ap, r_["slotv"].ap, 4.0e6, None, ALU.add, None, [r_["slotv"]], [r_["slotv"]])
            for k, ohn in enumerate(("oh1", "oh2")):
                tt("dve", r_["junk"].ap, r_[ohn].ap, r_["slotv"].ap, ALU.mult, [r_[ohn], r_["slotv"]], [r_["junk"]])
                S.add("dve", lambda e, k=k: e.tensor_reduce(out=r_["sl"].ap[:, k:k + 1], in_=r_["junk"].ap, axis=AX.X, op=ALU.add),
                      R(r_["junk"]), R(r_["sl"]))
                tt("dve", r_["junk"].ap, r_[ohn].ap, r_["val"].ap, ALU.mult, [r_[ohn], r_["val"]], [r_["junk"]])
                S.add("dve", lambda e, k=k: e.tensor_reduce(out=r_["vk"].ap[:, k:k + 1], in_=r_["junk"].ap, axis=AX.X, op=ALU.add),
                      R(r_["junk"]), R(r_["vk"]))
            cp("dve", sloti.ap[:, i, :], r_["sl"].ap, [r_["sl"]], [sloti])
            stt(wgt.ap[:, i, 0:1], r_["p1"].ap, r_["pgs"].ap[:, 0:1], r_["vk"].ap[:, 0:1], ALU.mult, ALU.mult,
                [r_["p1"], r_["pgs"], r_["vk"]], [wgt])
            stt(wgt.ap[:, i, 1:2], r_["p2"].ap, r_["pgs"].ap[:, 0:1], r_["vk"].ap[:, 1:2], ALU.mult, ALU.mult,
                [r_["p2"], r_["pgs"], r_["vk"]], [wgt])
            if dbg.get("tile") == i:
                dump("lg", lg)
                dump("rank", r_["rank"])
            for k in range(2):
                scat_ops.append(S.add("pool", lambda e, i=i, k=k: e.indirect_dma_start(
                    out=X_d, out_offset=bass.IndirectOffsetOnAxis(ap=sloti.ap[:, i, k:k + 1], axis=0),
                    in_=xn2.ap, in_offset=None, bounds_check=breg(e, NSLOT - 1), oob_is_err=False),
                    R(xn2, sloti), [r_X], dma_key="scat", cost=1.2, lat=4.0))

        import os as _os
        _skip = set(_os.environ.get('QSKIP', '').split(','))
        _w = lambda f, n: (lambda *a: None) if n in _skip else f
        Qpre, QB, QD, QC, Qcarry, Qtail = _w(Qpre, 'pre'), _w(QB, 'B'), _w(QD, 'D'), _w(QC, 'C'), _w(Qcarry, 'carry'), _w(Qtail, 'tail')
        for s_ in range(NTILES + 2):
            ip, iq, ir = s_, s_ - 1, s_ - 2
            hp = 0 <= ip < NTILES
            hq = 0 <= iq < NTILES
            hr = 0 <= ir < NTILES
            if hr:
                R0(ir)
            if hq:
                Qpre(iq)
                QB(iq, 0)
            if hr:
                R1(ir, 0)
                R1(ir, 1)
            if hp:
                P1(ip)
            if hq:
                QD(iq, 0)
            if hr:
                R1(ir, 2)
                R1(ir, 3)
                R2(ir, 0)
                R2(ir, 1)
            if hq:
                QB(iq, 1)
                QC(iq, 0)
            if hp:
                P2(ip)
            if hq:
                QD(iq, 1)
            if hr:
                R3(ir)
            if hq:
                QB(iq, 2)
                QC(iq, 1)
                QD(iq, 2)
            if hr:
                R4(ir)
            if hq:
                QB(iq, 3)
                QC(iq, 2)
            if hp:
                P3(ip)
            if hq:
                QD(iq, 3)
                QC(iq, 3)
                Qcarry(iq)
                Qtail(iq)
        if "route" in dbg:
            dump("wgt", wgt)
            dump("sloti", sloti)
        if stop_after == "A":
            S.finish(final_ops + store_ops + scat_ops)
            S.emit(st)
            return nc, dbg_outs

        S.barrier(dma_ops=[scat_ops[-1], store_ops[-1]])
        A.reset(mark_persist)
        w1b = [A.alloc("w1b%d" % i, [128, 8, 512], BF16) for i in range(2)]
        w3b = [A.alloc("w3b%d" % i, [128, 8, 512], BF16) for i in range(2)]
        w2b = [A.alloc("w2b%d" % i, [128, 4, D], BF16) for i in range(2)]
        Xblk = [A.alloc("Xblk%d" % i, [128, D], BF16) for i in range(3)]
        XT = [A.alloc("XT%d" % i, [128, 8, CAP], BF16) for i in range(2)]
        hidT = [A.alloc("hidT%d" % i, [128, 4, CAP], BF16) for i in range(2)]
        s1 = [A.alloc("s1_%d" % i, [128, 512], F32) for i in range(2)]
        Yblk = [A.alloc("Yblk%d" % i, [128, D], F32) for i in range(3)]
        NH = (CAP + 511) // 512
        HW_ = CAP // NH
        ystore = []
        cnt = {"x": 0, "y": 0, "h": 0}

        def W13(e_):
            sl_ = e_ % 2
            dma("pool", w1b[sl_].ap, w1_d[e_].rearrange("(kc p) n -> p kc n", p=128), [], [w1b[sl_]], "w1b%d" % sl_)
            dma("pool", w3b[sl_].ap, w3_d[e_].rearrange("(kc p) n -> p kc n", p=128), [], [w3b[sl_]], "w3b%d" % sl_)

        def W2(e_):
            sl_ = e_ % 2
            dma("pool", w2b[sl_].ap, w2_d[e_].rearrange("(kc p) n -> p kc n", p=128), [], [w2b[sl_]], "w2b%d" % sl_)

        def TX(e_):
            xt_ = XT[e_ % 2]
            for blk in range(NBLK):
                n_ = cnt["x"]
                cnt["x"] += 1
                xb_ = Xblk[n_ % 3]
                r0 = e_ * CAP + blk * 128
                dma("sp", xb_.ap, X_d[r0:r0 + 128, :], [r_X], [xb_], "xblk%d" % (n_ % 3))
                bk = n_ % 2
                pXT = psv(bk, [128, 8, 128], BF16)
                for kc in range(8):
                    tr(pXT[:, kc, :], xb_.ap[:, kc * 128:(kc + 1) * 128], ident_b.ap, [xb_, ident_b], [PSB[bk]])
                cp("act" if blk % 2 == 0 else "dve", xt_.ap[:, :, blk * 128:(blk + 1) * 128], pXT, [PSB[bk]], [xt_])

        def HH(e_):
            sl_ = e_ % 2
            xt_, hd_ = XT[e_ % 2], hidT[e_ % 2]
            for m in range(4):
                for nh in range(NH):
                    cs_ = slice(nh * HW_, (nh + 1) * HW_)
                    n_ = cnt["h"]
                    cnt["h"] += 1
                    b1 = 2 + n_ % 2
                    b3 = 4 + n_ % 2
                    p1_ = PSB[b1].ap[:, 0:HW_]
                    p3_ = PSB[b3].ap[:, 0:HW_]
                    for kc in range(8):
                        mm(p1_, w1b[sl_].ap[:, kc, m * 128:(m + 1) * 128], xt_.ap[:, kc, cs_], kc == 0, kc == 7,
                           [w1b[sl_], xt_], [PSB[b1]])
                    for kc in range(8):
                        mm(p3_, w3b[sl_].ap[:, kc, m * 128:(m + 1) * 128], xt_.ap[:, kc, cs_], kc == 0, kc == 7,
                           [w3b[sl_], xt_], [PSB[b3]])
                    s1_ = s1[n_ % 2]
                    act(s1_.ap[:, 0:HW_], p1_, AF.Silu, [PSB[b1]], [s1_])
                    tt("dve", hd_.ap[:, m, cs_], s1_.ap[:, 0:HW_], p3_, ALU.mult, [s1_, PSB[b3]], [hd_])

        def YY(e_):
            sl_ = e_ % 2
            hd_ = hidT[e_ % 2]
            for blk in range(NBLK):
                n_ = cnt["y"]
                cnt["y"] += 1
                yb_ = Yblk[n_ % 3]
                for half in range(2):
                    bk = 6 + half
                    for kc in range(4):
                        mm(PSB[bk].ap, hd_.ap[:, kc, blk * 128:(blk + 1) * 128], w2b[sl_].ap[:, kc, half * 512:(half + 1) * 512],
                           kc == 0, kc == 3, [hd_, w2b[sl_]], [PSB[bk]])
                    cp("act" if half == 0 else "dve", yb_.ap[:, half * 512:(half + 1) * 512], PSB[bk].ap, [PSB[bk]], [yb_])
                r0 = e_ * CAP + blk * 128
                ystore.append(dma("sp", Y_d[r0:r0 + 128, :], yb_.ap, [yb_], [r_Y], "yst%d" % (n_ % 3)))

        W13(0)
        W2(0)
        W13(1)
        W2(1)
        TX(0)
        for e_ in range(32):
            HH(e_)
            if e_ + 2 < 32:
                W13(e_ + 2)
            if e_ + 1 < 32:
                TX(e_ + 1)
            YY(e_)
            if e_ + 2 < 32:
                W2(e_ + 2)
        if stop_after == "B":
            S.finish(final_ops + ystore[-3:])
            S.emit(st)
            return nc, dbg_outs

        S.barrier(dma_ops=ystore[-3:])
        A.reset(mark_persist)
        NBC = 3
        Hc = [A.alloc("Hc%d" % i, [128, D], F32) for i in range(NBC)]
        Y0 = [A.alloc("Y0_%d" % i, [128, D], F32) for i in range(NBC)]
        Y1 = [A.alloc("Y1_%d" % i, [128, D], F32) for i in range(NBC)]
        acc = A.alloc("acc", [128, D], F32)
        ob = [A.alloc("ob%d" % i, [128, D], F32) for i in range(2)]
        gt2b = A.alloc("gt2b", [128, D], F32)
        gfb = A.alloc("gfb", [128, D], F32)
        jk = A.alloc("jk", [128, D], BF16)
        ssq3 = A.alloc("ssq3", [128, 1], F32)
        rstd3 = A.alloc("rstd3", [128, 1], F32)
        dma("sp", gfb.ap, gf_d.partition_broadcast(128), [], [gfb], "gfb")
        for s_ in range(NBC):
            memset("pool", Y0[s_].ap, 0.0, [Y0[s_]])
            memset("pool", Y1[s_].ap, 0.0, [Y1[s_]])

        def Cload(i):
            s_ = i % NBC
            dma("sp", Hc[s_].ap, H_d[i * 128:(i + 1) * 128, :], [r_H], [Hc[s_]], "hc%d" % s_)
            for k, Yk in enumerate((Y0[s_], Y1[s_])):
                S.add("pool", lambda e, i=i, k=k, Yk=Yk: e.indirect_dma_start(
                    out=Yk.ap, out_offset=None, in_=Y_d, in_offset=bass.IndirectOffsetOnAxis(ap=sloti.ap[:, i, k:k + 1], axis=0),
                    bounds_check=breg(e, NSLOT - 1), oob_is_err=False), R(r_Y, sloti), R(Yk), dma_key="gath%d_%d" % (k, s_),
                    cost=1.2, lat=5.0)

        Cload(0)
        if NTILES > 1:
            Cload(1)
        for i in range(NTILES):
            b, tau = i // NT, i % NT
            s_ = i % NBC
            if tau == 0:
                dma("sp", gt2b.ap, mod_d.ap[b:b + 1, 5 * D:6 * D].partition_broadcast(128), [mod_d], [gt2b], "gt2b")
            if i + 2 < NTILES:
                Cload(i + 2)
            ts("dve", acc.ap, Y0[s_].ap, wgt.ap[:, i, 0:1], None, ALU.mult, None, [Y0[s_], wgt], [acc])
            stt(acc.ap, Y1[s_].ap, wgt.ap[:, i, 1:2], acc.ap, ALU.mult, ALU.add, [Y1[s_], wgt, acc], [acc])
            tt("dve", acc.ap, acc.ap, gt2b.ap, ALU.mult, [acc, gt2b], [acc])
            tt("dve", acc.ap, acc.ap, Hc[s_].ap, ALU.add, [acc, Hc[s_]], [acc])
            rms_rstd(acc, ssq3, rstd3, jk.ap, jk)
            stt(ob[i % 2].ap, acc.ap, rstd3.ap[:, 0:1], gfb.ap, ALU.mult, ALU.mult, [acc, rstd3, gfb], [ob[i % 2]])
            final_ops.append(dma("sp", out_d[i * 128:(i + 1) * 128, :], ob[i % 2].ap, [ob[i % 2]], [], "ost%d" % (i % 2)))
        S.finish(final_ops)
        S.emit(st)
    return nc, dbg_outs


def prep_shared(inp):
    f = np.float32
    g = {}
    L = 0
    g["w_ada"] = np.ascontiguousarray(inp["w_ada"][L], f)
    g["g1T"] = np.ascontiguousarray(inp["norm1_g"][L].reshape(8, 128).T, f)
    g["w_in"] = np.ascontiguousarray(inp["w_in"][L], f)
    g["w_gate"] = np.ascontiguousarray(inp["w_gate"][L], f)
    g["b_gateT"] = np.ascontiguousarray(inp["b_gate"][L].reshape(16, 128).T, f)
    a_re, a_im, ls = inp["ssm_a_re"][L], inp["ssm_a_im"][L], inp["ssm_log_step"][L]
    def sm(v):
        return v.reshape(16, 2, 64).transpose(1, 2, 0).reshape(128, 16)
    lsx = np.repeat(ls[:, None], 64, axis=1)
    g["lam_sm"] = np.ascontiguousarray(np.stack([sm(a_re), sm(a_im), sm(lsx)], axis=1), f)
    Bsm = np.zeros((128, 2, 16, 128), f)
    for pi, Bsrc in enumerate((inp["ssm_b_re"][L], inp["ssm_b_im"][L])):
        for gi in range(32):
            j = gi // 2
            n0 = 64 * (gi % 2)
            c0 = 32 * (j % 4) + 16 * (gi % 2)
            Bsm[n0:n0 + 64, pi, j, c0:c0 + 16] = Bsrc[gi]
    g["Bsm"] = Bsm
    Csm = np.zeros((128, 2, 16, 128), f)
    for pi, Csrc in enumerate((inp["ssm_c_re"][L], inp["ssm_c_im"][L])):
        for gi in range(32):
            j = gi // 2
            n0 = 64 * (gi % 2)
            c0 = 32 * (j % 4) + 16 * (gi % 2)
            Csm[n0:n0 + 64, pi, j, c0:c0 + 16] = Csrc[gi].T
    g["Csm"] = Csm
    g["dT"] = np.ascontiguousarray(inp["ssm_d"][L].reshape(4, 128).T, f)
    g["w_glu"] = np.ascontiguousarray(inp["w_glu"][L], f)
    g["b_gluT"] = np.ascontiguousarray(inp["b_glu"][L].reshape(4, 128).T, f)
    g["wsT"] = np.ascontiguousarray(inp["sgu_w"][L].transpose(2, 0, 1), f)
    g["lngT"] = np.ascontiguousarray(inp["sgu_ln_g"][L].reshape(4, 128).T, f)
    g["lnbT"] = np.ascontiguousarray(inp["sgu_ln_b"][L].reshape(4, 128).T, f)
    bs = inp["sgu_b"][L]
    bsT = np.zeros((128, 4, 128), f)
    for q in range(4):
        bsT[0:64, q, :] = bs[2 * q][None, :]
        bsT[64:128, q, :] = bs[2 * q + 1][None, :]
    g["bsT"] = bsT
    g["w_bra"] = np.ascontiguousarray(inp["w_branch_a"][L], f)
    g["w_brb"] = np.ascontiguousarray(inp["w_branch_b"][L], f)
    g["w_out"] = np.ascontiguousarray(inp["w_out"][L], f)
    g["g2"] = np.ascontiguousarray(inp["norm2_g"][L].reshape(1, D), f)
    wr = np.concatenate([inp["w_router_group"][L], inp["w_router_expert"][L].transpose(1, 0, 2).reshape(D, 32)], axis=1)
    g["w_r"] = np.ascontiguousarray(wr.reshape(8, 128, 36).transpose(1, 0, 2), f)
    g["b_r"] = np.ascontiguousarray(np.concatenate([inp["b_router_group"][L], inp["b_router_expert"][L].reshape(32)]).reshape(1, 36), f)
    g["w1"] = np.ascontiguousarray(inp["w1"][L], f)
    g["w3"] = np.ascontiguousarray(inp["w3"][L], f)
    g["w2"] = np.ascontiguousarray(inp["w2"][L], f)
    g["gf"] = np.ascontiguousarray(inp["norm_f_g"].reshape(1, D), f)
    return g


def prep_core(inp, shared, b0, nseq):
    m = dict(shared)
    xs = inp["x"][b0:b0 + nseq]
    m["x"] = np.ascontiguousarray(xs.reshape(-1, D), np.float32)
    c = inp["c"][b0:b0 + nseq]
    m["cT"] = np.ascontiguousarray(c.reshape(nseq, 8, 128).transpose(2, 1, 0), np.float32)
    m["b_ada_rep"] = np.ascontiguousarray(np.repeat(inp["b_ada"][0][None, :], nseq, axis=0), np.float32)
    return m


_CACHE = {}


def kernel(**inputs):
    inp = {k: np.asarray(v) for k, v in inputs.items()}
    B, SEQ = inp["x"].shape[0], inp["x"].shape[1]
    nseq = B // N_CORES
    key = (nseq, SEQ)
    if key not in _CACHE:
        _CACHE[key] = build(NSEQ=nseq, SEQ=SEQ, CAP=768)[0]
    nc = _CACHE[key]
    shared = prep_shared(inp)
    in_maps = [prep_core(inp, shared, c * nseq, nseq) for c in range(N_CORES)]
    res = run_bass_kernel_spmd(nc, in_maps, core_ids=list(range(N_CORES)))
    outs = [np.asarray(r["out"]).reshape(nseq, SEQ, D) for r in res.results]
    return np.concatenate(outs, axis=0).astype(np.float32)
```

```python
import math
import os
from contextlib import ExitStack
import numpy as np
import concourse.bass as bass
import concourse.mybir as mybir
from concourse.bass_utils import run_bass_kernel_spmd

F32 = mybir.dt.float32
BF16 = mybir.dt.bfloat16
I32 = mybir.dt.int32
U8 = mybir.dt.uint8
AF = mybir.ActivationFunctionType
ALU = mybir.AluOpType
AX = mybir.AxisListType

ENGS = ("pe", "act", "dve", "pool", "sp")
N_CORES = 8
D = 1024
TWO_PI = 2.0 * math.pi
RMS_EPS = 1e-6
LN_EPS = 1e-5


class Res:
    __slots__ = ("name", "lastw", "readers")

    def __init__(self, name):
        self.name = name
        self.lastw = None
        self.readers = []


class Op:
    __slots__ = ("eng", "fn", "deps", "sdeps", "dma_key", "dma_val", "signal", "sig_idx", "cost", "lat", "idx", "succ", "nin", "fin")

    def __init__(self, eng, fn, dma_key):
        self.eng = eng
        self.fn = fn
        self.deps = []
        self.sdeps = []
        self.dma_key = dma_key
        self.dma_val = None
        self.signal = False
        self.sig_idx = None
        self.cost = 0.2
        self.lat = 0.0


class Sched:
    def __init__(self, nc):
        self.nc = nc
        self.ops = {e: [] for e in ENGS}
        self.all = []
        self.finals = []
        self.pending_barrier = {}
        import os as _o
        self.reorder = _o.environ.get("NOREORDER") is None

    def add(self, eng, fn, reads=(), writes=(), dma_key=None, cost=None, lat=0.0):
        op = Op(eng, fn, dma_key)
        if cost is not None:
            op.cost = cost
        op.lat = lat
        deps = []
        for r in reads:
            if r.lastw is not None:
                deps.append(r.lastw)
        for w in writes:
            if w.lastw is not None:
                deps.append(w.lastw)
            deps.extend(w.readers)
        if eng in self.pending_barrier:
            deps.extend(self.pending_barrier.pop(eng))
        seen = set()
        for d in deps:
            if d is op or id(d) in seen:
                continue
            seen.add(id(d))
            if d.eng == "pe" and eng == "pe" and d.dma_key is None and dma_key is None:
                op.sdeps.append(d)
                continue
            op.deps.append(d)
        for r in reads:
            r.readers.append(op)
        for w in writes:
            w.lastw = op
            w.readers = []
        op.idx = len(self.all)
        self.all.append(op)
        self.ops[eng].append(op)
        return op

    def barrier(self, dma_ops=()):
        lasts = [self.ops[e][-1] for e in ENGS if self.ops[e]]
        lasts = [o for o in lasts if o.dma_key is None] + list(dma_ops)
        for e in ENGS:
            self.pending_barrier.setdefault(e, []).extend(lasts)

    def finish(self, ops):
        self.finals.extend(ops)

    def schedule(self):
        import heapq
        ops = self.all
        for o in ops:
            o.succ = []
            o.nin = 0
        for o in ops:
            for d in o.deps + o.sdeps:
                d.succ.append(o)
                o.nin += 1
        import os as _o2
        SYNC = float(_o2.environ.get("SYNC", "1.0"))
        wait_h = {e: [] for e in ENGS}
        t_eng = {e: 0.0 for e in ENGS}
        order = {e: [] for e in ENGS}
        ready_at = {}
        for o in ops:
            if o.nin == 0:
                heapq.heappush(wait_h[o.eng], (0.0, o.idx))
        placed = 0
        n = len(ops)
        while placed < n:
            best = None
            for e in ENGS:
                h = wait_h[e]
                if not h:
                    continue
                te = t_eng[e]
                if h[0][0] <= te:
                    cand_idx = min(i for (r, i) in h if r <= te)
                    st = te
                else:
                    st, cand_idx = h[0]
                if best is None or (st, cand_idx) < (best[0], best[1]):
                    best = (st, cand_idx, e)
            st, ci, e = best
            h = wait_h[e]
            for k, (r, i) in enumerate(h):
                if i == ci:
                    h[k] = h[-1]
                    h.pop()
                    break
            heapq.heapify(h)
            o = ops[ci]
            t_eng[e] = st + o.cost
            o.fin = st + o.cost + o.lat
            order[e].append(o)
            placed += 1
            for sct in o.succ:
                sct.nin -= 1
                ra = max(ready_at.get(sct.idx, 0.0), o.fin + (SYNC if o.eng != sct.eng or o.dma_key is not None else 0.0))
                ready_at[sct.idx] = ra
                if sct.nin == 0:
                    heapq.heappush(wait_h[sct.eng], (ra, sct.idx))
        self.ops = order
        self.sim_time = max(t_eng.values())

    def emit(self, stack):
        nc = self.nc
        if self.reorder:
            self.schedule()
        for e in ENGS:
            for op in self.ops[e]:
                for d in op.deps:
                    if d.dma_key is None:
                        d.signal = True
        dma_cnt = {}
        for e in ENGS:
            for op in self.ops[e]:
                if op.dma_key is not None:
                    dma_cnt[op.dma_key] = dma_cnt.get(op.dma_key, 0) + 16
                    op.dma_val = dma_cnt[op.dma_key]
        sems = {e: stack.enter_context(nc.semaphore("sem_" + e)) for e in ENGS}
        dsem = {k: stack.enter_context(nc.semaphore("dsem_%s" % (k,))) for k in dma_cnt}
        for e in ENGS:
            n = 0
            for op in self.ops[e]:
                if op.dma_key is None and op.signal:
                    n += 1
                    op.sig_idx = n
        block = stack.enter_context(nc.Block())
        engobj = {"pe": "tensor", "act": "scalar", "dve": "vector", "pool": "gpsimd", "sp": "sync"}
        finals = self.finals

        def body_for(e):
            def body(eng):
                seen = {}
                for op in self.ops[e]:
                    need = {}
                    for d in op.deps:
                        if d.dma_key is not None:
                            s, v, key = dsem[d.dma_key], d.dma_val, ("d", d.dma_key)
                        else:
                            s, v, key = sems[d.eng], d.sig_idx, ("e", d.eng)
                        if key not in need or need[key][1] < v:
                            need[key] = (s, v)
                    for key, (s, v) in need.items():
                        if seen.get(key, 0) >= v:
                            continue
                        seen[key] = v
                        eng.wait_ge(s, v)
                    inst = op.fn(eng)
                    if op.dma_key is not None:
                        inst.then_inc(dsem[op.dma_key], 16)
                    elif op.signal:
                        inst.then_inc(sems[e], 1)
                if e == "sp":
                    for d in finals:
                        eng.wait_ge(dsem[d.dma_key], d.dma_val)
            return body

        for e in ENGS:
            getattr(block, engobj[e])(body_for(e))


class Tl:
    __slots__ = ("ap", "r")

    def __init__(self, ap, name):
        self.ap = ap
        self.r = Res(name)


class Arena:
    def __init__(self, nc, stack, nbytes):
        self.buf = stack.enter_context(nc.sbuf_tensor("arena", [128, nbytes], U8))
        self.off = 0
        self.cap = nbytes
        self.live = []

    def alloc(self, name, shape, dt):
        esz = {F32: 4, BF16: 2, I32: 4}[dt]
        n = 1
        for s in shape[1:]:
            n *= s
        nb = (n * esz + 31) // 32 * 32
        if os.environ.get("SIMONLY"):
            ro = self.off % (self.cap - nb)
            ro -= ro % 32
            v = self.buf[0:shape[0], ro:ro + n * esz].bitcast(dt)
            if len(shape) == 3:
                v = v.rearrange("p (a b) -> p a b", a=shape[1])
            elif len(shape) == 4:
                v = v.rearrange("p (a b c) -> p a b c", a=shape[1], b=shape[2])
            self.off += nb
            self.hi = max(getattr(self, "hi", 0), self.off)
            return Tl(v, name)
        assert self.off + nb <= self.cap, "SBUF arena overflow at %s: %d + %d > %d" % (name, self.off, nb, self.cap)
        v = self.buf[0:shape[0], self.off:self.off + n * esz].bitcast(dt)
        if len(shape) == 3:
            v = v.rearrange("p (a b) -> p a b", a=shape[1])
        elif len(shape) == 4:
            v = v.rearrange("p (a b c) -> p a b c", a=shape[1], b=shape[2])
        t = Tl(v, name)
        lo, hi = self.off, self.off + nb
        keep = []
        for (a, b, o) in self.live:
            if a < hi and lo < b:
                if o.r.lastw is not None:
                    t.r.readers.append(o.r.lastw)
                t.r.readers.extend(o.r.readers)
                if a >= lo and b <= hi:
                    continue
            keep.append((a, b, o))
        keep.append((lo, hi, t))
        self.live = keep
        self.off += nb
        return t

    def mark(self):
        return self.off

    def reset(self, m):
        self.off = m


def build(NSEQ=4, SEQ=2048, CAP=768, dbg=None, stop_after=None):
    nc = bass.Bass("TRN2", target_bir_lowering=False)
    NT = SEQ // 128
    NTOK = NSEQ * SEQ
    NTILES = NSEQ * NT
    NSLOT = 32 * CAP
    NBLK = CAP // 128
    dbg = dbg or {}
    dbg_outs = {}

    def din(name, shape, dt=F32):
        return nc.dram_tensor(name, list(shape), dt, kind="ExternalInput").ap()

    x_d = din("x", [NTOK, D])
    cT_d = din("cT", [128, 8, NSEQ])
    w_ada_d = din("w_ada", [D, 6 * D])
    b_ada_d = din("b_ada_rep", [NSEQ, 6 * D])
    g1T_d = din("g1T", [128, 8])
    w_in_d = din("w_in", [D, 1536])
    w_gate_d = din("w_gate", [D, 2048])
    b_gateT_d = din("b_gateT", [128, 16])
    lam_sm_d = din("lam_sm", [128, 3, 16])
    Bsm_d = din("Bsm", [128, 2, 16, 128])
    Csm_d = din("Csm", [128, 2, 16, 128])
    dT_d = din("dT", [128, 4])
    w_glu_d = din("w_glu", [512, 512])
    b_gluT_d = din("b_gluT", [128, 4])
    wsT_d = din("wsT", [128, 8, 128])
    lngT_d = din("lngT", [128, 4])
    lnbT_d = din("lnbT", [128, 4])
    bsT_d = din("bsT", [128, 4, 128])
    w_bra_d = din("w_bra", [512, D])
    w_brb_d = din("w_brb", [512, D])
    w_out_d = din("w_out", [D, D])
    g2_d = din("g2", [1, D])
    w_r_d = din("w_r", [128, 8, 36])
    b_r_d = din("b_r", [1, 36])
    w1_d = din("w1", [32, D, 512])
    w3_d = din("w3", [32, D, 512])
    w2_d = din("w2", [32, 512, D])
    gf_d = din("gf", [1, D])
    out_d = nc.dram_tensor("out", [NTOK, D], F32, kind="ExternalOutput").ap()
    mod_d = Tl(nc.dram_tensor("mod_d", [NSEQ, 6 * D], F32, kind="Internal").ap(), "mod_d")
    H_d = nc.dram_tensor("H_d", [NTOK, D], F32, kind="Internal").ap()
    X_d = nc.dram_tensor("X_d", [NSLOT, D], BF16, kind="Internal").ap()
    Y_d = nc.dram_tensor("Y_d", [NSLOT, D], F32, kind="Internal").ap()
    r_X = Res("X_d")
    r_Y = Res("Y_d")
    r_H = Res("H_d")

    S = Sched(nc)
    final_ops = []
    with ExitStack() as st:
        A = Arena(nc, st, int(os.environ.get("ARENA_CAP", "212800")))
        ps_all = st.enter_context(nc.psum_tensor("ps_all", [128, 8, 512], F32))
        PSB = [Tl(ps_all[:, b, :], "psb%d" % b) for b in range(8)]

        def psv(b, shape, dt=F32):
            v = PSB[b].ap
            if dt == BF16:
                v = v.bitcast(BF16)
            n = 1
            for s in shape[1:]:
                n *= s
            v = v[0:shape[0], 0:n]
            if len(shape) == 3:
                v = v.rearrange("p (a b) -> p a b", a=shape[1])
            elif len(shape) == 4:
                v = v.rearrange("p (a b c) -> p a b c", a=shape[1], b=shape[2])
            return v

        def psv2(b, shape):
            v = ps_all[:, b:b + 2, :].rearrange("p a b -> p (a b)")
            n = 1
            for s in shape[1:]:
                n *= s
            v = v[0:shape[0], 0:n]
            if len(shape) == 3:
                v = v.rearrange("p (a b) -> p a b", a=shape[1])
            elif len(shape) == 4:
                v = v.rearrange("p (a b c) -> p a b c", a=shape[1], b=shape[2])
            return v

        def R(*ts):
            return [t.r if isinstance(t, Tl) else t for t in ts]

        def fsz(ap):
            n = 1
            for s_ in ap.shape[1:]:
                n *= s_
            return n

        def isz(ap):
            return 2 if ap.dtype == BF16 else 4

        def ecost(eng, n):
            if eng == "dve":
                return 0.1 + n / 900.0
            if eng == "pool":
                return 0.25 + n / 430.0
            return 0.2 + n / 1150.0

        def dma(eng, out, in_, reads, writes, key, **kw):
            nb = out.shape[0] * fsz(out) * isz(out)
            return S.add(eng, lambda e: e.dma_start(out=out, in_=in_, **kw), R(*reads), R(*writes), dma_key=key,
                         cost=(1.0 if eng == "pool" else 0.07), lat=2.0 + nb / 180e3)

        def tt(eng, out, in0, in1, op, reads, writes):
            c = 0.45 if op == ALU.pow else ecost(eng, fsz(out))
            return S.add(eng, lambda e: e.tensor_tensor(out=out, in0=in0, in1=in1, op=op), R(*reads), R(*writes), cost=c)

        def ts(eng, out, in0, s1, s2, op0, op1, reads, writes, accum=None):
            c = ecost(eng, fsz(out))
            if op1 is None:
                return S.add(eng, lambda e: e.tensor_scalar(out=out, in0=in0, scalar1=s1, scalar2=None, op0=op0), R(*reads), R(*writes), cost=c)
            if accum is not None:
                return S.add(eng, lambda e: e.tensor_scalar(out=out, in0=in0, scalar1=s1, scalar2=s2, op0=op0, op1=op1, accum_out=accum), R(*reads), R(*writes), cost=c)
            return S.add(eng, lambda e: e.tensor_scalar(out=out, in0=in0, scalar1=s1, scalar2=s2, op0=op0, op1=op1), R(*reads), R(*writes), cost=c)

        def stt(out, in0, scalar, in1, op0, op1, reads, writes):
            return S.add("dve", lambda e: e.scalar_tensor_tensor(out=out, in0=in0, scalar=scalar, in1=in1, op0=op0, op1=op1), R(*reads), R(*writes),
                         cost=ecost("dve", fsz(out)))

        def act(out, in_, func, reads, writes, bias=None, scale=None, accum=None):
            kw = {}
            if bias is not None:
                kw["bias"] = bias
            if scale is not None:
                kw["scale"] = scale
            if accum is not None:
                kw["accum_out"] = accum
            return S.add("act", lambda e: e.activation(out=out, in_=in_, func=func, **kw), R(*reads), R(*writes),
                         cost=ecost("act", fsz(out)) + (0.1 if accum is not None else 0.0))

        def cp(eng, out, in_, reads, writes):
            c = ecost(eng, fsz(out))
            if eng == "act":
                return S.add("act", lambda e: e.copy(out=out, in_=in_), R(*reads), R(*writes), cost=c)
            return S.add(eng, lambda e: e.tensor_copy(out=out, in_=in_), R(*reads), R(*writes), cost=c)

        def mm(out, lhsT, rhs, start, stop, reads, writes):
            c = 0.03 + fsz(rhs) / 2400.0 * (4.0 if lhsT.dtype == F32 else 1.0)
            return S.add("pe", lambda e: e.matmul(out, lhsT=lhsT, rhs=rhs, start=start, stop=stop), R(*reads), R(*writes), cost=c)

        def tr(out, in_, ident, reads, writes):
            c = 0.03 + 128 / 2400.0 * (2.0 if in_.dtype == F32 else 1.0)
            return S.add("pe", lambda e: e.transpose(out=out, in_=in_, identity=ident), R(*reads), R(*writes), cost=c)

        def memset(eng, ap, val, writes):
            return S.add(eng, lambda e: e.memset(ap, val), [], R(*writes))

        _regs = {}

        def breg(e, val):
            if val not in _regs:
                _regs[val] = e.to_reg(val)
            return _regs[val]

        def dump(name, t, ap=None):
            ap = t.ap if ap is None else ap
            shp = list(ap.shape)
            o = nc.dram_tensor("dbg_" + name, shp, ap.dtype, kind="ExternalOutput").ap()
            dbg_outs[name] = "dbg_" + name
            final_ops.append(dma("sp", o, ap, [t], [], "dbg_" + name))

        ident_f = A.alloc("ident_f", [128, 128], F32)
        ident_b = A.alloc("ident_b", [128, 128], BF16)
        tri_b = A.alloc("tri_b", [128, 128], BF16)
        ones_b = A.alloc("ones_b", [128, 128], BF16)
        memset("pool", ident_f.ap, 0.0, [ident_f])
        S.add("pool", lambda e: e.affine_select(out=ident_f.ap, in_=ident_f.ap, pattern=[[-1, 128]], compare_op=ALU.not_equal,
                                                  fill=1.0, base=0, channel_multiplier=1), R(ident_f), R(ident_f))
        cp("pool", ident_b.ap, ident_f.ap, [ident_f], [ident_b])
        memset("pool", ones_b.ap, 1.0, [ones_b])
        S.add("pool", lambda e: e.affine_select(out=tri_b.ap, in_=ones_b.ap, pattern=[[1, 128]], compare_op=ALU.is_gt,
                                                  fill=0.0, base=0, channel_multiplier=-1), R(ones_b), R(tri_b))

        wgt = A.alloc("wgt", [128, NTILES, 2], F32)
        sloti = A.alloc("sloti", [128, NTILES, 2], I32)
        base = A.alloc("base", [128, 32], F32)
        ecap = A.alloc("ecap", [128, 32], F32)
        memset("pool", base.ap, 0.0, [base])
        ecap_i = A.alloc("ecap_i", [128, 32], I32)
        S.add("pool", lambda e: e.iota(ecap_i.ap, pattern=[[CAP, 32]], base=0, channel_multiplier=0), [], R(ecap_i))
        cp("pool", ecap.ap, ecap_i.ap, [ecap_i], [ecap])
        A1T = A.alloc("A1T", [128, 8, NSEQ], F32)
        sh1T = A.alloc("sh1T", [128, 8, NSEQ], F32)
        eps_rms = A.alloc("eps_rms", [128, 1], F32)
        eps_ln = A.alloc("eps_ln", [128, 1], F32)
        memset("pool", eps_rms.ap, 1e-6, [eps_rms])
        memset("pool", eps_ln.ap, 1e-5, [eps_ln])
        mhalf = A.alloc("mhalf", [128, 1], F32)
        memset("pool", mhalf.ap, -0.5, [mhalf])

        mark_persist = A.mark()

        cact = A.alloc("cact", [128, 8, NSEQ], F32)
        dma("sp", cact.ap, cT_d, [], [cact], "cact")
        act(cact.ap, cact.ap, AF.Silu, [cact], [cact])
        modrow = A.alloc("modrow", [NSEQ, 6 * D], F32)
        bada = A.alloc("bada", [NSEQ, 6 * D], F32)
        dma("sp", bada.ap, b_ada_d, [], [bada], "bada")
        wa = [A.alloc("wa%d" % i, [128, 8, 512], F32) for i in range(2)]
        wa_view = w_ada_d.rearrange("(kc p) n -> p kc n", p=128)
        for cb in range(12):
            w = wa[cb % 2]
            dma("sp", w.ap, wa_view[:, :, cb * 512:(cb + 1) * 512], [], [w], "wa%d" % (cb % 2))
            pb = PSB[cb % 2]
            for kc in range(8):
                mm(pb.ap[0:NSEQ, :], cact.ap[:, kc, :], w.ap[:, kc, :], kc == 0, kc == 7, [cact, w], [pb])
            tt("dve", modrow.ap[:, cb * 512:(cb + 1) * 512], pb.ap[0:NSEQ, :], bada.ap[:, cb * 512:(cb + 1) * 512], ALU.add,
               [pb, bada], [modrow])
        dma("sp", mod_d.ap, modrow.ap, [modrow], [mod_d], "mod_d")
        sc1T = A.alloc("sc1T", [128, 8, NSEQ], F32)
        g1T = A.alloc("g1T", [128, 8], F32)
        dma("sp", g1T.ap, g1T_d, [], [g1T], "g1T")
        for b in range(NSEQ):
            S.add("sp", lambda e, b=b: e.dma_start(out=sh1T.ap[:, :, b], in_=mod_d.ap[b, 0:D].rearrange("(kc p) -> p kc", p=128),
                                                  allow_slow_non_contiguous=True), R(mod_d), R(sh1T), dma_key="sh1T")
            S.add("sp", lambda e, b=b: e.dma_start(out=sc1T.ap[:, :, b], in_=mod_d.ap[b, D:2 * D].rearrange("(kc p) -> p kc", p=128),
                                                  allow_slow_non_contiguous=True), R(mod_d), R(sc1T), dma_key="sc1T")
        for b in range(NSEQ):
            stt(A1T.ap[:, :, b], sc1T.ap[:, :, b], 1.0, g1T.ap, ALU.add, ALU.mult, [sc1T, g1T], [A1T])
        if "mod" in dbg:
            dump("modrow", modrow)
            dump("A1T", A1T)
        A.reset(mark_persist)
        S.barrier()
        if stop_after == "mod":
            S.finish(final_ops)
            S.emit(st)
            return nc, dbg_outs

        w_r = A.alloc("w_r", [128, 8, 36], F32)
        dma("sp", w_r.ap, w_r_d, [], [w_r], "w_r")
        b_r = A.alloc("b_r", [128, 36], F32)
        dma("sp", b_r.ap, b_r_d.partition_broadcast(128), [], [b_r], "b_r")
        b_gateT = A.alloc("b_gateT", [128, 16], F32)
        dma("sp", b_gateT.ap, b_gateT_d, [], [b_gateT], "b_gateT")
        ts("pool", b_gateT.ap, b_gateT.ap, 0.5, 1.0, ALU.mult, ALU.mult, [b_gateT], [b_gateT])
        b_gluT = A.alloc("b_gluT", [128, 4], F32)
        dma("sp", b_gluT.ap, b_gluT_d, [], [b_gluT], "b_gluT")
        ts("pool", b_gluT.ap, b_gluT.ap, 0.5, 1.0, ALU.mult, ALU.mult, [b_gluT], [b_gluT])
        dT = A.alloc("dT", [128, 4], F32)
        dma("sp", dT.ap, dT_d, [], [dT], "dT")
        lngT = A.alloc("lngT", [128, 4], F32)
        dma("sp", lngT.ap, lngT_d, [], [lngT], "lngT")
        lnbT = A.alloc("lnbT", [128, 4], F32)
        dma("sp", lnbT.ap, lnbT_d, [], [lnbT], "lnbT")

        LC = 4
        NCH = 128 // LC
        tabC4 = A.alloc("tabC4", [128, 16, NCH + 1], F32)
        tabD4 = A.alloc("tabD4", [128, 16, NCH + 1], F32)
        Rtab = A.alloc("Rtab", [128, 16, 2, NCH], F32)
        rmagL = A.alloc("rmagL", [128, 16], F32)
        Pt = A.alloc("Pt", [128, 2, 8 * LC, 128], BF16)
        Qs = A.alloc("Qs", [128, 2, 16 * LC, 64], BF16)
        Kb = A.alloc("Kb", [128, 4 * LC, 128], BF16)
        tabC = Tl(None, "s5scr")
        mark_setup = A.mark()

        def range_reduce(ph, tmpf, tmpi, n):
            ts("dve", tmpi, ph, 1.0 / TWO_PI, None, ALU.mult, None, [tabC], [tabC])
            cp("dve", tmpf, tmpi, [tabC], [tabC])
            stt(ph, tmpf, -TWO_PI, ph, ALU.mult, ALU.add, [tabC], [tabC])
            wrap(ph, tmpf)

        def wrap(ph, tmpf):
            ts("dve", tmpf, ph, math.pi, None, ALU.is_gt, None, [tabC], [tabC])
            stt(ph, tmpf, -TWO_PI, ph, ALU.mult, ALU.add, [tabC], [tabC])
            ts("dve", tmpf, ph, -math.pi, None, ALU.is_lt, None, [tabC], [tabC])
            stt(ph, tmpf, TWO_PI, ph, ALU.mult, ALU.add, [tabC], [tabC])

        def scr(name, shape, dt):
            t = A.alloc(name, shape, dt)
            tabC.r.readers.extend(t.r.readers)
            t.r = tabC.r
            for k_, (a_, b_, o_) in enumerate(A.live):
                if o_ is t:
                    A.live[k_] = (a_, b_, t)
            return t

        TC = [tabC]
        lam = scr("lam", [128, 3, 16], F32)
        dma("sp", lam.ap, lam_sm_d, [], TC, "lam")
        zs = {n: scr("z_" + n, [128, 16], F32) for n in ("dt", "th", "lre", "lrdt", "den", "nr", "fre", "fim", "t0", "t1")}
        sv_i = scr("sv_i", [128, 129], I32)
        sv = scr("sv", [128, 129], F32)
        tabCf = scr("tabCf", [128, 16, 129], F32)
        tabDf = scr("tabDf", [128, 16, 129], F32)
        tmpf = scr("tmpf", [128, 16 * 129], F32)
        tmpi = scr("tmpi", [128, 16 * 129], I32)
        aim = lam.ap[:, 1, :]
        S.add("pool", lambda e: e.iota(sv_i.ap, pattern=[[1, 129]], base=0, channel_multiplier=0), [], R(tabC))
        cp("dve", sv.ap, sv_i.ap, TC, TC)
        ts("dve", zs["lre"].ap, lam.ap[:, 0, :], -1e-4, None, ALU.min, None, TC, TC)
        act(zs["dt"].ap, lam.ap[:, 2, :], AF.Exp, TC, TC)
        tt("dve", zs["lrdt"].ap, zs["lre"].ap, zs["dt"].ap, ALU.mult, TC, TC)
        tt("dve", zs["th"].ap, aim, zs["dt"].ap, ALU.mult, TC, TC)
        for j in range(16):
            ts("dve", tabDf.ap[:, j, :], sv.ap, zs["th"].ap[:, j:j + 1], None, ALU.mult, None, TC, TC)
        phD = tabDf.ap.rearrange("p a b -> p (a b)")
        phC = tabCf.ap.rearrange("p a b -> p (a b)")
        range_reduce(phD, tmpf.ap, tmpi.ap, 16 * 129)
        ts("dve", phC, phD, math.pi / 2, None, ALU.add, None, TC, TC)
        wrap(phC, tmpf.ap)
        act(phD, phD, AF.Sin, TC, TC)
        act(phC, phC, AF.Sin, TC, TC)
        cp("dve", tabC4.ap, tabCf.ap[:, :, 0:129:LC], TC, TC + [tabC4])
        cp("dve", tabD4.ap, tabDf.ap[:, :, 0:129:LC], TC, TC + [tabD4])
        rk = scr("rk", [128, 16, LC + 1], F32)
        pwr = scr("pwr", [128, 16, LC + 1], F32)
        pwi = scr("pwi", [128, 16, LC + 1], F32)
        for k in range(LC + 1):
            act(rk.ap[:, :, k], zs["lrdt"].ap, AF.Exp, TC, TC, scale=float(k))
        tt("dve", pwr.ap, rk.ap, tabCf.ap[:, :, 0:LC + 1], ALU.mult, TC, TC)
        tt("dve", pwi.ap, rk.ap, tabDf.ap[:, :, 0:LC + 1], ALU.mult, TC, TC)
        cp("dve", rmagL.ap, rk.ap[:, :, LC], TC, TC + [rmagL])
        cp("dve", Rtab.ap.rearrange("p a b c -> p a (b c)"), rmagL.ap.unsqueeze(2).to_broadcast([128, 16, 2 * NCH]),
           TC + [rmagL], TC + [Rtab])
        memset("dve", Rtab.ap[:, :, :, 0], 0.0, TC + [Rtab])
        abr, abi = pwr.ap[:, :, 1], pwi.ap[:, :, 1]
        lre_ = zs["lre"].ap
        tt("dve", zs["den"].ap, lre_, lre_, ALU.mult, TC, TC)
        tt("dve", zs["t0"].ap, aim, aim, ALU.mult, TC, TC)
        tt("dve", zs["den"].ap, zs["den"].ap, zs["t0"].ap, ALU.add, TC, TC)
        S.add("dve", lambda e: e.reciprocal(out=zs["den"].ap, in_=zs["den"].ap), R(tabC), R(tabC))
        ts("dve", zs["nr"].ap, abr, -1.0, None, ALU.add, None, TC, TC)
        tt("dve", zs["t0"].ap, zs["nr"].ap, lre_, ALU.mult, TC, TC)
        tt("dve", zs["t1"].ap, abi, aim, ALU.mult, TC, TC)
        tt("dve", zs["t0"].ap, zs["t0"].ap, zs["t1"].ap, ALU.add, TC, TC)
        tt("dve", zs["fre"].ap, zs["t0"].ap, zs["den"].ap, ALU.mult, TC, TC)
        tt("dve", zs["t0"].ap, abi, lre_, ALU.mult, TC, TC)
        tt("dve", zs["t1"].ap, zs["nr"].ap, aim, ALU.mult, TC, TC)
        tt("dve", zs["t0"].ap, zs["t0"].ap, zs["t1"].ap, ALU.subtract, TC, TC)
        tt("dve", zs["fim"].ap, zs["t0"].ap, zs["den"].ap, ALU.mult, TC, TC)
        Bsm = scr("Bsm", [128, 2, 16, 128], F32)
        Csm = scr("Csm", [128, 2, 16, 128], F32)
        dma("sp", Bsm.ap, Bsm_d, [], TC, "Bsm")
        dma("sp", Csm.ap, Csm_d, [], TC, "Csm")
        Btr = scr("Btr", [128, 16, 128], F32)
        Bti = scr("Bti", [128, 16, 128], F32)
        Gr = scr("Gr", [128, 16, 128], F32)
        Gi = scr("Gi", [128, 16, 128], F32)
        w0 = scr("w0", [128, 16, 128], F32)
        w1_ = scr("w1_", [128, 16, 128], F32)
        Psr = scr("Psr", [128, 16, 128], BF16)
        Psi = scr("Psi", [128, 16, 128], BF16)
        Kf = scr("Kf", [128, 128], F32)

        def bc(v):
            return v.unsqueeze(2).to_broadcast([128, 16, 128])

        def cmul(o_re, o_im, a_re, a_im, s_re, s_im, neg_im=False):
            tt("dve", w0.ap, a_re, bc(s_re), ALU.mult, TC, TC)
            tt("dve", w1_.ap, a_im, bc(s_im), ALU.mult, TC, TC)
            tt("dve", o_re, w0.ap, w1_.ap, ALU.subtract, TC, TC)
            tt("dve", w0.ap, a_re, bc(s_im), ALU.mult, TC, TC)
            tt("dve", w1_.ap, a_im, bc(s_re), ALU.mult, TC, TC)
            if neg_im:
                stt(o_im, w0.ap, -1.0, w1_.ap, ALU.mult, ALU.subtract, TC, TC)
            else:
                tt("dve", o_im, w0.ap, w1_.ap, ALU.add, TC, TC)

        cmul(Btr.ap, Bti.ap, Bsm.ap[:, 0], Bsm.ap[:, 1], zs["fre"].ap, zs["fim"].ap)
        memset("dve", Kb.ap, 0.0, TC + [Kb])
        for k in range(LC + 1):
            cmul(Gr.ap, Gi.ap, Csm.ap[:, 0], Csm.ap[:, 1], pwr.ap[:, :, k], pwi.ap[:, :, k], neg_im=True)
            if k >= 1:
                for j in range(16):
                    c0 = 64 * ((j % 4) // 2)
                    cp("act", Qs.ap[:, 0, j * LC + k - 1, :], Gr.ap[:, j, c0:c0 + 64], TC, TC + [Qs])
                    cp("act", Qs.ap[:, 1, j * LC + k - 1, :], Gi.ap[:, j, c0:c0 + 64], TC, TC + [Qs])
            if k < LC:
                for q in range(4):
                    pK = PSB[2 + q % 2]
                    for jm in range(4):
                        j = 4 * q + jm
                        mm(pK.ap[:, 0:128], Btr.ap[:, j, :], Gr.ap[:, j, :], jm == 0, False, TC, [pK])
                        mm(pK.ap[:, 0:128], Bti.ap[:, j, :], Gi.ap[:, j, :], False, jm == 3, TC, [pK])
                    if k == 0:
                        stt(Kf.ap, ident_f.ap, dT.ap[:, q:q + 1], pK.ap[:, 0:128], ALU.mult, ALU.add, [pK, ident_f, dT] + TC, TC)
                        cp("dve", Kb.ap[:, q * LC + k, :], Kf.ap, TC, TC + [Kb])
                    else:
                        cp("dve", Kb.ap[:, q * LC + k, :], pK.ap[:, 0:128], [pK] + TC, TC + [Kb])
                s_ = LC - 1 - k
                cmul(Psr.ap, Psi.ap, Btr.ap, Bti.ap, pwr.ap[:, :, k], pwi.ap[:, :, k])
                for part, Ps_ in enumerate((Psr, Psi)):
                    for pr in range(2):
                        bk = (2 * part + pr) % 2
                        pT = psv(bk, [128, 8, 128], BF16)
                        for idx in range(8):
                            j = 4 * (idx // 2) + 2 * pr + idx % 2
                            tr(pT[64 * pr:64 * pr + 64, idx, :], Ps_.ap[:, j, 64 * pr:64 * pr + 64], ident_b.ap, TC + [ident_b], [PSB[bk]])
                        cp("act", Pt.ap[64 * pr:64 * pr + 64, part, s_::LC, :], pT[64 * pr:64 * pr + 64, :, :], [PSB[bk]] + TC, TC + [Pt])
        if "s5setup" in dbg:
            dump("tabC4", tabC4)
            dump("Kb", Kb)
            dump("Pt", Pt)
            dump("Qs", Qs)
        A.reset(mark_setup)

        wsT_b = A.alloc("wsT_b", [128, 8, 128], BF16)
        sgub = A.alloc("sgub", [128, 4, 128], F32)
        mark_sgu = A.mark()
        wsf = A.alloc("wsf", [128, 8, 128], F32)
        dma("sp", wsf.ap, wsT_d, [], [wsf], "wsf")
        for h in range(8):
            S.add("pool", lambda e, h=h: e.affine_select(out=wsf.ap[:, h, :], in_=wsf.ap[:, h, :], pattern=[[1, 128]],
                                                           compare_op=ALU.is_ge, fill=0.0, base=0, channel_multiplier=-1),
                  R(wsf), R(wsf))
        cp("pool", wsT_b.ap, wsf.ap, [wsf], [wsT_b])
        bsT = A.alloc("bsT", [128, 4, 128], F32)
        dma("sp", bsT.ap, bsT_d, [], [bsT], "bsT")
        pmix0 = psv(3, [128, 4, 128])
        for h in range(8):
            po = (h % 2) * 64
            mm(pmix0[po:po + 64, h // 2, :], ones_b.ap[:, 0:64], wsT_b.ap[:, h, :], True, True, [ones_b, wsT_b], [PSB[3]])
        for q in range(4):
            stt(sgub.ap[:, q, :], pmix0[:, q, :], lnbT.ap[:, q:q + 1], bsT.ap[:, q, :], ALU.mult, ALU.add,
                [PSB[3], lnbT, bsT], [sgub])
        if "sgusetup" in dbg:
            dump("sgub", sgub)
            dump("wsT_b", wsT_b)
        A.reset(mark_sgu)

        if stop_after == "setup":
            S.finish(final_ops)
            S.emit(st)
            return nc, dbg_outs

        def wload(name, src_view, shape, key=None):
            t = A.alloc(name, shape, BF16)
            dma("pool", t.ap, src_view, [], [t], key or name)
            return t

        w_in_b = wload("w_in_b", w_in_d.rearrange("(kc p) n -> p kc n", p=128), [128, 8, 1536])
        w_gate_b = wload("w_gate_b", w_gate_d.rearrange("(kc p) n -> p kc n", p=128), [128, 8, 2048])
        w_glu_b = wload("w_glu_b", w_glu_d.rearrange("(kc p) n -> p kc n", p=128), [128, 4, 512])
        w_bra_b = wload("w_bra_b", w_bra_d.rearrange("(kc p) n -> p kc n", p=128), [128, 4, D])
        ts("pool", w_bra_b.ap, w_bra_b.ap, 0.5, 1.0, ALU.mult, ALU.mult, [w_bra_b], [w_bra_b])
        w_brb_b = wload("w_brb_b", w_brb_d.rearrange("(kc p) n -> p kc n", p=128), [128, 4, D])
        w_out_b = wload("w_out_b", w_out_d.rearrange("(kc p) n -> p kc n", p=128), [128, 8, D])
        def alias(name, shape, dt, of, off=0):
            t = Tl(None, name)
            n = 1
            for s_ in shape[1:]:
                n *= s_
            base_ = of.ap
            if len(base_.shape) == 3:
                base_ = base_.rearrange("p a b -> p (a b)")
            elif len(base_.shape) == 4:
                base_ = base_.rearrange("p a b c -> p (a b c)")
            if base_.dtype != dt:
                base_ = base_.bitcast(dt)
            v = base_[:, off:off + n]
            if len(shape) == 3:
                v = v.rearrange("p (a b) -> p a b", a=shape[1])
            t.ap = v
            t.r = of.r
            return t

        xt0 = A.alloc("xt0", [128, D], F32)
        NBR = int(os.environ.get("NBR", "1"))
        NBX = int(os.environ.get("NBX", str(NBR)))
        xr_l = [A.alloc("xr%d" % i, [128, D], F32) for i in range(NBX)]
        xsb = A.alloc("xsb", [128, D], BF16)
        ssq = A.alloc("ssq", [128, 1], F32)
        rstd = A.alloc("rstd", [128, 1], F32)
        xnT = [A.alloc("xnT%d" % i, [128, 8, 128], BF16) for i in range(3)]
        uT = [A.alloc("uT%d" % i, [128, 4, 128], BF16) for i in range(2)]
        guT = A.alloc("guT", [128, 4, 128], F32)
        gv = A.alloc("gv", [128, 512], F32)
        vst = A.alloc("vst", [128, 6], F32)
        vmv = A.alloc("vmv", [128, 2], F32)
        vrs = A.alloc("vrs", [128, 1], F32)
        vhat = A.alloc("vhat", [128, 512], BF16)
        mixT = alias("mixT", [128, 4, 128], F32, gv)
        ybT = [A.alloc("ybT%d" % i, [128, 4, 128], BF16) for i in range(3)]
        xtil = A.alloc("xtil", [128, 4, 2, NCH], F32)
        s5a = A.alloc("s5a", [128, 4, NCH], F32)
        s5b = A.alloc("s5b", [128, 4, NCH], F32)
        gsc = A.alloc("gsc", [128, 4, 2, NCH], F32)
        gp = A.alloc("gp", [128, 16, 2], F32)
        carry = A.alloc("carry", [128, 16, 2], F32)
        sm1 = A.alloc("sm1", [128, 16, 2], F32)
        cf2 = A.alloc("cf2", [128, 4, 2], F32)
        c4 = [A.alloc("c4_%d" % i, [128, 16], F32) for i in range(4)]
        Sprev = [A.alloc("Sprev%d" % i, [128, 4, 2, NCH], BF16) for i in range(2)]
        ygT = A.alloc("ygT", [128, 4, 128], BF16)
        sg = alias("sg", [128, 4, 128], F32, xsb) if os.environ.get("SGALIAS", "1") == "1" else A.alloc("sg", [128, 4, 128], F32)
        yaT = [A.alloc("yaT%d" % i, [128, 4, 128], BF16) for i in range(2)]
        gates_l = [A.alloc("gates%d" % i, [128, 16, 128], F32) for i in range(NBR)]
        if os.environ.get("XN2SEP", "1") == "1":
            xn2_l = [A.alloc("xn2_%d" % i, [128, D], F32) for i in range(NBR)]
            xn2T_l = [alias("xn2T%d" % i, [128, 8, 128], F32, xr_l[i % NBX]) for i in range(NBR)]
        else:
            xn2T_l = [alias("xn2T%d" % i, [128, 8, 128], F32, gates_l[i], off=1024) for i in range(NBR)]
            xn2_l = [alias("xn2_%d" % i, [128, D], F32, gates_l[i], off=0) for i in range(NBR)]
        NBM = int(os.environ.get("NBM", "1"))
        mergedT_l = [A.alloc("mergedT%d" % i, [128, 8, 128], BF16) for i in range(NBM)]
        jk2_l = [alias("jk2_%d" % i, [128, D], BF16, mergedT_l[i]) for i in range(NBM)]
        gt1b = A.alloc("gt1b", [128, D], F32)
        A2b = A.alloc("A2b", [128, D], F32)
        sh2b = A.alloc("sh2b", [128, D], F32)
        ssq2_l = [A.alloc("ssq2_%d" % i, [128, 1], F32) for i in range(NBR)]
        rstd2_l = [A.alloc("rstd2_%d" % i, [128, 1], F32) for i in range(NBR)]
        lg_l = [A.alloc("lg%d" % i, [128, 36], F32) for i in range(NBR)]
        rt_l = [{n: A.alloc("rt%d_" % i + n, [128, w_], F32) for n, w_ in
                 [("gmax", 1), ("ngmax", 1), ("maskg", 4), ("eg", 4), ("sume", 1), ("pgs", 1), ("pen", 4), ("lem", 32),
                  ("m1", 1), ("oh1", 32), ("lem2", 32), ("m2", 1), ("oh2", 32), ("dm", 1), ("e2", 1), ("p1", 1), ("p2", 1),
                  ("rank", 32), ("slotv", 32), ("junk", 32), ("sl", 2), ("val", 32), ("vk", 2)]} for i in range(NBR)]
        ohb_l = [A.alloc("ohb%d" % i, [128, 32], BF16) for i in range(NBR)]

        def rms_rstd(src, ssq_t, rstd_t, junk_ap, junk_t):
            act(junk_ap, src.ap, AF.Square, [src], [junk_t, ssq_t], accum=ssq_t.ap)
            ts("pool", ssq_t.ap, ssq_t.ap, 1.0 / D, RMS_EPS, ALU.mult, ALU.add, [ssq_t], [ssq_t])
            tt("pool", rstd_t.ap, ssq_t.ap, mhalf.ap, ALU.pow, [ssq_t, mhalf], [rstd_t])

        store_ops = []
        scat_ops = []
        c128 = tabC4.ap[:, :, NCH]
        d128 = tabD4.ap[:, :, NCH]

        def seqof(i):
            return i // NT

        def P1(i):
            b = seqof(i)
            X = xt0
            XN = xnT[i % 3]
            if i == 0:
                dma("sp", X.ap, x_d[0:128, :], [], [X], "xt0")
            rms_rstd(X, ssq, rstd, xsb.ap, xsb)
            act(xsb.ap, X.ap, AF.Copy, [X, rstd], [xsb], scale=rstd.ap[:, 0:1])
            if i + 1 < NTILES:
                dma("sp", X.ap, x_d[(i + 1) * 128:(i + 2) * 128, :], [], [X], "xt0")
            pX = psv(0, [128, 8, 128], BF16)
            for kc in range(8):
                tr(pX[:, kc, :], xsb.ap[:, kc * 128:(kc + 1) * 128], ident_b.ap, [xsb, ident_b], [PSB[0]])
            for kc in range(8):
                act(XN.ap[:, kc, :], pX[:, kc, :], AF.Identity, [PSB[0], A1T, sh1T], [XN],
                    bias=sh1T.ap[:, kc, b:b + 1], scale=A1T.ap[:, kc, b:b + 1])
            if dbg.get("tile") == i:
                dump("xnT", XN)

        def P2(i):
            XN = xnT[i % 3]
            UT = uT[i % 2]
            pZa = psv(1, [128, 4, 128])
            pZu = psv(0, [128, 4, 128])
            for m in range(4):
                for kc in range(8):
                    mm(pZa[:, m, :], w_in_b.ap[:, kc, m * 128:(m + 1) * 128], XN.ap[:, kc, :], kc == 0, kc == 7,
                       [w_in_b, XN], [PSB[1]])
            cp("act", UT.ap, pZa, [PSB[1]], [UT])
            for m in range(4):
                for kc in range(8):
                    mm(pZu[:, m, :], w_in_b.ap[:, kc, 512 + m * 128:512 + (m + 1) * 128], XN.ap[:, kc, :], kc == 0, kc == 7,
                       [w_in_b, XN], [PSB[0]])
            act(guT.ap, pZu, AF.Gelu_apprx_tanh, [PSB[0]], [guT])
            pV = psv(1, [128, 512])
            for kc in range(8):
                mm(pV, XN.ap[:, kc, :], w_in_b.ap[:, kc, 1024:1536], kc == 0, kc == 7, [w_in_b, XN], [PSB[1]])
            act(gv.ap, pV, AF.Gelu_apprx_tanh, [PSB[1]], [gv])
            if dbg.get("tile") == i:
                dump("uT", UT)

        def P3(i):
            YB = ybT[i % 3]
            S.add("dve", lambda e: e.bn_stats(out=vst.ap, in_=gv.ap), R(gv), R(vst))
            S.add("dve", lambda e: e.bn_aggr(out=vmv.ap, in_=vst.ap), R(vst), R(vmv))
            ts("pool", vrs.ap, vmv.ap[:, 1:2], 1.0, LN_EPS, ALU.mult, ALU.add, [vmv], [vrs])
            tt("pool", vrs.ap, vrs.ap, mhalf.ap, ALU.pow, [vrs, mhalf], [vrs])
            ts("dve", vhat.ap, gv.ap, vmv.ap[:, 0:1], vrs.ap[:, 0:1], ALU.subtract, ALU.mult, [gv, vmv, vrs], [vhat])
            pMix = psv(0, [128, 4, 128])
            for h in range(8):
                po = (h % 2) * 64
                mm(pMix[po:po + 64, h // 2, :], vhat.ap[:, h * 64:(h + 1) * 64], wsT_b.ap[:, h, :], True, True,
                   [vhat, wsT_b], [PSB[0]])
            for q in range(4):
                stt(mixT.ap[:, q, :], pMix[:, q, :], lngT.ap[:, q:q + 1], sgub.ap[:, q, :], ALU.mult, ALU.add,
                    [PSB[0], lngT, sgub], [mixT])
            tt("pool", YB.ap, guT.ap, mixT.ap, ALU.mult, [guT, mixT], [YB])
            if dbg.get("tile") == i:
                dump("ybT", YB)

        def pS5(q):
            o = (q % 2) * 4 * NCH
            return ps_all[:, 2:4, o:o + 4 * NCH].rearrange("p k (a b c) -> p k a b c", a=2, b=2)

        def Qpre(i):
            if i % NT == 0:
                memset("dve", carry.ap, 0.0, [carry])
            cT_, dT_ = tabC4.ap[:, :, 1], tabD4.ap[:, :, 1]
            tt("dve", c4[0].ap, carry.ap[:, :, 0], cT_, ALU.mult, [carry, tabC4], [c4[0]])
            tt("dve", c4[1].ap, carry.ap[:, :, 1], dT_, ALU.mult, [carry, tabD4], [c4[1]])
            tt("dve", c4[2].ap, carry.ap[:, :, 1], cT_, ALU.mult, [carry, tabC4], [c4[2]])
            tt("dve", c4[3].ap, carry.ap[:, :, 0], dT_, ALU.mult, [carry, tabD4], [c4[3]])
            tt("dve", sm1.ap[:, :, 0], c4[0].ap, c4[1].ap, ALU.add, [c4[0], c4[1]], [sm1])
            tt("dve", sm1.ap[:, :, 1], c4[2].ap, c4[3].ap, ALU.subtract, [c4[2], c4[3]], [sm1])

        def QB(i, q):
            UT = uT[i % 2]
            pS = pS5(q)
            for jj in range(4):
                ro = 64 * (jj // 2)
                for part in range(2):
                    for sx in range(LC):
                        mm(pS[:, jj // 2, jj % 2, part, :], Pt.ap[ro:ro + 64, part, (2 * q + jj % 2) * LC + sx, :],
                           UT.ap[ro:ro + 64, q, sx::LC], sx == 0, sx == LC - 1, [Pt, UT], [PSB[2 + jj // 2]])

        def QD(i, q):
            pS = pS5(q)
            SP = Sprev[q % 2]
            tc_ = tabC4.ap[:, 4 * q:4 * q + 4, 0:NCH]
            td_ = tabD4.ap[:, 4 * q:4 * q + 4, 0:NCH]
            tc4 = tc_.rearrange("p (k a) c -> p k a c", k=2)
            td4 = td_.rearrange("p (k a) c -> p k a c", k=2)
            a4 = s5a.ap.rearrange("p (k a) c -> p k a c", k=2)
            b4 = s5b.ap.rearrange("p (k a) c -> p k a c", k=2)
            PB = [PSB[2], PSB[3]]
            tt("dve", a4, pS[:, :, :, 0, :], tc4, ALU.mult, PB + [tabC4], [s5a])
            tt("dve", b4, pS[:, :, :, 1, :], td4, ALU.mult, PB + [tabD4], [s5b])
            tt("dve", xtil.ap[:, :, 0, :], s5a.ap, s5b.ap, ALU.add, [s5a, s5b], [xtil])
            tt("dve", a4, pS[:, :, :, 1, :], tc4, ALU.mult, PB + [tabC4, xtil], [s5a])
            tt("dve", b4, pS[:, :, :, 0, :], td4, ALU.mult, PB + [tabD4, xtil], [s5b])
            tt("dve", xtil.ap[:, :, 1, :], s5a.ap, s5b.ap, ALU.subtract, [s5a, s5b], [xtil])
            tt("dve", cf2.ap, carry.ap[:, 4 * q:4 * q + 4, :], rmagL.ap[:, 4 * q:4 * q + 4].unsqueeze(2).to_broadcast([128, 4, 2]),
               ALU.mult, [carry, rmagL], [cf2])
            tt("dve", xtil.ap[:, :, :, 0], xtil.ap[:, :, :, 0], cf2.ap, ALU.add, [xtil, cf2], [xtil])
            S.add("dve", lambda e: e.tensor_tensor_scan(
                out=gsc.ap.rearrange("p a b c -> p (a b c)"),
                data0=Rtab.ap[:, 4 * q:4 * q + 4, :, :].rearrange("p a b c -> p (a b c)"),
                data1=xtil.ap.rearrange("p a b c -> p (a b c)"), initial=0.0, op0=ALU.mult, op1=ALU.add),
                R(Rtab, xtil), R(gsc), cost=0.25 + 8 * NCH / 500.0)
            cp("dve", gp.ap[:, 4 * q:4 * q + 4, :], gsc.ap[:, :, :, NCH - 1], [gsc], [gp])
            n1 = NCH - 1
            tt("dve", s5a.ap[:, :, 0:n1], gsc.ap[:, :, 0, 0:n1], tc_[:, :, 0:n1], ALU.mult, [gsc, tabC4], [s5a])
            tt("dve", s5b.ap[:, :, 0:n1], gsc.ap[:, :, 1, 0:n1], td_[:, :, 0:n1], ALU.mult, [gsc, tabD4], [s5b])
            tt("dve", SP.ap[:, :, 0, 1:NCH], s5a.ap[:, :, 0:n1], s5b.ap[:, :, 0:n1], ALU.subtract, [s5a, s5b], [SP])
            tt("dve", s5a.ap[:, :, 0:n1], gsc.ap[:, :, 1, 0:n1], tc_[:, :, 0:n1], ALU.mult, [gsc, tabC4, SP], [s5a])
            tt("dve", s5b.ap[:, :, 0:n1], gsc.ap[:, :, 0, 0:n1], td_[:, :, 0:n1], ALU.mult, [gsc, tabD4, SP], [s5b])
            tt("dve", SP.ap[:, :, 1, 1:NCH], s5a.ap[:, :, 0:n1], s5b.ap[:, :, 0:n1], ALU.add, [s5a, s5b], [SP])
            cp("dve", SP.ap[:, :, :, 0], sm1.ap[:, 4 * q:4 * q + 4, :], [sm1], [SP])
            if dbg.get("tile") == i and q == 0:
                dump("gsc0", gsc)

        def QC(i, q):
            UT = uT[i % 2]
            SP = Sprev[q % 2]
            pY = psv(4, [128, 4, LC, NCH])
            for sp_ in range(LC):
                first = True
                for sx in range(sp_ + 1):
                    mm(pY[:, q, sp_, :], Kb.ap[:, q * LC + (sp_ - sx), :], UT.ap[:, q, sx::LC], first, False, [Kb, UT], [PSB[4]])
                    first = False
                for jj in range(4):
                    j = 4 * q + jj
                    ro = 64 * (jj // 2)
                    for part in range(2):
                        mm(pY[ro:ro + 64, q, sp_, :], Qs.ap[:, part, j * LC + sp_, :], SP.ap[:, jj, part, :],
                           False, jj == 3 and part == 1, [Qs, SP], [PSB[4]])

        def Qcarry(i):
            tt("dve", c4[0].ap, gp.ap[:, :, 0], c128, ALU.mult, [gp, tabC4], [c4[0]])
            tt("dve", c4[1].ap, gp.ap[:, :, 1], d128, ALU.mult, [gp, tabD4], [c4[1]])
            tt("dve", c4[2].ap, gp.ap[:, :, 1], c128, ALU.mult, [gp, tabC4], [c4[2]])
            tt("dve", c4[3].ap, gp.ap[:, :, 0], d128, ALU.mult, [gp, tabD4], [c4[3]])
            tt("dve", carry.ap[:, :, 0], c4[0].ap, c4[1].ap, ALU.subtract, [c4[0], c4[1]], [carry])
            tt("dve", carry.ap[:, :, 1], c4[2].ap, c4[3].ap, ALU.add, [c4[2], c4[3]], [carry])

        def Qtail(i):
            YA = yaT[i % 2]
            pY = psv(4, [128, 4, LC, NCH])
            for q in range(4):
                act(ygT.ap[:, q, :].rearrange("p (c s) -> p s c", s=LC), pY[:, q, :, :], AF.Gelu_apprx_tanh, [PSB[4]], [ygT])
            if dbg.get("tile") == i:
                dump("ypre", ygT)
            pG = psv(4, [128, 4, 128])
            for m in range(4):
                for kc in range(4):
                    mm(pG[:, m, :], w_glu_b.ap[:, kc, m * 128:(m + 1) * 128], ygT.ap[:, kc, :], kc == 0, kc == 3,
                       [w_glu_b, ygT], [PSB[4]])
            for m in range(4):
                act(sg.ap[:, m, :], pG[:, m, :], AF.Tanh, [PSB[4], b_gluT], [sg], bias=b_gluT.ap[:, m:m + 1], scale=0.5)
            stt(YA.ap, sg.ap, 1.0, ygT.ap, ALU.add, ALU.mult, [sg, ygT], [YA])
            if dbg.get("tile") == i:
                dump("yaT", YA)

        def R0(i):
            b = seqof(i)
            xr, xn2 = xr_l[i % NBX], xn2_l[i % NBR]
            if i % NT == 0:
                dma("sp", gt1b.ap, mod_d.ap[b:b + 1, 2 * D:3 * D].partition_broadcast(128), [mod_d], [gt1b], "gt1b")
                ts("dve", gt1b.ap, gt1b.ap, 0.5, None, ALU.mult, None, [gt1b], [gt1b])
                dma("sp", sh2b.ap, mod_d.ap[b:b + 1, 3 * D:4 * D].partition_broadcast(128), [mod_d], [sh2b], "sh2b")
                dma("sp", A2b.ap, mod_d.ap[b:b + 1, 4 * D:5 * D].partition_broadcast(128), [mod_d], [A2b], "A2b")
                dma("sp", xn2.ap, g2_d.partition_broadcast(128), [], [xn2], "g2tmp")
                stt(A2b.ap, A2b.ap, 1.0, xn2.ap, ALU.add, ALU.mult, [A2b, xn2], [A2b])
            dma("sp", xr.ap, x_d[i * 128:(i + 1) * 128, :], [], [xr], "xr")

        def R1(i, mg):
            XN = xnT[i % 3]
            gates = gates_l[i % NBR]
            bk = 5 if mg % 2 == 0 else 7
            pGt = psv(bk, [128, 4, 128])
            for mm_ in range(4):
                m = mg * 4 + mm_
                for kc in range(8):
                    mm(pGt[:, mm_, :], w_gate_b.ap[:, kc, m * 128:(m + 1) * 128], XN.ap[:, kc, :], kc == 0, kc == 7,
                       [w_gate_b, XN], [PSB[bk]])
            for mm_ in range(4):
                m = mg * 4 + mm_
                act(gates.ap[:, m, :], pGt[:, mm_, :], AF.Tanh, [PSB[bk], b_gateT], [gates],
                    bias=b_gateT.ap[:, m:m + 1], scale=0.5)

        def R2(i, half):
            YA, YB = yaT[i % 2], ybT[i % 3]
            gates, mergedT = gates_l[i % NBR], mergedT_l[i % NBM]
            pA = psv(5, [128, 4, 128])
            pB = psv(6, [128, 4, 128])
            for mm_ in range(4):
                m = half * 4 + mm_
                for kc in range(4):
                    mm(pA[:, mm_, :], w_bra_b.ap[:, kc, m * 128:(m + 1) * 128], YA.ap[:, kc, :], kc == 0, kc == 3,
                       [w_bra_b, YA], [PSB[5]])
            for mm_ in range(4):
                m = half * 4 + mm_
                for kc in range(4):
                    mm(pB[:, mm_, :], w_brb_b.ap[:, kc, m * 128:(m + 1) * 128], YB.ap[:, kc, :], kc == 0, kc == 3,
                       [w_brb_b, YB], [PSB[6]])
            ga = gates.ap[:, half * 4:half * 4 + 4, :]
            gb = gates.ap[:, 8 + half * 4:8 + half * 4 + 4, :]
            stt(ga, ga, 1.0, pA, ALU.add, ALU.mult, [gates, PSB[5]], [gates])
            stt(gb, gb, 1.0, pB, ALU.add, ALU.mult, [gates, PSB[6]], [gates])
            tt("dve", mergedT.ap[:, half * 4:half * 4 + 4, :], ga, gb, ALU.add, [gates], [mergedT])
            if dbg.get("tile") == i and half == 1:
                dump("mergedT", mergedT)

        def R3(i):
            xr, xn2, mergedT, jk2 = xr_l[i % NBX], xn2_l[i % NBR], mergedT_l[i % NBM], jk2_l[i % NBM]
            ssq2, rstd2 = ssq2_l[i % NBR], rstd2_l[i % NBR]
            for half in range(2):
                bk = 6 + half
                for kc in range(8):
                    mm(PSB[bk].ap, mergedT.ap[:, kc, :], w_out_b.ap[:, kc, half * 512:(half + 1) * 512], kc == 0, kc == 7,
                       [mergedT, w_out_b], [PSB[bk]])
                sl = slice(half * 512, (half + 1) * 512)
                tt("dve", xn2.ap[:, sl], PSB[bk].ap, gt1b.ap[:, sl], ALU.mult, [PSB[bk], gt1b], [xn2])
            tt("dve", xr.ap, xr.ap, xn2.ap, ALU.add, [xr, xn2], [xr])
            store_ops.append(dma("sp", H_d[i * 128:(i + 1) * 128, :], xr.ap, [xr], [r_H], "hst"))
            rms_rstd(xr, ssq2, rstd2, jk2.ap, jk2)
            stt(xn2.ap, xr.ap, rstd2.ap[:, 0:1], A2b.ap, ALU.mult, ALU.mult, [xr, rstd2, A2b], [xn2])
            tt("dve", xn2.ap, xn2.ap, sh2b.ap, ALU.add, [xn2, sh2b], [xn2])
            if dbg.get("tile") == i:
                dump("h", xr)
                dump("xn2", xn2)

        def R4(i):
            xn2, xn2T, lg, rt, ohb = xn2_l[i % NBR], xn2T_l[i % NBR], lg_l[i % NBR], rt_l[i % NBR], ohb_l[i % NBR]
            pX2 = psv2(6, [128, 8, 128])
            for kc in range(8):
                tr(pX2[:, kc, :], xn2.ap[:, kc * 128:(kc + 1) * 128], ident_f.ap, [xn2, ident_f], [PSB[6], PSB[7]])
            cp("act", xn2T.ap, pX2, [PSB[6], PSB[7]], [xn2T])
            pL = psv(5, [128, 36])
            for kc in range(8):
                mm(pL, xn2T.ap[:, kc, :], w_r.ap[:, kc, :], kc == 0, kc == 7, [xn2T, w_r], [PSB[5]])
            tt("dve", lg.ap, pL, b_r.ap, ALU.add, [PSB[5], b_r], [lg])
            r_ = rt
            BIG = 1.0e9
            S.add("dve", lambda e: e.tensor_reduce(out=r_["gmax"].ap, in_=lg.ap[:, 0:4], axis=AX.X, op=ALU.max), R(lg), R(r_["gmax"]))
            ts("dve", r_["maskg"].ap, lg.ap[:, 0:4], r_["gmax"].ap[:, 0:1], None, ALU.is_equal, None, [lg, r_["gmax"]], [r_["maskg"]])
            ts("dve", r_["ngmax"].ap, r_["gmax"].ap, -0.5, None, ALU.mult, None, [r_["gmax"]], [r_["ngmax"]])
            act(r_["eg"].ap, lg.ap[:, 0:4], AF.Tanh, [lg, r_["ngmax"]], [r_["eg"]], bias=r_["ngmax"].ap[:, 0:1], scale=0.5)
            ts("dve", r_["pen"].ap, r_["eg"].ap, -1.0, 1.0, ALU.mult, ALU.add, [r_["eg"]], [r_["pen"]])
            S.add("dve", lambda e: e.reciprocal(out=r_["pen"].ap, in_=r_["pen"].ap), R(r_["pen"]), R(r_["pen"]))
            stt(r_["eg"].ap, r_["eg"].ap, 1.0, r_["pen"].ap, ALU.add, ALU.mult, [r_["eg"], r_["pen"]], [r_["eg"]])
            S.add("dve", lambda e: e.tensor_reduce(out=r_["sume"].ap, in_=r_["eg"].ap, axis=AX.X, op=ALU.add), R(r_["eg"]), R(r_["sume"]))
            S.add("dve", lambda e: e.reciprocal(out=r_["pgs"].ap, in_=r_["sume"].ap), R(r_["sume"]), R(r_["pgs"]))
            ts("dve", r_["pen"].ap, r_["maskg"].ap, BIG, -BIG, ALU.mult, ALU.add, [r_["maskg"]], [r_["pen"]])
            for g in range(4):
                ts("dve", r_["lem"].ap[:, g * 8:(g + 1) * 8], lg.ap[:, 4 + g * 8:4 + (g + 1) * 8], r_["pen"].ap[:, g:g + 1], None,
                   ALU.add, None, [lg, r_["pen"]], [r_["lem"]])
            S.add("dve", lambda e: e.tensor_reduce(out=r_["m1"].ap, in_=r_["lem"].ap, axis=AX.X, op=ALU.max), R(r_["lem"]), R(r_["m1"]))
            ts("dve", r_["oh1"].ap, r_["lem"].ap, r_["m1"].ap[:, 0:1], None, ALU.is_equal, None, [r_["lem"], r_["m1"]], [r_["oh1"]])
            stt(r_["lem2"].ap, r_["oh1"].ap, -BIG, r_["lem"].ap, ALU.mult, ALU.add, [r_["oh1"], r_["lem"]], [r_["lem2"]])
            S.add("dve", lambda e: e.tensor_reduce(out=r_["m2"].ap, in_=r_["lem2"].ap, axis=AX.X, op=ALU.max), R(r_["lem2"]), R(r_["m2"]))
            ts("dve", r_["oh2"].ap, r_["lem2"].ap, r_["m2"].ap[:, 0:1], None, ALU.is_equal, None, [r_["lem2"], r_["m2"]], [r_["oh2"]])
            tt("dve", r_["dm"].ap, r_["m1"].ap, r_["m2"].ap, ALU.subtract, [r_["m1"], r_["m2"]], [r_["dm"]])
            act(r_["e2"].ap, r_["dm"].ap, AF.Tanh, [r_["dm"]], [r_["e2"]], scale=0.5)
            ts("dve", r_["p1"].ap, r_["e2"].ap, 0.5, 0.5, ALU.mult, ALU.add, [r_["e2"]], [r_["p1"]])
            ts("dve", r_["p2"].ap, r_["e2"].ap, -0.5, 0.5, ALU.mult, ALU.add, [r_["e2"]], [r_["p2"]])
            tt("dve", ohb.ap, r_["oh1"].ap, r_["oh2"].ap, ALU.add, [r_["oh1"], r_["oh2"]], [ohb])
            pR = psv(5, [128, 128])
            mm(pR[:, 64:96], tri_b.ap, ohb.ap, True, True, [tri_b, ohb], [PSB[5]])
            mm(pR[:, 96:128], ones_b.ap, ohb.ap, True, True, [ones_b, ohb], [PSB[5]])
            tt("dve", r_["rank"].ap, pR[:, 64:96], base.ap, ALU.add, [PSB[5], base], [r_["rank"]])
            tt("dve", base.ap, pR[:, 96:128], base.ap, ALU.add, [PSB[5], base, r_["rank"]], [base])
            ts("dve", r_["val"].ap, r_["rank"].ap, float(CAP), None, ALU.is_lt, None, [r_["rank"]], [r_["val"]])
            tt("dve", r_["slotv"].ap, r_["rank"].ap, ecap.ap, ALU.add, [r_["rank"], ecap], [r_["slotv"]])
            stt(r_["slotv"].ap, r_["val"].ap, -4.0e6, r_["slotv"].ap, ALU.mult, ALU.add, [r_["val"], r_["slotv"]], [r_["slotv"]])
            ts("dve", r_["slotv"].ap, r_["slotv"].ap, 4.0e6, None, ALU.add, None, [r_["slotv"]], [r_["slotv"]])
            for k, ohn in enumerate(("oh1", "oh2")):
                tt("dve", r_["junk"].ap, r_[ohn].ap, r_["slotv"].ap, ALU.mult, [r_[ohn], r_["slotv"]], [r_["junk"]])
                S.add("dve", lambda e, k=k: e.tensor_reduce(out=r_["sl"].ap[:, k:k + 1], in_=r_["junk"].ap, axis=AX.X, op=ALU.add),
                      R(r_["junk"]), R(r_["sl"]))
                tt("dve", r_["junk"].ap, r_[ohn].ap, r_["val"].ap, ALU.mult, [r_[ohn], r_["val"]], [r_["junk"]])
                S.add("dve", lambda e, k=k: e.tensor_reduce(out=r_["vk"].ap[:, k:k + 1], in_=r_["junk"].ap, axis=AX.X, op=ALU.add),
                      R(r_["junk"]), R(r_["vk"]))
            cp("dve", sloti.ap[:, i, :], r_["sl"].ap, [r_["sl"]], [sloti])
            stt(wgt.ap[:, i, 0:1], r_["p1"].ap, r_["pgs"].ap[:, 0:1], r_["vk"].ap[:, 0:1], ALU.mult, ALU.mult,
                [r_["p1"], r_["pgs"], r_["vk"]], [wgt])
            stt(wgt.ap[:, i, 1:2], r_["p2"].ap, r_["pgs"].ap[:, 0:1], r_["vk"].ap[:, 1:2], ALU.mult, ALU.mult,
                [r_["p2"], r_["pgs"], r_["vk"]], [wgt])
            if dbg.get("tile") == i:
                dump("lg", lg)
                dump("rank", r_["rank"])
            for k in range(2):
                scat_ops.append(S.add("pool", lambda e, i=i, k=k: e.indirect_dma_start(
                    out=X_d, out_offset=bass.IndirectOffsetOnAxis(ap=sloti.ap[:, i, k:k + 1], axis=0),
                    in_=xn2.ap, in_offset=None, bounds_check=breg(e, NSLOT - 1), oob_is_err=False),
                    R(xn2, sloti), [r_X], dma_key="scat", cost=1.2, lat=4.0))

        import os as _os
        _skip = set(_os.environ.get('QSKIP', '').split(','))
        _w = lambda f, n: (lambda *a: None) if n in _skip else f
        Qpre, QB, QD, QC, Qcarry, Qtail = _w(Qpre, 'pre'), _w(QB, 'B'), _w(QD, 'D'), _w(QC, 'C'), _w(Qcarry, 'carry'), _w(Qtail, 'tail')
        for s_ in range(NTILES + 2):
            ip, iq, ir = s_, s_ - 1, s_ - 2
            hp = 0 <= ip < NTILES
            hq = 0 <= iq < NTILES
            hr = 0 <= ir < NTILES
            if hr:
                R0(ir)
            if hq:
                Qpre(iq)
                QB(iq, 0)
            if hr:
                R1(ir, 0)
                R1(ir, 1)
            if hp:
                P1(ip)
            if hq:
                QD(iq, 0)
            if hr:
                R1(ir, 2)
                R1(ir, 3)
                R2(ir, 0)
                R2(ir, 1)
            if hq:
                QB(iq, 1)
                QC(iq, 0)
            if hp:
                P2(ip)
            if hq:
                QD(iq, 1)
            if hr:
                R3(ir)
            if hq:
                QB(iq, 2)
                QC(iq, 1)
                QD(iq, 2)
            if hr:
                R4(ir)
            if hq:
                QB(iq, 3)
                QC(iq, 2)
            if hp:
                P3(ip)
            if hq:
                QD(iq, 3)
                QC(iq, 3)
                Qcarry(iq)
                Qtail(iq)
        if "route" in dbg:
            dump("wgt", wgt)
            dump("sloti", sloti)
        if stop_after == "A":
            S.finish(final_ops + store_ops + scat_ops)
            S.emit(st)
            return nc, dbg_outs

        S.barrier(dma_ops=[scat_ops[-1], store_ops[-1]])
        A.reset(mark_persist)
        w1b = [A.alloc("w1b%d" % i, [128, 8, 512], BF16) for i in range(2)]
        w3b = [A.alloc("w3b%d" % i, [128, 8, 512], BF16) for i in range(2)]
        w2b = [A.alloc("w2b%d" % i, [128, 4, D], BF16) for i in range(2)]
        Xblk = [A.alloc("Xblk%d" % i, [128, D], BF16) for i in range(3)]
        XT = [A.alloc("XT%d" % i, [128, 8, CAP], BF16) for i in range(2)]
        hidT = [A.alloc("hidT%d" % i, [128, 4, CAP], BF16) for i in range(2)]
        s1 = [A.alloc("s1_%d" % i, [128, 512], F32) for i in range(2)]
        Yblk = [A.alloc("Yblk%d" % i, [128, D], F32) for i in range(3)]
        NH = (CAP + 511) // 512
        HW_ = CAP // NH
        ystore = []
        cnt = {"x": 0, "y": 0, "h": 0}

        def W13(e_):
            sl_ = e_ % 2
            dma("pool", w1b[sl_].ap, w1_d[e_].rearrange("(kc p) n -> p kc n", p=128), [], [w1b[sl_]], "w1b%d" % sl_)
            dma("pool", w3b[sl_].ap, w3_d[e_].rearrange("(kc p) n -> p kc n", p=128), [], [w3b[sl_]], "w3b%d" % sl_)

        def W2(e_):
            sl_ = e_ % 2
            dma("pool", w2b[sl_].ap, w2_d[e_].rearrange("(kc p) n -> p kc n", p=128), [], [w2b[sl_]], "w2b%d" % sl_)

        def TX(e_):
            xt_ = XT[e_ % 2]
            for blk in range(NBLK):
                n_ = cnt["x"]
                cnt["x"] += 1
                xb_ = Xblk[n_ % 3]
                r0 = e_ * CAP + blk * 128
                dma("sp", xb_.ap, X_d[r0:r0 + 128, :], [r_X], [xb_], "xblk%d" % (n_ % 3))
                bk = n_ % 2
                pXT = psv(bk, [128, 8, 128], BF16)
                for kc in range(8):
                    tr(pXT[:, kc, :], xb_.ap[:, kc * 128:(kc + 1) * 128], ident_b.ap, [xb_, ident_b], [PSB[bk]])
                cp("act" if blk % 2 == 0 else "dve", xt_.ap[:, :, blk * 128:(blk + 1) * 128], pXT, [PSB[bk]], [xt_])

        def HH(e_):
            sl_ = e_ % 2
            xt_, hd_ = XT[e_ % 2], hidT[e_ % 2]
            for m in range(4):
                for nh in range(NH):
                    cs_ = slice(nh * HW_, (nh + 1) * HW_)
                    n_ = cnt["h"]
                    cnt["h"] += 1
                    b1 = 2 + n_ % 2
                    b3 = 4 + n_ % 2
                    p1_ = PSB[b1].ap[:, 0:HW_]
                    p3_ = PSB[b3].ap[:, 0:HW_]
                    for kc in range(8):
                        mm(p1_, w1b[sl_].ap[:, kc, m * 128:(m + 1) * 128], xt_.ap[:, kc, cs_], kc == 0, kc == 7,
                           [w1b[sl_], xt_], [PSB[b1]])
                    for kc in range(8):
                        mm(p3_, w3b[sl_].ap[:, kc, m * 128:(m + 1) * 128], xt_.ap[:, kc, cs_], kc == 0, kc == 7,
                           [w3b[sl_], xt_], [PSB[b3]])
                    s1_ = s1[n_ % 2]
                    act(s1_.ap[:, 0:HW_], p1_, AF.Silu, [PSB[b1]], [s1_])
                    tt("dve", hd_.ap[:, m, cs_], s1_.ap[:, 0:HW_], p3_, ALU.mult, [s1_, PSB[b3]], [hd_])

        def YY(e_):
            sl_ = e_ % 2
            hd_ = hidT[e_ % 2]
            for blk in range(NBLK):
                n_ = cnt["y"]
                cnt["y"] += 1
                yb_ = Yblk[n_ % 3]
                for half in range(2):
                    bk = 6 + half
                    for kc in range(4):
                        mm(PSB[bk].ap, hd_.ap[:, kc, blk * 128:(blk + 1) * 128], w2b[sl_].ap[:, kc, half * 512:(half + 1) * 512],
                           kc == 0, kc == 3, [hd_, w2b[sl_]], [PSB[bk]])
                    cp("act" if half == 0 else "dve", yb_.ap[:, half * 512:(half + 1) * 512], PSB[bk].ap, [PSB[bk]], [yb_])
                r0 = e_ * CAP + blk * 128
                ystore.append(dma("sp", Y_d[r0:r0 + 128, :], yb_.ap, [yb_], [r_Y], "yst%d" % (n_ % 3)))

        W13(0)
        W2(0)
        W13(1)
        W2(1)
        TX(0)
        for e_ in range(32):
            HH(e_)
            if e_ + 2 < 32:
                W13(e_ + 2)
            if e_ + 1 < 32:
                TX(e_ + 1)
            YY(e_)
            if e_ + 2 < 32:
                W2(e_ + 2)
        if stop_after == "B":
            S.finish(final_ops + ystore[-3:])
            S.emit(st)
            return nc, dbg_outs

        S.barrier(dma_ops=ystore[-3:])
        A.reset(mark_persist)
        NBC = 3
        Hc = [A.alloc("Hc%d" % i, [128, D], F32) for i in range(NBC)]
        Y0 = [A.alloc("Y0_%d" % i, [128, D], F32) for i in range(NBC)]
        Y1 = [A.alloc("Y1_%d" % i, [128, D], F32) for i in range(NBC)]
        acc_l = [A.alloc("acc%d" % i, [128, D], F32) for i in range(2)]
        ob = [A.alloc("ob%d" % i, [128, D], F32) for i in range(2)]
        gt2b = A.alloc("gt2b", [128, D], F32)
        gfb = A.alloc("gfb", [128, D], F32)
        jk_l = [A.alloc("jk%d" % i, [128, D], BF16) for i in range(2)]
        ssq3_l = [A.alloc("ssq3_%d" % i, [128, 1], F32) for i in range(2)]
        rstd3_l = [A.alloc("rstd3_%d" % i, [128, 1], F32) for i in range(2)]
        dma("sp", gfb.ap, gf_d.partition_broadcast(128), [], [gfb], "gfb")
        for s_ in range(NBC):
            memset("pool", Y0[s_].ap, 0.0, [Y0[s_]])
            memset("pool", Y1[s_].ap, 0.0, [Y1[s_]])

        def Cload(i):
            s_ = i % NBC
            dma("sp", Hc[s_].ap, H_d[i * 128:(i + 1) * 128, :], [r_H], [Hc[s_]], "hc%d" % s_)
            for k, Yk in enumerate((Y0[s_], Y1[s_])):
                S.add("pool", lambda e, i=i, k=k, Yk=Yk: e.indirect_dma_start(
                    out=Yk.ap, out_offset=None, in_=Y_d, in_offset=bass.IndirectOffsetOnAxis(ap=sloti.ap[:, i, k:k + 1], axis=0),
                    bounds_check=breg(e, NSLOT - 1), oob_is_err=False), R(r_Y, sloti), R(Yk), dma_key="gath%d_%d" % (k, s_),
                    cost=1.2, lat=5.0)

        Cload(0)
        if NTILES > 1:
            Cload(1)
        for i in range(NTILES):
            b, tau = i // NT, i % NT
            s_ = i % NBC
            if tau == 0:
                dma("sp", gt2b.ap, mod_d.ap[b:b + 1, 5 * D:6 * D].partition_broadcast(128), [mod_d], [gt2b], "gt2b")
            if i + 2 < NTILES:
                Cload(i + 2)
            acc, jk, ssq3, rstd3 = acc_l[i % 2], jk_l[i % 2], ssq3_l[i % 2], rstd3_l[i % 2]
            ts("dve", acc.ap, Y0[s_].ap, wgt.ap[:, i, 0:1], None, ALU.mult, None, [Y0[s_], wgt], [acc])
            stt(acc.ap, Y1[s_].ap, wgt.ap[:, i, 1:2], acc.ap, ALU.mult, ALU.add, [Y1[s_], wgt, acc], [acc])
            tt("dve", acc.ap, acc.ap, gt2b.ap, ALU.mult, [acc, gt2b], [acc])
            tt("dve", acc.ap, acc.ap, Hc[s_].ap, ALU.add, [acc, Hc[s_]], [acc])
            rms_rstd(acc, ssq3, rstd3, jk.ap, jk)
            stt(ob[i % 2].ap, acc.ap, rstd3.ap[:, 0:1], gfb.ap, ALU.mult, ALU.mult, [acc, rstd3, gfb], [ob[i % 2]])
            final_ops.append(dma("sp", out_d[i * 128:(i + 1) * 128, :], ob[i % 2].ap, [ob[i % 2]], [], "ost%d" % (i % 2)))
        S.finish(final_ops)
        S.emit(st)
    return nc, dbg_outs


def prep_shared(inp):
    f = np.float32
    g = {}
    L = 0
    g["w_ada"] = np.ascontiguousarray(inp["w_ada"][L], f)
    g["g1T"] = np.ascontiguousarray(inp["norm1_g"][L].reshape(8, 128).T, f)
    g["w_in"] = np.ascontiguousarray(inp["w_in"][L], f)
    g["w_gate"] = np.ascontiguousarray(inp["w_gate"][L], f)
    g["b_gateT"] = np.ascontiguousarray(inp["b_gate"][L].reshape(16, 128).T, f)
    a_re, a_im, ls = inp["ssm_a_re"][L], inp["ssm_a_im"][L], inp["ssm_log_step"][L]
    def sm(v):
        return v.reshape(16, 2, 64).transpose(1, 2, 0).reshape(128, 16)
    lsx = np.repeat(ls[:, None], 64, axis=1)
    g["lam_sm"] = np.ascontiguousarray(np.stack([sm(a_re), sm(a_im), sm(lsx)], axis=1), f)
    Bsm = np.zeros((128, 2, 16, 128), f)
    for pi, Bsrc in enumerate((inp["ssm_b_re"][L], inp["ssm_b_im"][L])):
        for gi in range(32):
            j = gi // 2
            n0 = 64 * (gi % 2)
            c0 = 32 * (j % 4) + 16 * (gi % 2)
            Bsm[n0:n0 + 64, pi, j, c0:c0 + 16] = Bsrc[gi]
    g["Bsm"] = Bsm
    Csm = np.zeros((128, 2, 16, 128), f)
    for pi, Csrc in enumerate((inp["ssm_c_re"][L], inp["ssm_c_im"][L])):
        for gi in range(32):
            j = gi // 2
            n0 = 64 * (gi % 2)
            c0 = 32 * (j % 4) + 16 * (gi % 2)
            Csm[n0:n0 + 64, pi, j, c0:c0 + 16] = Csrc[gi].T
    g["Csm"] = Csm
    g["dT"] = np.ascontiguousarray(inp["ssm_d"][L].reshape(4, 128).T, f)
    g["w_glu"] = np.ascontiguousarray(inp["w_glu"][L], f)
    g["b_gluT"] = np.ascontiguousarray(inp["b_glu"][L].reshape(4, 128).T, f)
    g["wsT"] = np.ascontiguousarray(inp["sgu_w"][L].transpose(2, 0, 1), f)
    g["lngT"] = np.ascontiguousarray(inp["sgu_ln_g"][L].reshape(4, 128).T, f)
    g["lnbT"] = np.ascontiguousarray(inp["sgu_ln_b"][L].reshape(4, 128).T, f)
    bs = inp["sgu_b"][L]
    bsT = np.zeros((128, 4, 128), f)
    for q in range(4):
        bsT[0:64, q, :] = bs[2 * q][None, :]
        bsT[64:128, q, :] = bs[2 * q + 1][None, :]
    g["bsT"] = bsT
    g["w_bra"] = np.ascontiguousarray(inp["w_branch_a"][L], f)
    g["w_brb"] = np.ascontiguousarray(inp["w_branch_b"][L], f)
    g["w_out"] = np.ascontiguousarray(inp["w_out"][L], f)
    g["g2"] = np.ascontiguousarray(inp["norm2_g"][L].reshape(1, D), f)
    wr = np.concatenate([inp["w_router_group"][L], inp["w_router_expert"][L].transpose(1, 0, 2).reshape(D, 32)], axis=1)
    g["w_r"] = np.ascontiguousarray(wr.reshape(8, 128, 36).transpose(1, 0, 2), f)
    g["b_r"] = np.ascontiguousarray(np.concatenate([inp["b_router_group"][L], inp["b_router_expert"][L].reshape(32)]).reshape(1, 36), f)
    g["w1"] = np.ascontiguousarray(inp["w1"][L], f)
    g["w3"] = np.ascontiguousarray(inp["w3"][L], f)
    g["w2"] = np.ascontiguousarray(inp["w2"][L], f)
    g["gf"] = np.ascontiguousarray(inp["norm_f_g"].reshape(1, D), f)
    return g


def prep_core(inp, shared, b0, nseq):
    m = dict(shared)
    xs = inp["x"][b0:b0 + nseq]
    m["x"] = np.ascontiguousarray(xs.reshape(-1, D), np.float32)
    c = inp["c"][b0:b0 + nseq]
    m["cT"] = np.ascontiguousarray(c.reshape(nseq, 8, 128).transpose(2, 1, 0), np.float32)
    m["b_ada_rep"] = np.ascontiguousarray(np.repeat(inp["b_ada"][0][None, :], nseq, axis=0), np.float32)
    return m


_CACHE = {}


def kernel(**inputs):
    inp = {k: np.asarray(v) for k, v in inputs.items()}
    B, SEQ = inp["x"].shape[0], inp["x"].shape[1]
    nseq = B // N_CORES
    key = (nseq, SEQ)
    if key not in _CACHE:
        _CACHE[key] = build(NSEQ=nseq, SEQ=SEQ, CAP=768)[0]
    nc = _CACHE[key]
    shared = prep_shared(inp)
    in_maps = [prep_core(inp, shared, c * nseq, nseq) for c in range(N_CORES)]
    res = run_bass_kernel_spmd(nc, in_maps, core_ids=list(range(N_CORES)))
    outs = [np.asarray(r["out"]).reshape(nseq, SEQ, D) for r in res.results]
    return np.concatenate(outs, axis=0).astype(np.float32)
```

```python
import math
import os
from contextlib import ExitStack
import numpy as np
import concourse.bass as bass
import concourse.mybir as mybir
from concourse.bass_utils import run_bass_kernel_spmd

F32 = mybir.dt.float32
BF16 = mybir.dt.bfloat16
I32 = mybir.dt.int32
U8 = mybir.dt.uint8
AF = mybir.ActivationFunctionType
ALU = mybir.AluOpType
AX = mybir.AxisListType

ENGS = ("pe", "act", "dve", "pool", "sp")
N_CORES = 8
D = 1024
TWO_PI = 2.0 * math.pi
RMS_EPS = 1e-6
LN_EPS = 1e-5


class Res:
    __slots__ = ("name", "lastw", "readers")

    def __init__(self, name):
        self.name = name
        self.lastw = None
        self.readers = []


class Op:
    __slots__ = ("eng", "fn", "deps", "sdeps", "dma_key", "dma_val", "signal", "sig_idx", "cost", "lat", "idx", "succ", "nin", "fin")

    def __init__(self, eng, fn, dma_key):
        self.eng = eng
        self.fn = fn
        self.deps = []
        self.sdeps = []
        self.dma_key = dma_key
        self.dma_val = None
        self.signal = False
        self.sig_idx = None
        self.cost = 0.2
        self.lat = 0.0


class Sched:
    def __init__(self, nc):
        self.nc = nc
        self.ops = {e: [] for e in ENGS}
        self.all = []
        self.finals = []
        self.pending_barrier = {}
        import os as _o
        self.reorder = _o.environ.get("NOREORDER") is None

    def add(self, eng, fn, reads=(), writes=(), dma_key=None, cost=None, lat=0.0):
        op = Op(eng, fn, dma_key)
        if cost is not None:
            op.cost = cost
        op.lat = lat
        deps = []
        for r in reads:
            if r.lastw is not None:
                deps.append(r.lastw)
        for w in writes:
            if w.lastw is not None:
                deps.append(w.lastw)
            deps.extend(w.readers)
        if eng in self.pending_barrier:
            deps.extend(self.pending_barrier.pop(eng))
        seen = set()
        for d in deps:
            if d is op or id(d) in seen:
                continue
            seen.add(id(d))
            if d.eng == "pe" and eng == "pe" and d.dma_key is None and dma_key is None:
                op.sdeps.append(d)
                continue
            op.deps.append(d)
        for r in reads:
            r.readers.append(op)
        for w in writes:
            w.lastw = op
            w.readers = []
        op.idx = len(self.all)
        self.all.append(op)
        self.ops[eng].append(op)
        return op

    def barrier(self, dma_ops=()):
        lasts = [self.ops[e][-1] for e in ENGS if self.ops[e]]
        lasts = [o for o in lasts if o.dma_key is None] + list(dma_ops)
        for e in ENGS:
            self.pending_barrier.setdefault(e, []).extend(lasts)

    def finish(self, ops):
        self.finals.extend(ops)

    def schedule(self):
        import heapq
        ops = self.all
        for o in ops:
            o.succ = []
            o.nin = 0
        for o in ops:
            for d in o.deps + o.sdeps:
                d.succ.append(o)
                o.nin += 1
        import os as _o2
        SYNC = float(_o2.environ.get("SYNC", "1.0"))
        wait_h = {e: [] for e in ENGS}
        t_eng = {e: 0.0 for e in ENGS}
        order = {e: [] for e in ENGS}
        ready_at = {}
        for o in ops:
            if o.nin == 0:
                heapq.heappush(wait_h[o.eng], (0.0, o.idx))
        placed = 0
        n = len(ops)
        while placed < n:
            best = None
            for e in ENGS:
                h = wait_h[e]
                if not h:
                    continue
                te = t_eng[e]
                if h[0][0] <= te:
                    cand_idx = min(i for (r, i) in h if r <= te)
                    st = te
                else:
                    st, cand_idx = h[0]
                if best is None or (st, cand_idx) < (best[0], best[1]):
                    best = (st, cand_idx, e)
            st, ci, e = best
            h = wait_h[e]
            for k, (r, i) in enumerate(h):
                if i == ci:
                    h[k] = h[-1]
                    h.pop()
                    break
            heapq.heapify(h)
            o = ops[ci]
            t_eng[e] = st + o.cost
            o.fin = st + o.cost + o.lat
            order[e].append(o)
            placed += 1
            for sct in o.succ:
                sct.nin -= 1
                ra = max(ready_at.get(sct.idx, 0.0), o.fin + (SYNC if o.eng != sct.eng or o.dma_key is not None else 0.0))
                ready_at[sct.idx] = ra
                if sct.nin == 0:
                    heapq.heappush(wait_h[sct.eng], (ra, sct.idx))
        self.ops = order
        self.sim_time = max(t_eng.values())

    def emit(self, stack):
        nc = self.nc
        if self.reorder:
            self.schedule()
        for e in ENGS:
            for op in self.ops[e]:
                for d in op.deps:
                    if d.dma_key is None:
                        d.signal = True
        dma_cnt = {}
        for e in ENGS:
            for op in self.ops[e]:
                if op.dma_key is not None:
                    dma_cnt[op.dma_key] = dma_cnt.get(op.dma_key, 0) + 16
                    op.dma_val = dma_cnt[op.dma_key]
        sems = {e: stack.enter_context(nc.semaphore("sem_" + e)) for e in ENGS}
        dsem = {k: stack.enter_context(nc.semaphore("dsem_%s" % (k,))) for k in dma_cnt}
        for e in ENGS:
            n = 0
            for op in self.ops[e]:
                if op.dma_key is None and op.signal:
                    n += 1
                    op.sig_idx = n
        block = stack.enter_context(nc.Block())
        engobj = {"pe": "tensor", "act": "scalar", "dve": "vector", "pool": "gpsimd", "sp": "sync"}
        finals = self.finals

        def body_for(e):
            def body(eng):
                seen = {}
                for op in self.ops[e]:
                    need = {}
                    for d in op.deps:
                        if d.dma_key is not None:
                            s, v, key = dsem[d.dma_key], d.dma_val, ("d", d.dma_key)
                        else:
                            s, v, key = sems[d.eng], d.sig_idx, ("e", d.eng)
                        if key not in need or need[key][1] < v:
                            need[key] = (s, v)
                    for key, (s, v) in need.items():
                        if seen.get(key, 0) >= v:
                            continue
                        seen[key] = v
                        eng.wait_ge(s, v)
                    inst = op.fn(eng)
                    if op.dma_key is not None:
                        inst.then_inc(dsem[op.dma_key], 16)
                    elif op.signal:
                        inst.then_inc(sems[e], 1)
                if e == "sp":
                    for d in finals:
                        eng.wait_ge(dsem[d.dma_key], d.dma_val)
            return body

        for e in ENGS:
            getattr(block, engobj[e])(body_for(e))


class Tl:
    __slots__ = ("ap", "r")

    def __init__(self, ap, name):
        self.ap = ap
        self.r = Res(name)


class Arena:
    def __init__(self, nc, stack, nbytes):
        self.buf = stack.enter_context(nc.sbuf_tensor("arena", [128, nbytes], U8))
        self.off = 0
        self.cap = nbytes
        self.live = []

    def alloc(self, name, shape, dt):
        esz = {F32: 4, BF16: 2, I32: 4}[dt]
        n = 1
        for s in shape[1:]:
            n *= s
        nb = (n * esz + 31) // 32 * 32
        if os.environ.get("SIMONLY"):
            ro = self.off % (self.cap - nb)
            ro -= ro % 32
            v = self.buf[0:shape[0], ro:ro + n * esz].bitcast(dt)
            if len(shape) == 3:
                v = v.rearrange("p (a b) -> p a b", a=shape[1])
            elif len(shape) == 4:
                v = v.rearrange("p (a b c) -> p a b c", a=shape[1], b=shape[2])
            self.off += nb
            self.hi = max(getattr(self, "hi", 0), self.off)
            return Tl(v, name)
        assert self.off + nb <= self.cap, "SBUF arena overflow at %s: %d + %d > %d" % (name, self.off, nb, self.cap)
        v = self.buf[0:shape[0], self.off:self.off + n * esz].bitcast(dt)
        if len(shape) == 3:
            v = v.rearrange("p (a b) -> p a b", a=shape[1])
        elif len(shape) == 4:
            v = v.rearrange("p (a b c) -> p a b c", a=shape[1], b=shape[2])
        t = Tl(v, name)
        lo, hi = self.off, self.off + nb
        keep = []
        for (a, b, o) in self.live:
            if a < hi and lo < b:
                if o.r.lastw is not None:
                    t.r.readers.append(o.r.lastw)
                t.r.readers.extend(o.r.readers)
                if a >= lo and b <= hi:
                    continue
            keep.append((a, b, o))
        keep.append((lo, hi, t))
        self.live = keep
        self.off += nb
        return t

    def mark(self):
        return self.off

    def reset(self, m):
        self.off = m


def build(NSEQ=4, SEQ=2048, CAP=768, dbg=None, stop_after=None):
    nc = bass.Bass("TRN2", target_bir_lowering=False)
    NT = SEQ // 128
    NTOK = NSEQ * SEQ
    NTILES = NSEQ * NT
    NSLOT = 32 * CAP
    NBLK = CAP // 128
    dbg = dbg or {}
    dbg_outs = {}

    def din(name, shape, dt=F32):
        return nc.dram_tensor(name, list(shape), dt, kind="ExternalInput").ap()

    x_d = din("x", [NTOK, D])
    cT_d = din("cT", [128, 8, NSEQ])
    w_ada_d = din("w_ada", [D, 6 * D])
    b_ada_d = din("b_ada_rep", [NSEQ, 6 * D])
    g1T_d = din("g1T", [128, 8])
    w_in_d = din("w_in", [D, 1536])
    w_gate_d = din("w_gate", [D, 2048])
    b_gateT_d = din("b_gateT", [128, 16])
    lam_sm_d = din("lam_sm", [128, 3, 16])
    Bsm_d = din("Bsm", [128, 2, 16, 128])
    Csm_d = din("Csm", [128, 2, 16, 128])
    dT_d = din("dT", [128, 4])
    w_glu_d = din("w_glu", [512, 512])
    b_gluT_d = din("b_gluT", [128, 4])
    wsT_d = din("wsT", [128, 8, 128])
    lngT_d = din("lngT", [128, 4])
    lnbT_d = din("lnbT", [128, 4])
    bsT_d = din("bsT", [128, 4, 128])
    w_bra_d = din("w_bra", [512, D])
    w_brb_d = din("w_brb", [512, D])
    w_out_d = din("w_out", [D, D])
    g2_d = din("g2", [1, D])
    w_r_d = din("w_r", [128, 8, 36])
    b_r_d = din("b_r", [1, 36])
    w1_d = din("w1", [32, D, 512])
    w3_d = din("w3", [32, D, 512])
    w2_d = din("w2", [32, 512, D])
    gf_d = din("gf", [1, D])
    out_d = nc.dram_tensor("out", [NTOK, D], F32, kind="ExternalOutput").ap()
    mod_d = Tl(nc.dram_tensor("mod_d", [NSEQ, 6 * D], F32, kind="Internal").ap(), "mod_d")
    H_d = nc.dram_tensor("H_d", [NTOK, D], F32, kind="Internal").ap()
    X_d = nc.dram_tensor("X_d", [NSLOT, D], BF16, kind="Internal").ap()
    Y_d = nc.dram_tensor("Y_d", [NSLOT, D], F32, kind="Internal").ap()
    r_X = Res("X_d")
    r_Y = Res("Y_d")
    r_H = Res("H_d")

    S = Sched(nc)
    final_ops = []
    with ExitStack() as st:
        A = Arena(nc, st, int(os.environ.get("ARENA_CAP", "212800")))
        ps_all = st.enter_context(nc.psum_tensor("ps_all", [128, 8, 512], F32))
        PSB = [Tl(ps_all[:, b, :], "psb%d" % b) for b in range(8)]

        def psv(b, shape, dt=F32):
            v = PSB[b].ap
            if dt == BF16:
                v = v.bitcast(BF16)
            n = 1
            for s in shape[1:]:
                n *= s
            v = v[0:shape[0], 0:n]
            if len(shape) == 3:
                v = v.rearrange("p (a b) -> p a b", a=shape[1])
            elif len(shape) == 4:
                v = v.rearrange("p (a b c) -> p a b c", a=shape[1], b=shape[2])
            return v

        def psv2(b, shape):
            v = ps_all[:, b:b + 2, :].rearrange("p a b -> p (a b)")
            n = 1
            for s in shape[1:]:
                n *= s
            v = v[0:shape[0], 0:n]
            if len(shape) == 3:
                v = v.rearrange("p (a b) -> p a b", a=shape[1])
            elif len(shape) == 4:
                v = v.rearrange("p (a b c) -> p a b c", a=shape[1], b=shape[2])
            return v

        def R(*ts):
            return [t.r if isinstance(t, Tl) else t for t in ts]

        def fsz(ap):
            n = 1
            for s_ in ap.shape[1:]:
                n *= s_
            return n

        def isz(ap):
            return 2 if ap.dtype == BF16 else 4

        def ecost(eng, n):
            if eng == "dve":
                return 0.1 + n / 900.0
            if eng == "pool":
                return 0.25 + n / 430.0
            return 0.2 + n / 1150.0

        def dma(eng, out, in_, reads, writes, key, **kw):
            nb = out.shape[0] * fsz(out) * isz(out)
            return S.add(eng, lambda e: e.dma_start(out=out, in_=in_, **kw), R(*reads), R(*writes), dma_key=key,
                         cost=(1.0 if eng == "pool" else 0.07), lat=2.0 + nb / 180e3)

        def tt(eng, out, in0, in1, op, reads, writes):
            c = 0.45 if op == ALU.pow else ecost(eng, fsz(out))
            return S.add(eng, lambda e: e.tensor_tensor(out=out, in0=in0, in1=in1, op=op), R(*reads), R(*writes), cost=c)

        def ts(eng, out, in0, s1, s2, op0, op1, reads, writes, accum=None):
            c = ecost(eng, fsz(out))
            if op1 is None:
                return S.add(eng, lambda e: e.tensor_scalar(out=out, in0=in0, scalar1=s1, scalar2=None, op0=op0), R(*reads), R(*writes), cost=c)
            if accum is not None:
                return S.add(eng, lambda e: e.tensor_scalar(out=out, in0=in0, scalar1=s1, scalar2=s2, op0=op0, op1=op1, accum_out=accum), R(*reads), R(*writes), cost=c)
            return S.add(eng, lambda e: e.tensor_scalar(out=out, in0=in0, scalar1=s1, scalar2=s2, op0=op0, op1=op1), R(*reads), R(*writes), cost=c)

        def stt(out, in0, scalar, in1, op0, op1, reads, writes):
            return S.add("dve", lambda e: e.scalar_tensor_tensor(out=out, in0=in0, scalar=scalar, in1=in1, op0=op0, op1=op1), R(*reads), R(*writes),
                         cost=ecost("dve", fsz(out)))

        def act(out, in_, func, reads, writes, bias=None, scale=None, accum=None):
            kw = {}
            if bias is not None:
                kw["bias"] = bias
            if scale is not None:
                kw["scale"] = scale
            if accum is not None:
                kw["accum_out"] = accum
            return S.add("act", lambda e: e.activation(out=out, in_=in_, func=func, **kw), R(*reads), R(*writes),
                         cost=ecost("act", fsz(out)) + (0.1 if accum is not None else 0.0))

        def cp(eng, out, in_, reads, writes):
            c = ecost(eng, fsz(out))
            if eng == "act":
                return S.add("act", lambda e: e.copy(out=out, in_=in_), R(*reads), R(*writes), cost=c)
            return S.add(eng, lambda e: e.tensor_copy(out=out, in_=in_), R(*reads), R(*writes), cost=c)

        def mm(out, lhsT, rhs, start, stop, reads, writes):
            c = 0.03 + fsz(rhs) / 2400.0 * (4.0 if lhsT.dtype == F32 else 1.0)
            return S.add("pe", lambda e: e.matmul(out, lhsT=lhsT, rhs=rhs, start=start, stop=stop), R(*reads), R(*writes), cost=c)

        def tr(out, in_, ident, reads, writes):
            c = 0.03 + 128 / 2400.0 * (2.0 if in_.dtype == F32 else 1.0)
            return S.add("pe", lambda e: e.transpose(out=out, in_=in_, identity=ident), R(*reads), R(*writes), cost=c)

        def memset(eng, ap, val, writes):
            return S.add(eng, lambda e: e.memset(ap, val), [], R(*writes))

        _regs = {}

        def breg(e, val):
            if val not in _regs:
                _regs[val] = e.to_reg(val)
            return _regs[val]

        def dump(name, t, ap=None):
            ap = t.ap if ap is None else ap
            shp = list(ap.shape)
            o = nc.dram_tensor("dbg_" + name, shp, ap.dtype, kind="ExternalOutput").ap()
            dbg_outs[name] = "dbg_" + name
            final_ops.append(dma("sp", o, ap, [t], [], "dbg_" + name))

        ident_f = A.alloc("ident_f", [128, 128], F32)
        ident_b = A.alloc("ident_b", [128, 128], BF16)
        tri_b = A.alloc("tri_b", [128, 128], BF16)
        ones_b = A.alloc("ones_b", [128, 128], BF16)
        memset("pool", ident_f.ap, 0.0, [ident_f])
        S.add("pool", lambda e: e.affine_select(out=ident_f.ap, in_=ident_f.ap, pattern=[[-1, 128]], compare_op=ALU.not_equal,
                                                  fill=1.0, base=0, channel_multiplier=1), R(ident_f), R(ident_f))
        cp("pool", ident_b.ap, ident_f.ap, [ident_f], [ident_b])
        memset("pool", ones_b.ap, 1.0, [ones_b])
        S.add("pool", lambda e: e.affine_select(out=tri_b.ap, in_=ones_b.ap, pattern=[[1, 128]], compare_op=ALU.is_gt,
                                                  fill=0.0, base=0, channel_multiplier=-1), R(ones_b), R(tri_b))

        wgt = A.alloc("wgt", [128, NTILES, 2], F32)
        sloti = A.alloc("sloti", [128, NTILES, 2], I32)
        base = A.alloc("base", [128, 32], F32)
        ecap = A.alloc("ecap", [128, 32], F32)
        memset("pool", base.ap, 0.0, [base])
        ecap_i = A.alloc("ecap_i", [128, 32], I32)
        S.add("pool", lambda e: e.iota(ecap_i.ap, pattern=[[CAP, 32]], base=0, channel_multiplier=0), [], R(ecap_i))
        cp("pool", ecap.ap, ecap_i.ap, [ecap_i], [ecap])
        A1T = A.alloc("A1T", [128, 8, NSEQ], F32)
        sh1T = A.alloc("sh1T", [128, 8, NSEQ], F32)
        eps_rms = A.alloc("eps_rms", [128, 1], F32)
        eps_ln = A.alloc("eps_ln", [128, 1], F32)
        memset("pool", eps_rms.ap, 1e-6, [eps_rms])
        memset("pool", eps_ln.ap, 1e-5, [eps_ln])
        mhalf = A.alloc("mhalf", [128, 1], F32)
        memset("pool", mhalf.ap, -0.5, [mhalf])

        mark_persist = A.mark()

        w_r = A.alloc("w_r", [128, 8, 36], F32)
        dma("sp", w_r.ap, w_r_d, [], [w_r], "w_r")
        b_r = A.alloc("b_r", [128, 36], F32)
        dma("sp", b_r.ap, b_r_d.partition_broadcast(128), [], [b_r], "b_r")
        b_gateT = A.alloc("b_gateT", [128, 16], F32)
        dma("sp", b_gateT.ap, b_gateT_d, [], [b_gateT], "b_gateT")
        ts("pool", b_gateT.ap, b_gateT.ap, 0.5, 1.0, ALU.mult, ALU.mult, [b_gateT], [b_gateT])
        b_gluT = A.alloc("b_gluT", [128, 4], F32)
        dma("sp", b_gluT.ap, b_gluT_d, [], [b_gluT], "b_gluT")
        ts("pool", b_gluT.ap, b_gluT.ap, 0.5, 1.0, ALU.mult, ALU.mult, [b_gluT], [b_gluT])
        dT = A.alloc("dT", [128, 4], F32)
        dma("sp", dT.ap, dT_d, [], [dT], "dT")
        lngT = A.alloc("lngT", [128, 4], F32)
        dma("sp", lngT.ap, lngT_d, [], [lngT], "lngT")
        lnbT = A.alloc("lnbT", [128, 4], F32)
        dma("sp", lnbT.ap, lnbT_d, [], [lnbT], "lnbT")

        LC = 4
        NCH = 128 // LC
        tabC4 = A.alloc("tabC4", [128, 16, NCH + 1], F32)
        tabD4 = A.alloc("tabD4", [128, 16, NCH + 1], F32)
        Rtab = A.alloc("Rtab", [128, 16, 2, NCH], F32)
        rmagL = A.alloc("rmagL", [128, 16], F32)
        Pt = A.alloc("Pt", [128, 2, 8 * LC, 128], BF16)
        Qs = A.alloc("Qs", [128, 2, 16 * LC, 64], BF16)
        Kb = A.alloc("Kb", [128, 4 * LC, 128], BF16)
        tabC = Tl(None, "s5scr")
        mark_setup = A.mark()

        def range_reduce(ph, tmpf, tmpi, n):
            ts("dve", tmpi, ph, 1.0 / TWO_PI, None, ALU.mult, None, [tabC], [tabC])
            cp("dve", tmpf, tmpi, [tabC], [tabC])
            stt(ph, tmpf, -TWO_PI, ph, ALU.mult, ALU.add, [tabC], [tabC])
            wrap(ph, tmpf)

        def wrap(ph, tmpf):
            ts("dve", tmpf, ph, math.pi, None, ALU.is_gt, None, [tabC], [tabC])
            stt(ph, tmpf, -TWO_PI, ph, ALU.mult, ALU.add, [tabC], [tabC])
            ts("dve", tmpf, ph, -math.pi, None, ALU.is_lt, None, [tabC], [tabC])
            stt(ph, tmpf, TWO_PI, ph, ALU.mult, ALU.add, [tabC], [tabC])

        def scr(name, shape, dt):
            t = A.alloc(name, shape, dt)
            tabC.r.readers.extend(t.r.readers)
            t.r = tabC.r
            for k_, (a_, b_, o_) in enumerate(A.live):
                if o_ is t:
                    A.live[k_] = (a_, b_, t)
            return t

        TC = [tabC]
        lam = scr("lam", [128, 3, 16], F32)
        dma("sp", lam.ap, lam_sm_d, [], TC, "lam")
        zs = {n: scr("z_" + n, [128, 16], F32) for n in ("dt", "th", "lre", "lrdt", "den", "nr", "fre", "fim", "t0", "t1")}
        sv_i = scr("sv_i", [128, 129], I32)
        sv = scr("sv", [128, 129], F32)
        tabCf = scr("tabCf", [128, 16, 129], F32)
        tabDf = scr("tabDf", [128, 16, 129], F32)
        tmpf = scr("tmpf", [128, 16 * 129], F32)
        tmpi = scr("tmpi", [128, 16 * 129], I32)
        aim = lam.ap[:, 1, :]
        S.add("pool", lambda e: e.iota(sv_i.ap, pattern=[[1, 129]], base=0, channel_multiplier=0), [], R(tabC))
        cp("dve", sv.ap, sv_i.ap, TC, TC)
        ts("dve", zs["lre"].ap, lam.ap[:, 0, :], -1e-4, None, ALU.min, None, TC, TC)
        act(zs["dt"].ap, lam.ap[:, 2, :], AF.Exp, TC, TC)
        tt("dve", zs["lrdt"].ap, zs["lre"].ap, zs["dt"].ap, ALU.mult, TC, TC)
        tt("dve", zs["th"].ap, aim, zs["dt"].ap, ALU.mult, TC, TC)
        for j in range(16):
            ts("dve", tabDf.ap[:, j, :], sv.ap, zs["th"].ap[:, j:j + 1], None, ALU.mult, None, TC, TC)
        phD = tabDf.ap.rearrange("p a b -> p (a b)")
        phC = tabCf.ap.rearrange("p a b -> p (a b)")
        range_reduce(phD, tmpf.ap, tmpi.ap, 16 * 129)
        ts("dve", phC, phD, math.pi / 2, None, ALU.add, None, TC, TC)
        wrap(phC, tmpf.ap)
        act(phD, phD, AF.Sin, TC, TC)
        act(phC, phC, AF.Sin, TC, TC)
        cp("dve", tabC4.ap, tabCf.ap[:, :, 0:129:LC], TC, TC + [tabC4])
        cp("dve", tabD4.ap, tabDf.ap[:, :, 0:129:LC], TC, TC + [tabD4])
        rk = scr("rk", [128, 16, LC + 1], F32)
        pwr = scr("pwr", [128, 16, LC + 1], F32)
        pwi = scr("pwi", [128, 16, LC + 1], F32)
        for k in range(LC + 1):
            act(rk.ap[:, :, k], zs["lrdt"].ap, AF.Exp, TC, TC, scale=float(k))
        tt("dve", pwr.ap, rk.ap, tabCf.ap[:, :, 0:LC + 1], ALU.mult, TC, TC)
        tt("dve", pwi.ap, rk.ap, tabDf.ap[:, :, 0:LC + 1], ALU.mult, TC, TC)
        cp("dve", rmagL.ap, rk.ap[:, :, LC], TC, TC + [rmagL])
        cp("dve", Rtab.ap.rearrange("p a b c -> p a (b c)"), rmagL.ap.unsqueeze(2).to_broadcast([128, 16, 2 * NCH]),
           TC + [rmagL], TC + [Rtab])
        memset("dve", Rtab.ap[:, :, :, 0], 0.0, TC + [Rtab])
        abr, abi = pwr.ap[:, :, 1], pwi.ap[:, :, 1]
        lre_ = zs["lre"].ap
        tt("dve", zs["den"].ap, lre_, lre_, ALU.mult, TC, TC)
        tt("dve", zs["t0"].ap, aim, aim, ALU.mult, TC, TC)
        tt("dve", zs["den"].ap, zs["den"].ap, zs["t0"].ap, ALU.add, TC, TC)
        S.add("dve", lambda e: e.reciprocal(out=zs["den"].ap, in_=zs["den"].ap), R(tabC), R(tabC))
        ts("dve", zs["nr"].ap, abr, -1.0, None, ALU.add, None, TC, TC)
        tt("dve", zs["t0"].ap, zs["nr"].ap, lre_, ALU.mult, TC, TC)
        tt("dve", zs["t1"].ap, abi, aim, ALU.mult, TC, TC)
        tt("dve", zs["t0"].ap, zs["t0"].ap, zs["t1"].ap, ALU.add, TC, TC)
        tt("dve", zs["fre"].ap, zs["t0"].ap, zs["den"].ap, ALU.mult, TC, TC)
        tt("dve", zs["t0"].ap, abi, lre_, ALU.mult, TC, TC)
        tt("dve", zs["t1"].ap, zs["nr"].ap, aim, ALU.mult, TC, TC)
        tt("dve", zs["t0"].ap, zs["t0"].ap, zs["t1"].ap, ALU.subtract, TC, TC)
        tt("dve", zs["fim"].ap, zs["t0"].ap, zs["den"].ap, ALU.mult, TC, TC)
        Bsm = scr("Bsm", [128, 2, 16, 128], F32)
        Csm = scr("Csm", [128, 2, 16, 128], F32)
        dma("sp", Bsm.ap, Bsm_d, [], TC, "Bsm")
        dma("sp", Csm.ap, Csm_d, [], TC, "Csm")
        Btr = scr("Btr", [128, 16, 128], F32)
        Bti = scr("Bti", [128, 16, 128], F32)
        Gr = scr("Gr", [128, 16, 128], F32)
        Gi = scr("Gi", [128, 16, 128], F32)
        w0 = scr("w0", [128, 16, 128], F32)
        w1_ = scr("w1_", [128, 16, 128], F32)
        Psr = scr("Psr", [128, 16, 128], BF16)
        Psi = scr("Psi", [128, 16, 128], BF16)
        Kf = scr("Kf", [128, 128], F32)

        def bc(v):
            return v.unsqueeze(2).to_broadcast([128, 16, 128])

        def cmul(o_re, o_im, a_re, a_im, s_re, s_im, neg_im=False):
            tt("dve", w0.ap, a_re, bc(s_re), ALU.mult, TC, TC)
            tt("dve", w1_.ap, a_im, bc(s_im), ALU.mult, TC, TC)
            tt("dve", o_re, w0.ap, w1_.ap, ALU.subtract, TC, TC)
            tt("dve", w0.ap, a_re, bc(s_im), ALU.mult, TC, TC)
            tt("dve", w1_.ap, a_im, bc(s_re), ALU.mult, TC, TC)
            if neg_im:
                stt(o_im, w0.ap, -1.0, w1_.ap, ALU.mult, ALU.subtract, TC, TC)
            else:
                tt("dve", o_im, w0.ap, w1_.ap, ALU.add, TC, TC)

        cmul(Btr.ap, Bti.ap, Bsm.ap[:, 0], Bsm.ap[:, 1], zs["fre"].ap, zs["fim"].ap)
        memset("dve", Kb.ap, 0.0, TC + [Kb])
        for k in range(LC + 1):
            cmul(Gr.ap, Gi.ap, Csm.ap[:, 0], Csm.ap[:, 1], pwr.ap[:, :, k], pwi.ap[:, :, k], neg_im=True)
            if k >= 1:
                for j in range(16):
                    c0 = 64 * ((j % 4) // 2)
                    cp("act", Qs.ap[:, 0, j * LC + k - 1, :], Gr.ap[:, j, c0:c0 + 64], TC, TC + [Qs])
                    cp("act", Qs.ap[:, 1, j * LC + k - 1, :], Gi.ap[:, j, c0:c0 + 64], TC, TC + [Qs])
            if k < LC:
                for q in range(4):
                    pK = PSB[2 + q % 2]
                    for jm in range(4):
                        j = 4 * q + jm
                        mm(pK.ap[:, 0:128], Btr.ap[:, j, :], Gr.ap[:, j, :], jm == 0, False, TC, [pK])
                        mm(pK.ap[:, 0:128], Bti.ap[:, j, :], Gi.ap[:, j, :], False, jm == 3, TC, [pK])
                    if k == 0:
                        stt(Kf.ap, ident_f.ap, dT.ap[:, q:q + 1], pK.ap[:, 0:128], ALU.mult, ALU.add, [pK, ident_f, dT] + TC, TC)
                        cp("dve", Kb.ap[:, q * LC + k, :], Kf.ap, TC, TC + [Kb])
                    else:
                        cp("dve", Kb.ap[:, q * LC + k, :], pK.ap[:, 0:128], [pK] + TC, TC + [Kb])
                s_ = LC - 1 - k
                cmul(Psr.ap, Psi.ap, Btr.ap, Bti.ap, pwr.ap[:, :, k], pwi.ap[:, :, k])
                for part, Ps_ in enumerate((Psr, Psi)):
                    for pr in range(2):
                        bk = (2 * part + pr) % 2
                        pT = psv(bk, [128, 8, 128], BF16)
                        for idx in range(8):
                            j = 4 * (idx // 2) + 2 * pr + idx % 2
                            tr(pT[64 * pr:64 * pr + 64, idx, :], Ps_.ap[:, j, 64 * pr:64 * pr + 64], ident_b.ap, TC + [ident_b], [PSB[bk]])
                        cp("act", Pt.ap[64 * pr:64 * pr + 64, part, s_::LC, :], pT[64 * pr:64 * pr + 64, :, :], [PSB[bk]] + TC, TC + [Pt])
        if "s5setup" in dbg:
            dump("tabC4", tabC4)
            dump("Kb", Kb)
            dump("Pt", Pt)
            dump("Qs", Qs)
        cact = A.alloc("cact", [128, 8, NSEQ], F32)
        dma("sp", cact.ap, cT_d, [], [cact], "cact")
        act(cact.ap, cact.ap, AF.Silu, [cact], [cact])
        WB = 256
        bb = [A.alloc("bb%d" % i, [NSEQ, WB], F32) for i in range(2)]
        mo = [A.alloc("mo%d" % i, [NSEQ, WB], F32) for i in range(2)]
        wa = [A.alloc("wa%d" % i, [128, 8, WB], F32) for i in range(2)]
        wa_view = w_ada_d.rearrange("(kc p) n -> p kc n", p=128)
        for cb in range(6 * D // WB):
            w = wa[cb % 2]
            cs_ = slice(cb * WB, (cb + 1) * WB)
            dma("sp", w.ap, wa_view[:, :, cs_], [], [w], "wa%d" % (cb % 2))
            dma("sp", bb[cb % 2].ap, b_ada_d[:, cs_], [], [bb[cb % 2]], "bb%d" % (cb % 2))
            pb = PSB[6 + cb % 2]
            for kc in range(8):
                mm(pb.ap[0:NSEQ, 0:WB], cact.ap[:, kc, :], w.ap[:, kc, :], kc == 0, kc == 7, [cact, w], [pb])
            tt("dve", mo[cb % 2].ap, pb.ap[0:NSEQ, 0:WB], bb[cb % 2].ap, ALU.add, [pb, bb[cb % 2]], [mo[cb % 2]])
            dma("sp", mod_d.ap[:, cs_], mo[cb % 2].ap, [mo[cb % 2]], [mod_d], "mo%d" % (cb % 2))
        sc1T = A.alloc("sc1T", [128, 8, NSEQ], F32)
        g1T = A.alloc("g1T", [128, 8], F32)
        dma("sp", g1T.ap, g1T_d, [], [g1T], "g1T")
        for b in range(NSEQ):
            S.add("sp", lambda e, b=b: e.dma_start(out=sh1T.ap[:, :, b], in_=mod_d.ap[b, 0:D].rearrange("(kc p) -> p kc", p=128),
                                                  allow_slow_non_contiguous=True), R(mod_d), R(sh1T), dma_key="sh1T", cost=0.07, lat=6.0)
            S.add("sp", lambda e, b=b: e.dma_start(out=sc1T.ap[:, :, b], in_=mod_d.ap[b, D:2 * D].rearrange("(kc p) -> p kc", p=128),
                                                  allow_slow_non_contiguous=True), R(mod_d), R(sc1T), dma_key="sc1T", cost=0.07, lat=6.0)
        for b in range(NSEQ):
            stt(A1T.ap[:, :, b], sc1T.ap[:, :, b], 1.0, g1T.ap, ALU.add, ALU.mult, [sc1T, g1T], [A1T])
        if "mod" in dbg:
            dump("A1T", A1T)
        if stop_after == "mod":
            S.finish(final_ops)
            S.emit(st)
            return nc, dbg_outs
        A.reset(mark_setup)

        wsT_b = A.alloc("wsT_b", [128, 8, 128], BF16)
        sgub = A.alloc("sgub", [128, 4, 128], F32)
        mark_sgu = A.mark()
        wsf = A.alloc("wsf", [128, 8, 128], F32)
        dma("sp", wsf.ap, wsT_d, [], [wsf], "wsf")
        for h in range(8):
            S.add("pool", lambda e, h=h: e.affine_select(out=wsf.ap[:, h, :], in_=wsf.ap[:, h, :], pattern=[[1, 128]],
                                                           compare_op=ALU.is_ge, fill=0.0, base=0, channel_multiplier=-1),
                  R(wsf), R(wsf))
        cp("pool", wsT_b.ap, wsf.ap, [wsf], [wsT_b])
        bsT = A.alloc("bsT", [128, 4, 128], F32)
        dma("sp", bsT.ap, bsT_d, [], [bsT], "bsT")
        pmix0 = psv(3, [128, 4, 128])
        for h in range(8):
            po = (h % 2) * 64
            mm(pmix0[po:po + 64, h // 2, :], ones_b.ap[:, 0:64], wsT_b.ap[:, h, :], True, True, [ones_b, wsT_b], [PSB[3]])
        for q in range(4):
            stt(sgub.ap[:, q, :], pmix0[:, q, :], lnbT.ap[:, q:q + 1], bsT.ap[:, q, :], ALU.mult, ALU.add,
                [PSB[3], lnbT, bsT], [sgub])
        if "sgusetup" in dbg:
            dump("sgub", sgub)
            dump("wsT_b", wsT_b)
        A.reset(mark_sgu)

        if stop_after == "setup":
            S.finish(final_ops)
            S.emit(st)
            return nc, dbg_outs

        def wload(name, src_view, shape, key=None):
            t = A.alloc(name, shape, BF16)
            dma("pool", t.ap, src_view, [], [t], key or name)
            return t

        w_in_b = wload("w_in_b", w_in_d.rearrange("(kc p) n -> p kc n", p=128), [128, 8, 1536])
        w_gate_b = wload("w_gate_b", w_gate_d.rearrange("(kc p) n -> p kc n", p=128), [128, 8, 2048])
        w_glu_b = wload("w_glu_b", w_glu_d.rearrange("(kc p) n -> p kc n", p=128), [128, 4, 512])
        w_bra_b = wload("w_bra_b", w_bra_d.rearrange("(kc p) n -> p kc n", p=128), [128, 4, D])
        ts("pool", w_bra_b.ap, w_bra_b.ap, 0.5, 1.0, ALU.mult, ALU.mult, [w_bra_b], [w_bra_b])
        w_brb_b = wload("w_brb_b", w_brb_d.rearrange("(kc p) n -> p kc n", p=128), [128, 4, D])
        w_out_b = wload("w_out_b", w_out_d.rearrange("(kc p) n -> p kc n", p=128), [128, 8, D])
        def alias(name, shape, dt, of, off=0):
            t = Tl(None, name)
            n = 1
            for s_ in shape[1:]:
                n *= s_
            base_ = of.ap
            if len(base_.shape) == 3:
                base_ = base_.rearrange("p a b -> p (a b)")
            elif len(base_.shape) == 4:
                base_ = base_.rearrange("p a b c -> p (a b c)")
            if base_.dtype != dt:
                base_ = base_.bitcast(dt)
            v = base_[:, off:off + n]
            if len(shape) == 3:
                v = v.rearrange("p (a b) -> p a b", a=shape[1])
            t.ap = v
            t.r = of.r
            return t

        xt0 = A.alloc("xt0", [128, D], F32)
        NBR = int(os.environ.get("NBR", "1"))
        NBX = int(os.environ.get("NBX", str(NBR)))
        xr_l = [A.alloc("xr%d" % i, [128, D], F32) for i in range(NBX)]
        xsb = A.alloc("xsb", [128, D], BF16)
        ssq = A.alloc("ssq", [128, 1], F32)
        rstd = A.alloc("rstd", [128, 1], F32)
        xnT = [A.alloc("xnT%d" % i, [128, 8, 128], BF16) for i in range(3)]
        uT = [A.alloc("uT%d" % i, [128, 4, 128], BF16) for i in range(2)]
        guT = A.alloc("guT", [128, 4, 128], F32)
        gv = A.alloc("gv", [128, 512], F32)
        vst = A.alloc("vst", [128, 6], F32)
        vmv = A.alloc("vmv", [128, 2], F32)
        vrs = A.alloc("vrs", [128, 1], F32)
        vhat = A.alloc("vhat", [128, 512], BF16)
        mixT = alias("mixT", [128, 4, 128], F32, gv)
        ybT = [A.alloc("ybT%d" % i, [128, 4, 128], BF16) for i in range(3)]
        xtil = A.alloc("xtil", [128, 4, 2, NCH], F32)
        s5a = A.alloc("s5a", [128, 4, NCH], F32)
        s5b = A.alloc("s5b", [128, 4, NCH], F32)
        gsc = A.alloc("gsc", [128, 4, 2, NCH], F32)
        gp = A.alloc("gp", [128, 16, 2], F32)
        carry = A.alloc("carry", [128, 16, 2], F32)
        sm1 = A.alloc("sm1", [128, 16, 2], F32)
        cf2 = A.alloc("cf2", [128, 4, 2], F32)
        c4 = [A.alloc("c4_%d" % i, [128, 16], F32) for i in range(4)]
        Sprev = [A.alloc("Sprev%d" % i, [128, 4, 2, NCH], BF16) for i in range(2)]
        ygT = A.alloc("ygT", [128, 4, 128], BF16)
        sg = alias("sg", [128, 4, 128], F32, xsb) if os.environ.get("SGALIAS", "1") == "1" else A.alloc("sg", [128, 4, 128], F32)
        yaT = [A.alloc("yaT%d" % i, [128, 4, 128], BF16) for i in range(2)]
        gates_l = [A.alloc("gates%d" % i, [128, 16, 128], F32) for i in range(NBR)]
        if os.environ.get("XN2SEP", "1") == "1":
            xn2_l = [A.alloc("xn2_%d" % i, [128, D], F32) for i in range(NBR)]
            xn2T_l = [alias("xn2T%d" % i, [128, 8, 128], F32, xr_l[i % NBX]) for i in range(NBR)]
        else:
            xn2T_l = [alias("xn2T%d" % i, [128, 8, 128], F32, gates_l[i], off=1024) for i in range(NBR)]
            xn2_l = [alias("xn2_%d" % i, [128, D], F32, gates_l[i], off=0) for i in range(NBR)]
        NBM = int(os.environ.get("NBM", "1"))
        mergedT_l = [A.alloc("mergedT%d" % i, [128, 8, 128], BF16) for i in range(NBM)]
        jk2_l = [alias("jk2_%d" % i, [128, D], BF16, mergedT_l[i]) for i in range(NBM)]
        gt1b = A.alloc("gt1b", [128, D], F32)
        A2b = A.alloc("A2b", [128, D], F32)
        sh2b = A.alloc("sh2b", [128, D], F32)
        ssq2_l = [A.alloc("ssq2_%d" % i, [128, 1], F32) for i in range(NBR)]
        rstd2_l = [A.alloc("rstd2_%d" % i, [128, 1], F32) for i in range(NBR)]
        lg_l = [A.alloc("lg%d" % i, [128, 36], F32) for i in range(NBR)]
        rt_l = [{n: A.alloc("rt%d_" % i + n, [128, w_], F32) for n, w_ in
                 [("gmax", 1), ("ngmax", 1), ("maskg", 4), ("eg", 4), ("sume", 1), ("pgs", 1), ("pen", 4), ("lem", 32),
                  ("m1", 1), ("oh1", 32), ("lem2", 32), ("m2", 1), ("oh2", 32), ("dm", 1), ("e2", 1), ("p1", 1), ("p2", 1),
                  ("rank", 32), ("slotv", 32), ("junk", 32), ("sl", 2), ("val", 32), ("vk", 2)]} for i in range(NBR)]
        ohb_l = [A.alloc("ohb%d" % i, [128, 32], BF16) for i in range(NBR)]

        def rms_rstd(src, ssq_t, rstd_t, junk_ap, junk_t):
            act(junk_ap, src.ap, AF.Square, [src], [junk_t, ssq_t], accum=ssq_t.ap)
            ts("pool", ssq_t.ap, ssq_t.ap, 1.0 / D, RMS_EPS, ALU.mult, ALU.add, [ssq_t], [ssq_t])
            tt("pool", rstd_t.ap, ssq_t.ap, mhalf.ap, ALU.pow, [ssq_t, mhalf], [rstd_t])

        store_ops = []
        scat_ops = []
        c128 = tabC4.ap[:, :, NCH]
        d128 = tabD4.ap[:, :, NCH]

        def seqof(i):
            return i // NT

        def P1(i):
            b = seqof(i)
            X = xt0
            XN = xnT[i % 3]
            if i == 0:
                dma("sp", X.ap, x_d[0:128, :], [], [X], "xt0")
            rms_rstd(X, ssq, rstd, xsb.ap, xsb)
            act(xsb.ap, X.ap, AF.Copy, [X, rstd], [xsb], scale=rstd.ap[:, 0:1])
            if i + 1 < NTILES:
                dma("sp", X.ap, x_d[(i + 1) * 128:(i + 2) * 128, :], [], [X], "xt0")
            pX = psv(0, [128, 8, 128], BF16)
            for kc in range(8):
                tr(pX[:, kc, :], xsb.ap[:, kc * 128:(kc + 1) * 128], ident_b.ap, [xsb, ident_b], [PSB[0]])
            for kc in range(8):
                act(XN.ap[:, kc, :], pX[:, kc, :], AF.Identity, [PSB[0], A1T, sh1T], [XN],
                    bias=sh1T.ap[:, kc, b:b + 1], scale=A1T.ap[:, kc, b:b + 1])
            if dbg.get("tile") == i:
                dump("xnT", XN)

        def P2(i):
            XN = xnT[i % 3]
            UT = uT[i % 2]
            pZa = psv(1, [128, 4, 128])
            pZu = psv(0, [128, 4, 128])
            for m in range(4):
                for kc in range(8):
                    mm(pZa[:, m, :], w_in_b.ap[:, kc, m * 128:(m + 1) * 128], XN.ap[:, kc, :], kc == 0, kc == 7,
                       [w_in_b, XN], [PSB[1]])
            cp("act", UT.ap, pZa, [PSB[1]], [UT])
            for m in range(4):
                for kc in range(8):
                    mm(pZu[:, m, :], w_in_b.ap[:, kc, 512 + m * 128:512 + (m + 1) * 128], XN.ap[:, kc, :], kc == 0, kc == 7,
                       [w_in_b, XN], [PSB[0]])
            act(guT.ap, pZu, AF.Gelu_apprx_tanh, [PSB[0]], [guT])
            pV = psv(1, [128, 512])
            for kc in range(8):
                mm(pV, XN.ap[:, kc, :], w_in_b.ap[:, kc, 1024:1536], kc == 0, kc == 7, [w_in_b, XN], [PSB[1]])
            act(gv.ap, pV, AF.Gelu_apprx_tanh, [PSB[1]], [gv])
            if dbg.get("tile") == i:
                dump("uT", UT)

        def P3(i):
            YB = ybT[i % 3]
            S.add("dve", lambda e: e.bn_stats(out=vst.ap, in_=gv.ap), R(gv), R(vst))
            S.add("dve", lambda e: e.bn_aggr(out=vmv.ap, in_=vst.ap), R(vst), R(vmv))
            ts("pool", vrs.ap, vmv.ap[:, 1:2], 1.0, LN_EPS, ALU.mult, ALU.add, [vmv], [vrs])
            tt("pool", vrs.ap, vrs.ap, mhalf.ap, ALU.pow, [vrs, mhalf], [vrs])
            ts("dve", vhat.ap, gv.ap, vmv.ap[:, 0:1], vrs.ap[:, 0:1], ALU.subtract, ALU.mult, [gv, vmv, vrs], [vhat])
            pMix = psv(0, [128, 4, 128])
            for h in range(8):
                po = (h % 2) * 64
                mm(pMix[po:po + 64, h // 2, :], vhat.ap[:, h * 64:(h + 1) * 64], wsT_b.ap[:, h, :], True, True,
                   [vhat, wsT_b], [PSB[0]])
            for q in range(4):
                stt(mixT.ap[:, q, :], pMix[:, q, :], lngT.ap[:, q:q + 1], sgub.ap[:, q, :], ALU.mult, ALU.add,
                    [PSB[0], lngT, sgub], [mixT])
            tt("pool", YB.ap, guT.ap, mixT.ap, ALU.mult, [guT, mixT], [YB])
            if dbg.get("tile") == i:
                dump("ybT", YB)

        def pS5(q):
            o = (q % 2) * 4 * NCH
            return ps_all[:, 2:4, o:o + 4 * NCH].rearrange("p k (a b c) -> p k a b c", a=2, b=2)

        def Qpre(i):
            if i % NT == 0:
                memset("dve", carry.ap, 0.0, [carry])
            cT_, dT_ = tabC4.ap[:, :, 1], tabD4.ap[:, :, 1]
            tt("dve", c4[0].ap, carry.ap[:, :, 0], cT_, ALU.mult, [carry, tabC4], [c4[0]])
            tt("dve", c4[1].ap, carry.ap[:, :, 1], dT_, ALU.mult, [carry, tabD4], [c4[1]])
            tt("dve", c4[2].ap, carry.ap[:, :, 1], cT_, ALU.mult, [carry, tabC4], [c4[2]])
            tt("dve", c4[3].ap, carry.ap[:, :, 0], dT_, ALU.mult, [carry, tabD4], [c4[3]])
            tt("dve", sm1.ap[:, :, 0], c4[0].ap, c4[1].ap, ALU.add, [c4[0], c4[1]], [sm1])
            tt("dve", sm1.ap[:, :, 1], c4[2].ap, c4[3].ap, ALU.subtract, [c4[2], c4[3]], [sm1])

        def QB(i, q):
            UT = uT[i % 2]
            pS = pS5(q)
            for jj in range(4):
                ro = 64 * (jj // 2)
                for part in range(2):
                    for sx in range(LC):
                        mm(pS[:, jj // 2, jj % 2, part, :], Pt.ap[ro:ro + 64, part, (2 * q + jj % 2) * LC + sx, :],
                           UT.ap[ro:ro + 64, q, sx::LC], sx == 0, sx == LC - 1, [Pt, UT], [PSB[2 + jj // 2]])

        def QD(i, q):
            pS = pS5(q)
            SP = Sprev[q % 2]
            tc_ = tabC4.ap[:, 4 * q:4 * q + 4, 0:NCH]
            td_ = tabD4.ap[:, 4 * q:4 * q + 4, 0:NCH]
            tc4 = tc_.rearrange("p (k a) c -> p k a c", k=2)
            td4 = td_.rearrange("p (k a) c -> p k a c", k=2)
            a4 = s5a.ap.rearrange("p (k a) c -> p k a c", k=2)
            b4 = s5b.ap.rearrange("p (k a) c -> p k a c", k=2)
            PB = [PSB[2], PSB[3]]
            tt("dve", a4, pS[:, :, :, 0, :], tc4, ALU.mult, PB + [tabC4], [s5a])
            tt("dve", b4, pS[:, :, :, 1, :], td4, ALU.mult, PB + [tabD4], [s5b])
            tt("dve", xtil.ap[:, :, 0, :], s5a.ap, s5b.ap, ALU.add, [s5a, s5b], [xtil])
            tt("dve", a4, pS[:, :, :, 1, :], tc4, ALU.mult, PB + [tabC4, xtil], [s5a])
            tt("dve", b4, pS[:, :, :, 0, :], td4, ALU.mult, PB + [tabD4, xtil], [s5b])
            tt("dve", xtil.ap[:, :, 1, :], s5a.ap, s5b.ap, ALU.subtract, [s5a, s5b], [xtil])
            tt("dve", cf2.ap, carry.ap[:, 4 * q:4 * q + 4, :], rmagL.ap[:, 4 * q:4 * q + 4].unsqueeze(2).to_broadcast([128, 4, 2]),
               ALU.mult, [carry, rmagL], [cf2])
            tt("dve", xtil.ap[:, :, :, 0], xtil.ap[:, :, :, 0], cf2.ap, ALU.add, [xtil, cf2], [xtil])
            S.add("dve", lambda e: e.tensor_tensor_scan(
                out=gsc.ap.rearrange("p a b c -> p (a b c)"),
                data0=Rtab.ap[:, 4 * q:4 * q + 4, :, :].rearrange("p a b c -> p (a b c)"),
                data1=xtil.ap.rearrange("p a b c -> p (a b c)"), initial=0.0, op0=ALU.mult, op1=ALU.add),
                R(Rtab, xtil), R(gsc), cost=0.25 + 8 * NCH / 500.0)
            cp("dve", gp.ap[:, 4 * q:4 * q + 4, :], gsc.ap[:, :, :, NCH - 1], [gsc], [gp])
            n1 = NCH - 1
            tt("dve", s5a.ap[:, :, 0:n1], gsc.ap[:, :, 0, 0:n1], tc_[:, :, 0:n1], ALU.mult, [gsc, tabC4], [s5a])
            tt("dve", s5b.ap[:, :, 0:n1], gsc.ap[:, :, 1, 0:n1], td_[:, :, 0:n1], ALU.mult, [gsc, tabD4], [s5b])
            tt("dve", SP.ap[:, :, 0, 1:NCH], s5a.ap[:, :, 0:n1], s5b.ap[:, :, 0:n1], ALU.subtract, [s5a, s5b], [SP])
            tt("dve", s5a.ap[:, :, 0:n1], gsc.ap[:, :, 1, 0:n1], tc_[:, :, 0:n1], ALU.mult, [gsc, tabC4, SP], [s5a])
            tt("dve", s5b.ap[:, :, 0:n1], gsc.ap[:, :, 0, 0:n1], td_[:, :, 0:n1], ALU.mult, [gsc, tabD4, SP], [s5b])
            tt("dve", SP.ap[:, :, 1, 1:NCH], s5a.ap[:, :, 0:n1], s5b.ap[:, :, 0:n1], ALU.add, [s5a, s5b], [SP])
            cp("dve", SP.ap[:, :, :, 0], sm1.ap[:, 4 * q:4 * q + 4, :], [sm1], [SP])
            if dbg.get("tile") == i and q == 0:
                dump("gsc0", gsc)

        def QC(i, q):
            UT = uT[i % 2]
            SP = Sprev[q % 2]
            pY = psv(4, [128, 4, LC, NCH])
            for sp_ in range(LC):
                first = True
                for sx in range(sp_ + 1):
                    mm(pY[:, q, sp_, :], Kb.ap[:, q * LC + (sp_ - sx), :], UT.ap[:, q, sx::LC], first, False, [Kb, UT], [PSB[4]])
                    first = False
                for jj in range(4):
                    j = 4 * q + jj
                    ro = 64 * (jj // 2)
                    for part in range(2):
                        mm(pY[ro:ro + 64, q, sp_, :], Qs.ap[:, part, j * LC + sp_, :], SP.ap[:, jj, part, :],
                           False, jj == 3 and part == 1, [Qs, SP], [PSB[4]])

        def Qcarry(i):
            tt("dve", c4[0].ap, gp.ap[:, :, 0], c128, ALU.mult, [gp, tabC4], [c4[0]])
            tt("dve", c4[1].ap, gp.ap[:, :, 1], d128, ALU.mult, [gp, tabD4], [c4[1]])
            tt("dve", c4[2].ap, gp.ap[:, :, 1], c128, ALU.mult, [gp, tabC4], [c4[2]])
            tt("dve", c4[3].ap, gp.ap[:, :, 0], d128, ALU.mult, [gp, tabD4], [c4[3]])
            tt("dve", carry.ap[:, :, 0], c4[0].ap, c4[1].ap, ALU.subtract, [c4[0], c4[1]], [carry])
            tt("dve", carry.ap[:, :, 1], c4[2].ap, c4[3].ap, ALU.add, [c4[2], c4[3]], [carry])

        def Qtail(i):
            YA = yaT[i % 2]
            pY = psv(4, [128, 4, LC, NCH])
            for q in range(4):
                act(ygT.ap[:, q, :].rearrange("p (c s) -> p s c", s=LC), pY[:, q, :, :], AF.Gelu_apprx_tanh, [PSB[4]], [ygT])
            if dbg.get("tile") == i:
                dump("ypre", ygT)
            pG = psv(4, [128, 4, 128])
            for m in range(4):
                for kc in range(4):
                    mm(pG[:, m, :], w_glu_b.ap[:, kc, m * 128:(m + 1) * 128], ygT.ap[:, kc, :], kc == 0, kc == 3,
                       [w_glu_b, ygT], [PSB[4]])
            for m in range(4):
                act(sg.ap[:, m, :], pG[:, m, :], AF.Tanh, [PSB[4], b_gluT], [sg], bias=b_gluT.ap[:, m:m + 1], scale=0.5)
            stt(YA.ap, sg.ap, 1.0, ygT.ap, ALU.add, ALU.mult, [sg, ygT], [YA])
            if dbg.get("tile") == i:
                dump("yaT", YA)

        def R0(i):
            b = seqof(i)
            xr, xn2 = xr_l[i % NBX], xn2_l[i % NBR]
            if i % NT == 0:
                dma("sp", gt1b.ap, mod_d.ap[b:b + 1, 2 * D:3 * D].partition_broadcast(128), [mod_d], [gt1b], "gt1b")
                ts("dve", gt1b.ap, gt1b.ap, 0.5, None, ALU.mult, None, [gt1b], [gt1b])
                dma("sp", sh2b.ap, mod_d.ap[b:b + 1, 3 * D:4 * D].partition_broadcast(128), [mod_d], [sh2b], "sh2b")
                dma("sp", A2b.ap, mod_d.ap[b:b + 1, 4 * D:5 * D].partition_broadcast(128), [mod_d], [A2b], "A2b")
                dma("sp", xn2.ap, g2_d.partition_broadcast(128), [], [xn2], "g2tmp")
                stt(A2b.ap, A2b.ap, 1.0, xn2.ap, ALU.add, ALU.mult, [A2b, xn2], [A2b])
            dma("sp", xr.ap, x_d[i * 128:(i + 1) * 128, :], [], [xr], "xr")

        def R1(i, mg):
            XN = xnT[i % 3]
            gates = gates_l[i % NBR]
            bk = 5 if mg % 2 == 0 else 7
            pGt = psv(bk, [128, 4, 128])
            for mm_ in range(4):
                m = mg * 4 + mm_
                for kc in range(8):
                    mm(pGt[:, mm_, :], w_gate_b.ap[:, kc, m * 128:(m + 1) * 128], XN.ap[:, kc, :], kc == 0, kc == 7,
                       [w_gate_b, XN], [PSB[bk]])
            for mm_ in range(4):
                m = mg * 4 + mm_
                act(gates.ap[:, m, :], pGt[:, mm_, :], AF.Tanh, [PSB[bk], b_gateT], [gates],
                    bias=b_gateT.ap[:, m:m + 1], scale=0.5)

        def R2(i, half):
            YA, YB = yaT[i % 2], ybT[i % 3]
            gates, mergedT = gates_l[i % NBR], mergedT_l[i % NBM]
            pA = psv(5, [128, 4, 128])
            pB = psv(6, [128, 4, 128])
            for mm_ in range(4):
                m = half * 4 + mm_
                for kc in range(4):
                    mm(pA[:, mm_, :], w_bra_b.ap[:, kc, m * 128:(m + 1) * 128], YA.ap[:, kc, :], kc == 0, kc == 3,
                       [w_bra_b, YA], [PSB[5]])
            for mm_ in range(4):
                m = half * 4 + mm_
                for kc in range(4):
                    mm(pB[:, mm_, :], w_brb_b.ap[:, kc, m * 128:(m + 1) * 128], YB.ap[:, kc, :], kc == 0, kc == 3,
                       [w_brb_b, YB], [PSB[6]])
            ga = gates.ap[:, half * 4:half * 4 + 4, :]
            gb = gates.ap[:, 8 + half * 4:8 + half * 4 + 4, :]
            stt(ga, ga, 1.0, pA, ALU.add, ALU.mult, [gates, PSB[5]], [gates])
            stt(gb, gb, 1.0, pB, ALU.add, ALU.mult, [gates, PSB[6]], [gates])
            tt("dve", mergedT.ap[:, half * 4:half * 4 + 4, :], ga, gb, ALU.add, [gates], [mergedT])
            if dbg.get("tile") == i and half == 1:
                dump("mergedT", mergedT)

        def R3(i):
            xr, xn2, mergedT, jk2 = xr_l[i % NBX], xn2_l[i % NBR], mergedT_l[i % NBM], jk2_l[i % NBM]
            ssq2, rstd2 = ssq2_l[i % NBR], rstd2_l[i % NBR]
            for half in range(2):
                bk = 6 + half
                for kc in range(8):
                    mm(PSB[bk].ap, mergedT.ap[:, kc, :], w_out_b.ap[:, kc, half * 512:(half + 1) * 512], kc == 0, kc == 7,
                       [mergedT, w_out_b], [PSB[bk]])
                sl = slice(half * 512, (half + 1) * 512)
                tt("dve", xn2.ap[:, sl], PSB[bk].ap, gt1b.ap[:, sl], ALU.mult, [PSB[bk], gt1b], [xn2])
            tt("dve", xr.ap, xr.ap, xn2.ap, ALU.add, [xr, xn2], [xr])
            store_ops.append(dma("sp", H_d[i * 128:(i + 1) * 128, :], xr.ap, [xr], [r_H], "hst"))
            rms_rstd(xr, ssq2, rstd2, jk2.ap, jk2)
            stt(xn2.ap, xr.ap, rstd2.ap[:, 0:1], A2b.ap, ALU.mult, ALU.mult, [xr, rstd2, A2b], [xn2])
            tt("dve", xn2.ap, xn2.ap, sh2b.ap, ALU.add, [xn2, sh2b], [xn2])
            if dbg.get("tile") == i:
                dump("h", xr)
                dump("xn2", xn2)

        def R4(i):
            xn2, xn2T, lg, rt, ohb = xn2_l[i % NBR], xn2T_l[i % NBR], lg_l[i % NBR], rt_l[i % NBR], ohb_l[i % NBR]
            pX2 = psv2(6, [128, 8, 128])
            for kc in range(8):
                tr(pX2[:, kc, :], xn2.ap[:, kc * 128:(kc + 1) * 128], ident_f.ap, [xn2, ident_f], [PSB[6], PSB[7]])
            cp("act", xn2T.ap, pX2, [PSB[6], PSB[7]], [xn2T])
            pL = psv(5, [128, 36])
            for kc in range(8):
                mm(pL, xn2T.ap[:, kc, :], w_r.ap[:, kc, :], kc == 0, kc == 7, [xn2T, w_r], [PSB[5]])
            tt("dve", lg.ap, pL, b_r.ap, ALU.add, [PSB[5], b_r], [lg])
            r_ = rt
            BIG = 1.0e9
            S.add("dve", lambda e: e.tensor_reduce(out=r_["gmax"].ap, in_=lg.ap[:, 0:4], axis=AX.X, op=ALU.max), R(lg), R(r_["gmax"]))
            ts("dve", r_["maskg"].ap, lg.ap[:, 0:4], r_["gmax"].ap[:, 0:1], None, ALU.is_equal, None, [lg, r_["gmax"]], [r_["maskg"]])
            ts("dve", r_["ngmax"].ap, r_["gmax"].ap, -0.5, None, ALU.mult, None, [r_["gmax"]], [r_["ngmax"]])
            act(r_["eg"].ap, lg.ap[:, 0:4], AF.Tanh, [lg, r_["ngmax"]], [r_["eg"]], bias=r_["ngmax"].ap[:, 0:1], scale=0.5)
            ts("dve", r_["pen"].ap, r_["eg"].ap, -1.0, 1.0, ALU.mult, ALU.add, [r_["eg"]], [r_["pen"]])
            S.add("dve", lambda e: e.reciprocal(out=r_["pen"].ap, in_=r_["pen"].ap), R(r_["pen"]), R(r_["pen"]))
            stt(r_["eg"].ap, r_["eg"].ap, 1.0, r_["pen"].ap, ALU.add, ALU.mult, [r_["eg"], r_["pen"]], [r_["eg"]])
            S.add("dve", lambda e: e.tensor_reduce(out=r_["sume"].ap, in_=r_["eg"].ap, axis=AX.X, op=ALU.add), R(r_["eg"]), R(r_["sume"]))
            S.add("dve", lambda e: e.reciprocal(out=r_["pgs"].ap, in_=r_["sume"].ap), R(r_["sume"]), R(r_["pgs"]))
            ts("dve", r_["pen"].ap, r_["maskg"].ap, BIG, -BIG, ALU.mult, ALU.add, [r_["maskg"]], [r_["pen"]])
            for g in range(4):
                ts("dve", r_["lem"].ap[:, g * 8:(g + 1) * 8], lg.ap[:, 4 + g * 8:4 + (g + 1) * 8], r_["pen"].ap[:, g:g + 1], None,
                   ALU.add, None, [lg, r_["pen"]], [r_["lem"]])
            S.add("dve", lambda e: e.tensor_reduce(out=r_["m1"].ap, in_=r_["lem"].ap, axis=AX.X, op=ALU.max), R(r_["lem"]), R(r_["m1"]))
            ts("dve", r_["oh1"].ap, r_["lem"].ap, r_["m1"].ap[:, 0:1], None, ALU.is_equal, None, [r_["lem"], r_["m1"]], [r_["oh1"]])
            stt(r_["lem2"].ap, r_["oh1"].ap, -BIG, r_["lem"].ap, ALU.mult, ALU.add, [r_["oh1"], r_["lem"]], [r_["lem2"]])
            S.add("dve", lambda e: e.tensor_reduce(out=r_["m2"].ap, in_=r_["lem2"].ap, axis=AX.X, op=ALU.max), R(r_["lem2"]), R(r_["m2"]))
            ts("dve", r_["oh2"].ap, r_["lem2"].ap, r_["m2"].ap[:, 0:1], None, ALU.is_equal, None, [r_["lem2"], r_["m2"]], [r_["oh2"]])
            tt("dve", r_["dm"].ap, r_["m1"].ap, r_["m2"].ap, ALU.subtract, [r_["m1"], r_["m2"]], [r_["dm"]])
            act(r_["e2"].ap, r_["dm"].ap, AF.Tanh, [r_["dm"]], [r_["e2"]], scale=0.5)
            ts("dve", r_["p1"].ap, r_["e2"].ap, 0.5, 0.5, ALU.mult, ALU.add, [r_["e2"]], [r_["p1"]])
            ts("dve", r_["p2"].ap, r_["e2"].ap, -0.5, 0.5, ALU.mult, ALU.add, [r_["e2"]], [r_["p2"]])
            tt("dve", ohb.ap, r_["oh1"].ap, r_["oh2"].ap, ALU.add, [r_["oh1"], r_["oh2"]], [ohb])
            pR = psv(5, [128, 128])
            mm(pR[:, 64:96], tri_b.ap, ohb.ap, True, True, [tri_b, ohb], [PSB[5]])
            mm(pR[:, 96:128], ones_b.ap, ohb.ap, True, True, [ones_b, ohb], [PSB[5]])
            tt("dve", r_["rank"].ap, pR[:, 64:96], base.ap, ALU.add, [PSB[5], base], [r_["rank"]])
            tt("dve", base.ap, pR[:, 96:128], base.ap, ALU.add, [PSB[5], base, r_["rank"]], [base])
            ts("dve", r_["val"].ap, r_["rank"].ap, float(CAP), None, ALU.is_lt, None, [r_["rank"]], [r_["val"]])
            tt("dve", r_["slotv"].ap, r_["rank"].ap, ecap.ap, ALU.add, [r_["rank"], ecap], [r_["slotv"]])
            stt(r_["slotv"].ap, r_["val"].ap, -4.0e6, r_["slotv"].ap, ALU.mult, ALU.add, [r_["val"], r_["slotv"]], [r_["slotv"]])
            ts("dve", r_["slotv"].ap, r_["slotv"].ap, 4.0e6, None, ALU.add, None, [r_["slotv"]], [r_["slotv"]])
            for k, ohn in enumerate(("oh1", "oh2")):
                tt("dve", r_["junk"].ap, r_[ohn].ap, r_["slotv"].ap, ALU.mult, [r_[ohn], r_["slotv"]], [r_["junk"]])
                S.add("dve", lambda e, k=k: e.tensor_reduce(out=r_["sl"].ap[:, k:k + 1], in_=r_["junk"].ap, axis=AX.X, op=ALU.add),
                      R(r_["junk"]), R(r_["sl"]))
                tt("dve", r_["junk"].ap, r_[ohn].ap, r_["val"].ap, ALU.mult, [r_[ohn], r_["val"]], [r_["junk"]])
                S.add("dve", lambda e, k=k: e.tensor_reduce(out=r_["vk"].ap[:, k:k + 1], in_=r_["junk"].ap, axis=AX.X, op=ALU.add),
                      R(r_["junk"]), R(r_["vk"]))
            cp("dve", sloti.ap[:, i, :], r_["sl"].ap, [r_["sl"]], [sloti])
            stt(wgt.ap[:, i, 0:1], r_["p1"].ap, r_["pgs"].ap[:, 0:1], r_["vk"].ap[:, 0:1], ALU.mult, ALU.mult,
                [r_["p1"], r_["pgs"], r_["vk"]], [wgt])
            stt(wgt.ap[:, i, 1:2], r_["p2"].ap, r_["pgs"].ap[:, 0:1], r_["vk"].ap[:, 1:2], ALU.mult, ALU.mult,
                [r_["p2"], r_["pgs"], r_["vk"]], [wgt])
            if dbg.get("tile") == i:
                dump("lg", lg)
                dump("rank", r_["rank"])
            for k in range(2):
                scat_ops.append(S.add("pool", lambda e, i=i, k=k: e.indirect_dma_start(
                    out=X_d, out_offset=bass.IndirectOffsetOnAxis(ap=sloti.ap[:, i, k:k + 1], axis=0),
                    in_=xn2.ap, in_offset=None, bounds_check=breg(e, NSLOT - 1), oob_is_err=False),
                    R(xn2, sloti), [r_X], dma_key="scat", cost=1.2, lat=4.0))

        import os as _os
        _skip = set(_os.environ.get('QSKIP', '').split(','))
        _w = lambda f, n: (lambda *a: None) if n in _skip else f
        Qpre, QB, QD, QC, Qcarry, Qtail = _w(Qpre, 'pre'), _w(QB, 'B'), _w(QD, 'D'), _w(QC, 'C'), _w(Qcarry, 'carry'), _w(Qtail, 'tail')
        for s_ in range(NTILES + 2):
            ip, iq, ir = s_, s_ - 1, s_ - 2
            hp = 0 <= ip < NTILES
            hq = 0 <= iq < NTILES
            hr = 0 <= ir < NTILES
            if hr:
                R0(ir)
            if hq:
                Qpre(iq)
                QB(iq, 0)
            if hr:
                R1(ir, 0)
                R1(ir, 1)
            if hp:
                P1(ip)
            if hq:
                QD(iq, 0)
            if hr:
                R1(ir, 2)
                R1(ir, 3)
                R2(ir, 0)
                R2(ir, 1)
            if hq:
                QB(iq, 1)
                QC(iq, 0)
            if hp:
                P2(ip)
            if hq:
                QD(iq, 1)
            if hr:
                R3(ir)
            if hq:
                QB(iq, 2)
                QC(iq, 1)
                QD(iq, 2)
            if hr:
                R4(ir)
            if hq:
                QB(iq, 3)
                QC(iq, 2)
            if hp:
                P3(ip)
            if hq:
                QD(iq, 3)
                QC(iq, 3)
                Qcarry(iq)
                Qtail(iq)
        if "route" in dbg:
            dump("wgt", wgt)
            dump("sloti", sloti)
        if stop_after == "A":
            S.finish(final_ops + store_ops + scat_ops)
            S.emit(st)
            return nc, dbg_outs

        S.barrier(dma_ops=[scat_ops[-1], store_ops[-1]])
        A.reset(mark_persist)
        w1b = [A.alloc("w1b%d" % i, [128, 8, 512], BF16) for i in range(2)]
        w3b = [A.alloc("w3b%d" % i, [128, 8, 512], BF16) for i in range(2)]
        w2b = [A.alloc("w2b%d" % i, [128, 4, D], BF16) for i in range(2)]
        Xblk = [A.alloc("Xblk%d" % i, [128, D], BF16) for i in range(3)]
        XT = [A.alloc("XT%d" % i, [128, 8, CAP], BF16) for i in range(2)]
        hidT = [A.alloc("hidT%d" % i, [128, 4, CAP], BF16) for i in range(2)]
        s1 = [A.alloc("s1_%d" % i, [128, 512], F32) for i in range(2)]
        Yblk = [A.alloc("Yblk%d" % i, [128, D], F32) for i in range(3)]
        NH = (CAP + 511) // 512
        HW_ = CAP // NH
        ystore = []
        cnt = {"x": 0, "y": 0, "h": 0}

        def W13(e_):
            sl_ = e_ % 2
            dma("pool", w1b[sl_].ap, w1_d[e_].rearrange("(kc p) n -> p kc n", p=128), [], [w1b[sl_]], "w1b%d" % sl_)
            dma("pool", w3b[sl_].ap, w3_d[e_].rearrange("(kc p) n -> p kc n", p=128), [], [w3b[sl_]], "w3b%d" % sl_)

        def W2(e_):
            sl_ = e_ % 2
            dma("pool", w2b[sl_].ap, w2_d[e_].rearrange("(kc p) n -> p kc n", p=128), [], [w2b[sl_]], "w2b%d" % sl_)

        def TX(e_):
            xt_ = XT[e_ % 2]
            for blk in range(NBLK):
                n_ = cnt["x"]
                cnt["x"] += 1
                xb_ = Xblk[n_ % 3]
                r0 = e_ * CAP + blk * 128
                dma("sp", xb_.ap, X_d[r0:r0 + 128, :], [r_X], [xb_], "xblk%d" % (n_ % 3))
                bk = n_ % 2
                pXT = psv(bk, [128, 8, 128], BF16)
                for kc in range(8):
                    tr(pXT[:, kc, :], xb_.ap[:, kc * 128:(kc + 1) * 128], ident_b.ap, [xb_, ident_b], [PSB[bk]])
                cp("act" if blk % 2 == 0 else "dve", xt_.ap[:, :, blk * 128:(blk + 1) * 128], pXT, [PSB[bk]], [xt_])

        def HH(e_):
            sl_ = e_ % 2
            xt_, hd_ = XT[e_ % 2], hidT[e_ % 2]
            for m in range(4):
                for nh in range(NH):
                    cs_ = slice(nh * HW_, (nh + 1) * HW_)
                    n_ = cnt["h"]
                    cnt["h"] += 1
                    b1 = 2 + n_ % 2
                    b3 = 4 + n_ % 2
                    p1_ = PSB[b1].ap[:, 0:HW_]
                    p3_ = PSB[b3].ap[:, 0:HW_]
                    for kc in range(8):
                        mm(p1_, w1b[sl_].ap[:, kc, m * 128:(m + 1) * 128], xt_.ap[:, kc, cs_], kc == 0, kc == 7,
                           [w1b[sl_], xt_], [PSB[b1]])
                    for kc in range(8):
                        mm(p3_, w3b[sl_].ap[:, kc, m * 128:(m + 1) * 128], xt_.ap[:, kc, cs_], kc == 0, kc == 7,
                           [w3b[sl_], xt_], [PSB[b3]])
                    s1_ = s1[n_ % 2]
                    act(s1_.ap[:, 0:HW_], p1_, AF.Silu, [PSB[b1]], [s1_])
                    tt("dve", hd_.ap[:, m, cs_], s1_.ap[:, 0:HW_], p3_, ALU.mult, [s1_, PSB[b3]], [hd_])

        def YY(e_):
            sl_ = e_ % 2
            hd_ = hidT[e_ % 2]
            for blk in range(NBLK):
                n_ = cnt["y"]
                cnt["y"] += 1
                yb_ = Yblk[n_ % 3]
                for half in range(2):
                    bk = 6 + half
                    for kc in range(4):
                        mm(PSB[bk].ap, hd_.ap[:, kc, blk * 128:(blk + 1) * 128], w2b[sl_].ap[:, kc, half * 512:(half + 1) * 512],
                           kc == 0, kc == 3, [hd_, w2b[sl_]], [PSB[bk]])
                    cp("act" if half == 0 else "dve", yb_.ap[:, half * 512:(half + 1) * 512], PSB[bk].ap, [PSB[bk]], [yb_])
                r0 = e_ * CAP + blk * 128
                ystore.append(dma("sp", Y_d[r0:r0 + 128, :], yb_.ap, [yb_], [r_Y], "yst%d" % (n_ % 3)))

        W13(0)
        W2(0)
        W13(1)
        W2(1)
        TX(0)
        for e_ in range(32):
            HH(e_)
            if e_ + 2 < 32:
                W13(e_ + 2)
            if e_ + 1 < 32:
                TX(e_ + 1)
            YY(e_)
            if e_ + 2 < 32:
                W2(e_ + 2)
        if stop_after == "B":
            S.finish(final_ops + ystore[-3:])
            S.emit(st)
            return nc, dbg_outs

        S.barrier(dma_ops=ystore[-3:])
        A.reset(mark_persist)
        NBC = int(os.environ.get('NBC', '3'))
        Hc = [A.alloc("Hc%d" % i, [128, D], F32) for i in range(NBC)]
        Y0 = [A.alloc("Y0_%d" % i, [128, D], F32) for i in range(NBC)]
        Y1 = [A.alloc("Y1_%d" % i, [128, D], F32) for i in range(NBC)]
        acc_l = [A.alloc("acc%d" % i, [128, D], F32) for i in range(2)]
        ob = [A.alloc("ob%d" % i, [128, D], F32) for i in range(2)]
        gt2b = A.alloc("gt2b", [128, D], F32)
        gfb = A.alloc("gfb", [128, D], F32)
        jk_l = [A.alloc("jk%d" % i, [128, D], BF16) for i in range(2)]
        ssq3_l = [A.alloc("ssq3_%d" % i, [128, 1], F32) for i in range(2)]
        rstd3_l = [A.alloc("rstd3_%d" % i, [128, 1], F32) for i in range(2)]
        dma("sp", gfb.ap, gf_d.partition_broadcast(128), [], [gfb], "gfb")
        for s_ in range(NBC):
            memset("pool", Y0[s_].ap, 0.0, [Y0[s_]])
            memset("pool", Y1[s_].ap, 0.0, [Y1[s_]])

        def Cload(i):
            s_ = i % NBC
            dma("sp", Hc[s_].ap, H_d[i * 128:(i + 1) * 128, :], [r_H], [Hc[s_]], "hc%d" % s_)
            for k, Yk in enumerate((Y0[s_], Y1[s_])):
                S.add("pool", lambda e, i=i, k=k, Yk=Yk: e.indirect_dma_start(
                    out=Yk.ap, out_offset=None, in_=Y_d, in_offset=bass.IndirectOffsetOnAxis(ap=sloti.ap[:, i, k:k + 1], axis=0),
                    bounds_check=breg(e, NSLOT - 1), oob_is_err=False), R(r_Y, sloti), R(Yk), dma_key="gath%d_%d" % (k, s_),
                    cost=1.2, lat=5.0)

        for i_ in range(min(NBC - 1, NTILES)):
            Cload(i_)
        for i in range(NTILES):
            b, tau = i // NT, i % NT
            s_ = i % NBC
            if tau == 0:
                dma("sp", gt2b.ap, mod_d.ap[b:b + 1, 5 * D:6 * D].partition_broadcast(128), [mod_d], [gt2b], "gt2b")
            if i + NBC - 1 < NTILES:
                Cload(i + NBC - 1)
            acc, jk, ssq3, rstd3 = acc_l[i % 2], jk_l[i % 2], ssq3_l[i % 2], rstd3_l[i % 2]
            act(acc.ap, Y0[s_].ap, AF.Copy, [Y0[s_], wgt], [acc], scale=wgt.ap[:, i, 0:1])
            stt(acc.ap, Y1[s_].ap, wgt.ap[:, i, 1:2], acc.ap, ALU.mult, ALU.add, [Y1[s_], wgt, acc], [acc])
            tt("dve", acc.ap, acc.ap, gt2b.ap, ALU.mult, [acc, gt2b], [acc])
            tt("dve", acc.ap, acc.ap, Hc[s_].ap, ALU.add, [acc, Hc[s_]], [acc])
            rms_rstd(acc, ssq3, rstd3, jk.ap, jk)
            stt(ob[i % 2].ap, acc.ap, rstd3.ap[:, 0:1], gfb.ap, ALU.mult, ALU.mult, [acc, rstd3, gfb], [ob[i % 2]])
            final_ops.append(dma("sp", out_d[i * 128:(i + 1) * 128, :], ob[i % 2].ap, [ob[i % 2]], [], "ost%d" % (i % 2)))
        S.finish(final_ops)
        S.emit(st)
    return nc, dbg_outs


def prep_shared(inp):
    f = np.float32
    g = {}
    L = 0
    g["w_ada"] = np.ascontiguousarray(inp["w_ada"][L], f)
    g["g1T"] = np.ascontiguousarray(inp["norm1_g"][L].reshape(8, 128).T, f)
    g["w_in"] = np.ascontiguousarray(inp["w_in"][L], f)
    g["w_gate"] = np.ascontiguousarray(inp["w_gate"][L], f)
    g["b_gateT"] = np.ascontiguousarray(inp["b_gate"][L].reshape(16, 128).T, f)
    a_re, a_im, ls = inp["ssm_a_re"][L], inp["ssm_a_im"][L], inp["ssm_log_step"][L]
    def sm(v):
        return v.reshape(16, 2, 64).transpose(1, 2, 0).reshape(128, 16)
    lsx = np.repeat(ls[:, None], 64, axis=1)
    g["lam_sm"] = np.ascontiguousarray(np.stack([sm(a_re), sm(a_im), sm(lsx)], axis=1), f)
    Bsm = np.zeros((128, 2, 16, 128), f)
    for pi, Bsrc in enumerate((inp["ssm_b_re"][L], inp["ssm_b_im"][L])):
        for gi in range(32):
            j = gi // 2
            n0 = 64 * (gi % 2)
            c0 = 32 * (j % 4) + 16 * (gi % 2)
            Bsm[n0:n0 + 64, pi, j, c0:c0 + 16] = Bsrc[gi]
    g["Bsm"] = Bsm
    Csm = np.zeros((128, 2, 16, 128), f)
    for pi, Csrc in enumerate((inp["ssm_c_re"][L], inp["ssm_c_im"][L])):
        for gi in range(32):
            j = gi // 2
            n0 = 64 * (gi % 2)
            c0 = 32 * (j % 4) + 16 * (gi % 2)
            Csm[n0:n0 + 64, pi, j, c0:c0 + 16] = Csrc[gi].T
    g["Csm"] = Csm
    g["dT"] = np.ascontiguousarray(inp["ssm_d"][L].reshape(4, 128).T, f)
    g["w_glu"] = np.ascontiguousarray(inp["w_glu"][L], f)
    g["b_gluT"] = np.ascontiguousarray(inp["b_glu"][L].reshape(4, 128).T, f)
    g["wsT"] = np.ascontiguousarray(inp["sgu_w"][L].transpose(2, 0, 1), f)
    g["lngT"] = np.ascontiguousarray(inp["sgu_ln_g"][L].reshape(4, 128).T, f)
    g["lnbT"] = np.ascontiguousarray(inp["sgu_ln_b"][L].reshape(4, 128).T, f)
    bs = inp["sgu_b"][L]
    bsT = np.zeros((128, 4, 128), f)
    for q in range(4):
        bsT[0:64, q, :] = bs[2 * q][None, :]
        bsT[64:128, q, :] = bs[2 * q + 1][None, :]
    g["bsT"] = bsT
    g["w_bra"] = np.ascontiguousarray(inp["w_branch_a"][L], f)
    g["w_brb"] = np.ascontiguousarray(inp["w_branch_b"][L], f)
    g["w_out"] = np.ascontiguousarray(inp["w_out"][L], f)
    g["g2"] = np.ascontiguousarray(inp["norm2_g"][L].reshape(1, D), f)
    wr = np.concatenate([inp["w_router_group"][L], inp["w_router_expert"][L].transpose(1, 0, 2).reshape(D, 32)], axis=1)
    g["w_r"] = np.ascontiguousarray(wr.reshape(8, 128, 36).transpose(1, 0, 2), f)
    g["b_r"] = np.ascontiguousarray(np.concatenate([inp["b_router_group"][L], inp["b_router_expert"][L].reshape(32)]).reshape(1, 36), f)
    g["w1"] = np.ascontiguousarray(inp["w1"][L], f)
    g["w3"] = np.ascontiguousarray(inp["w3"][L], f)
    g["w2"] = np.ascontiguousarray(inp["w2"][L], f)
    g["gf"] = np.ascontiguousarray(inp["norm_f_g"].reshape(1, D), f)
    return g


def prep_core(inp, shared, b0, nseq):
    m = dict(shared)
    xs = inp["x"][b0:b0 + nseq]
    m["x"] = np.ascontiguousarray(xs.reshape(-1, D), np.float32)
    c = inp["c"][b0:b0 + nseq]
    m["cT"] = np.ascontiguousarray(c.reshape(nseq, 8, 128).transpose(2, 1, 0), np.float32)
    m["b_ada_rep"] = np.ascontiguousarray(np.repeat(inp["b_ada"][0][None, :], nseq, axis=0), np.float32)
    return m


_CACHE = {}


def kernel(**inputs):
    inp = {k: np.asarray(v) for k, v in inputs.items()}
    B, SEQ = inp["x"].shape[0], inp["x"].shape[1]
    nseq = B // N_CORES
    key = (nseq, SEQ)
    if key not in _CACHE:
        _CACHE[key] = build(NSEQ=nseq, SEQ=SEQ, CAP=768)[0]
    nc = _CACHE[key]
    shared = prep_shared(inp)
    in_maps = [prep_core(inp, shared, c * nseq, nseq) for c in range(N_CORES)]
    res = run_bass_kernel_spmd(nc, in_maps, core_ids=list(range(N_CORES)))
    outs = [np.asarray(r["out"]).reshape(nseq, SEQ, D) for r in res.results]
    return np.concatenate(outs, axis=0).astype(np.float32)
```
